# Optimizing a Trainium2 kernel written in Bass

```python
import math
import jax
import jax.numpy as jnp
from jax import lax
import numpy as np

D_MODEL = 4096
BATCH = 1
SEQ = 8192
DEPTH = 2

N_EVEN = (DEPTH + 1) // 2
N_ODD = DEPTH // 2

A_WIDTH = D_MODEL // 2
A_VDIM = 128
A_HEADS = A_WIDTH // A_VDIM
A_QKDIM = A_VDIM // 2
Q_BLOCK = 128
POOL_WINDOWS = (2, 4, 8, 16)
B_WIDTH = D_MODEL - A_WIDTH
B_GROUPS = len(POOL_WINDOWS)
B_GROUP_DIM = B_WIDTH // B_GROUPS
C_KDIM = 128
C_HEADS = D_MODEL // C_KDIM
C_VDIM = D_MODEL // C_HEADS
C_CHUNK = 64
FF_DENSE = 256 * math.ceil(8 * D_MODEL / 3 / 256)
N_EXPERTS = 8
TOP_K = 2
FF_EXPERT = D_MODEL
DN_ALPHA = (2 * DEPTH) ** 0.25
DN_BETA = (8 * DEPTH) ** -0.25
LN_EPS = 1e-5
RMS_EPS = 1e-6
NEG_INF = -1e30

kernel_name = 'hybrid_diffattn_pool_hgrn2_moe_deepnorm'


def _post_norm_residual(x, y, gate, g, b):
    r = DN_ALPHA * x.astype(jnp.float32) + (1.0 + gate[:, None, :]) * y.astype(jnp.float32)
    mu = jnp.mean(r, axis=-1, keepdims=True)
    var = jnp.mean(jnp.square(r - mu), axis=-1, keepdims=True)
    return ((r - mu) * lax.rsqrt(var + LN_EPS) * g + b).astype(x.dtype)


def _rms_norm(x, g):
    xf = x.astype(jnp.float32)
    return xf * lax.rsqrt(jnp.mean(jnp.square(xf), axis=-1, keepdims=True) + RMS_EPS) * g


def _modulate(x, shift, scale):
    return (x * (1.0 + scale[:, None, :]) + shift[:, None, :]).astype(x.dtype)


def _swiglu(h, w_in, w_out):
    a, b = jnp.split(h @ w_in, 2, axis=-1)
    return (jax.nn.silu(a) * b) @ w_out


def _alibi_slopes(n_heads):
    return 2.0 ** (-8.0 * jnp.arange(1, n_heads + 1, dtype=jnp.float32) / n_heads)


def _diff_attention(q, k, v, lam):
    b_, s_, h_, _, dq = q.shape
    nb = s_ // Q_BLOCK
    slopes = _alibi_slopes(h_)
    scale = dq ** -0.5
    kf = k.astype(jnp.float32)
    vf = v.astype(jnp.float32)
    qb = q.astype(jnp.float32).reshape(b_, nb, Q_BLOCK, h_, 2, dq).transpose(1, 0, 2, 3, 4, 5)
    kpos = jnp.arange(s_)

    def block(args):
        i, qi = args
        qpos = i * Q_BLOCK + jnp.arange(Q_BLOCK)
        dist = qpos[:, None] - kpos[None, :]
        bias = -slopes[:, None, None] * dist.astype(jnp.float32)
        s = jnp.einsum('bqhmd,bkhmd->bhmqk', qi, kf) * scale + bias[None, :, None]
        s = jnp.where(dist >= 0, s, NEG_INF)
        p = jax.nn.softmax(s, axis=-1)
        w = p[:, :, 0] - lam * p[:, :, 1]
        return jnp.einsum('bhqk,bkhd->bqhd', w, vf)

    o = lax.map(block, (jnp.arange(nb), qb))
    return o.transpose(1, 0, 2, 3, 4).reshape(b_, s_, h_, vf.shape[-1])


def _multiscale_pool(u, pool_w, pool_scale):
    b_, s_, _ = u.shape
    uf = u.astype(jnp.float32).reshape(b_, s_, B_GROUPS, B_GROUP_DIM)
    csum = jnp.concatenate([jnp.zeros((b_, 1, B_GROUPS, B_GROUP_DIM), jnp.float32),
                            jnp.cumsum(uf, axis=1)], axis=1)
    t = jnp.arange(s_)[:, None]
    win = jnp.array(POOL_WINDOWS, dtype=jnp.int32)[None, :]
    start = jnp.maximum(t + 1 - win, 0)
    count = (t + 1 - start).astype(jnp.float32)
    lower = csum[:, start, jnp.arange(B_GROUPS)[None, :], :]
    mean = (csum[:, 1:] - lower) / count[None, :, :, None]
    y = jnp.einsum('bsgc,gce->bsge', mean - uf, pool_w.astype(jnp.float32))
    return y.reshape(b_, s_, B_WIDTH) * pool_scale.astype(jnp.float32)


def _even_mixer(h, w_in, lam_q1, lam_k1, lam_q2, lam_k2, subln_g, pool_w, pool_scale, w_out, lam_init):
    b_, s_, _ = h.shape
    proj = h @ w_in
    q, k, v, u = jnp.split(proj, [A_WIDTH, 2 * A_WIDTH, 3 * A_WIDTH], axis=-1)
    q = q.reshape(b_, s_, A_HEADS, 2, A_QKDIM)
    k = k.reshape(b_, s_, A_HEADS, 2, A_QKDIM)
    v = v.reshape(b_, s_, A_HEADS, A_VDIM)
    lam = (jnp.exp(jnp.sum(lam_q1.astype(jnp.float32) * lam_k1.astype(jnp.float32)))
           - jnp.exp(jnp.sum(lam_q2.astype(jnp.float32) * lam_k2.astype(jnp.float32))) + lam_init)
    o_a = _diff_attention(q, k, v, lam)
    o_a = (_rms_norm(o_a, subln_g) * (1.0 - lam_init)).reshape(b_, s_, A_WIDTH)
    o_b = _multiscale_pool(u, pool_w, pool_scale)
    return (jnp.concatenate([o_a, o_b], axis=-1) @ w_out).astype(h.dtype)


def _hgrn2_chunked(q, k, logf, v):
    b_, h_, s_, dk = q.shape
    dv = v.shape[-1]
    n = s_ // C_CHUNK

    def to_chunks(a):
        return a.reshape(b_, h_, n, C_CHUNK, a.shape[-1]).transpose(2, 0, 1, 3, 4)

    tri = jnp.tril(jnp.ones((C_CHUNK, C_CHUNK), dtype=bool))[:, :, None]

    def step(state, inp):
        qc, kc, gc, vc = inp
        bcum = jnp.cumsum(gc, axis=-2)
        diff = bcum[..., :, None, :] - bcum[..., None, :, :]
        decay = jnp.exp(jnp.where(tri, diff, -jnp.inf))
        attn = jnp.einsum('bhtk,bhsk,bhtsk->bhts', qc, kc, decay)
        o = (jnp.einsum('bhts,bhsv->bhtv', attn, vc)
             + jnp.einsum('bhtk,bhkv->bhtv', qc * jnp.exp(bcum), state))
        b_last = bcum[..., -1:, :]
        new_state = (jnp.exp(b_last)[..., 0, :, None] * state
                     + jnp.einsum('bhsk,bhsv->bhkv', kc * jnp.exp(b_last - bcum), vc))
        return new_state, o

    s0 = jnp.zeros((b_, h_, dk, dv), jnp.float32)
    _, o = lax.scan(step, s0, (to_chunks(q), to_chunks(k), to_chunks(logf), to_chunks(v)))
    return o.transpose(1, 2, 0, 3, 4).reshape(b_, h_, s_, dv)


def _odd_mixer(h, w_in, lb, gnorm_g, w_out):
    b_, s_, _ = h.shape
    q, f, i, g = jnp.split(h @ w_in, 4, axis=-1)
    q = jax.nn.silu(q.astype(jnp.float32))
    forget = lb + (1.0 - lb) * jax.nn.sigmoid(f.astype(jnp.float32))
    k = 1.0 - forget
    logf = jnp.log(forget)

    def heads(a):
        return a.reshape(b_, s_, C_HEADS, -1).transpose(0, 2, 1, 3)

    o = _hgrn2_chunked(heads(q), heads(k), heads(logf), heads(i.astype(jnp.float32)))
    o = o.transpose(0, 2, 1, 3)
    o = _rms_norm(o, gnorm_g) * jax.nn.silu(g.astype(jnp.float32).reshape(b_, s_, C_HEADS, C_VDIM))
    return (o.reshape(b_, s_, D_MODEL) @ w_out).astype(h.dtype)


def _moe_swiglu(h, router_w, w_in, w_out):
    b_, s_, d_ = h.shape
    t = h.reshape(b_ * s_, d_)
    logits = (t @ router_w).astype(jnp.float32)
    top_v, top_i = lax.top_k(logits, TOP_K)
    top_g = jax.nn.softmax(top_v, axis=-1)
    gate = jnp.sum(jax.nn.one_hot(top_i, N_EXPERTS, dtype=jnp.float32) * top_g[..., None], axis=1)
    y = jnp.zeros((b_ * s_, d_), jnp.float32)
    for e in range(N_EXPERTS):
        y = y + gate[:, e:e + 1] * _swiglu(t, w_in[e], w_out[e]).astype(jnp.float32)
    return y.reshape(b_, s_, d_).astype(h.dtype)


def setup_inputs(seed: int = 0) -> dict:
    key = jax.random.key(seed)
    ks = iter(jax.random.split(key, 40))

    def nrm(shape, scale):
        return jax.random.normal(next(ks), shape, jnp.float32) * scale

    D = D_MODEL
    sD = D ** -0.5
    x = nrm((BATCH, SEQ, D), 1.0)
    c = nrm((BATCH, D), 1.0)
    ada_w = nrm((DEPTH, D, 6 * D), 0.1 * sD)
    ada_b = nrm((DEPTH, 6 * D), 0.02)
    ln_g = 1.0 + nrm((DEPTH, 2, D), 0.02)
    ln_b = nrm((DEPTH, 2, D), 0.02)
    even_w_in = jnp.concatenate([nrm((N_EVEN, D, A_WIDTH), sD),
                                 nrm((N_EVEN, D, A_WIDTH), sD),
                                 nrm((N_EVEN, D, A_WIDTH), sD * DN_BETA),
                                 nrm((N_EVEN, D, B_WIDTH), sD)], axis=-1)
    lam_q1 = nrm((N_EVEN, A_QKDIM), 0.1)
    lam_k1 = nrm((N_EVEN, A_QKDIM), 0.1)
    lam_q2 = nrm((N_EVEN, A_QKDIM), 0.1)
    lam_k2 = nrm((N_EVEN, A_QKDIM), 0.1)
    subln_g = 1.0 + nrm((N_EVEN, A_VDIM), 0.02)
    pool_w = nrm((N_EVEN, B_GROUPS, B_GROUP_DIM, B_GROUP_DIM), B_GROUP_DIM ** -0.5 * DN_BETA)
    pool_scale = 1.0 + nrm((N_EVEN, B_WIDTH), 0.02)
    even_w_out = nrm((N_EVEN, D, D), sD * DN_BETA)
    ffn_w_in = nrm((N_EVEN, D, 2 * FF_DENSE), sD)
    ffn_w_out = nrm((N_EVEN, FF_DENSE, D), FF_DENSE ** -0.5 * DN_BETA)
    odd_w_in = jnp.concatenate([nrm((N_ODD, D, D), sD),
                                nrm((N_ODD, D, D), sD),
                                nrm((N_ODD, D, D), sD * DN_BETA),
                                nrm((N_ODD, D, D), sD)], axis=-1)
    lb_raw = nrm((DEPTH, D), 0.5)
    gnorm_g = 1.0 + nrm((N_ODD, C_VDIM), 0.02)
    odd_w_out = nrm((N_ODD, D, D), sD * DN_BETA)
    router_w = nrm((N_ODD, D, N_EXPERTS), sD)
    exp_w_in = nrm((N_ODD, N_EXPERTS, D, 2 * FF_EXPERT), sD)
    exp_w_out = nrm((N_ODD, N_EXPERTS, FF_EXPERT, D), FF_EXPERT ** -0.5 * DN_BETA)
    return {'x': x, 'c': c, 'ada_w': ada_w, 'ada_b': ada_b, 'ln_g': ln_g, 'ln_b': ln_b,
            'even_w_in': even_w_in, 'lam_q1': lam_q1, 'lam_k1': lam_k1, 'lam_q2': lam_q2,
            'lam_k2': lam_k2, 'subln_g': subln_g, 'pool_w': pool_w, 'pool_scale': pool_scale,
            'even_w_out': even_w_out, 'ffn_w_in': ffn_w_in, 'ffn_w_out': ffn_w_out,
            'odd_w_in': odd_w_in, 'lb_raw': lb_raw, 'gnorm_g': gnorm_g, 'odd_w_out': odd_w_out,
            'router_w': router_w, 'exp_w_in': exp_w_in, 'exp_w_out': exp_w_out}


def reference(x, c, ada_w, ada_b, ln_g, ln_b, even_w_in, lam_q1, lam_k1, lam_q2, lam_k2,
              subln_g, pool_w, pool_scale, even_w_out, ffn_w_in, ffn_w_out, odd_w_in, lb_raw,
              gnorm_g, odd_w_out, router_w, exp_w_in, exp_w_out):
    lb_all = jnp.cumsum(jax.nn.softmax(lb_raw.astype(jnp.float32), axis=0), axis=0)
    lb_all = lb_all - lb_all[0]
    c_act = jax.nn.silu(c.astype(jnp.float32))
    for l in range(DEPTH):
        j = l // 2
        mod = c_act @ ada_w[l] + ada_b[l]
        sh1, sc1, g1, sh2, sc2, g2 = jnp.split(mod, 6, axis=-1)
        h = _modulate(x, sh1, sc1)
        if l % 2 == 0:
            lam_init = 0.8 - 0.6 * math.exp(-0.3 * l)
            y = _even_mixer(h, even_w_in[j], lam_q1[j], lam_k1[j], lam_q2[j], lam_k2[j],
                            subln_g[j], pool_w[j], pool_scale[j], even_w_out[j], lam_init)
        else:
            y = _odd_mixer(h, odd_w_in[j], lb_all[l], gnorm_g[j], odd_w_out[j])
        x = _post_norm_residual(x, y, g1, ln_g[l, 0], ln_b[l, 0])
        h = _modulate(x, sh2, sc2)
        if l % 2 == 0:
            y = _swiglu(h, ffn_w_in[j], ffn_w_out[j])
        else:
            y = _moe_swiglu(h, router_w[j], exp_w_in[j], exp_w_out[j])
        x = _post_norm_residual(x, y, g2, ln_g[l, 1], ln_b[l, 1])
    return x
```

```python
import numpy as np
import ml_dtypes
import concourse.bass as bass
import concourse.mybir as mybir
from concourse.bass_utils import run_bass_kernel_spmd
from contextlib import ExitStack

F32 = mybir.dt.float32
BF16 = mybir.dt.bfloat16
AF = mybir.ActivationFunctionType
ALU = mybir.AluOpType
AX = mybir.AxisListType


class Sched:
    ENG = ('pe', 'act', 'dve', 'pool', 'sp')

    def __init__(self, nc, es, immediate=False):
        self.immediate = immediate
        self.nc = nc
        self.es = es
        self.eng = {'pe': nc.tensor, 'act': nc.scalar, 'dve': nc.vector,
                    'pool': nc.gpsimd, 'sp': nc.sync}
        self.sem = {e: es.enter_context(nc.semaphore("s_" + e)) for e in self.ENG}
        self.cnt = {e: 0 for e in self.ENG}
        self.prog = {e: [] for e in self.ENG}
        self.waited = {}
        self.lastw = {}
        self.reads = {}
        self.dsem = {}
        self.semobj = {e: self.sem[e] for e in self.ENG}
        self.ninst = 0

    def _dma_sem(self, key):
        if key not in self.dsem:
            s = self.es.enter_context(self.nc.semaphore("d_%d" % len(self.dsem)))
            k = ('d', key)
            self.semobj[k] = s
            self.dsem[key] = [k, 0]
        return self.dsem[key]

    def _deps(self, eng, reads, writes):
        deps = {}
        def add(d):
            if d is None:
                return
            k, v, e = d
            if e == 'pe' and eng == 'pe':
                return
            if deps.get(k, 0) < v:
                deps[k] = v
        for r in reads:
            add(self.lastw.get(r))
        for w in writes:
            add(self.lastw.get(w))
            for d in self.reads.get(w, ()):
                add(d)
        out = []
        for k, v in deps.items():
            if self.waited.get((eng, k), 0) >= v:
                continue
            self.waited[(eng, k)] = v
            out.append((self.semobj[k], v))
        return out

    def _commit(self, ident, reads, writes):
        for w in writes:
            self.lastw[w] = ident
            self.reads[w] = []
        for r in reads:
            if r in writes:
                continue
            self.reads.setdefault(r, []).append(ident)

    def op(self, eng, fn, reads=(), writes=(), inc=True):
        waits = self._deps(eng, reads, writes)
        if inc:
            self.cnt[eng] += 1
            val = self.cnt[eng]
        else:
            val = self.cnt[eng] + 1
        sem = self.sem[eng]
        e = self.eng[eng]
        def emit():
            for s, v in waits:
                e.wait_ge(s, v)
            i = fn(e)
            if inc:
                i.then_inc(sem, 1)
        if self.immediate:
            emit()
        else:
            self.prog[eng].append(emit)
        self._commit((eng, val, eng), reads, writes)
        self.ninst += 1

    def dma(self, eng, fn, slot, reads=(), writes=()):
        waits = self._deps(eng, reads, writes)
        ds = self._dma_sem(slot)
        ds[1] += 16
        k, val = ds[0], ds[1]
        sem = self.semobj[k]
        e = self.eng[eng]
        def emit():
            for s, v in waits:
                e.wait_ge(s, v)
            fn(e).then_inc(sem, 16)
        if self.immediate:
            emit()
        else:
            self.prog[eng].append(emit)
        self._commit((k, val, 'dma'), reads, writes)
        self.ninst += 1

    def finish(self, final_res=None):
        waits = [(self.sem[k], self.cnt[k]) for k in self.ENG if self.cnt[k] > 0]
        waits += [(self.semobj[k], v) for (k, v) in self.dsem.values()]
        e = self.eng['sp']
        def emit():
            for s, v in waits:
                e.wait_ge(s, v)
        self.prog['sp'].append(emit)
        nc = self.nc
        allsems = [self.sem[k] for k in self.ENG] + [self.semobj[k] for (k, v) in self.dsem.values()]
        with nc.Block() as b0:
            @b0.sync
            def _(t):
                for s_ in allsems:
                    t.sem_clear(s_)
        with nc.Block() as block:
            @block.tensor
            def _(t):
                for f in self.prog['pe']:
                    f()
            @block.scalar
            def _(t):
                for f in self.prog['act']:
                    f()
            @block.vector
            def _(t):
                for f in self.prog['dve']:
                    f()
            @block.gpsimd
            def _(t):
                for f in self.prog['pool']:
                    f()
            @block.sync
            def _(t):
                for f in self.prog['sp']:
                    f()


    def barrier(self):
        waits = [(k, self.cnt[k]) for k in self.ENG if self.cnt[k] > 0]
        waits += [(k, v) for (k, v) in self.dsem.values()]
        for eng in self.ENG:
            ws = []
            for k, v in waits:
                if self.waited.get((eng, k), 0) >= v:
                    continue
                self.waited[(eng, k)] = v
                ws.append((self.semobj[k], v))
            e = self.eng[eng]
            def emit(ws=ws, e=e):
                for s, v in ws:
                    e.wait_ge(s, v)
            self.prog[eng].append(emit)
        self.lastw = {}
        self.reads = {}


class Arena:
    def __init__(self, nc, es, name, kbytes):
        self.t = es.enter_context(nc.sbuf_tensor(name, [128, kbytes * 256], F32))
        self.n = kbytes * 256
        self.off = 0
        self.marks = []

    def alloc(self, shape, dt, parts=128):
        nel = 1
        for s_ in shape:
            nel *= s_
        esz = 4 if dt == F32 else 2
        nw = (nel * esz + 3) // 4
        nw = (nw + 7) // 8 * 8
        assert self.off + nw <= self.n, "arena overflow %d + %d > %d" % (self.off, nw, self.n)
        v = self.t[0:parts, self.off:self.off + nw]
        self.off += nw
        if dt != F32:
            v = v.bitcast(dt)
        v = v[:, 0:nel]
        if len(shape) == 2:
            v = v.rearrange("p (a b) -> p a b", a=shape[0])
        elif len(shape) == 3:
            v = v.rearrange("p (a b c) -> p a b c", a=shape[0], b=shape[1])
        return v

    def mark(self):
        return self.off

    def reset(self, mark):
        self.off = mark


IMM = False
def build_a(NCOL=6144):
    nc = bass.Bass("TRN2", target_bir_lowering=False)
    D = 4096
    aw = nc.dram_tensor("aw", [D, NCOL], F32, kind="ExternalInput").ap()
    ab = nc.dram_tensor("ab", [1, NCOL], F32, kind="ExternalInput").ap()
    cv = nc.dram_tensor("cv", [128, 32], F32, kind="ExternalInput").ap()
    mo = nc.dram_tensor("mo", [1, NCOL], F32, kind="ExternalOutput").ap()
    with ExitStack() as es:
        S = Sched(nc, es, immediate=IMM)
        sb = lambda name, shape, dt: es.enter_context(nc.sbuf_tensor(name, shape, dt))
        NB = 8
        wt = [sb("wt%d" % i, [128, 512], F32) for i in range(NB)]
        cs = sb("cs", [128, 32], F32)
        ca = sb("ca", [128, 32], F32)
        abs_ = sb("abs", [1, NCOL], F32)
        mos = sb("mos", [1, NCOL], F32)
        ps = [es.enter_context(nc.psum_tensor("ps%d" % i, [128, 512], F32)) for i in range(2)]
        S.dma('sp', lambda e: e.dma_start(out=cs[:], in_=cv), slot='cs', writes=['cs'])
        S.dma('sp', lambda e: e.dma_start(out=abs_[:], in_=ab), slot='abs', writes=['abs'])
        S.op('act', lambda e: e.activation(out=ca[:], in_=cs[:], func=AF.Silu), reads=['cs'], writes=['ca'])
        it = 0
        for n in range(NCOL // 512):
            bank = n % 2
            for k in range(32):
                b = it % NB
                it += 1
                S.dma('sp', lambda e, b=b, k=k, n=n: e.dma_start(out=wt[b][:], in_=aw[k * 128:(k + 1) * 128, n * 512:(n + 1) * 512]),
                      slot='wt%d' % b, writes=['wt%d' % b])
                S.op('pe', lambda e, b=b, k=k, bank=bank: e.matmul(ps[bank][0:1, :], lhsT=ca[:, k:k + 1], rhs=wt[b][:], start=(k == 0), stop=(k == 31)),
                     reads=['wt%d' % b, 'ca'], writes=['ps%d' % bank])
            S.op('dve', lambda e, n=n, bank=bank: e.tensor_tensor(out=mos[0:1, n * 512:(n + 1) * 512], in0=ps[bank][0:1, :], in1=abs_[0:1, n * 512:(n + 1) * 512], op=ALU.add),
                 reads=['ps%d' % bank, 'abs'], writes=[('mos', n)])
        S.dma('sp', lambda e: e.dma_start(out=mo, in_=mos[:]), slot='out', reads=[('mos', n) for n in range(NCOL // 512)], writes=[])
        S.finish()
    return nc


D = 4096
KC = 32
S_ = 8192
T = 512
NTG = S_ // T
RMS_EPS = 1e-6


def build_b(NTG_RUN=NTG, heads=(0, 1)):
    nc = bass.Bass("TRN2", target_bir_lowering=False)
    dt_in = lambda name, shape, dt=F32: nc.dram_tensor(name, shape, dt, kind="ExternalInput").ap()
    xT = dt_in("xT", [D, S_])
    mv = dt_in("mv", [128, 2, KC])
    wq = dt_in("wq", [D, 1280])
    pw_d = dt_in("pw", [512, 256])
    psc_d = dt_in("psc", [128, 2])
    sel_d = dt_in("sel4", [128, 4])
    rc_d = dt_in("rc", [128, 2, T])
    bt_d = dt_in("bt", [128, 2, 5, T])
    off_d = dt_in("offt", [128, 2, 64])
    lam_d = dt_in("lamv", [128, 4, 64])
    sg_d = dt_in("sg", [128, 128])
    idb_d = dt_in("identb", [128, 128], BF16)
    oTc = nc.dram_tensor("oTc", [512, S_], BF16, kind="ExternalOutput").ap()
    qk_scr = nc.dram_tensor("qk_scr", [4, 128, S_], BF16, kind="Internal").ap()
    v_scr = nc.dram_tensor("v_scr", [S_, 2, 130], BF16, kind="Internal").ap()

    with ExitStack() as es:
        S = Sched(nc, es)
        ar = Arena(nc, es, "arena", 200)
        psf = [es.enter_context(nc.psum_tensor("ps%d" % i, [128, 512], F32)) for i in range(8)]
        ps7b = psf[7][:].bitcast(BF16)

        mvs = ar.alloc([2, KC], F32)
        sc1p = ar.alloc([KC], F32)
        psc = ar.alloc([2], F32)
        sel4 = ar.alloc([4], F32)
        lamv = ar.alloc([4, 64], F32)
        lamt = ar.alloc([2, 64], F32)
        lsc = ar.alloc([8], F32)
        SG = ar.alloc([128], F32)
        identb = ar.alloc([128], BF16)
        for (dst, src, nm) in [(mvs, mv, 'mvs'), (psc, psc_d, 'psc'), (sel4, sel_d, 'sel4'), (lamv, lam_d, 'lamv'),
                               (SG, sg_d, 'SG'), (identb, idb_d, 'identb')]:
            S.dma('sp', lambda e, dst=dst, src=src: e.dma_start(out=dst, in_=src), slot=nm, writes=[nm])
        S.op('dve', lambda e: e.tensor_scalar(out=sc1p, in0=mvs[:, 1, :], scalar1=1.0, scalar2=None, op0=ALU.add), reads=['mvs'], writes=['sc1p'])
        S.op('dve', lambda e: e.tensor_tensor(out=lamt[:, 0, :], in0=lamv[:, 0, :], in1=lamv[:, 1, :], op=ALU.mult), reads=['lamv'], writes=['lamt0'])
        S.op('dve', lambda e: e.tensor_tensor(out=lamt[:, 1, :], in0=lamv[:, 2, :], in1=lamv[:, 3, :], op=ALU.mult), reads=['lamv'], writes=['lamt1'])
        S.op('dve', lambda e: e.reduce_sum(out=lsc[:, 0:1], in_=lamt[:, 0, :], axis=AX.X), reads=['lamt0'], writes=['lsc0'])
        S.op('dve', lambda e: e.reduce_sum(out=lsc[:, 1:2], in_=lamt[:, 1, :], axis=AX.X), reads=['lamt1'], writes=['lsc1'])
        S.op('act', lambda e: e.activation(out=lsc[:, 2:4], in_=lsc[:, 0:2], func=AF.Exp), reads=['lsc0', 'lsc1'], writes=['lsc23'])
        S.op('dve', lambda e: e.tensor_tensor(out=lsc[:, 4:5], in0=lsc[:, 2:3], in1=lsc[:, 3:4], op=ALU.subtract), reads=['lsc23'], writes=['lsc4'])
        S.op('dve', lambda e: e.tensor_scalar(out=lsc[:, 4:5], in0=lsc[:, 4:5], scalar1=0.2, scalar2=-1.0, op0=ALU.add, op1=ALU.mult), reads=['lsc4'], writes=['lsc4'])
        S.op('dve', lambda e: e.tensor_scalar(out=SG, in0=SG, scalar1=0.8, scalar2=None, op0=ALU.mult), reads=['SG'], writes=['SG'])
        pmark = ar.mark()

        W = ar.alloc([KC, 1280], BF16)
        hT = ar.alloc([KC, T], BF16)
        NXS = 3
        xs = [ar.alloc([2, T], F32) for i in range(NXS)]
        pw = ar.alloc([4, 256], BF16)
        rc = ar.alloc([2, T], F32)
        ue = [ar.alloc([4, 528], F32) for i in range(2)]
        NPT = 2
        pA = [ar.alloc([528], F32) for i in range(NPT)]
        pB = [ar.alloc([528], F32) for i in range(NPT)]
        pS = [ar.alloc([T], F32) for i in range(NPT)]
        z = ar.alloc([4, T], BF16)
        NST = 3
        qst = [ar.alloc([T], BF16) for i in range(NST)]
        vst = [ar.alloc([2, 130], BF16) for i in range(2)]
        ost = [ar.alloc([T], BF16) for i in range(2)]

        for k in range(KC):
            S.dma('pool', lambda e, k=k: e.dma_start(out=W[:, k, :], in_=wq[k * 128:(k + 1) * 128, :]), slot='W%d' % k, writes=[('W', k)])
        S.dma('pool', lambda e: e.dma_start(out=pw, in_=pw_d.rearrange("(c p) n -> p c n", p=128)), slot='pw', writes=['pw'])
        S.dma('sp', lambda e: e.dma_start(out=rc, in_=rc_d), slot='rc', writes=['rc'])
        for i in range(2):
            S.op('dve', lambda e, i=i: e.memset(vst[i][:, :, 128:130], 1.0), writes=['vst%d' % i])
        S.op('dve', lambda e: e.memset(ue[0][:, :, 0:16], 0.0), writes=[('ue', 0, j) for j in range(4)])
        WR = [('W', k) for k in range(KC)]
        xv = xT.rearrange("(c q) t -> q c t", q=128)
        xit = 0
        qit = 0
        for tg in range(NTG_RUN):
            tsl = slice(tg * T, (tg + 1) * T)
            for c2 in range(KC // 2):
                b = xit % NXS
                xit += 1
                S.dma('sp', lambda e, b=b, c2=c2, tsl=tsl: e.dma_start(out=xs[b], in_=xv[:, c2 * 2:c2 * 2 + 2, tsl]), slot='xs%d' % b, writes=['xs%d' % b])
                for i in range(2):
                    c = c2 * 2 + i
                    S.op('act', lambda e, b=b, i=i, c=c: e.activation(out=hT[:, c, :], in_=xs[b][:, i, :], func=AF.Identity,
                                                                   scale=sc1p[:, c:c + 1], bias=mvs[:, 0, c:c + 1]),
                         reads=['xs%d' % b, 'sc1p', 'mvs'], writes=[('hT', c)])
            for k in range(KC):
                for j in range(4):
                    S.op('pe', lambda e, k=k, j=j: e.matmul(psf[j][:], lhsT=W[:, k, j * 128:(j + 1) * 128], rhs=hT[:, k, :], start=(k == 0), stop=(k == KC - 1)),
                         reads=[('W', k), ('hT', k)], writes=['ps%d' % j], inc=(k == KC - 1))
            for j in range(4):
                b = qit % NST
                qit += 1
                if j % 2 == 0:
                    S.op('act', lambda e, b=b, j=j: e.activation(out=qst[b], in_=psf[j][:], func=AF.Copy), reads=['ps%d' % j], writes=['qst%d' % b])
                else:
                    S.op('dve', lambda e, b=b, j=j: e.tensor_copy(out=qst[b], in_=psf[j][:]), reads=['ps%d' % j], writes=['qst%d' % b])
                S.dma('sp', lambda e, b=b, j=j, tsl=tsl: e.dma_start(out=qk_scr[j, :, tsl], in_=qst[b]), slot='qst%d' % b, reads=['qst%d' % b], writes=[])
            U = ue[tg % 2]
            Un = ue[(tg + 1) % 2]
            for k in range(KC):
                for j in range(4):
                    S.op('pe', lambda e, k=k, j=j: e.matmul(psf[4 + j][:], lhsT=W[:, k, 512 + j * 128:512 + (j + 1) * 128], rhs=hT[:, k, :], start=(k == 0), stop=(k == KC - 1)),
                         reads=[('W', k), ('hT', k)], writes=['ps%d' % (4 + j)], inc=(k == KC - 1))
            for j in range(4):
                ur = ('ue', tg % 2, j)
                urn = ('ue', (tg + 1) % 2, j)
                S.op('act', lambda e, j=j, U=U: e.activation(out=U[:, j, 16:528], in_=psf[4 + j][:], func=AF.Copy), reads=['ps%d' % (4 + j)], writes=[ur])
                S.op('pool', lambda e, j=j, U=U, Un=Un: e.tensor_copy(out=Un[:, j, 0:16], in_=U[:, j, 512:528]), reads=[ur], writes=[urn])
                eng = 'dve'
                i2 = j % NPT
                A, B, SS = pA[i2], pB[i2], pS[i2]
                Ar, Br, Sr = 'pA%d' % i2, 'pB%d' % i2, 'pS%d' % i2
                E_ = lambda lo, hi, U=U, j=j: U[:, j, lo:hi]
                S.op(eng, lambda e, A=A, E_=E_: e.tensor_tensor(out=A[:, 1:528], in0=E_(1, 528), in1=E_(0, 527), op=ALU.add), reads=[ur], writes=[Ar])
                S.op(eng, lambda e, A=A, SS=SS: e.tensor_scalar(out=SS, in0=A[:, 16:528], scalar1=sel4[:, 0:1], scalar2=None, op0=ALU.mult), reads=[Ar, 'sel4'], writes=[Sr])
                S.op(eng, lambda e, A=A, B=B: e.tensor_tensor(out=B[:, 3:528], in0=A[:, 3:528], in1=A[:, 1:526], op=ALU.add), reads=[Ar], writes=[Br])
                S.op(eng, lambda e, B=B, SS=SS: e.scalar_tensor_tensor(out=SS, in0=B[:, 16:528], scalar=sel4[:, 1:2], in1=SS, op0=ALU.mult, op1=ALU.add), reads=[Br, Sr, 'sel4'], writes=[Sr])
                S.op(eng, lambda e, A=A, B=B: e.tensor_tensor(out=A[:, 7:528], in0=B[:, 7:528], in1=B[:, 3:524], op=ALU.add), reads=[Br], writes=[Ar])
                S.op(eng, lambda e, A=A, SS=SS: e.scalar_tensor_tensor(out=SS, in0=A[:, 16:528], scalar=sel4[:, 2:3], in1=SS, op0=ALU.mult, op1=ALU.add), reads=[Ar, Sr, 'sel4'], writes=[Sr])
                S.op(eng, lambda e, A=A, B=B: e.tensor_tensor(out=B[:, 15:528], in0=A[:, 15:528], in1=A[:, 7:520], op=ALU.add), reads=[Ar], writes=[Br])
                S.op(eng, lambda e, B=B, SS=SS: e.scalar_tensor_tensor(out=SS, in0=B[:, 16:528], scalar=sel4[:, 3:4], in1=SS, op0=ALU.mult, op1=ALU.add), reads=[Br, Sr, 'sel4'], writes=[Sr])
                rci = 0 if tg == 0 else 1
                S.op(eng, lambda e, SS=SS, rci=rci: e.tensor_tensor(out=SS, in0=SS, in1=rc[:, rci, :], op=ALU.mult), reads=[Sr, 'rc'], writes=[Sr])
                S.op(eng, lambda e, SS=SS, j=j, E_=E_: e.tensor_tensor(out=z[:, j, :], in0=SS, in1=E_(16, 528), op=ALU.subtract), reads=[Sr, ur], writes=[('z', j)])
            for tb in range(4):
                for k in range(KC):
                    S.op('pe', lambda e, k=k, tb=tb: e.matmul(psf[tb][:, 0:256], lhsT=hT[:, k, tb * 128:(tb + 1) * 128], rhs=W[:, k, 1024:1280], start=(k == 0), stop=(k == KC - 1)),
                         reads=[('W', k), ('hT', k)], writes=['ps%d' % tb], inc=(k == KC - 1))
            for tb in range(4):
                b = tb % 2
                S.op('dve', lambda e, b=b, tb=tb: e.tensor_copy(out=vst[b][:, :, 0:128], in_=psf[tb][:, 0:256].rearrange("p (h d) -> p h d", h=2)),
                     reads=['ps%d' % tb], writes=['vst%d' % b])
                r0 = tg * T + tb * 128
                S.dma('sp', lambda e, b=b, r0=r0: e.dma_start(out=v_scr[r0:r0 + 128, :, :], in_=vst[b]), slot='vst%d' % b, reads=['vst%d' % b], writes=[])
            for oc in range(2):
                for kc in range(4):
                    S.op('pe', lambda e, oc=oc, kc=kc: e.matmul(psf[4 + oc][:], lhsT=pw[:, kc, oc * 128:(oc + 1) * 128], rhs=z[:, kc, :], start=(kc == 0), stop=(kc == 3)),
                         reads=['pw', ('z', kc)], writes=['ps%d' % (4 + oc)], inc=(kc == 3))
                S.op('act', lambda e, oc=oc: e.activation(out=ost[oc], in_=psf[4 + oc][:], func=AF.Identity, scale=psc[:, oc:oc + 1]),
                     reads=['ps%d' % (4 + oc), 'psc'], writes=['ost%d' % oc])
                S.dma('sp', lambda e, oc=oc, tsl=tsl: e.dma_start(out=oTc[256 + oc * 128:256 + (oc + 1) * 128, tsl], in_=ost[oc]), slot='ost%d' % oc, reads=['ost%d' % oc], writes=[])

        S.barrier()
        ar.reset(pmark)
        QK = ar.alloc([4, S_], BF16)
        Vx = ar.alloc([64, 2, 130], BF16)
        bt = ar.alloc([2, 5, T], F32)
        offt = ar.alloc([2, 64], F32)
        NSB = 3
        Sb = [ar.alloc([T], F32) for i in range(NSB)]
        NPT2 = 4
        PT = [ar.alloc([T], BF16) for i in range(NPT2)]
        rl = ar.alloc([4, 4], F32)
        t2 = [ar.alloc([128], F32) for i in range(2)]
        o_ = [ar.alloc([128], F32) for i in range(2)]
        junk = ar.alloc([128], F32)
        on = [ar.alloc([128], BF16) for i in range(2)]
        ost2 = [ar.alloc([T], BF16) for i in range(2)]
        for j in range(4):
            S.dma('sp', lambda e, j=j: e.dma_start(out=QK[:, j, :], in_=qk_scr[j]), slot='QK%d' % j, writes=[('QK', j)])
        S.dma('sp', lambda e: e.dma_start(out=Vx, in_=v_scr.rearrange("(b p) h d -> p b h d", p=128)), slot='Vx', writes=['Vx'])
        S.dma('sp', lambda e: e.dma_start(out=bt, in_=bt_d), slot='bt', writes=['bt'])
        S.dma('sp', lambda e: e.dma_start(out=offt, in_=off_d), slot='offt', writes=['offt'])

        accpos = {}
        lst = [(m, s) for m in range(2) for s in range(4)]
        for i, (m, s) in enumerate(lst):
            accpos[(m, s)] = (4 + i // 3, (i % 3) * 130)
        tcount = 0
        epi = 0
        for h in heads:
            for ib in range(NTG_RUN):
                for bk in (4, 5, 6):
                    S.op('dve', lambda e, bk=bk: e.memset(psf[bk][:, 0:390], 0.0), writes=['ps%d' % bk])
                tiles = [(m, jb) for jb in range(4 * ib + 4) for m in range(2)]
                SKEW = 3
                def emit_qk(ti, h=h, ib=ib, tiles=tiles, tcount=tcount):
                    m, jb = tiles[ti]
                    bank = (tcount + ti) % 4
                    S.op('pe', lambda e: e.matmul(psf[bank][:], lhsT=QK[m * 64:(m + 1) * 64, 2 + h, jb * 128:(jb + 1) * 128],
                                                  rhs=QK[m * 64:(m + 1) * 64, h, ib * T:(ib + 1) * T], start=True, stop=True),
                         reads=[('QK', 2 + h), ('QK', h)], writes=['ps%d' % bank])
                    d = jb - 4 * ib
                    var = 0 if d < 0 else d + 1
                    n = 4 * ib - jb if d < 0 else 0
                    sbi = (tcount + ti) % NSB
                    pti = (tcount + ti) % NPT2
                    S.op('dve', lambda e: e.scalar_tensor_tensor(out=Sb[sbi], in0=psf[bank][:], scalar=0.125, in1=bt[:, h, var, :], op0=ALU.mult, op1=ALU.add),
                         reads=['ps%d' % bank, 'bt'], writes=['Sb%d' % sbi])
                    S.op('act', lambda e: e.activation(out=PT[pti], in_=Sb[sbi], func=AF.Exp, bias=offt[:, h, n:n + 1], scale=1.0),
                         reads=['Sb%d' % sbi, 'offt'], writes=['PT%d' % pti])
                def emit_pv(ti, h=h, ib=ib, tiles=tiles, tcount=tcount):
                    m, jb = tiles[ti]
                    d = jb - 4 * ib
                    pti = (tcount + ti) % NPT2
                    subs = [s for s in range(4) if not (d >= 0 and s < d)]
                    for s in subs:
                        bk, c0 = accpos[(m, s)]
                        S.op('pe', lambda e, s=s, bk=bk, c0=c0: e.matmul(psf[bk][:, c0:c0 + 129], lhsT=PT[pti][:, s * 128:(s + 1) * 128], rhs=Vx[:, jb, h, 0:129],
                                                                         start=False, stop=False, skip_group_check=True),
                             reads=['PT%d' % pti, 'Vx'], writes=['ps%d' % bk], inc=(s == subs[-1]))
                nt = len(tiles)
                for ti in range(nt + SKEW):
                    if ti < nt:
                        emit_qk(ti)
                    if ti - SKEW >= 0:
                        emit_pv(ti - SKEW)
                tcount += nt
                for s in range(4):
                    b1, c1 = accpos[(0, s)]
                    b2, c2 = accpos[(1, s)]
                    e2 = epi % 2
                    epi += 1
                    A1 = psf[b1][:, c1:c1 + 129]
                    A2 = psf[b2][:, c2:c2 + 129]
                    rr = ('rl', s)
                    S.op('dve', lambda e, s=s, A1=A1: e.reciprocal(out=rl[:, s, 0:1], in_=A1[:, 128:129]), reads=['ps%d' % b1], writes=[rr])
                    S.op('dve', lambda e, s=s, A2=A2: e.reciprocal(out=rl[:, s, 1:2], in_=A2[:, 128:129]), reads=['ps%d' % b2], writes=[rr])
                    S.op('dve', lambda e, s=s: e.tensor_tensor(out=rl[:, s, 1:2], in0=rl[:, s, 1:2], in1=lsc[:, 4:5], op=ALU.mult), reads=[rr, 'lsc4'], writes=[rr])
                    S.op('act', lambda e, s=s, A2=A2, e2=e2: e.activation(out=t2[e2], in_=A2[:, 0:128], func=AF.Identity, scale=rl[:, s, 1:2]),
                         reads=['ps%d' % b2, rr], writes=['t2%d' % e2])
                    S.op('dve', lambda e, s=s, A1=A1, e2=e2: e.scalar_tensor_tensor(out=o_[e2], in0=A1[:, 0:128], scalar=rl[:, s, 0:1], in1=t2[e2], op0=ALU.mult, op1=ALU.add),
                         reads=['ps%d' % b1, rr, 't2%d' % e2], writes=['o%d' % e2])
                    S.op('act', lambda e, s=s, e2=e2: e.activation(out=junk, in_=o_[e2], func=AF.Square, accum_out=rl[:, s, 2:3]),
                         reads=['o%d' % e2], writes=['junk', ('rl2', s)])
                    S.op('dve', lambda e, s=s: e.tensor_scalar(out=rl[:, s, 2:3], in0=rl[:, s, 2:3], scalar1=1.0 / 128, scalar2=RMS_EPS, op0=ALU.mult, op1=ALU.add),
                         reads=[('rl2', s)], writes=[('rl2', s)])
                    S.op('act', lambda e, s=s: e.activation(out=rl[:, s, 3:4], in_=rl[:, s, 2:3], func=AF.Sqrt), reads=[('rl2', s)], writes=[('rl3', s)])
                    S.op('dve', lambda e, s=s: e.reciprocal(out=rl[:, s, 3:4], in_=rl[:, s, 3:4]), reads=[('rl3', s)], writes=[('rl3', s)])
                    S.op('dve', lambda e, s=s, e2=e2: e.scalar_tensor_tensor(out=on[e2], in0=o_[e2], scalar=rl[:, s, 3:4], in1=SG, op0=ALU.mult, op1=ALU.mult),
                         reads=['o%d' % e2, ('rl3', s), 'SG'], writes=['on%d' % e2])
                    S.op('pe', lambda e, s=s, e2=e2: e.transpose(out=ps7b[:, s * 128:(s + 1) * 128], in_=on[e2], identity=identb),
                         reads=['on%d' % e2, 'identb'], writes=['ps7'])
                ob = (epi // 4) % 2
                S.op('act', lambda e, ob=ob: e.activation(out=ost2[ob], in_=ps7b[:, 0:512], func=AF.Copy), reads=['ps7'], writes=['ost2%d' % ob])
                S.dma('sp', lambda e, ob=ob, h=h, ib=ib: e.dma_start(out=oTc[h * 128:(h + 1) * 128, ib * T:(ib + 1) * T], in_=ost2[ob]),
                      slot='ost2%d' % ob, reads=['ost2%d' % ob], writes=[])
        S.finish()
        print("stage b ninst", S.ninst)
    return nc


D = 4096
KC = 32
S_ = 8192
T = 512
NTG = S_ // T
RMS_EPS = 1e-6
CH = 128


def build_d(NTG_RUN=NTG, heads=(0, 1, 2, 3)):
    nc = bass.Bass("TRN2", target_bir_lowering=False)
    dt_in = lambda name, shape, dt=F32: nc.dram_tensor(name, shape, dt, kind="ExternalInput").ap()
    xT = dt_in("xT", [D, S_])
    mv = dt_in("mv", [128, 2, KC])
    wd = dt_in("wd", [D, 2048])
    lbr_d = dt_in("lbr", [128, 2, 4])
    gn_d = dt_in("gn4", [128, 512])
    mask_d = dt_in("mask01", [128, 128])
    idb_d = dt_in("identb", [128, 128], BF16)
    oTd = nc.dram_tensor("oTd", [512, S_], BF16, kind="ExternalOutput").ap()
    qt_scr = nc.dram_tensor("qt_scr", [4, 128, S_], BF16, kind="Internal").ap()
    kt_scr = nc.dram_tensor("kt_scr", [4, 128, S_], BF16, kind="Internal").ap()
    kh_scr = nc.dram_tensor("kh_scr", [S_, 4, 128], BF16, kind="Internal").ap()
    v_scr = nc.dram_tensor("v_scr", [S_, 4, 128], BF16, kind="Internal").ap()
    sg_scr = nc.dram_tensor("sg_scr", [S_, 4, 128], BF16, kind="Internal").ap()
    NCHK = S_ // CH

    with ExitStack() as es:
        S = Sched(nc, es)
        ar = Arena(nc, es, "arena", 200)
        psf = [es.enter_context(nc.psum_tensor("ps%d" % i, [128, 512], F32)) for i in range(8)]
        psb = [psf[i][:].bitcast(BF16) for i in range(8)]

        mvs = ar.alloc([2, KC], F32)
        sc1p = ar.alloc([KC], F32)
        lbr = ar.alloc([2, 4], F32)
        lbt = ar.alloc([3, 4], F32)
        GN4 = ar.alloc([512], F32)
        mask01 = ar.alloc([128], F32)
        identb = ar.alloc([128], BF16)
        ones = ar.alloc([128], F32)
        dtab = ar.alloc([2, 4, NCHK], F32)
        for (dst, src, nm) in [(mvs, mv, 'mvs'), (lbr, lbr_d, 'lbr'), (GN4, gn_d, 'GN4'), (mask01, mask_d, 'mask01'), (identb, idb_d, 'identb')]:
            S.dma('sp', lambda e, dst=dst, src=src: e.dma_start(out=dst, in_=src), slot=nm, writes=[nm])
        S.op('dve', lambda e: e.memset(ones, 1.0), writes=['ones'])
        S.op('dve', lambda e: e.tensor_scalar(out=sc1p, in0=mvs[:, 1, :], scalar1=1.0, scalar2=None, op0=ALU.add), reads=['mvs'], writes=['sc1p'])
        S.op('dve', lambda e: e.tensor_tensor(out=lbt[:, 2, :], in0=lbr[:, 0, :], in1=lbr[:, 1, :], op=ALU.subtract), reads=['lbr'], writes=['lbt2'])
        S.op('act', lambda e: e.activation(out=lbt[:, 2, :], in_=lbt[:, 2, :], func=AF.Exp), reads=['lbt2'], writes=['lbt2'])
        S.op('dve', lambda e: e.tensor_scalar(out=lbt[:, 2, :], in0=lbt[:, 2, :], scalar1=1.0, scalar2=None, op0=ALU.add), reads=['lbt2'], writes=['lbt2'])
        S.op('dve', lambda e: e.reciprocal(out=lbt[:, 0, :], in_=lbt[:, 2, :]), reads=['lbt2'], writes=['lbt0'])
        S.op('dve', lambda e: e.tensor_scalar(out=lbt[:, 1, :], in0=lbt[:, 0, :], scalar1=-1.0, scalar2=1.0, op0=ALU.mult, op1=ALU.add), reads=['lbt0'], writes=['lbt1'])
        LB = ['lbt0', 'lbt1']
        pmark = ar.mark()

        W = ar.alloc([KC, 1024], BF16)
        hT = ar.alloc([KC, T], BF16)
        NXS = 3
        xs = [ar.alloc([2, T], F32) for i in range(NXS)]
        NTP = 2
        tmp = [[ar.alloc([T], F32) for i in range(6)] for p in range(NTP)]
        NST = 3
        qst = [ar.alloc([T], BF16) for i in range(NST)]
        khfm = [ar.alloc([T], BF16) for i in range(2)]
        khst = [ar.alloc([4, 128], BF16) for i in range(2)]
        vst = [ar.alloc([T], BF16) for i in range(2)]
        sgst = [ar.alloc([T], BF16) for i in range(2)]
        xv = xT.rearrange("(c q) t -> q c t", q=128)
        xit = [0]
        qit = [0]

        def load_W(col0):
            for k in range(KC):
                S.dma('pool', lambda e, k=k: e.dma_start(out=W[:, k, :], in_=wd[k * 128:(k + 1) * 128, col0:col0 + 1024]), slot='W%d' % k, writes=[('W', k)])

        def load_mod(tg):
            tsl = slice(tg * T, (tg + 1) * T)
            for c2 in range(KC // 2):
                b = xit[0] % NXS
                xit[0] += 1
                S.dma('sp', lambda e, b=b, c2=c2, tsl=tsl: e.dma_start(out=xs[b], in_=xv[:, c2 * 2:c2 * 2 + 2, tsl]), slot='xs%d' % b, writes=['xs%d' % b])
                for i in range(2):
                    c = c2 * 2 + i
                    S.op('act', lambda e, b=b, i=i, c=c: e.activation(out=hT[:, c, :], in_=xs[b][:, i, :], func=AF.Identity,
                                                                   scale=sc1p[:, c:c + 1], bias=mvs[:, 0, c:c + 1]),
                         reads=['xs%d' % b, 'sc1p', 'mvs'], writes=[('hT', c)])

        load_W(0)
        hcount = 0
        for tg in range(NTG_RUN):
            tsl = slice(tg * T, (tg + 1) * T)
            load_mod(tg)
            for grp in range(2):
                for k in range(KC):
                    for j in range(4):
                        bank = grp * 4 + j
                        S.op('pe', lambda e, k=k, j=j, grp=grp, bank=bank: e.matmul(psf[bank][:], lhsT=W[:, k, grp * 512 + j * 128:grp * 512 + (j + 1) * 128], rhs=hT[:, k, :],
                                                                                start=(k == 0), stop=(k == KC - 1)),
                             reads=[('W', k), ('hT', k)], writes=['ps%d' % bank], inc=(k == KC - 1))
            for j in range(4):
                pp = hcount % NTP
                hcount += 1
                t0, t1, t2, t3, t4, t5 = tmp[pp]
                R = lambda i, pp=pp: 't%d_%d' % (pp, i)
                qb, fb = 'ps%d' % j, 'ps%d' % (4 + j)
                qps, fps = psf[j], psf[4 + j]
                S.op('act', lambda e, t0=t0, fps=fps: e.activation(out=t0, in_=fps[:], func=AF.Exp, scale=-1.0), reads=[fb], writes=[R(0)])
                S.op('dve', lambda e, t0=t0: e.tensor_scalar(out=t0, in0=t0, scalar1=1.0, scalar2=None, op0=ALU.add), reads=[R(0)], writes=[R(0)])
                S.op('dve', lambda e, t0=t0: e.reciprocal(out=t0, in_=t0), reads=[R(0)], writes=[R(0)])
                S.op('dve', lambda e, t0=t0, t1=t1, j=j: e.tensor_scalar(out=t1, in0=t0, scalar1=lbt[:, 1, j:j + 1], scalar2=lbt[:, 0, j:j + 1], op0=ALU.mult, op1=ALU.add),
                     reads=[R(0)] + LB, writes=[R(1)])
                S.op('dve', lambda e, t1=t1, t2=t2: e.tensor_scalar(out=t2, in0=t1, scalar1=-1.0, scalar2=1.0, op0=ALU.mult, op1=ALU.add), reads=[R(1)], writes=[R(2)])
                S.op('act', lambda e, t0=t0, t1=t1: e.activation(out=t0, in_=t1, func=AF.Ln), reads=[R(1)], writes=[R(0)])
                for c in range(4):
                    cs = slice(c * CH, (c + 1) * CH)
                    S.op('dve', lambda e, t0=t0, t3=t3, cs=cs: e.tensor_tensor_scan(out=t3[:, cs], data0=ones, data1=t0[:, cs], initial=0.0, op0=ALU.mult, op1=ALU.add),
                         reads=[R(0), 'ones'], writes=[R(3)])
                S.op('act', lambda e, t4=t4, qps=qps: e.activation(out=t4, in_=qps[:], func=AF.Exp, scale=-1.0), reads=[qb], writes=[R(4)])
                S.op('dve', lambda e, t4=t4: e.tensor_scalar(out=t4, in0=t4, scalar1=1.0, scalar2=None, op0=ALU.add), reads=[R(4)], writes=[R(4)])
                S.op('dve', lambda e, t4=t4: e.reciprocal(out=t4, in_=t4), reads=[R(4)], writes=[R(4)])
                S.op('dve', lambda e, t4=t4, qps=qps: e.tensor_tensor(out=t4, in0=qps[:], in1=t4, op=ALU.mult), reads=[R(4), qb], writes=[R(4)])
                B3 = t3.rearrange("p (c t) -> p c t", c=4)
                Bref = t3[:, 63:T:CH].unsqueeze(2).to_broadcast([128, 4, CH])
                Blast = t3[:, CH - 1:T:CH].unsqueeze(2).to_broadcast([128, 4, CH])
                t53 = t5.rearrange("p (c t) -> p c t", c=4)
                S.op('dve', lambda e, B3=B3, Bref=Bref, t53=t53: e.tensor_tensor(out=t53, in0=B3, in1=Bref, op=ALU.subtract), reads=[R(3)], writes=[R(5)])
                S.op('act', lambda e, t5=t5: e.activation(out=t5, in_=t5, func=AF.Exp), reads=[R(5)], writes=[R(5)])
                b = qit[0] % NST
                qit[0] += 1
                S.op('dve', lambda e, t4=t4, t5=t5, b=b: e.tensor_tensor(out=qst[b], in0=t4, in1=t5, op=ALU.mult), reads=[R(4), R(5)], writes=['qst%d' % b])
                S.dma('sp', lambda e, b=b, j=j, tsl=tsl: e.dma_start(out=qt_scr[j, :, tsl], in_=qst[b]), slot='qst%d' % b, reads=['qst%d' % b], writes=[])
                S.op('dve', lambda e, B3=B3, Bref=Bref, t53=t53: e.tensor_tensor(out=t53, in0=Bref, in1=B3, op=ALU.subtract), reads=[R(3)], writes=[R(5)])
                S.op('act', lambda e, t5=t5: e.activation(out=t5, in_=t5, func=AF.Exp), reads=[R(5)], writes=[R(5)])
                b = qit[0] % NST
                qit[0] += 1
                S.op('dve', lambda e, t2=t2, t5=t5, b=b: e.scalar_tensor_tensor(out=qst[b], in0=t5, scalar=1e30, in1=t2, op0=ALU.min, op1=ALU.mult), reads=[R(2), R(5)], writes=['qst%d' % b])
                S.dma('sp', lambda e, b=b, j=j, tsl=tsl: e.dma_start(out=kt_scr[j, :, tsl], in_=qst[b]), slot='qst%d' % b, reads=['qst%d' % b], writes=[])
                S.op('dve', lambda e, B3=B3, Blast=Blast, t53=t53: e.tensor_tensor(out=t53, in0=Blast, in1=B3, op=ALU.subtract), reads=[R(3)], writes=[R(5)])
                S.op('act', lambda e, t5=t5: e.activation(out=t5, in_=t5, func=AF.Exp), reads=[R(5)], writes=[R(5)])
                kb = hcount % 2
                S.op('dve', lambda e, t2=t2, t5=t5, kb=kb: e.tensor_tensor(out=khfm[kb], in0=t2, in1=t5, op=ALU.mult), reads=[R(2), R(5)], writes=['khfm%d' % kb])
                for c in range(4):
                    S.op('pe', lambda e, c=c, kb=kb, j=j: e.transpose(out=psb[j][:, c * 128:(c + 1) * 128], in_=khfm[kb][:, c * CH:(c + 1) * CH], identity=identb),
                         reads=['khfm%d' % kb, 'identb'], writes=[qb])
                S.op('act', lambda e, kb=kb, j=j: e.activation(out=khst[kb], in_=psb[j][:, 0:512].rearrange("p (c d) -> p c d", c=4), func=AF.Copy),
                     reads=[qb], writes=['khst%d' % kb])
                S.dma('sp', lambda e, kb=kb, j=j, tg=tg: e.dma_start(out=kh_scr.rearrange("(b p) h d -> p b h d", p=128)[:, tg * 4:(tg + 1) * 4, j, :], in_=khst[kb]),
                      slot='khst%d' % kb, reads=['khst%d' % kb], writes=[])
                S.op('act', lambda e, t3=t3, j=j, tg=tg: e.activation(out=dtab[:, 0, j, tg * 4:(tg + 1) * 4], in_=t3[:, CH - 1:T:CH], func=AF.Exp), reads=[R(3)], writes=[('dtab', j, tg)])
                S.op('act', lambda e, t3=t3, j=j, tg=tg: e.activation(out=dtab[:, 1, j, tg * 4:(tg + 1) * 4], in_=t3[:, 63:T:CH], func=AF.Exp), reads=[R(3)], writes=[('dtab', j, tg)])

        load_W(1024)
        vit = 0
        for tg in range(NTG_RUN):
            load_mod(tg)
            for k in range(KC):
                for tb in range(4):
                    for grp in range(2):
                        bank = grp * 4 + tb
                        S.op('pe', lambda e, k=k, tb=tb, grp=grp, bank=bank: e.matmul(psf[bank][:], lhsT=hT[:, k, tb * 128:(tb + 1) * 128], rhs=W[:, k, grp * 512:(grp + 1) * 512],
                                                                                 start=(k == 0), stop=(k == KC - 1)),
                             reads=[('W', k), ('hT', k)], writes=['ps%d' % bank], inc=(k == KC - 1))
            for tb in range(4):
                vb = vit % 2
                vit += 1
                r0 = tg * T + tb * 128
                S.op('act', lambda e, vb=vb, tb=tb: e.activation(out=vst[vb], in_=psf[tb][:], func=AF.Copy), reads=['ps%d' % tb], writes=['vst%d' % vb])
                S.dma('sp', lambda e, vb=vb, r0=r0: e.dma_start(out=v_scr[r0:r0 + 128, :, :].rearrange("p h d -> p (h d)"), in_=vst[vb]), slot='vst%d' % vb, reads=['vst%d' % vb], writes=[])
                pp = vit % NTP
                t0 = tmp[pp][0]
                tr = 't%d_0' % pp
                gb = 'ps%d' % (4 + tb)
                S.op('act', lambda e, t0=t0, tb=tb: e.activation(out=t0, in_=psf[4 + tb][:], func=AF.Exp, scale=-1.0), reads=[gb], writes=[tr])
                S.op('dve', lambda e, t0=t0: e.tensor_scalar(out=t0, in0=t0, scalar1=1.0, scalar2=None, op0=ALU.add), reads=[tr], writes=[tr])
                S.op('dve', lambda e, t0=t0: e.reciprocal(out=t0, in_=t0), reads=[tr], writes=[tr])
                S.op('dve', lambda e, t0=t0, tb=tb: e.tensor_tensor(out=t0, in0=psf[4 + tb][:], in1=t0, op=ALU.mult), reads=[tr, gb], writes=[tr])
                S.op('dve', lambda e, t0=t0, vb=vb: e.tensor_tensor(out=sgst[vb], in0=t0, in1=GN4, op=ALU.mult), reads=[tr, 'GN4'], writes=['sgst%d' % vb])
                S.dma('sp', lambda e, vb=vb, r0=r0: e.dma_start(out=sg_scr[r0:r0 + 128, :, :].rearrange("p h d -> p (h d)"), in_=sgst[vb]), slot='sgst%d' % vb, reads=['sgst%d' % vb], writes=[])

        S.barrier()
        ar.reset(pmark)
        NC_RUN = NTG_RUN * 4
        QT = [ar.alloc([S_], BF16) for i in range(2)]
        KT = [ar.alloc([S_], BF16) for i in range(2)]
        KH = [ar.alloc([NCHK, 128], BF16) for i in range(2)]
        VV = [ar.alloc([NCHK, 128], BF16) for i in range(2)]
        SG = [ar.alloc([NCHK, 128], BF16) for i in range(2)]
        St = ar.alloc([128], F32)
        Sbf = [ar.alloc([128], BF16) for i in range(2)]
        ATs = [ar.alloc([128], BF16) for i in range(2)]
        sc = ar.alloc([4, 4], F32)
        junk = ar.alloc([128], F32)
        on = [ar.alloc([128], BF16) for i in range(2)]
        ost = [ar.alloc([T], BF16) for i in range(2)]
        for i in range(2):
            S.op('dve', lambda e, i=i: e.memset(ATs[i], 0.0), writes=['ATs%d' % i])
        tokv = lambda scr: scr.rearrange("(b p) h d -> p b h d", p=128)
        oi = 0
        for hi, h in enumerate(heads):
            hb = hi % 2
            S.dma('sp', lambda e, h=h, hb=hb: e.dma_start(out=QT[hb], in_=qt_scr[h]), slot='QT%d' % hb, writes=['QT%d' % hb])
            S.dma('sp', lambda e, h=h, hb=hb: e.dma_start(out=KT[hb], in_=kt_scr[h]), slot='KT%d' % hb, writes=['KT%d' % hb])
            S.dma('sp', lambda e, h=h, hb=hb: e.dma_start(out=KH[hb], in_=tokv(kh_scr)[:, :, h, :]), slot='KH%d' % hb, writes=['KH%d' % hb])
            S.dma('sp', lambda e, h=h, hb=hb: e.dma_start(out=VV[hb], in_=tokv(v_scr)[:, :, h, :]), slot='VV%d' % hb, writes=['VV%d' % hb])
            S.dma('sp', lambda e, h=h, hb=hb: e.dma_start(out=SG[hb], in_=tokv(sg_scr)[:, :, h, :]), slot='SG%d' % hb, writes=['SG%d' % hb])
            S.op('dve', lambda e: e.memset(St, 0.0), writes=['St'])
            S.op('dve', lambda e: e.memset(Sbf[0], 0.0), writes=['Sbf0'])

            def emit_AU(c, h=h, hb=hb):
                cs = slice(c * CH, (c + 1) * CH)
                a = c % 2
                S.op('pe', lambda e: e.matmul(psf[a][:, 0:128], lhsT=KT[hb][:, cs], rhs=QT[hb][:, cs], start=True, stop=True),
                     reads=['KT%d' % hb, 'QT%d' % hb], writes=['ps%d' % a])
                S.op('dve', lambda e: e.copy_predicated(out=ATs[a], mask=mask01.bitcast(mybir.dt.uint32), data=psf[a][:, 0:128]), reads=['ps%d' % a, 'mask01', 'ATs%d' % a], writes=['ATs%d' % a])
                S.op('pe', lambda e: e.matmul(psf[2 + a][:, 0:128], lhsT=KH[hb][:, c, :], rhs=VV[hb][:, c, :], start=True, stop=True),
                     reads=['KH%d' % hb, 'VV%d' % hb], writes=['ps%d' % (2 + a)])

            emit_AU(0)
            for c in range(NC_RUN):
                a = c % 2
                cs = slice(c * CH, (c + 1) * CH)
                if c + 1 < NC_RUN:
                    emit_AU(c + 1)
                ob = 'ps%d' % (4 + a)
                S.op('pe', lambda e, a=a, c=c, hb=hb: e.matmul(psf[4 + a][:, 0:128], lhsT=ATs[a], rhs=VV[hb][:, c, :], start=True, stop=False),
                     reads=['ATs%d' % a, 'VV%d' % hb], writes=[ob], inc=False)
                S.op('pe', lambda e, a=a, cs=cs, hb=hb: e.matmul(psf[4 + a][:, 0:128], lhsT=QT[hb][:, cs], rhs=Sbf[a], start=False, stop=True),
                     reads=['QT%d' % hb, 'Sbf%d' % a], writes=[ob])
                S.op('dve', lambda e, a=a, c=c, h=h: e.scalar_tensor_tensor(out=St, in0=St, scalar=dtab[:, 0, h, c:c + 1], in1=psf[2 + a][:, 0:128], op0=ALU.mult, op1=ALU.add),
                     reads=['St', 'ps%d' % (2 + a), ('dtab', h, c // 4)], writes=['St'])
                if c + 1 < NC_RUN:
                    S.op('act', lambda e, a=a, c=c, h=h: e.activation(out=Sbf[1 - a], in_=St, func=AF.Identity, scale=dtab[:, 1, h, c + 1:c + 2]),
                         reads=['St', ('dtab', h, (c + 1) // 4)], writes=['Sbf%d' % (1 - a)])
                s4 = c % 4
                S.op('act', lambda e, a=a, s4=s4: e.activation(out=junk, in_=psf[4 + a][:, 0:128], func=AF.Square, accum_out=sc[:, s4, 0:1]), reads=[ob], writes=['junk', ('sc0', s4)])
                S.op('dve', lambda e, s4=s4: e.tensor_scalar(out=sc[:, s4, 0:1], in0=sc[:, s4, 0:1], scalar1=1.0 / 128, scalar2=RMS_EPS, op0=ALU.mult, op1=ALU.add),
                     reads=[('sc0', s4)], writes=[('sc0', s4)])
                S.op('act', lambda e, s4=s4: e.activation(out=sc[:, s4, 1:2], in_=sc[:, s4, 0:1], func=AF.Sqrt), reads=[('sc0', s4)], writes=[('sc1', s4)])
                S.op('dve', lambda e, s4=s4: e.reciprocal(out=sc[:, s4, 1:2], in_=sc[:, s4, 1:2]), reads=[('sc1', s4)], writes=[('sc1', s4)])
                S.op('dve', lambda e, a=a, s4=s4, c=c, hb=hb: e.scalar_tensor_tensor(out=on[a], in0=psf[4 + a][:, 0:128], scalar=sc[:, s4, 1:2], in1=SG[hb][:, c, :], op0=ALU.mult, op1=ALU.mult),
                     reads=[ob, ('sc1', s4), 'SG%d' % hb], writes=['on%d' % a])
                tbk = 6 + (c // 4) % 2
                S.op('pe', lambda e, a=a, s4=s4, tbk=tbk: e.transpose(out=psb[tbk][:, s4 * 128:(s4 + 1) * 128], in_=on[a], identity=identb),
                     reads=['on%d' % a, 'identb'], writes=['ps%d' % tbk])
                if s4 == 3:
                    o2 = oi % 2
                    oi += 1
                    g4 = c // 4
                    S.op('act', lambda e, o2=o2, tbk=tbk: e.activation(out=ost[o2], in_=psb[tbk][:, 0:512], func=AF.Copy), reads=['ps%d' % tbk], writes=['ost%d' % o2])
                    S.dma('sp', lambda e, o2=o2, h=h, g4=g4: e.dma_start(out=oTd[h * 128:(h + 1) * 128, g4 * T:(g4 + 1) * T], in_=ost[o2]),
                          slot='ost%d' % o2, reads=['ost%d' % o2], writes=[])
        S.finish()
        print("stage d ninst", S.ninst)
    return nc


D = 4096
KC = 32
T = 512
ALPHA = 4 ** 0.25
LN_EPS = 1e-5


def build_ff(moe, TOK=1024, n_groups_limit=None):
    nc = bass.Bass("TRN2", target_bir_lowering=False)
    NPASS = TOK // T
    oT = nc.dram_tensor("oT", [D, TOK], BF16, kind="ExternalInput").ap()
    xT = nc.dram_tensor("xT", [D, TOK], F32, kind="ExternalInput").ap()
    pv = nc.dram_tensor("pv", [128, 8, KC], F32, kind="ExternalInput").ap()
    w_o = nc.dram_tensor("w_o", [D, D], F32, kind="ExternalInput").ap()
    ones_d = nc.dram_tensor("ones", [128, 128], F32, kind="ExternalInput").ap()
    if moe:
        NE, H = 8, 4096
        w_in = nc.dram_tensor("w_in", [NE, D, 2 * H], F32, kind="ExternalInput").ap()
        w_o2 = nc.dram_tensor("w_o2", [NE, H, D], F32, kind="ExternalInput").ap()
        rw_d = nc.dram_tensor("rw", [128, KC, 8], F32, kind="ExternalInput").ap()
        sel_d = nc.dram_tensor("sel", [8, 8, 128], F32, kind="ExternalInput").ap()
        ident_d = nc.dram_tensor("ident", [128, 128], F32, kind="ExternalInput").ap()
        groups = []
        for e in range(NE):
            groups.append(dict(win=w_in[e].rearrange("k (s c) -> k s c", s=2), wo=w_o2[e],
                               pairs=list(range(16)), gate=e))
    else:
        H = 11008
        w_in = nc.dram_tensor("w_in", [D, 2 * H], F32, kind="ExternalInput").ap()
        w_o2 = nc.dram_tensor("w_o2", [H, D], F32, kind="ExternalInput").ap()
        win_v = w_in.rearrange("k (s c) -> k s c", s=2)
        groups = [dict(win=win_v, wo=w_o2, pairs=list(range(0, 16)), gate=None),
                  dict(win=win_v, wo=w_o2, pairs=list(range(16, 32)), gate=None),
                  dict(win=win_v, wo=w_o2, pairs=list(range(32, 43)), gate=None)]
    if n_groups_limit is not None:
        groups = groups[:n_groups_limit]
    yT = nc.dram_tensor("yT", [D, TOK], F32, kind="ExternalOutput").ap()

    with ExitStack() as es:
        S = Sched(nc, es)
        sb = lambda name, shape, dt: es.enter_context(nc.sbuf_tensor(name, shape, dt))
        acc = sb("acc", [128, KC, T], F32)
        actT = sb("actT", [128, KC, T], BF16)
        h2T = sb("h2T", [128, KC, T], BF16)
        NB = 8
        wt = [sb("wt%d" % i, [128, 512], BF16) for i in range(NB)]
        pvs = sb("pvs", [128, 8, KC], F32)
        dv = sb("dv", [128, 6, KC], F32)
        ones = sb("ones_s", [128, 128], F32)
        s1 = sb("s1", [128, T], F32)
        s2 = sb("s2", [128, T], F32)
        mean = sb("mean", [128, T], F32)
        msq = sb("msq", [128, T], F32)
        rstd = sb("rstd", [128, T], F32)
        NTB = 3
        tb_ = [sb("tb%d" % i, [128, T], F32) for i in range(NTB)]
        sa_ = [sb("sa%d" % i, [128, T], F32) for i in range(NTB)]
        yo_ = [sb("yo%d" % i, [128, T], F32) for i in range(NTB)]
        ps = [es.enter_context(nc.psum_tensor("ps%d" % i, [128, 512], F32)) for i in range(8)]
        if moe:
            rw = sb("rw_s", [128, KC, 8], F32)
            sel = sb("sel_s", [8, 8, 128], F32)
            ident = sb("ident_s", [128, 128], F32)
            h2f_ = [sb("h2f%d" % i, [128, T], F32) for i in range(2)]
            G_ = [sb("G%d" % i, [128, T], F32) for i in range(2)]
            bg_ = [sb("bg%d" % i, [128, T], F32) for i in range(NTB)]
            gT = sb("gT", [8, T], F32)
            Lsb = sb("Lsb", [128, 4, 8], F32)
            m8 = sb("m8", [128, 4, 8], F32)
            nv1 = sb("nv1", [128, 4], F32)
            msk = sb("msk", [128, 4, 8], F32)
            ex = sb("ex", [128, 4, 8], F32)
            me = sb("me", [128, 4, 8], F32)
            den = sb("den", [128, 4], F32)
            gsb = sb("gsb", [128, 4, 8], F32)

        S.dma('sp', lambda e: e.dma_start(out=pvs[:], in_=pv), slot='pvs', writes=['pvs'])
        S.dma('sp', lambda e: e.dma_start(out=ones[:], in_=ones_d), slot='ones', writes=['ones'])
        if moe:
            S.dma('sp', lambda e: e.dma_start(out=rw[:], in_=rw_d), slot='rw', writes=['rw'])
            S.dma('sp', lambda e: e.dma_start(out=sel[:], in_=sel_d), slot='sel', writes=['sel'])
            S.dma('sp', lambda e: e.dma_start(out=ident[:], in_=ident_d), slot='ident', writes=['ident'])
        V = lambda i: pvs[:, i, :]
        S.op('dve', lambda e: e.tensor_scalar(out=dv[:, 0, :], in0=V(0), scalar1=1.0, scalar2=None, op0=ALU.add), reads=['pvs'], writes=['dv0'])
        S.op('dve', lambda e: e.tensor_scalar(out=dv[:, 5, :], in0=V(2), scalar1=1.0, scalar2=None, op0=ALU.add), reads=['pvs'], writes=['dv5'])
        S.op('dve', lambda e: e.tensor_tensor(out=dv[:, 1, :], in0=V(4), in1=dv[:, 5, :], op=ALU.mult), reads=['pvs', 'dv5'], writes=['dv1'])
        S.op('dve', lambda e: e.tensor_tensor(out=dv[:, 2, :], in0=V(5), in1=dv[:, 5, :], op=ALU.mult), reads=['pvs', 'dv5'], writes=['dv2'])
        S.op('dve', lambda e: e.tensor_tensor(out=dv[:, 2, :], in0=dv[:, 2, :], in1=V(1), op=ALU.add), reads=['pvs', 'dv2'], writes=['dv2'])
        S.op('dve', lambda e: e.tensor_scalar(out=dv[:, 3, :], in0=V(4), scalar1=ALPHA, scalar2=None, op0=ALU.mult), reads=['pvs'], writes=['dv3'])
        S.op('dve', lambda e: e.tensor_scalar(out=dv[:, 4, :], in0=V(5), scalar1=ALPHA, scalar2=None, op0=ALU.mult), reads=['pvs'], writes=['dv4'])
        S.op('dve', lambda e: e.tensor_scalar(out=dv[:, 5, :], in0=V(3), scalar1=1.0, scalar2=None, op0=ALU.add), reads=['pvs', 'dv1', 'dv2'], writes=['dv5'])
        DVR = ['dv0', 'dv1', 'dv2', 'dv3', 'dv4', 'dv5', 'pvs']

        witer = [0]

        def wtile_load(src_ap, three=False):
            b = witer[0] % NB
            witer[0] += 1
            if three:
                S.dma('pool', lambda e: e.dma_start(out=wt[b][:].rearrange("p (s c) -> p s c", s=2), in_=src_ap),
                      slot='wt%d' % b, writes=['wt%d' % b])
            else:
                S.dma('pool', lambda e: e.dma_start(out=wt[b][:], in_=src_ap), slot='wt%d' % b, writes=['wt%d' % b])
            return b

        bankset = [0]

        def gemm_group(tiles, rhs_of, rhs_res, evac):
            base = (bankset[0] % 2) * 4
            bankset[0] += 1
            nk = len(tiles)
            for ki, (src, three) in enumerate(tiles):
                b = wtile_load(src, three)
                for j in range(4):
                    bank = base + j
                    S.op('pe', lambda e, b=b, j=j, ki=ki, bank=bank: e.matmul(
                        ps[bank][:], lhsT=wt[b][:, j * 128:(j + 1) * 128], rhs=rhs_of(ki),
                        start=(ki == 0), stop=(ki == nk - 1)),
                        reads=['wt%d' % b] + rhs_res(ki), writes=['ps%d' % bank], inc=(j == 3))
            for j in range(4):
                evac(j, base + j)

        def layernorm(final, ps_pass):
            for c in range(KC):
                if c == 0:
                    S.op('dve', lambda e: e.tensor_copy(out=s1[:], in_=acc[:, 0, :]), reads=[('acc', 0)], writes=['s1'])
                    S.op('act', lambda e: e.activation(out=s2[:], in_=acc[:, 0, :], func=AF.Square), reads=[('acc', 0)], writes=['s2'])
                else:
                    t = tb_[c % NTB]
                    tr = 'tb%d' % (c % NTB)
                    S.op('act', lambda e, c=c, t=t: e.activation(out=t[:], in_=acc[:, c, :], func=AF.Square), reads=[('acc', c)], writes=[tr])
                    S.op('dve', lambda e, c=c: e.tensor_tensor(out=s1[:], in0=s1[:], in1=acc[:, c, :], op=ALU.add), reads=[('acc', c), 's1'], writes=['s1'])
                    S.op('dve', lambda e, t=t: e.tensor_tensor(out=s2[:], in0=s2[:], in1=t[:], op=ALU.add), reads=[tr, 's2'], writes=['s2'])
            S.op('pe', lambda e: e.matmul(ps[4][:], lhsT=ones[:], rhs=s1[:], start=True, stop=True), reads=['ones', 's1'], writes=['ps4'])
            S.op('pe', lambda e: e.matmul(ps[5][:], lhsT=ones[:], rhs=s2[:], start=True, stop=True), reads=['ones', 's2'], writes=['ps5'])
            S.op('act', lambda e: e.activation(out=mean[:], in_=ps[4][:], func=AF.Copy, scale=1.0 / D), reads=['ps4'], writes=['mean'])
            S.op('dve', lambda e: e.tensor_tensor(out=msq[:], in0=mean[:], in1=mean[:], op=ALU.mult), reads=['mean'], writes=['msq'])
            S.op('dve', lambda e: e.scalar_tensor_tensor(out=msq[:], in0=ps[5][:], scalar=1.0 / D, in1=msq[:], op0=ALU.mult, op1=ALU.subtract),
                 reads=['ps5', 'msq'], writes=['msq'])
            S.op('dve', lambda e: e.tensor_scalar(out=msq[:], in0=msq[:], scalar1=LN_EPS, scalar2=None, op0=ALU.add), reads=['msq'], writes=['msq'])
            S.op('act', lambda e: e.activation(out=rstd[:], in_=msq[:], func=AF.Sqrt), reads=['msq'], writes=['rstd'])
            S.op('dve', lambda e: e.reciprocal(out=rstd[:], in_=rstd[:]), reads=['rstd'], writes=['rstd'])
            for c in range(KC):
                t = tb_[c % NTB]
                tr = 'tb%d' % (c % NTB)
                S.op('dve', lambda e, c=c, t=t: e.tensor_tensor(out=t[:], in0=acc[:, c, :], in1=mean[:], op=ALU.subtract), reads=[('acc', c), 'mean'], writes=[tr])
                S.op('dve', lambda e, t=t: e.tensor_tensor(out=t[:], in0=t[:], in1=rstd[:], op=ALU.mult), reads=[tr, 'rstd'], writes=[tr])
                if not final:
                    S.op('act', lambda e, c=c, t=t: e.activation(out=h2T[:, c, :], in_=t[:], func=AF.Identity, scale=dv[:, 1, c:c + 1], bias=dv[:, 2, c:c + 1]),
                         reads=[tr] + DVR, writes=[('h2T', c)])
                    if moe:
                        hf = h2f_[c % 2]
                        hr = 'h2f%d' % (c % 2)
                        S.op('dve', lambda e, c=c, t=t, hf=hf: e.tensor_scalar(out=hf[:], in0=t[:], scalar1=dv[:, 1, c:c + 1], scalar2=dv[:, 2, c:c + 1], op0=ALU.mult, op1=ALU.add),
                             reads=[tr] + DVR, writes=[hr])
                        for q in range(4):
                            S.op('pe', lambda e, c=c, q=q, hf=hf: e.matmul(ps[q][:, 0:8], lhsT=hf[:, q * 128:(q + 1) * 128], rhs=rw[:, c, :],
                                                                     start=(c == 0), stop=(c == KC - 1)),
                                 reads=[hr, 'rw'], writes=['ps%d' % q])
                    S.op('act', lambda e, c=c, t=t: e.activation(out=acc[:, c, :], in_=t[:], func=AF.Identity, scale=dv[:, 3, c:c + 1], bias=dv[:, 4, c:c + 1]),
                         reads=[tr] + DVR, writes=[('acc', c)])
                else:
                    yo = yo_[c % NTB]
                    yr = 'yo%d' % (c % NTB)
                    S.op('act', lambda e, c=c, t=t, yo=yo: e.activation(out=yo[:], in_=t[:], func=AF.Identity, scale=pvs[:, 6, c:c + 1], bias=pvs[:, 7, c:c + 1]),
                         reads=[tr] + DVR, writes=[yr])
                    S.dma('sp', lambda e, c=c, yo=yo: e.dma_start(out=yT[c * 128:(c + 1) * 128, ps_pass * T:(ps_pass + 1) * T], in_=yo[:]),
                          slot=yr, reads=[yr], writes=[])

        for p in range(NPASS):
            tsl = slice(p * T, (p + 1) * T)
            xv = xT.rearrange("(c q) t -> q c t", q=128)
            ov = oT.rearrange("(c q) t -> q c t", q=128)
            for g4 in range(4):
                cs = slice(g4 * 8, (g4 + 1) * 8)
                S.dma('sp', lambda e, cs=cs, tsl=tsl, xv=xv: e.dma_start(out=acc[:, cs, :], in_=xv[:, cs, tsl]), slot='accld%d' % g4,
                      writes=[('acc', c) for c in range(g4 * 8, g4 * 8 + 8)])
                S.dma('sp', lambda e, cs=cs, tsl=tsl, ov=ov: e.dma_start(out=actT[:, cs, :], in_=ov[:, cs, tsl]), slot='actld%d' % g4,
                      writes=[('actT', c) for c in range(g4 * 8, g4 * 8 + 8)])
            for c in range(KC):
                S.op('act', lambda e, c=c: e.activation(out=acc[:, c, :], in_=acc[:, c, :], func=AF.Copy, scale=ALPHA),
                     reads=[('acc', c)], writes=[('acc', c)])
            for ng in range(D // 512):
                tiles = [(w_o[k * 128:(k + 1) * 128, ng * 512:(ng + 1) * 512], False) for k in range(KC)]

                def evac(j, bank, ng=ng):
                    n = ng * 4 + j
                    S.op('dve', lambda e: e.scalar_tensor_tensor(out=acc[:, n, :], in0=ps[bank][:], scalar=dv[:, 0, n:n + 1], in1=acc[:, n, :],
                                                                 op0=ALU.mult, op1=ALU.add),
                         reads=['ps%d' % bank, ('acc', n)] + DVR, writes=[('acc', n)])
                gemm_group(tiles, lambda ki: actT[:, ki, :], lambda ki: [('actT', ki)], evac)
            layernorm(False, p)
            if moe:
                for q in range(4):
                    S.op('dve', lambda e, q=q: e.tensor_copy(out=Lsb[:, q, :], in_=ps[q][:, 0:8]), reads=['ps%d' % q], writes=[('L', q)])
                    S.op('dve', lambda e, q=q: e.max(out=m8[:, q, :], in_=Lsb[:, q, :]), reads=[('L', q)], writes=[('m8', q)])
                    S.op('dve', lambda e, q=q: e.tensor_scalar(out=nv1[:, q:q + 1], in0=m8[:, q, 0:1], scalar1=-1.0, scalar2=None, op0=ALU.mult),
                         reads=[('m8', q)], writes=[('nv1', q)])
                    S.op('dve', lambda e, q=q: e.tensor_scalar(out=msk[:, q, :], in0=Lsb[:, q, :], scalar1=m8[:, q, 1:2], scalar2=None, op0=ALU.is_ge),
                         reads=[('m8', q), ('L', q)], writes=[('msk', q)])
                    S.op('act', lambda e, q=q: e.activation(out=ex[:, q, :], in_=Lsb[:, q, :], func=AF.Exp, bias=nv1[:, q:q + 1], scale=1.0),
                         reads=[('L', q), ('nv1', q)], writes=[('ex', q)])
                    S.op('dve', lambda e, q=q: e.tensor_tensor(out=me[:, q, :], in0=msk[:, q, :], in1=ex[:, q, :], op=ALU.mult),
                         reads=[('msk', q), ('ex', q)], writes=[('me', q)])
                    S.op('dve', lambda e, q=q: e.reduce_sum(out=den[:, q:q + 1], in_=me[:, q, :], axis=AX.X),
                         reads=[('me', q)], writes=[('den', q)])
                    S.op('dve', lambda e, q=q: e.reciprocal(out=den[:, q:q + 1], in_=den[:, q:q + 1]), reads=[('den', q)], writes=[('den', q)])
                    S.op('dve', lambda e, q=q: e.tensor_scalar(out=gsb[:, q, :], in0=me[:, q, :], scalar1=den[:, q:q + 1], scalar2=None, op0=ALU.mult),
                         reads=[('me', q), ('den', q)], writes=[('gsb', q)])
                    S.op('pe', lambda e, q=q: e.transpose(out=ps[6][0:8, q * 128:(q + 1) * 128], in_=gsb[:, q, :], identity=ident[:]),
                         reads=[('gsb', q), 'ident'], writes=['ps6'])
                S.op('act', lambda e: e.activation(out=gT[:], in_=ps[6][0:8, :], func=AF.Copy), reads=['ps6'], writes=['gT'])
            for gi, g in enumerate(groups):
                if g['gate'] is not None:
                    G = G_[gi % 2]
                    Gr = 'G%d' % (gi % 2)
                    ge = g['gate']
                    S.op('pe', lambda e, ge=ge: e.matmul(ps[7][:], lhsT=sel[:, ge, :], rhs=gT[:], start=True, stop=True),
                         reads=['sel', 'gT'], writes=['ps7'])
                    S.op('act', lambda e, G=G: e.activation(out=G[:], in_=ps[7][:], func=AF.Copy), reads=['ps7'], writes=[Gr])
                pairs = g['pairs']
                chunks = []
                for pi, P in enumerate(pairs):
                    tiles = [(g['win'][k * 128:(k + 1) * 128, :, P * 256:(P + 1) * 256], True) for k in range(KC)]
                    lj0 = pi * 2

                    def evac(j, bank, lj0=lj0, g=g):
                        if j >= 2:
                            return
                        lj = lj0 + j
                        abank, bbank = bank, bank + 2
                        i3 = lj % NTB
                        sa = sa_[i3]
                        S.op('act', lambda e: e.activation(out=sa[:], in_=ps[abank][:], func=AF.Silu), reads=['ps%d' % abank], writes=['sa%d' % i3])
                        if g['gate'] is not None:
                            bg = bg_[i3]
                            Gg = G_[gi % 2]
                            S.op('dve', lambda e: e.tensor_tensor(out=bg[:], in0=ps[bbank][:], in1=Gg[:], op=ALU.mult),
                                 reads=['ps%d' % bbank, 'G%d' % (gi % 2)], writes=['bg%d' % i3])
                            S.op('dve', lambda e: e.tensor_tensor(out=actT[:, lj, :], in0=sa[:], in1=bg[:], op=ALU.mult),
                                 reads=['sa%d' % i3, 'bg%d' % i3], writes=[('actT', lj)])
                        else:
                            S.op('dve', lambda e: e.tensor_tensor(out=actT[:, lj, :], in0=sa[:], in1=ps[bbank][:], op=ALU.mult),
                                 reads=['sa%d' % i3, 'ps%d' % bbank], writes=[('actT', lj)])
                    gemm_group(tiles, lambda ki: h2T[:, ki, :], lambda ki: [('h2T', ki)], evac)
                    chunks += [(lj0, P * 2), (lj0 + 1, P * 2 + 1)]
                for ng in range(D // 512):
                    tiles = [(g['wo'][gj * 128:(gj + 1) * 128, ng * 512:(ng + 1) * 512], False) for (lj, gj) in chunks]

                    def evac2(j, bank, ng=ng):
                        n = ng * 4 + j
                        S.op('dve', lambda e: e.scalar_tensor_tensor(out=acc[:, n, :], in0=ps[bank][:], scalar=dv[:, 5, n:n + 1], in1=acc[:, n, :],
                                                                     op0=ALU.mult, op1=ALU.add),
                             reads=['ps%d' % bank, ('acc', n)] + DVR, writes=[('acc', n)])
                    gemm_group(tiles, lambda ki, chunks=chunks: actT[:, chunks[ki][0], :], lambda ki, chunks=chunks: [('actT', chunks[ki][0])], evac2)
            layernorm(True, p)
        S.finish()
        print("ff ninst", S.ninst)
    return nc

POOL_WINDOWS = (2, 4, 8, 16)

def pc(v):
    return np.ascontiguousarray(np.asarray(v, np.float32).reshape(-1, 128).T)

def stage_b_inputs(core, xT, sh1, sc1, even_w_in, lam_q1, lam_k1, lam_q2, lam_k2, subln_g, pool_w, pool_scale):
    hA, hB = 2 * core, 2 * core + 1
    g, half = core // 2, core % 2
    w = even_w_in
    cols = np.concatenate([np.arange(hA * 128, hA * 128 + 128), np.arange(hB * 128, hB * 128 + 128),
                           2048 + np.arange(hA * 128, hA * 128 + 128), 2048 + np.arange(hB * 128, hB * 128 + 128),
                           6144 + g * 512 + np.arange(512),
                           4096 + np.arange(hA * 128, hA * 128 + 128), 4096 + np.arange(hB * 128, hB * 128 + 128)])
    wq = np.ascontiguousarray(w[:, cols])
    pw = np.ascontiguousarray(pool_w[g][:, half * 256:(half + 1) * 256])
    ps = pool_scale[g * 512 + half * 256: g * 512 + (half + 1) * 256]
    psc = np.ascontiguousarray(ps.reshape(2, 128).T)
    sel4 = np.zeros((128, 4), np.float32); sel4[:, g] = 1
    wdw = POOL_WINDOWS[g]
    t = np.arange(512)
    rc = np.zeros((128, 2, 512), np.float32)
    rc[:, 0, :] = (1.0 / np.minimum(t + 1, wdw)).astype(np.float32)
    rc[:, 1, :] = np.float32(1.0 / wdw)
    slopes = (2.0 ** (-8.0 * np.arange(1, 17, dtype=np.float32) / 16)).astype(np.float32)
    bt = np.zeros((128, 2, 5, 512), np.float32)
    offt = np.zeros((128, 2, 64), np.float32)
    j = np.arange(128)[:, None].astype(np.float32); i = np.arange(512)[None, :].astype(np.float32)
    for hl, h in enumerate((hA, hB)):
        sl = slopes[h]
        bt[:, hl, 0, :] = -sl * (i - j)
        for d in range(4):
            dist = i - j - 128 * d
            bt[:, hl, d + 1, :] = np.where(dist >= 0, -sl * dist, np.float32(-1e30))
        offt[:, hl, :] = (-sl * 128 * np.arange(64, dtype=np.float32))[None, :]
    lamv = np.stack([np.broadcast_to(v, (128, 64)) for v in (lam_q1, lam_k1, lam_q2, lam_k2)], axis=1).astype(np.float32)
    sg = np.broadcast_to(subln_g, (128, 128)).astype(np.float32)
    return dict(xT=xT, mv=np.ascontiguousarray(np.stack([pc(sh1), pc(sc1)], axis=1)), wq=wq, pw=pw, psc=psc, sel4=sel4, rc=rc,
                bt=bt, offt=offt, lamv=np.ascontiguousarray(lamv), sg=np.ascontiguousarray(sg),
                identb=np.eye(128, dtype=np.float32).astype(ml_dtypes.bfloat16))

def stage_d_inputs(core, xT, sh1, sc1, odd_w_in, lb_raw, gnorm_g):
    c0 = core * 512
    cols = np.concatenate([k * 4096 + c0 + np.arange(512) for k in range(4)])
    wd = np.ascontiguousarray(odd_w_in[:, cols])
    lbr = np.ascontiguousarray(lb_raw[:, c0:c0 + 512].reshape(2, 4, 128).transpose(2, 0, 1)).astype(np.float32)
    gn4 = np.ascontiguousarray(np.broadcast_to(np.tile(gnorm_g, 4), (128, 512))).astype(np.float32)
    s = np.arange(128)[:, None]; t = np.arange(128)[None, :]
    mask01 = (s <= t).astype(np.float32)
    return dict(xT=xT, mv=np.ascontiguousarray(np.stack([pc(sh1), pc(sc1)], axis=1)), wd=wd, lbr=lbr, gn4=gn4, mask01=mask01,
                identb=np.eye(128, dtype=np.float32).astype(ml_dtypes.bfloat16))

def _run(nc, in_maps):
    return run_bass_kernel_spmd(nc, in_maps, core_ids=list(range(8))).results


def kernel(**inputs):
    g = lambda k: np.asarray(inputs[k])
    x = np.asarray(g('x'), np.float32)[0]
    c = np.asarray(g('c'), np.float32)[0]
    ada_w, ada_b = g('ada_w'), g('ada_b')
    ln_g, ln_b = g('ln_g'), g('ln_b')
    NCORE = 8
    in_maps = []
    cvp = pc(c)
    for core in range(NCORE):
        l, cb = core // 4, core % 4
        in_maps.append(dict(aw=np.ascontiguousarray(ada_w[l][:, cb * 6144:(cb + 1) * 6144]),
                            ab=np.ascontiguousarray(ada_b[l][cb * 6144:(cb + 1) * 6144][None, :]), cv=cvp))
    res = _run(build_a(), in_maps)
    mod = np.concatenate([res[core]['mo'][0] for core in range(NCORE)]).reshape(2, 6, 4096)
    del in_maps
    ones = np.ones((128, 128), np.float32)
    xT = np.ascontiguousarray(x.T)
    in_maps = [stage_b_inputs(core, xT, mod[0, 0], mod[0, 1], g('even_w_in')[0], g('lam_q1')[0], g('lam_k1')[0], g('lam_q2')[0],
                              g('lam_k2')[0], g('subln_g')[0], g('pool_w')[0], g('pool_scale')[0]) for core in range(NCORE)]
    res = _run(build_b(), in_maps)
    ocT = np.empty((4096, 8192), ml_dtypes.bfloat16)
    for core in range(NCORE):
        o = res[core]['oTc']
        ocT[core * 256:(core + 1) * 256] = o[0:256]
        ocT[2048 + core * 256:2048 + (core + 1) * 256] = o[256:512]
    del in_maps, res

    def ff_stage(moe, l, ocT, xT_in, w_o, extra):
        pv = np.ascontiguousarray(np.stack([pc(mod[l, 2]), pc(mod[l, 3]), pc(mod[l, 4]), pc(mod[l, 5]),
                                            pc(ln_g[l, 0]), pc(ln_b[l, 0]), pc(ln_g[l, 1]), pc(ln_b[l, 1])], axis=1))
        in_maps = []
        for core in range(NCORE):
            sl = slice(core * 1024, (core + 1) * 1024)
            m = dict(oT=np.ascontiguousarray(ocT[:, sl]), xT=np.ascontiguousarray(xT_in[:, sl]), pv=pv, w_o=w_o, ones=ones)
            m.update(extra)
            in_maps.append(m)
        res = _run(build_ff(moe), in_maps)
        return np.concatenate([res[core]['yT'] for core in range(NCORE)], axis=1)

    x1T = ff_stage(False, 0, ocT, xT, np.ascontiguousarray(g('even_w_out')[0]),
                   dict(w_in=np.ascontiguousarray(g('ffn_w_in')[0]), w_o2=np.ascontiguousarray(g('ffn_w_out')[0])))
    del xT
    in_maps = [stage_d_inputs(core, x1T, mod[1, 0], mod[1, 1], g('odd_w_in')[0], g('lb_raw'), g('gnorm_g')[0]) for core in range(NCORE)]
    res = _run(build_d(), in_maps)
    oT = np.concatenate([res[core]['oTd'] for core in range(NCORE)], axis=0)
    del in_maps, res
    sel = np.zeros((8, 8, 128), np.float32)
    for e in range(8):
        sel[e, e, :] = 1
    rw = np.ascontiguousarray(np.asarray(g('router_w')[0], np.float32).reshape(32, 128, 8).transpose(1, 0, 2))
    x2T = ff_stage(True, 1, oT, x1T, np.ascontiguousarray(g('odd_w_out')[0]),
                   dict(w_in=np.ascontiguousarray(g('exp_w_in')[0]), w_o2=np.ascontiguousarray(g('exp_w_out')[0]), rw=rw, sel=sel,
                        ident=np.eye(128, dtype=np.float32)))
    return np.ascontiguousarray(x2T.T)[None].astype(np.float32)
```

```python
import numpy as np
import ml_dtypes
import concourse.bass as bass
import concourse.mybir as mybir
from concourse.bass_utils import run_bass_kernel_spmd
from contextlib import ExitStack

F32 = mybir.dt.float32
BF16 = mybir.dt.bfloat16
AF = mybir.ActivationFunctionType
ALU = mybir.AluOpType
AX = mybir.AxisListType


class Sched:
    ENG = ('pe', 'act', 'dve', 'pool', 'sp')

    def __init__(self, nc, es, immediate=False):
        self.immediate = immediate
        self.nc = nc
        self.es = es
        self.eng = {'pe': nc.tensor, 'act': nc.scalar, 'dve': nc.vector,
                    'pool': nc.gpsimd, 'sp': nc.sync}
        self.sem = {e: es.enter_context(nc.semaphore("s_" + e)) for e in self.ENG}
        self.cnt = {e: 0 for e in self.ENG}
        self.prog = {e: [] for e in self.ENG}
        self.waited = {}
        self.lastw = {}
        self.reads = {}
        self.dsem = {}
        self.semobj = {e: self.sem[e] for e in self.ENG}
        self.ninst = 0

    def _dma_sem(self, key):
        if key not in self.dsem:
            s = self.es.enter_context(self.nc.semaphore("d_%d" % len(self.dsem)))
            k = ('d', key)
            self.semobj[k] = s
            self.dsem[key] = [k, 0]
        return self.dsem[key]

    def _deps(self, eng, reads, writes):
        deps = {}
        def add(d):
            if d is None:
                return
            k, v, e = d
            if e == 'pe' and eng == 'pe':
                return
            if deps.get(k, 0) < v:
                deps[k] = v
        for r in reads:
            add(self.lastw.get(r))
        for w in writes:
            add(self.lastw.get(w))
            for d in self.reads.get(w, ()):
                add(d)
        out = []
        for k, v in deps.items():
            if self.waited.get((eng, k), 0) >= v:
                continue
            self.waited[(eng, k)] = v
            out.append((self.semobj[k], v))
        return out

    def _commit(self, ident, reads, writes):
        for w in writes:
            self.lastw[w] = ident
            self.reads[w] = []
        for r in reads:
            if r in writes:
                continue
            self.reads.setdefault(r, []).append(ident)

    def op(self, eng, fn, reads=(), writes=(), inc=True):
        waits = self._deps(eng, reads, writes)
        if inc:
            self.cnt[eng] += 1
            val = self.cnt[eng]
        else:
            val = self.cnt[eng] + 1
        sem = self.sem[eng]
        e = self.eng[eng]
        def emit():
            for s, v in waits:
                e.wait_ge(s, v)
            i = fn(e)
            if inc:
                i.then_inc(sem, 1)
        if self.immediate:
            emit()
        else:
            self.prog[eng].append(emit)
        self._commit((eng, val, eng), reads, writes)
        self.ninst += 1

    def dma(self, eng, fn, slot, reads=(), writes=()):
        waits = self._deps(eng, reads, writes)
        ds = self._dma_sem(slot)
        ds[1] += 16
        k, val = ds[0], ds[1]
        sem = self.semobj[k]
        e = self.eng[eng]
        def emit():
            for s, v in waits:
                e.wait_ge(s, v)
            fn(e).then_inc(sem, 16)
        if self.immediate:
            emit()
        else:
            self.prog[eng].append(emit)
        self._commit((k, val, 'dma'), reads, writes)
        self.ninst += 1

    def finish(self, final_res=None):
        waits = [(self.sem[k], self.cnt[k]) for k in self.ENG if self.cnt[k] > 0]
        waits += [(self.semobj[k], v) for (k, v) in self.dsem.values()]
        e = self.eng['sp']
        def emit():
            for s, v in waits:
                e.wait_ge(s, v)
        self.prog['sp'].append(emit)
        nc = self.nc
        allsems = [self.sem[k] for k in self.ENG] + [self.semobj[k] for (k, v) in self.dsem.values()]
        with nc.Block() as b0:
            @b0.sync
            def _(t):
                for s_ in allsems:
                    t.sem_clear(s_)
        with nc.Block() as block:
            @block.tensor
            def _(t):
                for f in self.prog['pe']:
                    f()
            @block.scalar
            def _(t):
                for f in self.prog['act']:
                    f()
            @block.vector
            def _(t):
                for f in self.prog['dve']:
                    f()
            @block.gpsimd
            def _(t):
                for f in self.prog['pool']:
                    f()
            @block.sync
            def _(t):
                for f in self.prog['sp']:
                    f()


    def barrier(self):
        waits = [(k, self.cnt[k]) for k in self.ENG if self.cnt[k] > 0]
        waits += [(k, v) for (k, v) in self.dsem.values()]
        for eng in self.ENG:
            ws = []
            for k, v in waits:
                if self.waited.get((eng, k), 0) >= v:
                    continue
                self.waited[(eng, k)] = v
                ws.append((self.semobj[k], v))
            e = self.eng[eng]
            def emit(ws=ws, e=e):
                for s, v in ws:
                    e.wait_ge(s, v)
            self.prog[eng].append(emit)
        self.lastw = {}
        self.reads = {}


class Arena:
    def __init__(self, nc, es, name, kbytes):
        self.t = es.enter_context(nc.sbuf_tensor(name, [128, kbytes * 256], F32))
        self.n = kbytes * 256
        self.off = 0
        self.marks = []

    def alloc(self, shape, dt, parts=128):
        nel = 1
        for s_ in shape:
            nel *= s_
        esz = 4 if dt == F32 else 2
        nw = (nel * esz + 3) // 4
        nw = (nw + 7) // 8 * 8
        assert self.off + nw <= self.n, "arena overflow %d + %d > %d" % (self.off, nw, self.n)
        v = self.t[0:parts, self.off:self.off + nw]
        self.off += nw
        if dt != F32:
            v = v.bitcast(dt)
        v = v[:, 0:nel]
        if len(shape) == 2:
            v = v.rearrange("p (a b) -> p a b", a=shape[0])
        elif len(shape) == 3:
            v = v.rearrange("p (a b c) -> p a b c", a=shape[0], b=shape[1])
        return v

    def mark(self):
        return self.off

    def reset(self, mark):
        self.off = mark


IMM = False
def build_a(NCOL=6144):
    nc = bass.Bass("TRN2", target_bir_lowering=False)
    D = 4096
    aw = nc.dram_tensor("aw", [D, NCOL], F32, kind="ExternalInput").ap()
    ab = nc.dram_tensor("ab", [1, NCOL], F32, kind="ExternalInput").ap()
    cv = nc.dram_tensor("cv", [128, 32], F32, kind="ExternalInput").ap()
    mo = nc.dram_tensor("mo", [1, NCOL], F32, kind="ExternalOutput").ap()
    with ExitStack() as es:
        S = Sched(nc, es, immediate=IMM)
        sb = lambda name, shape, dt: es.enter_context(nc.sbuf_tensor(name, shape, dt))
        NB = 8
        wt = [sb("wt%d" % i, [128, 512], F32) for i in range(NB)]
        cs = sb("cs", [128, 32], F32)
        ca = sb("ca", [128, 32], F32)
        abs_ = sb("abs", [1, NCOL], F32)
        mos = sb("mos", [1, NCOL], F32)
        ps = [es.enter_context(nc.psum_tensor("ps%d" % i, [128, 512], F32)) for i in range(2)]
        S.dma('sp', lambda e: e.dma_start(out=cs[:], in_=cv), slot='cs', writes=['cs'])
        S.dma('sp', lambda e: e.dma_start(out=abs_[:], in_=ab), slot='abs', writes=['abs'])
        S.op('act', lambda e: e.activation(out=ca[:], in_=cs[:], func=AF.Silu), reads=['cs'], writes=['ca'])
        it = 0
        for n in range(NCOL // 512):
            bank = n % 2
            for k in range(32):
                b = it % NB
                it += 1
                S.dma('sp', lambda e, b=b, k=k, n=n: e.dma_start(out=wt[b][:], in_=aw[k * 128:(k + 1) * 128, n * 512:(n + 1) * 512]),
                      slot='wt%d' % b, writes=['wt%d' % b])
                S.op('pe', lambda e, b=b, k=k, bank=bank: e.matmul(ps[bank][0:1, :], lhsT=ca[:, k:k + 1], rhs=wt[b][:], start=(k == 0), stop=(k == 31)),
                     reads=['wt%d' % b, 'ca'], writes=['ps%d' % bank])
            S.op('dve', lambda e, n=n, bank=bank: e.tensor_tensor(out=mos[0:1, n * 512:(n + 1) * 512], in0=ps[bank][0:1, :], in1=abs_[0:1, n * 512:(n + 1) * 512], op=ALU.add),
                 reads=['ps%d' % bank, 'abs'], writes=[('mos', n)])
        S.dma('sp', lambda e: e.dma_start(out=mo, in_=mos[:]), slot='out', reads=[('mos', n) for n in range(NCOL // 512)], writes=[])
        S.finish()
    return nc


D = 4096
KC = 32
S_ = 8192
T = 512
NTG = S_ // T
RMS_EPS = 1e-6


def build_b(NTG_RUN=NTG, heads=(0, 1)):
    nc = bass.Bass("TRN2", target_bir_lowering=False)
    dt_in = lambda name, shape, dt=F32: nc.dram_tensor(name, shape, dt, kind="ExternalInput").ap()
    xT = dt_in("xT", [D, S_])
    mv = dt_in("mv", [128, 2, KC])
    wq = dt_in("wq", [D, 1280])
    pw_d = dt_in("pw", [512, 256])
    psc_d = dt_in("psc", [128, 2])
    sel_d = dt_in("sel4", [128, 4])
    rc_d = dt_in("rc", [128, 2, T])
    bt_d = dt_in("bt", [128, 2, 5, T])
    off_d = dt_in("offt", [128, 2, 64])
    lam_d = dt_in("lamv", [128, 4, 64])
    sg_d = dt_in("sg", [128, 128])
    idb_d = dt_in("identb", [128, 128], BF16)
    oTc = nc.dram_tensor("oTc", [512, S_], BF16, kind="ExternalOutput").ap()
    qk_scr = nc.dram_tensor("qk_scr", [4, 128, S_], BF16, kind="Internal").ap()
    v_scr = nc.dram_tensor("v_scr", [S_, 2, 130], BF16, kind="Internal").ap()

    with ExitStack() as es:
        S = Sched(nc, es)
        ar = Arena(nc, es, "arena", 200)
        psf = [es.enter_context(nc.psum_tensor("ps%d" % i, [128, 512], F32)) for i in range(8)]
        ps7b = psf[7][:].bitcast(BF16)

        mvs = ar.alloc([2, KC], F32)
        sc1p = ar.alloc([KC], F32)
        psc = ar.alloc([2], F32)
        sel4 = ar.alloc([4], F32)
        lamv = ar.alloc([4, 64], F32)
        lamt = ar.alloc([2, 64], F32)
        lsc = ar.alloc([8], F32)
        SG = ar.alloc([128], F32)
        identb = ar.alloc([128], BF16)
        for (dst, src, nm) in [(mvs, mv, 'mvs'), (psc, psc_d, 'psc'), (sel4, sel_d, 'sel4'), (lamv, lam_d, 'lamv'),
                               (SG, sg_d, 'SG'), (identb, idb_d, 'identb')]:
            S.dma('sp', lambda e, dst=dst, src=src: e.dma_start(out=dst, in_=src), slot=nm, writes=[nm])
        S.op('dve', lambda e: e.tensor_scalar(out=sc1p, in0=mvs[:, 1, :], scalar1=1.0, scalar2=None, op0=ALU.add), reads=['mvs'], writes=['sc1p'])
        S.op('dve', lambda e: e.tensor_tensor(out=lamt[:, 0, :], in0=lamv[:, 0, :], in1=lamv[:, 1, :], op=ALU.mult), reads=['lamv'], writes=['lamt0'])
        S.op('dve', lambda e: e.tensor_tensor(out=lamt[:, 1, :], in0=lamv[:, 2, :], in1=lamv[:, 3, :], op=ALU.mult), reads=['lamv'], writes=['lamt1'])
        S.op('dve', lambda e: e.reduce_sum(out=lsc[:, 0:1], in_=lamt[:, 0, :], axis=AX.X), reads=['lamt0'], writes=['lsc0'])
        S.op('dve', lambda e: e.reduce_sum(out=lsc[:, 1:2], in_=lamt[:, 1, :], axis=AX.X), reads=['lamt1'], writes=['lsc1'])
        S.op('act', lambda e: e.activation(out=lsc[:, 2:4], in_=lsc[:, 0:2], func=AF.Exp), reads=['lsc0', 'lsc1'], writes=['lsc23'])
        S.op('dve', lambda e: e.tensor_tensor(out=lsc[:, 4:5], in0=lsc[:, 2:3], in1=lsc[:, 3:4], op=ALU.subtract), reads=['lsc23'], writes=['lsc4'])
        S.op('dve', lambda e: e.tensor_scalar(out=lsc[:, 4:5], in0=lsc[:, 4:5], scalar1=0.2, scalar2=-1.0, op0=ALU.add, op1=ALU.mult), reads=['lsc4'], writes=['lsc4'])
        S.op('dve', lambda e: e.tensor_scalar(out=SG, in0=SG, scalar1=0.8, scalar2=None, op0=ALU.mult), reads=['SG'], writes=['SG'])
        pmark = ar.mark()

        W = ar.alloc([KC, 1280], BF16)
        hT = ar.alloc([KC, T], BF16)
        NXS = 3
        xs = [ar.alloc([2, T], F32) for i in range(NXS)]
        pw = ar.alloc([4, 256], BF16)
        rc = ar.alloc([2, T], F32)
        ue = [ar.alloc([4, 528], F32) for i in range(2)]
        NPT = 2
        pA = [ar.alloc([528], F32) for i in range(NPT)]
        pB = [ar.alloc([528], F32) for i in range(NPT)]
        pS = [ar.alloc([T], F32) for i in range(NPT)]
        z = ar.alloc([4, T], BF16)
        NST = 3
        qst = [ar.alloc([T], BF16) for i in range(NST)]
        vst = [ar.alloc([2, 130], BF16) for i in range(2)]
        ost = [ar.alloc([T], BF16) for i in range(2)]

        for k in range(KC):
            S.dma('pool', lambda e, k=k: e.dma_start(out=W[:, k, :], in_=wq[k * 128:(k + 1) * 128, :]), slot='W%d' % k, writes=[('W', k)])
        S.dma('pool', lambda e: e.dma_start(out=pw, in_=pw_d.rearrange("(c p) n -> p c n", p=128)), slot='pw', writes=['pw'])
        S.dma('sp', lambda e: e.dma_start(out=rc, in_=rc_d), slot='rc', writes=['rc'])
        for i in range(2):
            S.op('dve', lambda e, i=i: e.memset(vst[i][:, :, 128:130], 1.0), writes=['vst%d' % i])
        S.op('dve', lambda e: e.memset(ue[0][:, :, 0:16], 0.0), writes=[('ue', 0, j) for j in range(4)])
        WR = [('W', k) for k in range(KC)]
        xv = xT.rearrange("(c q) t -> q c t", q=128)
        xit = 0
        qit = 0
        for tg in range(NTG_RUN):
            tsl = slice(tg * T, (tg + 1) * T)
            for c2 in range(KC // 2):
                b = xit % NXS
                xit += 1
                S.dma('sp', lambda e, b=b, c2=c2, tsl=tsl: e.dma_start(out=xs[b], in_=xv[:, c2 * 2:c2 * 2 + 2, tsl]), slot='xs%d' % b, writes=['xs%d' % b])
                for i in range(2):
                    c = c2 * 2 + i
                    S.op('act', lambda e, b=b, i=i, c=c: e.activation(out=hT[:, c, :], in_=xs[b][:, i, :], func=AF.Identity,
                                                                   scale=sc1p[:, c:c + 1], bias=mvs[:, 0, c:c + 1]),
                         reads=['xs%d' % b, 'sc1p', 'mvs'], writes=[('hT', c)])
            for k in range(KC):
                for j in range(4):
                    S.op('pe', lambda e, k=k, j=j: e.matmul(psf[j][:], lhsT=W[:, k, j * 128:(j + 1) * 128], rhs=hT[:, k, :], start=(k == 0), stop=(k == KC - 1)),
                         reads=[('W', k), ('hT', k)], writes=['ps%d' % j], inc=(k == KC - 1))
            for j in range(4):
                b = qit % NST
                qit += 1
                if j % 2 == 0:
                    S.op('act', lambda e, b=b, j=j: e.activation(out=qst[b], in_=psf[j][:], func=AF.Copy), reads=['ps%d' % j], writes=['qst%d' % b])
                else:
                    S.op('dve', lambda e, b=b, j=j: e.tensor_copy(out=qst[b], in_=psf[j][:]), reads=['ps%d' % j], writes=['qst%d' % b])
                S.dma('sp', lambda e, b=b, j=j, tsl=tsl: e.dma_start(out=qk_scr[j, :, tsl], in_=qst[b]), slot='qst%d' % b, reads=['qst%d' % b], writes=[])
            U = ue[tg % 2]
            Un = ue[(tg + 1) % 2]
            for k in range(KC):
                for j in range(4):
                    S.op('pe', lambda e, k=k, j=j: e.matmul(psf[4 + j][:], lhsT=W[:, k, 512 + j * 128:512 + (j + 1) * 128], rhs=hT[:, k, :], start=(k == 0), stop=(k == KC - 1)),
                         reads=[('W', k), ('hT', k)], writes=['ps%d' % (4 + j)], inc=(k == KC - 1))
            for j in range(4):
                ur = ('ue', tg % 2, j)
                urn = ('ue', (tg + 1) % 2, j)
                S.op('act', lambda e, j=j, U=U: e.activation(out=U[:, j, 16:528], in_=psf[4 + j][:], func=AF.Copy), reads=['ps%d' % (4 + j)], writes=[ur])
                S.op('pool', lambda e, j=j, U=U, Un=Un: e.tensor_copy(out=Un[:, j, 0:16], in_=U[:, j, 512:528]), reads=[ur], writes=[urn])
                eng = 'dve'
                i2 = j % NPT
                A, B, SS = pA[i2], pB[i2], pS[i2]
                Ar, Br, Sr = 'pA%d' % i2, 'pB%d' % i2, 'pS%d' % i2
                E_ = lambda lo, hi, U=U, j=j: U[:, j, lo:hi]
                S.op(eng, lambda e, A=A, E_=E_: e.tensor_tensor(out=A[:, 1:528], in0=E_(1, 528), in1=E_(0, 527), op=ALU.add), reads=[ur], writes=[Ar])
                S.op(eng, lambda e, A=A, SS=SS: e.tensor_scalar(out=SS, in0=A[:, 16:528], scalar1=sel4[:, 0:1], scalar2=None, op0=ALU.mult), reads=[Ar, 'sel4'], writes=[Sr])
                S.op(eng, lambda e, A=A, B=B: e.tensor_tensor(out=B[:, 3:528], in0=A[:, 3:528], in1=A[:, 1:526], op=ALU.add), reads=[Ar], writes=[Br])
                S.op(eng, lambda e, B=B, SS=SS: e.scalar_tensor_tensor(out=SS, in0=B[:, 16:528], scalar=sel4[:, 1:2], in1=SS, op0=ALU.mult, op1=ALU.add), reads=[Br, Sr, 'sel4'], writes=[Sr])
                S.op(eng, lambda e, A=A, B=B: e.tensor_tensor(out=A[:, 7:528], in0=B[:, 7:528], in1=B[:, 3:524], op=ALU.add), reads=[Br], writes=[Ar])
                S.op(eng, lambda e, A=A, SS=SS: e.scalar_tensor_tensor(out=SS, in0=A[:, 16:528], scalar=sel4[:, 2:3], in1=SS, op0=ALU.mult, op1=ALU.add), reads=[Ar, Sr, 'sel4'], writes=[Sr])
                S.op(eng, lambda e, A=A, B=B: e.tensor_tensor(out=B[:, 15:528], in0=A[:, 15:528], in1=A[:, 7:520], op=ALU.add), reads=[Ar], writes=[Br])
                S.op(eng, lambda e, B=B, SS=SS: e.scalar_tensor_tensor(out=SS, in0=B[:, 16:528], scalar=sel4[:, 3:4], in1=SS, op0=ALU.mult, op1=ALU.add), reads=[Br, Sr, 'sel4'], writes=[Sr])
                rci = 0 if tg == 0 else 1
                S.op(eng, lambda e, SS=SS, rci=rci: e.tensor_tensor(out=SS, in0=SS, in1=rc[:, rci, :], op=ALU.mult), reads=[Sr, 'rc'], writes=[Sr])
                S.op(eng, lambda e, SS=SS, j=j, E_=E_: e.tensor_tensor(out=z[:, j, :], in0=SS, in1=E_(16, 528), op=ALU.subtract), reads=[Sr, ur], writes=[('z', j)])
            for tb in range(4):
                for k in range(KC):
                    S.op('pe', lambda e, k=k, tb=tb: e.matmul(psf[tb][:, 0:256], lhsT=hT[:, k, tb * 128:(tb + 1) * 128], rhs=W[:, k, 1024:1280], start=(k == 0), stop=(k == KC - 1)),
                         reads=[('W', k), ('hT', k)], writes=['ps%d' % tb], inc=(k == KC - 1))
            for tb in range(4):
                b = tb % 2
                S.op('dve', lambda e, b=b, tb=tb: e.tensor_copy(out=vst[b][:, :, 0:128], in_=psf[tb][:, 0:256].rearrange("p (h d) -> p h d", h=2)),
                     reads=['ps%d' % tb], writes=['vst%d' % b])
                r0 = tg * T + tb * 128
                S.dma('sp', lambda e, b=b, r0=r0: e.dma_start(out=v_scr[r0:r0 + 128, :, :], in_=vst[b]), slot='vst%d' % b, reads=['vst%d' % b], writes=[])
            for oc in range(2):
                for kc in range(4):
                    S.op('pe', lambda e, oc=oc, kc=kc: e.matmul(psf[4 + oc][:], lhsT=pw[:, kc, oc * 128:(oc + 1) * 128], rhs=z[:, kc, :], start=(kc == 0), stop=(kc == 3)),
                         reads=['pw', ('z', kc)], writes=['ps%d' % (4 + oc)], inc=(kc == 3))
                S.op('act', lambda e, oc=oc: e.activation(out=ost[oc], in_=psf[4 + oc][:], func=AF.Identity, scale=psc[:, oc:oc + 1]),
                     reads=['ps%d' % (4 + oc), 'psc'], writes=['ost%d' % oc])
                S.dma('sp', lambda e, oc=oc, tsl=tsl: e.dma_start(out=oTc[256 + oc * 128:256 + (oc + 1) * 128, tsl], in_=ost[oc]), slot='ost%d' % oc, reads=['ost%d' % oc], writes=[])

        S.barrier()
        ar.reset(pmark)
        QK = ar.alloc([4, S_], BF16)
        Vx = ar.alloc([64, 2, 130], BF16)
        bt = ar.alloc([2, 5, T], F32)
        offt = ar.alloc([2, 64], F32)
        NSB = 3
        Sb = [ar.alloc([T], F32) for i in range(NSB)]
        NPT2 = 4
        PT = [ar.alloc([T], BF16) for i in range(NPT2)]
        rl = ar.alloc([4, 4], F32)
        t2 = [ar.alloc([128], F32) for i in range(2)]
        o_ = [ar.alloc([128], F32) for i in range(2)]
        junk = ar.alloc([128], F32)
        on = [ar.alloc([128], BF16) for i in range(2)]
        ost2 = [ar.alloc([T], BF16) for i in range(2)]
        for j in range(4):
            S.dma('sp', lambda e, j=j: e.dma_start(out=QK[:, j, :], in_=qk_scr[j]), slot='QK%d' % j, writes=[('QK', j)])
        S.dma('sp', lambda e: e.dma_start(out=Vx, in_=v_scr.rearrange("(b p) h d -> p b h d", p=128)), slot='Vx', writes=['Vx'])
        S.dma('sp', lambda e: e.dma_start(out=bt, in_=bt_d), slot='bt', writes=['bt'])
        S.dma('sp', lambda e: e.dma_start(out=offt, in_=off_d), slot='offt', writes=['offt'])

        accpos = {}
        lst = [(m, s) for m in range(2) for s in range(4)]
        for i, (m, s) in enumerate(lst):
            accpos[(m, s)] = (4 + i // 3, (i % 3) * 130)
        tcount = 0
        epi = 0
        for h in heads:
            for ib in range(NTG_RUN):
                for bk in (4, 5, 6):
                    S.op('dve', lambda e, bk=bk: e.memset(psf[bk][:, 0:390], 0.0), writes=['ps%d' % bk])
                tiles = [(m, jb) for jb in range(4 * ib + 4) for m in range(2)]
                SKEW = 3
                def emit_qk(ti, h=h, ib=ib, tiles=tiles, tcount=tcount):
                    m, jb = tiles[ti]
                    bank = (tcount + ti) % 4
                    S.op('pe', lambda e: e.matmul(psf[bank][:], lhsT=QK[m * 64:(m + 1) * 64, 2 + h, jb * 128:(jb + 1) * 128],
                                                  rhs=QK[m * 64:(m + 1) * 64, h, ib * T:(ib + 1) * T], start=True, stop=True),
                         reads=[('QK', 2 + h), ('QK', h)], writes=['ps%d' % bank])
                    d = jb - 4 * ib
                    var = 0 if d < 0 else d + 1
                    n = 4 * ib - jb if d < 0 else 0
                    sbi = (tcount + ti) % NSB
                    pti = (tcount + ti) % NPT2
                    S.op('dve', lambda e: e.scalar_tensor_tensor(out=Sb[sbi], in0=psf[bank][:], scalar=0.125, in1=bt[:, h, var, :], op0=ALU.mult, op1=ALU.add),
                         reads=['ps%d' % bank, 'bt'], writes=['Sb%d' % sbi])
                    S.op('act', lambda e: e.activation(out=PT[pti], in_=Sb[sbi], func=AF.Exp, bias=offt[:, h, n:n + 1], scale=1.0),
                         reads=['Sb%d' % sbi, 'offt'], writes=['PT%d' % pti])
                def emit_pv(ti, h=h, ib=ib, tiles=tiles, tcount=tcount):
                    m, jb = tiles[ti]
                    d = jb - 4 * ib
                    pti = (tcount + ti) % NPT2
                    subs = [s for s in range(4) if not (d >= 0 and s < d)]
                    for s in subs:
                        bk, c0 = accpos[(m, s)]
                        S.op('pe', lambda e, s=s, bk=bk, c0=c0: e.matmul(psf[bk][:, c0:c0 + 129], lhsT=PT[pti][:, s * 128:(s + 1) * 128], rhs=Vx[:, jb, h, 0:129],
                                                                         start=False, stop=False, skip_group_check=True),
                             reads=['PT%d' % pti, 'Vx'], writes=['ps%d' % bk], inc=(s == subs[-1]))
                nt = len(tiles)
                for ti in range(nt + SKEW):
                    if ti < nt:
                        emit_qk(ti)
                    if ti - SKEW >= 0:
                        emit_pv(ti - SKEW)
                tcount += nt
                for s in range(4):
                    b1, c1 = accpos[(0, s)]
                    b2, c2 = accpos[(1, s)]
                    e2 = epi % 2
                    epi += 1
                    A1 = psf[b1][:, c1:c1 + 129]
                    A2 = psf[b2][:, c2:c2 + 129]
                    rr = ('rl', s)
                    S.op('dve', lambda e, s=s, A1=A1: e.reciprocal(out=rl[:, s, 0:1], in_=A1[:, 128:129]), reads=['ps%d' % b1], writes=[rr])
                    S.op('dve', lambda e, s=s, A2=A2: e.reciprocal(out=rl[:, s, 1:2], in_=A2[:, 128:129]), reads=['ps%d' % b2], writes=[rr])
                    S.op('dve', lambda e, s=s: e.tensor_tensor(out=rl[:, s, 1:2], in0=rl[:, s, 1:2], in1=lsc[:, 4:5], op=ALU.mult), reads=[rr, 'lsc4'], writes=[rr])
                    S.op('act', lambda e, s=s, A2=A2, e2=e2: e.activation(out=t2[e2], in_=A2[:, 0:128], func=AF.Identity, scale=rl[:, s, 1:2]),
                         reads=['ps%d' % b2, rr], writes=['t2%d' % e2])
                    S.op('dve', lambda e, s=s, A1=A1, e2=e2: e.scalar_tensor_tensor(out=o_[e2], in0=A1[:, 0:128], scalar=rl[:, s, 0:1], in1=t2[e2], op0=ALU.mult, op1=ALU.add),
                         reads=['ps%d' % b1, rr, 't2%d' % e2], writes=['o%d' % e2])
                    S.op('act', lambda e, s=s, e2=e2: e.activation(out=junk, in_=o_[e2], func=AF.Square, accum_out=rl[:, s, 2:3]),
                         reads=['o%d' % e2], writes=['junk', ('rl2', s)])
                    S.op('dve', lambda e, s=s: e.tensor_scalar(out=rl[:, s, 2:3], in0=rl[:, s, 2:3], scalar1=1.0 / 128, scalar2=RMS_EPS, op0=ALU.mult, op1=ALU.add),
                         reads=[('rl2', s)], writes=[('rl2', s)])
                    S.op('act', lambda e, s=s: e.activation(out=rl[:, s, 3:4], in_=rl[:, s, 2:3], func=AF.Sqrt), reads=[('rl2', s)], writes=[('rl3', s)])
                    S.op('dve', lambda e, s=s: e.reciprocal(out=rl[:, s, 3:4], in_=rl[:, s, 3:4]), reads=[('rl3', s)], writes=[('rl3', s)])
                    S.op('dve', lambda e, s=s, e2=e2: e.scalar_tensor_tensor(out=on[e2], in0=o_[e2], scalar=rl[:, s, 3:4], in1=SG, op0=ALU.mult, op1=ALU.mult),
                         reads=['o%d' % e2, ('rl3', s), 'SG'], writes=['on%d' % e2])
                    S.op('pe', lambda e, s=s, e2=e2: e.transpose(out=ps7b[:, s * 128:(s + 1) * 128], in_=on[e2], identity=identb),
                         reads=['on%d' % e2, 'identb'], writes=['ps7'])
                ob = (epi // 4) % 2
                S.op('act', lambda e, ob=ob: e.activation(out=ost2[ob], in_=ps7b[:, 0:512], func=AF.Copy), reads=['ps7'], writes=['ost2%d' % ob])
                S.dma('sp', lambda e, ob=ob, h=h, ib=ib: e.dma_start(out=oTc[h * 128:(h + 1) * 128, ib * T:(ib + 1) * T], in_=ost2[ob]),
                      slot='ost2%d' % ob, reads=['ost2%d' % ob], writes=[])
        S.finish()
        print("stage b ninst", S.ninst)
    return nc


D = 4096
KC = 32
S_ = 8192
T = 512
NTG = S_ // T
RMS_EPS = 1e-6
CH = 128


def build_d(NTG_RUN=NTG, heads=(0, 1, 2, 3)):
    nc = bass.Bass("TRN2", target_bir_lowering=False)
    dt_in = lambda name, shape, dt=F32: nc.dram_tensor(name, shape, dt, kind="ExternalInput").ap()
    xT = dt_in("xT", [D, S_])
    mv = dt_in("mv", [128, 2, KC])
    wd = dt_in("wd", [D, 2048])
    lbr_d = dt_in("lbr", [128, 2, 4])
    gn_d = dt_in("gn4", [128, 512])
    mask_d = dt_in("mask01", [128, 128])
    idb_d = dt_in("identb", [128, 128], BF16)
    oTd = nc.dram_tensor("oTd", [512, S_], BF16, kind="ExternalOutput").ap()
    qt_scr = nc.dram_tensor("qt_scr", [4, 128, S_], BF16, kind="Internal").ap()
    kt_scr = nc.dram_tensor("kt_scr", [4, 128, S_], BF16, kind="Internal").ap()
    kh_scr = nc.dram_tensor("kh_scr", [S_, 4, 128], BF16, kind="Internal").ap()
    v_scr = nc.dram_tensor("v_scr", [S_, 4, 128], BF16, kind="Internal").ap()
    sg_scr = nc.dram_tensor("sg_scr", [S_, 4, 128], BF16, kind="Internal").ap()
    NCHK = S_ // CH

    with ExitStack() as es:
        S = Sched(nc, es)
        ar = Arena(nc, es, "arena", 200)
        psf = [es.enter_context(nc.psum_tensor("ps%d" % i, [128, 512], F32)) for i in range(8)]
        psb = [psf[i][:].bitcast(BF16) for i in range(8)]

        mvs = ar.alloc([2, KC], F32)
        sc1p = ar.alloc([KC], F32)
        lbr = ar.alloc([2, 4], F32)
        lbt = ar.alloc([3, 4], F32)
        GN4 = ar.alloc([512], F32)
        mask01 = ar.alloc([128], F32)
        identb = ar.alloc([128], BF16)
        ones = ar.alloc([128], F32)
        dtab = ar.alloc([2, 4, NCHK], F32)
        for (dst, src, nm) in [(mvs, mv, 'mvs'), (lbr, lbr_d, 'lbr'), (GN4, gn_d, 'GN4'), (mask01, mask_d, 'mask01'), (identb, idb_d, 'identb')]:
            S.dma('sp', lambda e, dst=dst, src=src: e.dma_start(out=dst, in_=src), slot=nm, writes=[nm])
        S.op('dve', lambda e: e.memset(ones, 1.0), writes=['ones'])
        S.op('dve', lambda e: e.tensor_scalar(out=sc1p, in0=mvs[:, 1, :], scalar1=1.0, scalar2=None, op0=ALU.add), reads=['mvs'], writes=['sc1p'])
        S.op('dve', lambda e: e.tensor_tensor(out=lbt[:, 2, :], in0=lbr[:, 0, :], in1=lbr[:, 1, :], op=ALU.subtract), reads=['lbr'], writes=['lbt2'])
        S.op('act', lambda e: e.activation(out=lbt[:, 2, :], in_=lbt[:, 2, :], func=AF.Exp), reads=['lbt2'], writes=['lbt2'])
        S.op('dve', lambda e: e.tensor_scalar(out=lbt[:, 2, :], in0=lbt[:, 2, :], scalar1=1.0, scalar2=None, op0=ALU.add), reads=['lbt2'], writes=['lbt2'])
        S.op('dve', lambda e: e.reciprocal(out=lbt[:, 0, :], in_=lbt[:, 2, :]), reads=['lbt2'], writes=['lbt0'])
        S.op('dve', lambda e: e.tensor_scalar(out=lbt[:, 1, :], in0=lbt[:, 0, :], scalar1=-1.0, scalar2=1.0, op0=ALU.mult, op1=ALU.add), reads=['lbt0'], writes=['lbt1'])
        LB = ['lbt0', 'lbt1']
        pmark = ar.mark()

        W = ar.alloc([KC, 1024], BF16)
        hT = ar.alloc([KC, T], BF16)
        NXS = 3
        xs = [ar.alloc([2, T], F32) for i in range(NXS)]
        NTP = 2
        tmp = [[ar.alloc([T], F32) for i in range(6)] for p in range(NTP)]
        NST = 3
        qst = [ar.alloc([T], BF16) for i in range(NST)]
        khfm = [ar.alloc([T], BF16) for i in range(2)]
        khst = [ar.alloc([4, 128], BF16) for i in range(2)]
        vst = [ar.alloc([T], BF16) for i in range(2)]
        sgst = [ar.alloc([T], BF16) for i in range(2)]
        xv = xT.rearrange("(c q) t -> q c t", q=128)
        xit = [0]
        qit = [0]

        def load_W(col0):
            for k in range(KC):
                S.dma('pool', lambda e, k=k: e.dma_start(out=W[:, k, :], in_=wd[k * 128:(k + 1) * 128, col0:col0 + 1024]), slot='W%d' % k, writes=[('W', k)])

        def load_mod(tg):
            tsl = slice(tg * T, (tg + 1) * T)
            for c2 in range(KC // 2):
                b = xit[0] % NXS
                xit[0] += 1
                S.dma('sp', lambda e, b=b, c2=c2, tsl=tsl: e.dma_start(out=xs[b], in_=xv[:, c2 * 2:c2 * 2 + 2, tsl]), slot='xs%d' % b, writes=['xs%d' % b])
                for i in range(2):
                    c = c2 * 2 + i
                    S.op('act', lambda e, b=b, i=i, c=c: e.activation(out=hT[:, c, :], in_=xs[b][:, i, :], func=AF.Identity,
                                                                   scale=sc1p[:, c:c + 1], bias=mvs[:, 0, c:c + 1]),
                         reads=['xs%d' % b, 'sc1p', 'mvs'], writes=[('hT', c)])

        load_W(0)
        hcount = 0
        for tg in range(NTG_RUN):
            tsl = slice(tg * T, (tg + 1) * T)
            load_mod(tg)
            for grp in range(2):
                for k in range(KC):
                    for j in range(4):
                        bank = grp * 4 + j
                        S.op('pe', lambda e, k=k, j=j, grp=grp, bank=bank: e.matmul(psf[bank][:], lhsT=W[:, k, grp * 512 + j * 128:grp * 512 + (j + 1) * 128], rhs=hT[:, k, :],
                                                                                start=(k == 0), stop=(k == KC - 1)),
                             reads=[('W', k), ('hT', k)], writes=['ps%d' % bank], inc=(k == KC - 1))
            for j in range(4):
                pp = hcount % NTP
                hcount += 1
                t0, t1, t2, t3, t4, t5 = tmp[pp]
                R = lambda i, pp=pp: 't%d_%d' % (pp, i)
                qb, fb = 'ps%d' % j, 'ps%d' % (4 + j)
                qps, fps = psf[j], psf[4 + j]
                S.op('act', lambda e, t0=t0, fps=fps: e.activation(out=t0, in_=fps[:], func=AF.Exp, scale=-1.0), reads=[fb], writes=[R(0)])
                S.op('dve', lambda e, t0=t0: e.tensor_scalar(out=t0, in0=t0, scalar1=1.0, scalar2=None, op0=ALU.add), reads=[R(0)], writes=[R(0)])
                S.op('dve', lambda e, t0=t0: e.reciprocal(out=t0, in_=t0), reads=[R(0)], writes=[R(0)])
                S.op('dve', lambda e, t0=t0, t1=t1, j=j: e.tensor_scalar(out=t1, in0=t0, scalar1=lbt[:, 1, j:j + 1], scalar2=lbt[:, 0, j:j + 1], op0=ALU.mult, op1=ALU.add),
                     reads=[R(0)] + LB, writes=[R(1)])
                S.op('dve', lambda e, t1=t1, t2=t2: e.tensor_scalar(out=t2, in0=t1, scalar1=-1.0, scalar2=1.0, op0=ALU.mult, op1=ALU.add), reads=[R(1)], writes=[R(2)])
                S.op('act', lambda e, t0=t0, t1=t1: e.activation(out=t0, in_=t1, func=AF.Ln), reads=[R(1)], writes=[R(0)])
                for c in range(4):
                    cs = slice(c * CH, (c + 1) * CH)
                    S.op('dve', lambda e, t0=t0, t3=t3, cs=cs: e.tensor_tensor_scan(out=t3[:, cs], data0=ones, data1=t0[:, cs], initial=0.0, op0=ALU.mult, op1=ALU.add),
                         reads=[R(0), 'ones'], writes=[R(3)])
                S.op('act', lambda e, t4=t4, qps=qps: e.activation(out=t4, in_=qps[:], func=AF.Exp, scale=-1.0), reads=[qb], writes=[R(4)])
                S.op('dve', lambda e, t4=t4: e.tensor_scalar(out=t4, in0=t4, scalar1=1.0, scalar2=None, op0=ALU.add), reads=[R(4)], writes=[R(4)])
                S.op('dve', lambda e, t4=t4: e.reciprocal(out=t4, in_=t4), reads=[R(4)], writes=[R(4)])
                S.op('dve', lambda e, t4=t4, qps=qps: e.tensor_tensor(out=t4, in0=qps[:], in1=t4, op=ALU.mult), reads=[R(4), qb], writes=[R(4)])
                B3 = t3.rearrange("p (c t) -> p c t", c=4)
                Bref = t3[:, 63:T:CH].unsqueeze(2).to_broadcast([128, 4, CH])
                Blast = t3[:, CH - 1:T:CH].unsqueeze(2).to_broadcast([128, 4, CH])
                t53 = t5.rearrange("p (c t) -> p c t", c=4)
                S.op('dve', lambda e, B3=B3, Bref=Bref, t53=t53: e.tensor_tensor(out=t53, in0=B3, in1=Bref, op=ALU.subtract), reads=[R(3)], writes=[R(5)])
                S.op('act', lambda e, t5=t5: e.activation(out=t5, in_=t5, func=AF.Exp), reads=[R(5)], writes=[R(5)])
                b = qit[0] % NST
                qit[0] += 1
                S.op('dve', lambda e, t4=t4, t5=t5, b=b: e.tensor_tensor(out=qst[b], in0=t4, in1=t5, op=ALU.mult), reads=[R(4), R(5)], writes=['qst%d' % b])
                S.dma('sp', lambda e, b=b, j=j, tsl=tsl: e.dma_start(out=qt_scr[j, :, tsl], in_=qst[b]), slot='qst%d' % b, reads=['qst%d' % b], writes=[])
                S.op('dve', lambda e, B3=B3, Bref=Bref, t53=t53: e.tensor_tensor(out=t53, in0=Bref, in1=B3, op=ALU.subtract), reads=[R(3)], writes=[R(5)])
                S.op('act', lambda e, t5=t5: e.activation(out=t5, in_=t5, func=AF.Exp), reads=[R(5)], writes=[R(5)])
                b = qit[0] % NST
                qit[0] += 1
                S.op('dve', lambda e, t2=t2, t5=t5, b=b: e.scalar_tensor_tensor(out=qst[b], in0=t5, scalar=1e30, in1=t2, op0=ALU.min, op1=ALU.mult), reads=[R(2), R(5)], writes=['qst%d' % b])
                S.dma('sp', lambda e, b=b, j=j, tsl=tsl: e.dma_start(out=kt_scr[j, :, tsl], in_=qst[b]), slot='qst%d' % b, reads=['qst%d' % b], writes=[])
                S.op('dve', lambda e, B3=B3, Blast=Blast, t53=t53: e.tensor_tensor(out=t53, in0=Blast, in1=B3, op=ALU.subtract), reads=[R(3)], writes=[R(5)])
                S.op('act', lambda e, t5=t5: e.activation(out=t5, in_=t5, func=AF.Exp), reads=[R(5)], writes=[R(5)])
                kb = hcount % 2
                S.op('dve', lambda e, t2=t2, t5=t5, kb=kb: e.tensor_tensor(out=khfm[kb], in0=t2, in1=t5, op=ALU.mult), reads=[R(2), R(5)], writes=['khfm%d' % kb])
                for c in range(4):
                    S.op('pe', lambda e, c=c, kb=kb, j=j: e.transpose(out=psb[j][:, c * 128:(c + 1) * 128], in_=khfm[kb][:, c * CH:(c + 1) * CH], identity=identb),
                         reads=['khfm%d' % kb, 'identb'], writes=[qb])
                S.op('act', lambda e, kb=kb, j=j: e.activation(out=khst[kb], in_=psb[j][:, 0:512].rearrange("p (c d) -> p c d", c=4), func=AF.Copy),
                     reads=[qb], writes=['khst%d' % kb])
                S.dma('sp', lambda e, kb=kb, j=j, tg=tg: e.dma_start(out=kh_scr.rearrange("(b p) h d -> p b h d", p=128)[:, tg * 4:(tg + 1) * 4, j, :], in_=khst[kb]),
                      slot='khst%d' % kb, reads=['khst%d' % kb], writes=[])
                S.op('act', lambda e, t3=t3, j=j, tg=tg: e.activation(out=dtab[:, 0, j, tg * 4:(tg + 1) * 4], in_=t3[:, CH - 1:T:CH], func=AF.Exp), reads=[R(3)], writes=[('dtab', j, tg)])
                S.op('act', lambda e, t3=t3, j=j, tg=tg: e.activation(out=dtab[:, 1, j, tg * 4:(tg + 1) * 4], in_=t3[:, 63:T:CH], func=AF.Exp), reads=[R(3)], writes=[('dtab', j, tg)])

        load_W(1024)
        vit = 0
        for tg in range(NTG_RUN):
            load_mod(tg)
            for k in range(KC):
                for tb in range(4):
                    for grp in range(2):
                        bank = grp * 4 + tb
                        S.op('pe', lambda e, k=k, tb=tb, grp=grp, bank=bank: e.matmul(psf[bank][:], lhsT=hT[:, k, tb * 128:(tb + 1) * 128], rhs=W[:, k, grp * 512:(grp + 1) * 512],
                                                                                 start=(k == 0), stop=(k == KC - 1)),
                             reads=[('W', k), ('hT', k)], writes=['ps%d' % bank], inc=(k == KC - 1))
            for tb in range(4):
                vb = vit % 2
                vit += 1
                r0 = tg * T + tb * 128
                S.op('act', lambda e, vb=vb, tb=tb: e.activation(out=vst[vb], in_=psf[tb][:], func=AF.Copy), reads=['ps%d' % tb], writes=['vst%d' % vb])
                S.dma('sp', lambda e, vb=vb, r0=r0: e.dma_start(out=v_scr[r0:r0 + 128, :, :].rearrange("p h d -> p (h d)"), in_=vst[vb]), slot='vst%d' % vb, reads=['vst%d' % vb], writes=[])
                pp = vit % NTP
                t0 = tmp[pp][0]
                tr = 't%d_0' % pp
                gb = 'ps%d' % (4 + tb)
                S.op('act', lambda e, t0=t0, tb=tb: e.activation(out=t0, in_=psf[4 + tb][:], func=AF.Exp, scale=-1.0), reads=[gb], writes=[tr])
                S.op('dve', lambda e, t0=t0: e.tensor_scalar(out=t0, in0=t0, scalar1=1.0, scalar2=None, op0=ALU.add), reads=[tr], writes=[tr])
                S.op('dve', lambda e, t0=t0: e.reciprocal(out=t0, in_=t0), reads=[tr], writes=[tr])
                S.op('dve', lambda e, t0=t0, tb=tb: e.tensor_tensor(out=t0, in0=psf[4 + tb][:], in1=t0, op=ALU.mult), reads=[tr, gb], writes=[tr])
                S.op('dve', lambda e, t0=t0, vb=vb: e.tensor_tensor(out=sgst[vb], in0=t0, in1=GN4, op=ALU.mult), reads=[tr, 'GN4'], writes=['sgst%d' % vb])
                S.dma('sp', lambda e, vb=vb, r0=r0: e.dma_start(out=sg_scr[r0:r0 + 128, :, :].rearrange("p h d -> p (h d)"), in_=sgst[vb]), slot='sgst%d' % vb, reads=['sgst%d' % vb], writes=[])

        S.barrier()
        ar.reset(pmark)
        NC_RUN = NTG_RUN * 4
        QT = [ar.alloc([S_], BF16) for i in range(2)]
        KT = [ar.alloc([S_], BF16) for i in range(2)]
        KH = [ar.alloc([NCHK, 128], BF16) for i in range(2)]
        VV = [ar.alloc([NCHK, 128], BF16) for i in range(2)]
        SG = [ar.alloc([NCHK, 128], BF16) for i in range(2)]
        St = ar.alloc([128], F32)
        Sbf = [ar.alloc([128], BF16) for i in range(2)]
        ATs = [ar.alloc([128], BF16) for i in range(2)]
        sc = ar.alloc([4, 4], F32)
        junk = ar.alloc([128], F32)
        on = [ar.alloc([128], BF16) for i in range(2)]
        ost = [ar.alloc([T], BF16) for i in range(2)]
        for i in range(2):
            S.op('dve', lambda e, i=i: e.memset(ATs[i], 0.0), writes=['ATs%d' % i])
        tokv = lambda scr: scr.rearrange("(b p) h d -> p b h d", p=128)
        oi = 0
        for hi, h in enumerate(heads):
            hb = hi % 2
            S.dma('sp', lambda e, h=h, hb=hb: e.dma_start(out=QT[hb], in_=qt_scr[h]), slot='QT%d' % hb, writes=['QT%d' % hb])
            S.dma('sp', lambda e, h=h, hb=hb: e.dma_start(out=KT[hb], in_=kt_scr[h]), slot='KT%d' % hb, writes=['KT%d' % hb])
            S.dma('sp', lambda e, h=h, hb=hb: e.dma_start(out=KH[hb], in_=tokv(kh_scr)[:, :, h, :]), slot='KH%d' % hb, writes=['KH%d' % hb])
            S.dma('sp', lambda e, h=h, hb=hb: e.dma_start(out=VV[hb], in_=tokv(v_scr)[:, :, h, :]), slot='VV%d' % hb, writes=['VV%d' % hb])
            S.dma('sp', lambda e, h=h, hb=hb: e.dma_start(out=SG[hb], in_=tokv(sg_scr)[:, :, h, :]), slot='SG%d' % hb, writes=['SG%d' % hb])
            S.op('dve', lambda e: e.memset(St, 0.0), writes=['St'])
            S.op('dve', lambda e: e.memset(Sbf[0], 0.0), writes=['Sbf0'])

            def emit_AU(c, h=h, hb=hb):
                cs = slice(c * CH, (c + 1) * CH)
                a = c % 2
                S.op('pe', lambda e: e.matmul(psf[a][:, 0:128], lhsT=KT[hb][:, cs], rhs=QT[hb][:, cs], start=True, stop=True),
                     reads=['KT%d' % hb, 'QT%d' % hb], writes=['ps%d' % a])
                S.op('dve', lambda e: e.copy_predicated(out=ATs[a], mask=mask01.bitcast(mybir.dt.uint32), data=psf[a][:, 0:128]), reads=['ps%d' % a, 'mask01', 'ATs%d' % a], writes=['ATs%d' % a])
                S.op('pe', lambda e: e.matmul(psf[2 + a][:, 0:128], lhsT=KH[hb][:, c, :], rhs=VV[hb][:, c, :], start=True, stop=True),
                     reads=['KH%d' % hb, 'VV%d' % hb], writes=['ps%d' % (2 + a)])

            emit_AU(0)
            for c in range(NC_RUN):
                a = c % 2
                cs = slice(c * CH, (c + 1) * CH)
                if c + 1 < NC_RUN:
                    emit_AU(c + 1)
                ob = 'ps%d' % (4 + a)
                S.op('pe', lambda e, a=a, c=c, hb=hb: e.matmul(psf[4 + a][:, 0:128], lhsT=ATs[a], rhs=VV[hb][:, c, :], start=True, stop=False),
                     reads=['ATs%d' % a, 'VV%d' % hb], writes=[ob], inc=False)
                S.op('pe', lambda e, a=a, cs=cs, hb=hb: e.matmul(psf[4 + a][:, 0:128], lhsT=QT[hb][:, cs], rhs=Sbf[a], start=False, stop=True),
                     reads=['QT%d' % hb, 'Sbf%d' % a], writes=[ob])
                S.op('dve', lambda e, a=a, c=c, h=h: e.scalar_tensor_tensor(out=St, in0=St, scalar=dtab[:, 0, h, c:c + 1], in1=psf[2 + a][:, 0:128], op0=ALU.mult, op1=ALU.add),
                     reads=['St', 'ps%d' % (2 + a), ('dtab', h, c // 4)], writes=['St'])
                if c + 1 < NC_RUN:
                    S.op('act', lambda e, a=a, c=c, h=h: e.activation(out=Sbf[1 - a], in_=St, func=AF.Identity, scale=dtab[:, 1, h, c + 1:c + 2]),
                         reads=['St', ('dtab', h, (c + 1) // 4)], writes=['Sbf%d' % (1 - a)])
                s4 = c % 4
                S.op('act', lambda e, a=a, s4=s4: e.activation(out=junk, in_=psf[4 + a][:, 0:128], func=AF.Square, accum_out=sc[:, s4, 0:1]), reads=[ob], writes=['junk', ('sc0', s4)])
                S.op('dve', lambda e, s4=s4: e.tensor_scalar(out=sc[:, s4, 0:1], in0=sc[:, s4, 0:1], scalar1=1.0 / 128, scalar2=RMS_EPS, op0=ALU.mult, op1=ALU.add),
                     reads=[('sc0', s4)], writes=[('sc0', s4)])
                S.op('act', lambda e, s4=s4: e.activation(out=sc[:, s4, 1:2], in_=sc[:, s4, 0:1], func=AF.Sqrt), reads=[('sc0', s4)], writes=[('sc1', s4)])
                S.op('dve', lambda e, s4=s4: e.reciprocal(out=sc[:, s4, 1:2], in_=sc[:, s4, 1:2]), reads=[('sc1', s4)], writes=[('sc1', s4)])
                S.op('dve', lambda e, a=a, s4=s4, c=c, hb=hb: e.scalar_tensor_tensor(out=on[a], in0=psf[4 + a][:, 0:128], scalar=sc[:, s4, 1:2], in1=SG[hb][:, c, :], op0=ALU.mult, op1=ALU.mult),
                     reads=[ob, ('sc1', s4), 'SG%d' % hb], writes=['on%d' % a])
                tbk = 6 + (c // 4) % 2
                S.op('pe', lambda e, a=a, s4=s4, tbk=tbk: e.transpose(out=psb[tbk][:, s4 * 128:(s4 + 1) * 128], in_=on[a], identity=identb),
                     reads=['on%d' % a, 'identb'], writes=['ps%d' % tbk])
                if s4 == 3:
                    o2 = oi % 2
                    oi += 1
                    g4 = c // 4
                    S.op('act', lambda e, o2=o2, tbk=tbk: e.activation(out=ost[o2], in_=psb[tbk][:, 0:512], func=AF.Copy), reads=['ps%d' % tbk], writes=['ost%d' % o2])
                    S.dma('sp', lambda e, o2=o2, h=h, g4=g4: e.dma_start(out=oTd[h * 128:(h + 1) * 128, g4 * T:(g4 + 1) * T], in_=ost[o2]),
                          slot='ost%d' % o2, reads=['ost%d' % o2], writes=[])
        S.finish()
        print("stage d ninst", S.ninst)
    return nc


D = 4096
KC = 32
T = 512
ALPHA = 4 ** 0.25
LN_EPS = 1e-5


def build_ff(moe, TOK=1024, n_groups_limit=None, mode='full'):
    nc = bass.Bass("TRN2", target_bir_lowering=False)
    NPASS = TOK // T
    oT = nc.dram_tensor("oT", [D, TOK], BF16, kind="ExternalInput").ap()
    xT = nc.dram_tensor("xT", [D, TOK], F32, kind="ExternalInput").ap()
    pv = nc.dram_tensor("pv", [128, 8, KC], F32, kind="ExternalInput").ap()
    w_o = nc.dram_tensor("w_o", [D, D], F32, kind="ExternalInput").ap()
    ones_d = nc.dram_tensor("ones", [128, 128], F32, kind="ExternalInput").ap()
    if moe:
        NE, H = 8, 4096
        rw_d = nc.dram_tensor("rw", [128, KC, 8], F32, kind="ExternalInput").ap()
        groups = []
        if mode == 'full':
            w_in = nc.dram_tensor("w_in", [NE, D, 2 * H], F32, kind="ExternalInput").ap()
            w_o2 = nc.dram_tensor("w_o2", [NE, H, D], F32, kind="ExternalInput").ap()
            sel_d = nc.dram_tensor("sel", [8, 8, 128], F32, kind="ExternalInput").ap()
            ident_d = nc.dram_tensor("ident", [128, 128], F32, kind="ExternalInput").ap()
            for e in range(NE):
                groups.append(dict(win=w_in[e].rearrange("k (s c) -> k s c", s=2), wo=w_o2[e],
                                   pairs=list(range(16)), gate=e))
    else:
        H = 11008
        w_in = nc.dram_tensor("w_in", [D, 2 * H], F32, kind="ExternalInput").ap()
        w_o2 = nc.dram_tensor("w_o2", [H, D], F32, kind="ExternalInput").ap()
        win_v = w_in.rearrange("k (s c) -> k s c", s=2)
        groups = [dict(win=win_v, wo=w_o2, pairs=list(range(0, 16)), gate=None),
                  dict(win=win_v, wo=w_o2, pairs=list(range(16, 32)), gate=None),
                  dict(win=win_v, wo=w_o2, pairs=list(range(32, 43)), gate=None)]
    if n_groups_limit is not None:
        groups = groups[:n_groups_limit]
    if mode == 'full':
        yT = nc.dram_tensor("yT", [D, TOK], F32, kind="ExternalOutput").ap()
    else:
        h2o = nc.dram_tensor("h2o", [D, TOK], BF16, kind="ExternalOutput").ap()
        acco = nc.dram_tensor("acco", [D, TOK], F32, kind="ExternalOutput").ap()
        gout = nc.dram_tensor("gout", [TOK, 8], F32, kind="ExternalOutput").ap()

    with ExitStack() as es:
        S = Sched(nc, es)
        sb = lambda name, shape, dt: es.enter_context(nc.sbuf_tensor(name, shape, dt))
        acc = sb("acc", [128, KC, T], F32)
        actT = sb("actT", [128, KC, T], BF16)
        h2T = sb("h2T", [128, KC, T], BF16)
        NB = 8
        wt = [sb("wt%d" % i, [128, 512], BF16) for i in range(NB)]
        pvs = sb("pvs", [128, 8, KC], F32)
        dv = sb("dv", [128, 6, KC], F32)
        ones = sb("ones_s", [128, 128], F32)
        s1 = sb("s1", [128, T], F32)
        s2 = sb("s2", [128, T], F32)
        mean = sb("mean", [128, T], F32)
        msq = sb("msq", [128, T], F32)
        rstd = sb("rstd", [128, T], F32)
        NTB = 3
        tb_ = [sb("tb%d" % i, [128, T], F32) for i in range(NTB)]
        sa_ = [sb("sa%d" % i, [128, T], F32) for i in range(NTB)]
        yo_ = [sb("yo%d" % i, [128, T], F32) for i in range(NTB)]
        ps = [es.enter_context(nc.psum_tensor("ps%d" % i, [128, 512], F32)) for i in range(8)]
        if moe:
            rw = sb("rw_s", [128, KC, 8], F32)
            if mode == 'full':
                sel = sb("sel_s", [8, 8, 128], F32)
                ident = sb("ident_s", [128, 128], F32)
            h2f_ = [sb("h2f%d" % i, [128, T], F32) for i in range(2)]
            G_ = [sb("G%d" % i, [128, T], F32) for i in range(2)]
            bg_ = [sb("bg%d" % i, [128, T], F32) for i in range(NTB)]
            gT = sb("gT", [8, T], F32)
            Lsb = sb("Lsb", [128, 4, 8], F32)
            m8 = sb("m8", [128, 4, 8], F32)
            nv1 = sb("nv1", [128, 4], F32)
            msk = sb("msk", [128, 4, 8], F32)
            ex = sb("ex", [128, 4, 8], F32)
            me = sb("me", [128, 4, 8], F32)
            den = sb("den", [128, 4], F32)
            gsb = sb("gsb", [128, 4, 8], F32)

        S.dma('sp', lambda e: e.dma_start(out=pvs[:], in_=pv), slot='pvs', writes=['pvs'])
        S.dma('sp', lambda e: e.dma_start(out=ones[:], in_=ones_d), slot='ones', writes=['ones'])
        if moe:
            S.dma('sp', lambda e: e.dma_start(out=rw[:], in_=rw_d), slot='rw', writes=['rw'])
            if mode == 'full':
                S.dma('sp', lambda e: e.dma_start(out=sel[:], in_=sel_d), slot='sel', writes=['sel'])
                S.dma('sp', lambda e: e.dma_start(out=ident[:], in_=ident_d), slot='ident', writes=['ident'])
        V = lambda i: pvs[:, i, :]
        S.op('dve', lambda e: e.tensor_scalar(out=dv[:, 0, :], in0=V(0), scalar1=1.0, scalar2=None, op0=ALU.add), reads=['pvs'], writes=['dv0'])
        S.op('dve', lambda e: e.tensor_scalar(out=dv[:, 5, :], in0=V(2), scalar1=1.0, scalar2=None, op0=ALU.add), reads=['pvs'], writes=['dv5'])
        S.op('dve', lambda e: e.tensor_tensor(out=dv[:, 1, :], in0=V(4), in1=dv[:, 5, :], op=ALU.mult), reads=['pvs', 'dv5'], writes=['dv1'])
        S.op('dve', lambda e: e.tensor_tensor(out=dv[:, 2, :], in0=V(5), in1=dv[:, 5, :], op=ALU.mult), reads=['pvs', 'dv5'], writes=['dv2'])
        S.op('dve', lambda e: e.tensor_tensor(out=dv[:, 2, :], in0=dv[:, 2, :], in1=V(1), op=ALU.add), reads=['pvs', 'dv2'], writes=['dv2'])
        S.op('dve', lambda e: e.tensor_scalar(out=dv[:, 3, :], in0=V(4), scalar1=ALPHA, scalar2=None, op0=ALU.mult), reads=['pvs'], writes=['dv3'])
        S.op('dve', lambda e: e.tensor_scalar(out=dv[:, 4, :], in0=V(5), scalar1=ALPHA, scalar2=None, op0=ALU.mult), reads=['pvs'], writes=['dv4'])
        S.op('dve', lambda e: e.tensor_scalar(out=dv[:, 5, :], in0=V(3), scalar1=1.0, scalar2=None, op0=ALU.add), reads=['pvs', 'dv1', 'dv2'], writes=['dv5'])
        DVR = ['dv0', 'dv1', 'dv2', 'dv3', 'dv4', 'dv5', 'pvs']

        witer = [0]

        def wtile_load(src_ap, three=False):
            b = witer[0] % NB
            witer[0] += 1
            if three:
                S.dma('pool', lambda e: e.dma_start(out=wt[b][:].rearrange("p (s c) -> p s c", s=2), in_=src_ap),
                      slot='wt%d' % b, writes=['wt%d' % b])
            else:
                S.dma('pool', lambda e: e.dma_start(out=wt[b][:], in_=src_ap), slot='wt%d' % b, writes=['wt%d' % b])
            return b

        bankset = [0]

        def gemm_group(tiles, rhs_of, rhs_res, evac):
            base = (bankset[0] % 2) * 4
            bankset[0] += 1
            nk = len(tiles)
            for ki, (src, three) in enumerate(tiles):
                b = wtile_load(src, three)
                for j in range(4):
                    bank = base + j
                    S.op('pe', lambda e, b=b, j=j, ki=ki, bank=bank: e.matmul(
                        ps[bank][:], lhsT=wt[b][:, j * 128:(j + 1) * 128], rhs=rhs_of(ki),
                        start=(ki == 0), stop=(ki == nk - 1)),
                        reads=['wt%d' % b] + rhs_res(ki), writes=['ps%d' % bank], inc=(j == 3))
            for j in range(4):
                evac(j, base + j)

        def layernorm(final, ps_pass):
            for c in range(KC):
                if c == 0:
                    S.op('dve', lambda e: e.tensor_copy(out=s1[:], in_=acc[:, 0, :]), reads=[('acc', 0)], writes=['s1'])
                    S.op('act', lambda e: e.activation(out=s2[:], in_=acc[:, 0, :], func=AF.Square), reads=[('acc', 0)], writes=['s2'])
                else:
                    t = tb_[c % NTB]
                    tr = 'tb%d' % (c % NTB)
                    S.op('act', lambda e, c=c, t=t: e.activation(out=t[:], in_=acc[:, c, :], func=AF.Square), reads=[('acc', c)], writes=[tr])
                    S.op('dve', lambda e, c=c: e.tensor_tensor(out=s1[:], in0=s1[:], in1=acc[:, c, :], op=ALU.add), reads=[('acc', c), 's1'], writes=['s1'])
                    S.op('dve', lambda e, t=t: e.tensor_tensor(out=s2[:], in0=s2[:], in1=t[:], op=ALU.add), reads=[tr, 's2'], writes=['s2'])
            S.op('pe', lambda e: e.matmul(ps[4][:], lhsT=ones[:], rhs=s1[:], start=True, stop=True), reads=['ones', 's1'], writes=['ps4'])
            S.op('pe', lambda e: e.matmul(ps[5][:], lhsT=ones[:], rhs=s2[:], start=True, stop=True), reads=['ones', 's2'], writes=['ps5'])
            S.op('act', lambda e: e.activation(out=mean[:], in_=ps[4][:], func=AF.Copy, scale=1.0 / D), reads=['ps4'], writes=['mean'])
            S.op('dve', lambda e: e.tensor_tensor(out=msq[:], in0=mean[:], in1=mean[:], op=ALU.mult), reads=['mean'], writes=['msq'])
            S.op('dve', lambda e: e.scalar_tensor_tensor(out=msq[:], in0=ps[5][:], scalar=1.0 / D, in1=msq[:], op0=ALU.mult, op1=ALU.subtract),
                 reads=['ps5', 'msq'], writes=['msq'])
            S.op('dve', lambda e: e.tensor_scalar(out=msq[:], in0=msq[:], scalar1=LN_EPS, scalar2=None, op0=ALU.add), reads=['msq'], writes=['msq'])
            S.op('act', lambda e: e.activation(out=rstd[:], in_=msq[:], func=AF.Sqrt), reads=['msq'], writes=['rstd'])
            S.op('dve', lambda e: e.reciprocal(out=rstd[:], in_=rstd[:]), reads=['rstd'], writes=['rstd'])
            for c in range(KC):
                t = tb_[c % NTB]
                tr = 'tb%d' % (c % NTB)
                S.op('dve', lambda e, c=c, t=t: e.tensor_tensor(out=t[:], in0=acc[:, c, :], in1=mean[:], op=ALU.subtract), reads=[('acc', c), 'mean'], writes=[tr])
                S.op('dve', lambda e, t=t: e.tensor_tensor(out=t[:], in0=t[:], in1=rstd[:], op=ALU.mult), reads=[tr, 'rstd'], writes=[tr])
                if not final:
                    S.op('act', lambda e, c=c, t=t: e.activation(out=h2T[:, c, :], in_=t[:], func=AF.Identity, scale=dv[:, 1, c:c + 1], bias=dv[:, 2, c:c + 1]),
                         reads=[tr] + DVR, writes=[('h2T', c)])
                    if moe:
                        hf = h2f_[c % 2]
                        hr = 'h2f%d' % (c % 2)
                        S.op('dve', lambda e, c=c, t=t, hf=hf: e.tensor_scalar(out=hf[:], in0=t[:], scalar1=dv[:, 1, c:c + 1], scalar2=dv[:, 2, c:c + 1], op0=ALU.mult, op1=ALU.add),
                             reads=[tr] + DVR, writes=[hr])
                        for q in range(4):
                            S.op('pe', lambda e, c=c, q=q, hf=hf: e.matmul(ps[q][:, 0:8], lhsT=hf[:, q * 128:(q + 1) * 128], rhs=rw[:, c, :],
                                                                     start=(c == 0), stop=(c == KC - 1)),
                                 reads=[hr, 'rw'], writes=['ps%d' % q])
                    S.op('act', lambda e, c=c, t=t: e.activation(out=acc[:, c, :], in_=t[:], func=AF.Identity, scale=dv[:, 3, c:c + 1], bias=dv[:, 4, c:c + 1]),
                         reads=[tr] + DVR, writes=[('acc', c)])
                else:
                    yo = yo_[c % NTB]
                    yr = 'yo%d' % (c % NTB)
                    S.op('act', lambda e, c=c, t=t, yo=yo: e.activation(out=yo[:], in_=t[:], func=AF.Identity, scale=pvs[:, 6, c:c + 1], bias=pvs[:, 7, c:c + 1]),
                         reads=[tr] + DVR, writes=[yr])
                    S.dma('sp', lambda e, c=c, yo=yo: e.dma_start(out=yT[c * 128:(c + 1) * 128, ps_pass * T:(ps_pass + 1) * T], in_=yo[:]),
                          slot=yr, reads=[yr], writes=[])

        for p in range(NPASS):
            tsl = slice(p * T, (p + 1) * T)
            xv = xT.rearrange("(c q) t -> q c t", q=128)
            ov = oT.rearrange("(c q) t -> q c t", q=128)
            for g4 in range(4):
                cs = slice(g4 * 8, (g4 + 1) * 8)
                S.dma('sp', lambda e, cs=cs, tsl=tsl, xv=xv: e.dma_start(out=acc[:, cs, :], in_=xv[:, cs, tsl]), slot='accld%d' % g4,
                      writes=[('acc', c) for c in range(g4 * 8, g4 * 8 + 8)])
                S.dma('sp', lambda e, cs=cs, tsl=tsl, ov=ov: e.dma_start(out=actT[:, cs, :], in_=ov[:, cs, tsl]), slot='actld%d' % g4,
                      writes=[('actT', c) for c in range(g4 * 8, g4 * 8 + 8)])
            for c in range(KC):
                S.op('act', lambda e, c=c: e.activation(out=acc[:, c, :], in_=acc[:, c, :], func=AF.Copy, scale=ALPHA),
                     reads=[('acc', c)], writes=[('acc', c)])
            for ng in range(D // 512):
                tiles = [(w_o[k * 128:(k + 1) * 128, ng * 512:(ng + 1) * 512], False) for k in range(KC)]

                def evac(j, bank, ng=ng):
                    n = ng * 4 + j
                    S.op('dve', lambda e: e.scalar_tensor_tensor(out=acc[:, n, :], in0=ps[bank][:], scalar=dv[:, 0, n:n + 1], in1=acc[:, n, :],
                                                                 op0=ALU.mult, op1=ALU.add),
                         reads=['ps%d' % bank, ('acc', n)] + DVR, writes=[('acc', n)])
                gemm_group(tiles, lambda ki: actT[:, ki, :], lambda ki: [('actT', ki)], evac)
            layernorm(False, p)
            if moe:
                for q in range(4):
                    S.op('dve', lambda e, q=q: e.tensor_copy(out=Lsb[:, q, :], in_=ps[q][:, 0:8]), reads=['ps%d' % q], writes=[('L', q)])
                    S.op('dve', lambda e, q=q: e.max(out=m8[:, q, :], in_=Lsb[:, q, :]), reads=[('L', q)], writes=[('m8', q)])
                    S.op('dve', lambda e, q=q: e.tensor_scalar(out=nv1[:, q:q + 1], in0=m8[:, q, 0:1], scalar1=-1.0, scalar2=None, op0=ALU.mult),
                         reads=[('m8', q)], writes=[('nv1', q)])
                    S.op('dve', lambda e, q=q: e.tensor_scalar(out=msk[:, q, :], in0=Lsb[:, q, :], scalar1=m8[:, q, 1:2], scalar2=None, op0=ALU.is_ge),
                         reads=[('m8', q), ('L', q)], writes=[('msk', q)])
                    S.op('act', lambda e, q=q: e.activation(out=ex[:, q, :], in_=Lsb[:, q, :], func=AF.Exp, bias=nv1[:, q:q + 1], scale=1.0),
                         reads=[('L', q), ('nv1', q)], writes=[('ex', q)])
                    S.op('dve', lambda e, q=q: e.tensor_tensor(out=me[:, q, :], in0=msk[:, q, :], in1=ex[:, q, :], op=ALU.mult),
                         reads=[('msk', q), ('ex', q)], writes=[('me', q)])
                    S.op('dve', lambda e, q=q: e.reduce_sum(out=den[:, q:q + 1], in_=me[:, q, :], axis=AX.X),
                         reads=[('me', q)], writes=[('den', q)])
                    S.op('dve', lambda e, q=q: e.reciprocal(out=den[:, q:q + 1], in_=den[:, q:q + 1]), reads=[('den', q)], writes=[('den', q)])
                    S.op('dve', lambda e, q=q: e.tensor_scalar(out=gsb[:, q, :], in0=me[:, q, :], scalar1=den[:, q:q + 1], scalar2=None, op0=ALU.mult),
                         reads=[('me', q), ('den', q)], writes=[('gsb', q)])
                    if mode == 'full':
                        S.op('pe', lambda e, q=q: e.transpose(out=ps[6][0:8, q * 128:(q + 1) * 128], in_=gsb[:, q, :], identity=ident[:]),
                             reads=[('gsb', q), 'ident'], writes=['ps6'])
                if mode == 'full':
                    S.op('act', lambda e: e.activation(out=gT[:], in_=ps[6][0:8, :], func=AF.Copy), reads=['ps6'], writes=['gT'])
            if mode == 'e1':
                hv = h2o.rearrange("(c q) t -> q c t", q=128)
                av = acco.rearrange("(c q) t -> q c t", q=128)
                for g4 in range(4):
                    cs = slice(g4 * 8, (g4 + 1) * 8)
                    S.dma('sp', lambda e, cs=cs, tsl=tsl, hv=hv: e.dma_start(out=hv[:, cs, tsl], in_=h2T[:, cs, :]), slot='h2o%d' % g4,
                          reads=[('h2T', c) for c in range(g4 * 8, g4 * 8 + 8)], writes=[])
                    S.dma('sp', lambda e, cs=cs, tsl=tsl, av=av: e.dma_start(out=av[:, cs, tsl], in_=acc[:, cs, :]), slot='acco%d' % g4,
                          reads=[('acc', c) for c in range(g4 * 8, g4 * 8 + 8)], writes=[])
                S.dma('sp', lambda e, p=p: e.dma_start(out=gout.rearrange("(q r) x -> r q x", r=128)[:, p * 4:(p + 1) * 4, :], in_=gsb[:]), slot='gout',
                      reads=[('gsb', q) for q in range(4)], writes=[])
                continue
            for gi, g in enumerate(groups):
                if g['gate'] is not None:
                    G = G_[gi % 2]
                    Gr = 'G%d' % (gi % 2)
                    ge = g['gate']
                    S.op('pe', lambda e, ge=ge: e.matmul(ps[7][:], lhsT=sel[:, ge, :], rhs=gT[:], start=True, stop=True),
                         reads=['sel', 'gT'], writes=['ps7'])
                    S.op('act', lambda e, G=G: e.activation(out=G[:], in_=ps[7][:], func=AF.Copy), reads=['ps7'], writes=[Gr])
                pairs = g['pairs']
                chunks = []
                for pi, P in enumerate(pairs):
                    tiles = [(g['win'][k * 128:(k + 1) * 128, :, P * 256:(P + 1) * 256], True) for k in range(KC)]
                    lj0 = pi * 2

                    def evac(j, bank, lj0=lj0, g=g):
                        if j >= 2:
                            return
                        lj = lj0 + j
                        abank, bbank = bank, bank + 2
                        i3 = lj % NTB
                        sa = sa_[i3]
                        S.op('act', lambda e: e.activation(out=sa[:], in_=ps[abank][:], func=AF.Silu), reads=['ps%d' % abank], writes=['sa%d' % i3])
                        if g['gate'] is not None:
                            bg = bg_[i3]
                            Gg = G_[gi % 2]
                            S.op('dve', lambda e: e.tensor_tensor(out=bg[:], in0=ps[bbank][:], in1=Gg[:], op=ALU.mult),
                                 reads=['ps%d' % bbank, 'G%d' % (gi % 2)], writes=['bg%d' % i3])
                            S.op('dve', lambda e: e.tensor_tensor(out=actT[:, lj, :], in0=sa[:], in1=bg[:], op=ALU.mult),
                                 reads=['sa%d' % i3, 'bg%d' % i3], writes=[('actT', lj)])
                        else:
                            S.op('dve', lambda e: e.tensor_tensor(out=actT[:, lj, :], in0=sa[:], in1=ps[bbank][:], op=ALU.mult),
                                 reads=['sa%d' % i3, 'ps%d' % bbank], writes=[('actT', lj)])
                    gemm_group(tiles, lambda ki: h2T[:, ki, :], lambda ki: [('h2T', ki)], evac)
                    chunks += [(lj0, P * 2), (lj0 + 1, P * 2 + 1)]
                for ng in range(D // 512):
                    tiles = [(g['wo'][gj * 128:(gj + 1) * 128, ng * 512:(ng + 1) * 512], False) for (lj, gj) in chunks]

                    def evac2(j, bank, ng=ng):
                        n = ng * 4 + j
                        S.op('dve', lambda e: e.scalar_tensor_tensor(out=acc[:, n, :], in0=ps[bank][:], scalar=dv[:, 5, n:n + 1], in1=acc[:, n, :],
                                                                     op0=ALU.mult, op1=ALU.add),
                             reads=['ps%d' % bank, ('acc', n)] + DVR, writes=[('acc', n)])
                    gemm_group(tiles, lambda ki, chunks=chunks: actT[:, chunks[ki][0], :], lambda ki, chunks=chunks: [('actT', chunks[ki][0])], evac2)
            layernorm(True, p)
        S.finish()
        print("ff ninst", S.ninst)
    return nc


def build_e2(NP):
    nc = bass.Bass("TRN2", target_bir_lowering=False)
    H = 4096
    hT_d = nc.dram_tensor("hT", [D, NP], BF16, kind="ExternalInput").ap()
    gb_d = nc.dram_tensor("gb", [128, NP], F32, kind="ExternalInput").ap()
    w_in = nc.dram_tensor("w_in", [D, 2 * H], F32, kind="ExternalInput").ap()
    w_o2 = nc.dram_tensor("w_o2", [H, D], F32, kind="ExternalInput").ap()
    yT = nc.dram_tensor("yT", [D, NP], F32, kind="ExternalOutput").ap()
    win_v = w_in.rearrange("k (s c) -> k s c", s=2)
    with ExitStack() as es:
        S = Sched(nc, es)
        sb = lambda name, shape, dt: es.enter_context(nc.sbuf_tensor(name, shape, dt))
        actT = sb("actT", [128, KC, T], BF16)
        h2T = [sb("h2T%d" % i, [128, KC, T], BF16) for i in range(2)]
        NB = 8
        wt = [sb("wt%d" % i, [128, 512], BF16) for i in range(NB)]
        NTB = 3
        sa_ = [sb("sa%d" % i, [128, T], F32) for i in range(NTB)]
        bg_ = [sb("bg%d" % i, [128, T], F32) for i in range(NTB)]
        yo_ = [sb("yo%d" % i, [128, T], F32) for i in range(4)]
        G_ = [sb("G%d" % i, [128, T], F32) for i in range(2)]
        ps = [es.enter_context(nc.psum_tensor("ps%d" % i, [128, 512], F32)) for i in range(8)]
        witer = [0]
        bankset = [0]
        yit = [0]

        def wtile_load(src_ap, three):
            b = witer[0] % NB
            witer[0] += 1
            if three:
                S.dma('pool', lambda e: e.dma_start(out=wt[b][:].rearrange("p (s c) -> p s c", s=2), in_=src_ap), slot='wt%d' % b, writes=['wt%d' % b])
            else:
                S.dma('pool', lambda e: e.dma_start(out=wt[b][:], in_=src_ap), slot='wt%d' % b, writes=['wt%d' % b])
            return b

        def gemm_group(tiles, rhs_of, rhs_res, evac):
            base = (bankset[0] % 2) * 4
            bankset[0] += 1
            nk = len(tiles)
            for ki, (src, three) in enumerate(tiles):
                b = wtile_load(src, three)
                for j in range(4):
                    bank = base + j
                    S.op('pe', lambda e, b=b, j=j, ki=ki, bank=bank: e.matmul(ps[bank][:], lhsT=wt[b][:, j * 128:(j + 1) * 128], rhs=rhs_of(ki),
                                                                          start=(ki == 0), stop=(ki == nk - 1)),
                         reads=['wt%d' % b] + rhs_res(ki), writes=['ps%d' % bank], inc=(j == 3))
            for j in range(4):
                evac(j, base + j)

        hv = hT_d.rearrange("(c q) t -> q c t", q=128)
        for p in range(NP // T):
            tsl = slice(p * T, (p + 1) * T)
            hb = p % 2
            hT = h2T[hb]
            G = G_[hb]
            for g4 in range(4):
                cs = slice(g4 * 8, (g4 + 1) * 8)
                S.dma('sp', lambda e, cs=cs, tsl=tsl, hT=hT: e.dma_start(out=hT[:, cs, :], in_=hv[:, cs, tsl]), slot='hld%d_%d' % (hb, g4),
                      writes=[('h2T', hb, c) for c in range(g4 * 8, g4 * 8 + 8)])
            S.dma('sp', lambda e, tsl=tsl, G=G: e.dma_start(out=G[:], in_=gb_d[:, tsl]), slot='G%d' % hb, writes=['G%d' % hb])
            chunks = []
            for P in range(16):
                tiles = [(win_v[k * 128:(k + 1) * 128, :, P * 256:(P + 1) * 256], True) for k in range(KC)]

                def evac(j, bank, P=P, hb=hb, G=G):
                    if j >= 2:
                        return
                    lj = P * 2 + j
                    abank, bbank = bank, bank + 2
                    i3 = lj % NTB
                    sa, bg = sa_[i3], bg_[i3]
                    S.op('act', lambda e: e.activation(out=sa[:], in_=ps[abank][:], func=AF.Silu), reads=['ps%d' % abank], writes=['sa%d' % i3])
                    S.op('dve', lambda e: e.tensor_tensor(out=bg[:], in0=ps[bbank][:], in1=G[:], op=ALU.mult), reads=['ps%d' % bbank, 'G%d' % hb], writes=['bg%d' % i3])
                    S.op('dve', lambda e: e.tensor_tensor(out=actT[:, lj, :], in0=sa[:], in1=bg[:], op=ALU.mult), reads=['sa%d' % i3, 'bg%d' % i3], writes=[('actT', lj)])
                gemm_group(tiles, lambda ki, hT=hT: hT[:, ki, :], lambda ki, hb=hb: [('h2T', hb, ki)], evac)
            for ng in range(D // 512):
                tiles = [(w_o2[j * 128:(j + 1) * 128, ng * 512:(ng + 1) * 512], False) for j in range(KC)]

                def evac2(j, bank, ng=ng, tsl=tsl):
                    n = ng * 4 + j
                    yi = yit[0] % 4
                    yit[0] += 1
                    yo = yo_[yi]
                    if j % 2 == 0:
                        S.op('act', lambda e: e.activation(out=yo[:], in_=ps[bank][:], func=AF.Copy), reads=['ps%d' % bank], writes=['yo%d' % yi])
                    else:
                        S.op('dve', lambda e: e.tensor_copy(out=yo[:], in_=ps[bank][:]), reads=['ps%d' % bank], writes=['yo%d' % yi])
                    S.dma('sp', lambda e: e.dma_start(out=yT[n * 128:(n + 1) * 128, tsl], in_=yo[:]), slot='yo%d' % yi, reads=['yo%d' % yi], writes=[])
                gemm_group(tiles, lambda ki: actT[:, ki, :], lambda ki: [('actT', ki)], evac2)
        S.finish()
        print("e2 ninst", S.ninst)
    return nc


def build_e3(TOK=1024):
    nc = bass.Bass("TRN2", target_bir_lowering=False)
    acc_d = nc.dram_tensor("accT", [D, TOK], F32, kind="ExternalInput").ap()
    ya_d = nc.dram_tensor("yA", [D, TOK], F32, kind="ExternalInput").ap()
    yb_d = nc.dram_tensor("yB", [D, TOK], F32, kind="ExternalInput").ap()
    pv = nc.dram_tensor("pv", [128, 8, KC], F32, kind="ExternalInput").ap()
    ones_d = nc.dram_tensor("ones", [128, 128], F32, kind="ExternalInput").ap()
    yT = nc.dram_tensor("yT", [D, TOK], F32, kind="ExternalOutput").ap()
    with ExitStack() as es:
        S = Sched(nc, es)
        sb = lambda name, shape, dt: es.enter_context(nc.sbuf_tensor(name, shape, dt))
        acc = sb("acc", [128, KC, T], F32)
        pvs = sb("pvs", [128, 8, KC], F32)
        g2p = sb("g2p", [128, KC], F32)
        ones = sb("ones_s", [128, 128], F32)
        s1 = sb("s1", [128, T], F32)
        s2 = sb("s2", [128, T], F32)
        mean = sb("mean", [128, T], F32)
        msq = sb("msq", [128, T], F32)
        rstd = sb("rstd", [128, T], F32)
        NTB = 3
        tb_ = [sb("tb%d" % i, [128, T], F32) for i in range(NTB)]
        yo_ = [sb("yo%d" % i, [128, T], F32) for i in range(NTB)]
        ya_ = [sb("ya%d" % i, [128, T], F32) for i in range(NTB)]
        yb_ = [sb("yb%d" % i, [128, T], F32) for i in range(NTB)]
        ps = [es.enter_context(nc.psum_tensor("ps%d" % i, [128, 512], F32)) for i in range(2)]
        S.dma('sp', lambda e: e.dma_start(out=pvs[:], in_=pv), slot='pvs', writes=['pvs'])
        S.dma('sp', lambda e: e.dma_start(out=ones[:], in_=ones_d), slot='ones', writes=['ones'])
        S.op('dve', lambda e: e.tensor_scalar(out=g2p[:], in0=pvs[:, 3, :], scalar1=1.0, scalar2=None, op0=ALU.add), reads=['pvs'], writes=['g2p'])
        av = acc_d.rearrange("(c q) t -> q c t", q=128)
        for p in range(TOK // T):
            tsl = slice(p * T, (p + 1) * T)
            for g4 in range(4):
                cs = slice(g4 * 8, (g4 + 1) * 8)
                S.dma('sp', lambda e, cs=cs, tsl=tsl: e.dma_start(out=acc[:, cs, :], in_=av[:, cs, tsl]), slot='accld%d' % g4,
                      writes=[('acc', c) for c in range(g4 * 8, g4 * 8 + 8)])
            for c in range(KC):
                i3 = c % NTB
                ya, yb = ya_[i3], yb_[i3]
                S.dma('sp', lambda e, c=c, tsl=tsl, ya=ya: e.dma_start(out=ya[:], in_=ya_d[c * 128:(c + 1) * 128, tsl]), slot='ya%d' % i3, writes=['ya%d' % i3])
                S.dma('sp', lambda e, c=c, tsl=tsl, yb=yb: e.dma_start(out=yb[:], in_=yb_d[c * 128:(c + 1) * 128, tsl]), slot='yb%d' % i3, writes=['yb%d' % i3])
                S.op('dve', lambda e, ya=ya, yb=yb: e.tensor_tensor(out=ya[:], in0=ya[:], in1=yb[:], op=ALU.add), reads=['ya%d' % i3, 'yb%d' % i3], writes=['ya%d' % i3])
                S.op('dve', lambda e, c=c, ya=ya: e.scalar_tensor_tensor(out=acc[:, c, :], in0=ya[:], scalar=g2p[:, c:c + 1], in1=acc[:, c, :], op0=ALU.mult, op1=ALU.add),
                     reads=['ya%d' % i3, 'g2p', ('acc', c)], writes=[('acc', c)])
            for c in range(KC):
                if c == 0:
                    S.op('dve', lambda e: e.tensor_copy(out=s1[:], in_=acc[:, 0, :]), reads=[('acc', 0)], writes=['s1'])
                    S.op('act', lambda e: e.activation(out=s2[:], in_=acc[:, 0, :], func=AF.Square), reads=[('acc', 0)], writes=['s2'])
                else:
                    t = tb_[c % NTB]
                    tr = 'tb%d' % (c % NTB)
                    S.op('act', lambda e, c=c, t=t: e.activation(out=t[:], in_=acc[:, c, :], func=AF.Square), reads=[('acc', c)], writes=[tr])
                    S.op('dve', lambda e, c=c: e.tensor_tensor(out=s1[:], in0=s1[:], in1=acc[:, c, :], op=ALU.add), reads=[('acc', c), 's1'], writes=['s1'])
                    S.op('dve', lambda e, t=t: e.tensor_tensor(out=s2[:], in0=s2[:], in1=t[:], op=ALU.add), reads=[tr, 's2'], writes=['s2'])
            S.op('pe', lambda e: e.matmul(ps[0][:], lhsT=ones[:], rhs=s1[:], start=True, stop=True), reads=['ones', 's1'], writes=['ps0'])
            S.op('pe', lambda e: e.matmul(ps[1][:], lhsT=ones[:], rhs=s2[:], start=True, stop=True), reads=['ones', 's2'], writes=['ps1'])
            S.op('act', lambda e: e.activation(out=mean[:], in_=ps[0][:], func=AF.Copy, scale=1.0 / D), reads=['ps0'], writes=['mean'])
            S.op('dve', lambda e: e.tensor_tensor(out=msq[:], in0=mean[:], in1=mean[:], op=ALU.mult), reads=['mean'], writes=['msq'])
            S.op('dve', lambda e: e.scalar_tensor_tensor(out=msq[:], in0=ps[1][:], scalar=1.0 / D, in1=msq[:], op0=ALU.mult, op1=ALU.subtract), reads=['ps1', 'msq'], writes=['msq'])
            S.op('dve', lambda e: e.tensor_scalar(out=msq[:], in0=msq[:], scalar1=LN_EPS, scalar2=None, op0=ALU.add), reads=['msq'], writes=['msq'])
            S.op('act', lambda e: e.activation(out=rstd[:], in_=msq[:], func=AF.Sqrt), reads=['msq'], writes=['rstd'])
            S.op('dve', lambda e: e.reciprocal(out=rstd[:], in_=rstd[:]), reads=['rstd'], writes=['rstd'])
            for c in range(KC):
                t = tb_[c % NTB]
                tr = 'tb%d' % (c % NTB)
                yo = yo_[c % NTB]
                yr = 'yo%d' % (c % NTB)
                S.op('dve', lambda e, c=c, t=t: e.tensor_tensor(out=t[:], in0=acc[:, c, :], in1=mean[:], op=ALU.subtract), reads=[('acc', c), 'mean'], writes=[tr])
                S.op('dve', lambda e, t=t: e.tensor_tensor(out=t[:], in0=t[:], in1=rstd[:], op=ALU.mult), reads=[tr, 'rstd'], writes=[tr])
                S.op('act', lambda e, c=c, t=t, yo=yo: e.activation(out=yo[:], in_=t[:], func=AF.Identity, scale=pvs[:, 6, c:c + 1], bias=pvs[:, 7, c:c + 1]),
                     reads=[tr, 'pvs'], writes=[yr])
                S.dma('sp', lambda e, c=c, yo=yo, tsl=tsl: e.dma_start(out=yT[c * 128:(c + 1) * 128, tsl], in_=yo[:]), slot=yr, reads=[yr], writes=[])
        S.finish()
    return nc

POOL_WINDOWS = (2, 4, 8, 16)

def pc(v):
    return np.ascontiguousarray(np.asarray(v, np.float32).reshape(-1, 128).T)

def stage_b_inputs(core, xT, sh1, sc1, even_w_in, lam_q1, lam_k1, lam_q2, lam_k2, subln_g, pool_w, pool_scale):
    hA, hB = 2 * core, 2 * core + 1
    g, half = core // 2, core % 2
    w = even_w_in
    cols = np.concatenate([np.arange(hA * 128, hA * 128 + 128), np.arange(hB * 128, hB * 128 + 128),
                           2048 + np.arange(hA * 128, hA * 128 + 128), 2048 + np.arange(hB * 128, hB * 128 + 128),
                           6144 + g * 512 + np.arange(512),
                           4096 + np.arange(hA * 128, hA * 128 + 128), 4096 + np.arange(hB * 128, hB * 128 + 128)])
    wq = np.ascontiguousarray(w[:, cols])
    pw = np.ascontiguousarray(pool_w[g][:, half * 256:(half + 1) * 256])
    ps = pool_scale[g * 512 + half * 256: g * 512 + (half + 1) * 256]
    psc = np.ascontiguousarray(ps.reshape(2, 128).T)
    sel4 = np.zeros((128, 4), np.float32); sel4[:, g] = 1
    wdw = POOL_WINDOWS[g]
    t = np.arange(512)
    rc = np.zeros((128, 2, 512), np.float32)
    rc[:, 0, :] = (1.0 / np.minimum(t + 1, wdw)).astype(np.float32)
    rc[:, 1, :] = np.float32(1.0 / wdw)
    slopes = (2.0 ** (-8.0 * np.arange(1, 17, dtype=np.float32) / 16)).astype(np.float32)
    bt = np.zeros((128, 2, 5, 512), np.float32)
    offt = np.zeros((128, 2, 64), np.float32)
    j = np.arange(128)[:, None].astype(np.float32); i = np.arange(512)[None, :].astype(np.float32)
    for hl, h in enumerate((hA, hB)):
        sl = slopes[h]
        bt[:, hl, 0, :] = -sl * (i - j)
        for d in range(4):
            dist = i - j - 128 * d
            bt[:, hl, d + 1, :] = np.where(dist >= 0, -sl * dist, np.float32(-1e30))
        offt[:, hl, :] = (-sl * 128 * np.arange(64, dtype=np.float32))[None, :]
    lamv = np.stack([np.broadcast_to(v, (128, 64)) for v in (lam_q1, lam_k1, lam_q2, lam_k2)], axis=1).astype(np.float32)
    sg = np.broadcast_to(subln_g, (128, 128)).astype(np.float32)
    return dict(xT=xT, mv=np.ascontiguousarray(np.stack([pc(sh1), pc(sc1)], axis=1)), wq=wq, pw=pw, psc=psc, sel4=sel4, rc=rc,
                bt=bt, offt=offt, lamv=np.ascontiguousarray(lamv), sg=np.ascontiguousarray(sg),
                identb=np.eye(128, dtype=np.float32).astype(ml_dtypes.bfloat16))

def stage_d_inputs(core, xT, sh1, sc1, odd_w_in, lb_raw, gnorm_g):
    c0 = core * 512
    cols = np.concatenate([k * 4096 + c0 + np.arange(512) for k in range(4)])
    wd = np.ascontiguousarray(odd_w_in[:, cols])
    lbr = np.ascontiguousarray(lb_raw[:, c0:c0 + 512].reshape(2, 4, 128).transpose(2, 0, 1)).astype(np.float32)
    gn4 = np.ascontiguousarray(np.broadcast_to(np.tile(gnorm_g, 4), (128, 512))).astype(np.float32)
    s = np.arange(128)[:, None]; t = np.arange(128)[None, :]
    mask01 = (s <= t).astype(np.float32)
    return dict(xT=xT, mv=np.ascontiguousarray(np.stack([pc(sh1), pc(sc1)], axis=1)), wd=wd, lbr=lbr, gn4=gn4, mask01=mask01,
                identb=np.eye(128, dtype=np.float32).astype(ml_dtypes.bfloat16))

def moe_layout(gate, h2T, PASS=512):
    sel = gate != 0
    ok = bool((sel.sum(1) == 2).all())
    idx = [np.nonzero(sel[:, e])[0] for e in range(gate.shape[1])]
    nmax = max(1, max(len(i) for i in idx))
    NP = ((nmax + PASS - 1) // PASS) * PASS
    per = []
    for e, ie in enumerate(idx):
        n = len(ie)
        hT = np.zeros((h2T.shape[0], NP), h2T.dtype)
        hT[:, :n] = h2T[:, ie]
        gb = np.zeros((128, NP), np.float32)
        gb[:, :n] = gate[ie, e][None, :]
        per.append(dict(hT=hT, gb=gb))
    rank = np.cumsum(sel, axis=1) - 1
    return ok, idx, NP, per, rank

def moe_scatter(ys, idx, rank, N):
    Dm = ys[0].shape[0]
    YA = np.zeros((Dm, N), np.float32)
    YB = np.zeros((Dm, N), np.float32)
    for e, ie in enumerate(idx):
        n = len(ie)
        r = rank[ie, e]
        y = ys[e][:, :n]
        YA[:, ie[r == 0]] = y[:, r == 0]
        YB[:, ie[r == 1]] = y[:, r == 1]
    return YA, YB

def _run(nc, in_maps):
    return run_bass_kernel_spmd(nc, in_maps, core_ids=list(range(8))).results


def kernel(**inputs):
    g = lambda k: np.asarray(inputs[k])
    x = np.asarray(g('x'), np.float32)[0]
    c = np.asarray(g('c'), np.float32)[0]
    ada_w, ada_b = g('ada_w'), g('ada_b')
    ln_g, ln_b = g('ln_g'), g('ln_b')
    NCORE = 8
    in_maps = []
    cvp = pc(c)
    for core in range(NCORE):
        l, cb = core // 4, core % 4
        in_maps.append(dict(aw=np.ascontiguousarray(ada_w[l][:, cb * 6144:(cb + 1) * 6144]),
                            ab=np.ascontiguousarray(ada_b[l][cb * 6144:(cb + 1) * 6144][None, :]), cv=cvp))
    res = _run(build_a(), in_maps)
    mod = np.concatenate([res[core]['mo'][0] for core in range(NCORE)]).reshape(2, 6, 4096)
    del in_maps
    ones = np.ones((128, 128), np.float32)
    xT = np.ascontiguousarray(x.T)
    in_maps = [stage_b_inputs(core, xT, mod[0, 0], mod[0, 1], g('even_w_in')[0], g('lam_q1')[0], g('lam_k1')[0], g('lam_q2')[0],
                              g('lam_k2')[0], g('subln_g')[0], g('pool_w')[0], g('pool_scale')[0]) for core in range(NCORE)]
    res = _run(build_b(), in_maps)
    ocT = np.empty((4096, 8192), ml_dtypes.bfloat16)
    for core in range(NCORE):
        o = res[core]['oTc']
        ocT[core * 256:(core + 1) * 256] = o[0:256]
        ocT[2048 + core * 256:2048 + (core + 1) * 256] = o[256:512]
    del in_maps, res

    def ff_stage(moe, l, ocT, xT_in, w_o, extra):
        pv = np.ascontiguousarray(np.stack([pc(mod[l, 2]), pc(mod[l, 3]), pc(mod[l, 4]), pc(mod[l, 5]),
                                            pc(ln_g[l, 0]), pc(ln_b[l, 0]), pc(ln_g[l, 1]), pc(ln_b[l, 1])], axis=1))
        in_maps = []
        for core in range(NCORE):
            sl = slice(core * 1024, (core + 1) * 1024)
            m = dict(oT=np.ascontiguousarray(ocT[:, sl]), xT=np.ascontiguousarray(xT_in[:, sl]), pv=pv, w_o=w_o, ones=ones)
            m.update(extra)
            in_maps.append(m)
        res = _run(build_ff(moe), in_maps)
        return np.concatenate([res[core]['yT'] for core in range(NCORE)], axis=1)

    x1T = ff_stage(False, 0, ocT, xT, np.ascontiguousarray(g('even_w_out')[0]),
                   dict(w_in=np.ascontiguousarray(g('ffn_w_in')[0]), w_o2=np.ascontiguousarray(g('ffn_w_out')[0])))
    del xT
    in_maps = [stage_d_inputs(core, x1T, mod[1, 0], mod[1, 1], g('odd_w_in')[0], g('lb_raw'), g('gnorm_g')[0]) for core in range(NCORE)]
    res = _run(build_d(), in_maps)
    oT = np.concatenate([res[core]['oTd'] for core in range(NCORE)], axis=0)
    del in_maps, res
    rw = np.ascontiguousarray(np.asarray(g('router_w')[0], np.float32).reshape(32, 128, 8).transpose(1, 0, 2))
    w_o1 = np.ascontiguousarray(g('odd_w_out')[0])
    pv1 = np.ascontiguousarray(np.stack([pc(mod[1, 2]), pc(mod[1, 3]), pc(mod[1, 4]), pc(mod[1, 5]),
                                         pc(ln_g[1, 0]), pc(ln_b[1, 0]), pc(ln_g[1, 1]), pc(ln_b[1, 1])], axis=1))
    in_maps = []
    for core in range(NCORE):
        sl = slice(core * 1024, (core + 1) * 1024)
        in_maps.append(dict(oT=np.ascontiguousarray(oT[:, sl]), xT=np.ascontiguousarray(x1T[:, sl]), pv=pv1, w_o=w_o1, ones=ones, rw=rw))
    res = _run(build_ff(True, mode='e1'), in_maps)
    h2T = np.concatenate([res[core]['h2o'] for core in range(NCORE)], axis=1)
    accT = np.concatenate([res[core]['acco'] for core in range(NCORE)], axis=1)
    gate = np.concatenate([res[core]['gout'] for core in range(NCORE)], axis=0)
    del in_maps, res
    ok, idx, NP, per, rank = moe_layout(gate, h2T)
    if not ok:
        sel = np.zeros((8, 8, 128), np.float32)
        for e in range(8):
            sel[e, e, :] = 1
        x2T = ff_stage(True, 1, oT, x1T, w_o1,
                       dict(w_in=np.ascontiguousarray(g('exp_w_in')[0]), w_o2=np.ascontiguousarray(g('exp_w_out')[0]), rw=rw, sel=sel,
                            ident=np.eye(128, dtype=np.float32)))
        return np.ascontiguousarray(x2T.T)[None].astype(np.float32)
    ewi, ewo = g('exp_w_in')[0], g('exp_w_out')[0]
    in_maps = [dict(hT=per[e]['hT'], gb=per[e]['gb'], w_in=np.ascontiguousarray(ewi[e]), w_o2=np.ascontiguousarray(ewo[e])) for e in range(NCORE)]
    res = _run(build_e2(NP), in_maps)
    YA, YB = moe_scatter([res[e]['yT'] for e in range(NCORE)], idx, rank, 8192)
    del in_maps, res, per
    in_maps = []
    for core in range(NCORE):
        sl = slice(core * 1024, (core + 1) * 1024)
        in_maps.append(dict(accT=np.ascontiguousarray(accT[:, sl]), yA=np.ascontiguousarray(YA[:, sl]), yB=np.ascontiguousarray(YB[:, sl]), pv=pv1, ones=ones))
    res = _run(build_e3(), in_maps)
    x2T = np.concatenate([res[core]['yT'] for core in range(NCORE)], axis=1)
    return np.ascontiguousarray(x2T.T)[None].astype(np.float32)
```

```python
import numpy as np
import ml_dtypes
import concourse.bass as bass
import concourse.mybir as mybir
from concourse.bass_utils import run_bass_kernel_spmd
from contextlib import ExitStack

F32 = mybir.dt.float32
BF16 = mybir.dt.bfloat16
AF = mybir.ActivationFunctionType
ALU = mybir.AluOpType
AX = mybir.AxisListType


class Sched:
    ENG = ('pe', 'act', 'dve', 'pool', 'sp')

    def __init__(self, nc, es, immediate=False):
        self.immediate = immediate
        self.nc = nc
        self.es = es
        self.eng = {'pe': nc.tensor, 'act': nc.scalar, 'dve': nc.vector,
                    'pool': nc.gpsimd, 'sp': nc.sync}
        self.sem = {e: es.enter_context(nc.semaphore("s_" + e)) for e in self.ENG}
        self.cnt = {e: 0 for e in self.ENG}
        self.prog = {e: [] for e in self.ENG}
        self.waited = {}
        self.lastw = {}
        self.reads = {}
        self.dsem = {}
        self.semobj = {e: self.sem[e] for e in self.ENG}
        self.ninst = 0
        self._rec = None

    def record(self, fn):
        saved = self._rec
        self._rec = []
        try:
            fn()
            out = self._rec
        finally:
            self._rec = saved
        return out

    def replay(self, lists):
        n = max([len(l) for l in lists] + [0])
        for i in range(n):
            for l in lists:
                if i < len(l):
                    kind, a, kw = l[i]
                    getattr(self, kind)(*a, **kw)

    def _dma_sem(self, key):
        if key not in self.dsem:
            s = self.es.enter_context(self.nc.semaphore("d_%d" % len(self.dsem)))
            k = ('d', key)
            self.semobj[k] = s
            self.dsem[key] = [k, 0]
        return self.dsem[key]

    def _deps(self, eng, reads, writes):
        deps = {}
        def add(d):
            if d is None:
                return
            k, v, e = d
            if e == 'pe' and eng == 'pe':
                return
            if deps.get(k, 0) < v:
                deps[k] = v
        for r in reads:
            add(self.lastw.get(r))
        for w in writes:
            add(self.lastw.get(w))
            for d in self.reads.get(w, ()):
                add(d)
        out = []
        for k, v in deps.items():
            if self.waited.get((eng, k), 0) >= v:
                continue
            self.waited[(eng, k)] = v
            out.append((self.semobj[k], v))
        return out

    def _commit(self, ident, reads, writes):
        for w in writes:
            self.lastw[w] = ident
            self.reads[w] = []
        for r in reads:
            if r in writes:
                continue
            self.reads.setdefault(r, []).append(ident)

    def op(self, eng, fn, reads=(), writes=(), inc=True):
        if self._rec is not None:
            self._rec.append(('op', (eng, fn), dict(reads=reads, writes=writes, inc=inc)))
            return
        waits = self._deps(eng, reads, writes)
        if inc:
            self.cnt[eng] += 1
            val = self.cnt[eng]
        else:
            val = self.cnt[eng] + 1
        sem = self.sem[eng]
        e = self.eng[eng]
        def emit():
            for s, v in waits:
                e.wait_ge(s, v)
            i = fn(e)
            if inc:
                i.then_inc(sem, 1)
        if self.immediate:
            emit()
        else:
            self.prog[eng].append(emit)
        self._commit((eng, val, eng), reads, writes)
        self.ninst += 1

    def dma(self, eng, fn, slot, reads=(), writes=()):
        if self._rec is not None:
            self._rec.append(('dma', (eng, fn, slot), dict(reads=reads, writes=writes)))
            return
        waits = self._deps(eng, reads, writes)
        ds = self._dma_sem(slot)
        ds[1] += 16
        k, val = ds[0], ds[1]
        sem = self.semobj[k]
        e = self.eng[eng]
        def emit():
            for s, v in waits:
                e.wait_ge(s, v)
            fn(e).then_inc(sem, 16)
        if self.immediate:
            emit()
        else:
            self.prog[eng].append(emit)
        self._commit((k, val, 'dma'), reads, writes)
        self.ninst += 1

    def finish(self, final_res=None):
        waits = [(self.sem[k], self.cnt[k]) for k in self.ENG if self.cnt[k] > 0]
        waits += [(self.semobj[k], v) for (k, v) in self.dsem.values()]
        e = self.eng['sp']
        def emit():
            for s, v in waits:
                e.wait_ge(s, v)
        self.prog['sp'].append(emit)
        nc = self.nc
        allsems = [self.sem[k] for k in self.ENG] + [self.semobj[k] for (k, v) in self.dsem.values()]
        with nc.Block() as b0:
            @b0.sync
            def _(t):
                for s_ in allsems:
                    t.sem_clear(s_)
        with nc.Block() as block:
            @block.tensor
            def _(t):
                for f in self.prog['pe']:
                    f()
            @block.scalar
            def _(t):
                for f in self.prog['act']:
                    f()
            @block.vector
            def _(t):
                for f in self.prog['dve']:
                    f()
            @block.gpsimd
            def _(t):
                for f in self.prog['pool']:
                    f()
            @block.sync
            def _(t):
                for f in self.prog['sp']:
                    f()


    def barrier(self):
        waits = [(k, self.cnt[k]) for k in self.ENG if self.cnt[k] > 0]
        waits += [(k, v) for (k, v) in self.dsem.values()]
        for eng in self.ENG:
            ws = []
            for k, v in waits:
                if self.waited.get((eng, k), 0) >= v:
                    continue
                self.waited[(eng, k)] = v
                ws.append((self.semobj[k], v))
            e = self.eng[eng]
            def emit(ws=ws, e=e):
                for s, v in ws:
                    e.wait_ge(s, v)
            self.prog[eng].append(emit)
        self.lastw = {}
        self.reads = {}


class Arena:
    def __init__(self, nc, es, name, kbytes):
        self.t = es.enter_context(nc.sbuf_tensor(name, [128, kbytes * 256], F32))
        self.n = kbytes * 256
        self.off = 0
        self.marks = []

    def alloc(self, shape, dt, parts=128):
        nel = 1
        for s_ in shape:
            nel *= s_
        esz = 4 if dt == F32 else 2
        nw = (nel * esz + 3) // 4
        nw = (nw + 7) // 8 * 8
        assert self.off + nw <= self.n, "arena overflow %d + %d > %d" % (self.off, nw, self.n)
        v = self.t[0:parts, self.off:self.off + nw]
        self.off += nw
        if dt != F32:
            v = v.bitcast(dt)
        v = v[:, 0:nel]
        if len(shape) == 2:
            v = v.rearrange("p (a b) -> p a b", a=shape[0])
        elif len(shape) == 3:
            v = v.rearrange("p (a b c) -> p a b c", a=shape[0], b=shape[1])
        return v

    def mark(self):
        return self.off

    def reset(self, mark):
        self.off = mark


IMM = False
def build_a(NCOL=6144):
    nc = bass.Bass("TRN2", target_bir_lowering=False)
    D = 4096
    aw = nc.dram_tensor("aw", [D, NCOL], F32, kind="ExternalInput").ap()
    ab = nc.dram_tensor("ab", [1, NCOL], F32, kind="ExternalInput").ap()
    cv = nc.dram_tensor("cv", [128, 32], F32, kind="ExternalInput").ap()
    mo = nc.dram_tensor("mo", [1, NCOL], F32, kind="ExternalOutput").ap()
    with ExitStack() as es:
        S = Sched(nc, es, immediate=IMM)
        sb = lambda name, shape, dt: es.enter_context(nc.sbuf_tensor(name, shape, dt))
        NB = 8
        wt = [sb("wt%d" % i, [128, 512], F32) for i in range(NB)]
        cs = sb("cs", [128, 32], F32)
        ca = sb("ca", [128, 32], F32)
        abs_ = sb("abs", [1, NCOL], F32)
        mos = sb("mos", [1, NCOL], F32)
        ps = [es.enter_context(nc.psum_tensor("ps%d" % i, [128, 512], F32)) for i in range(2)]
        S.dma('sp', lambda e: e.dma_start(out=cs[:], in_=cv), slot='cs', writes=['cs'])
        S.dma('sp', lambda e: e.dma_start(out=abs_[:], in_=ab), slot='abs', writes=['abs'])
        S.op('act', lambda e: e.activation(out=ca[:], in_=cs[:], func=AF.Silu), reads=['cs'], writes=['ca'])
        it = 0
        for n in range(NCOL // 512):
            bank = n % 2
            for k in range(32):
                b = it % NB
                it += 1
                S.dma('sp', lambda e, b=b, k=k, n=n: e.dma_start(out=wt[b][:], in_=aw[k * 128:(k + 1) * 128, n * 512:(n + 1) * 512]),
                      slot='wt%d' % b, writes=['wt%d' % b])
                S.op('pe', lambda e, b=b, k=k, bank=bank: e.matmul(ps[bank][0:1, :], lhsT=ca[:, k:k + 1], rhs=wt[b][:], start=(k == 0), stop=(k == 31)),
                     reads=['wt%d' % b, 'ca'], writes=['ps%d' % bank])
            S.op('dve', lambda e, n=n, bank=bank: e.tensor_tensor(out=mos[0:1, n * 512:(n + 1) * 512], in0=ps[bank][0:1, :], in1=abs_[0:1, n * 512:(n + 1) * 512], op=ALU.add),
                 reads=['ps%d' % bank, 'abs'], writes=[('mos', n)])
        S.dma('sp', lambda e: e.dma_start(out=mo, in_=mos[:]), slot='out', reads=[('mos', n) for n in range(NCOL // 512)], writes=[])
        S.finish()
    return nc


D = 4096
KC = 32
S_ = 8192
T = 512
NTG = S_ // T
RMS_EPS = 1e-6


def build_b(NTG_RUN=NTG, heads=(0, 1)):
    nc = bass.Bass("TRN2", target_bir_lowering=False)
    dt_in = lambda name, shape, dt=F32: nc.dram_tensor(name, shape, dt, kind="ExternalInput").ap()
    xT = dt_in("xT", [D, S_])
    mv = dt_in("mv", [128, 2, KC])
    wq = dt_in("wq", [D, 1280])
    pw_d = dt_in("pw", [512, 256])
    psc_d = dt_in("psc", [128, 2])
    sel_d = dt_in("sel4", [128, 4])
    rc_d = dt_in("rc", [128, 2, T])
    bt_d = dt_in("bt", [128, 2, 5, T])
    off_d = dt_in("offt", [128, 2, 64])
    lam_d = dt_in("lamv", [128, 4, 64])
    sg_d = dt_in("sg", [128, 128])
    idb_d = dt_in("identb", [128, 128], BF16)
    oTc = nc.dram_tensor("oTc", [512, S_], BF16, kind="ExternalOutput").ap()
    qk_scr = nc.dram_tensor("qk_scr", [4, 128, S_], BF16, kind="Internal").ap()
    v_scr = nc.dram_tensor("v_scr", [S_, 2, 130], BF16, kind="Internal").ap()

    with ExitStack() as es:
        S = Sched(nc, es)
        ar = Arena(nc, es, "arena", 200)
        psf = [es.enter_context(nc.psum_tensor("ps%d" % i, [128, 512], F32)) for i in range(8)]
        ps7b = psf[7][:].bitcast(BF16)

        mvs = ar.alloc([2, KC], F32)
        sc1p = ar.alloc([KC], F32)
        psc = ar.alloc([2], F32)
        sel4 = ar.alloc([4], F32)
        lamv = ar.alloc([4, 64], F32)
        lamt = ar.alloc([2, 64], F32)
        lsc = ar.alloc([8], F32)
        SG = ar.alloc([128], F32)
        identb = ar.alloc([128], BF16)
        for (dst, src, nm) in [(mvs, mv, 'mvs'), (psc, psc_d, 'psc'), (sel4, sel_d, 'sel4'), (lamv, lam_d, 'lamv'),
                               (SG, sg_d, 'SG'), (identb, idb_d, 'identb')]:
            S.dma('sp', lambda e, dst=dst, src=src: e.dma_start(out=dst, in_=src), slot=nm, writes=[nm])
        S.op('dve', lambda e: e.tensor_scalar(out=sc1p, in0=mvs[:, 1, :], scalar1=1.0, scalar2=None, op0=ALU.add), reads=['mvs'], writes=['sc1p'])
        S.op('dve', lambda e: e.tensor_tensor(out=lamt[:, 0, :], in0=lamv[:, 0, :], in1=lamv[:, 1, :], op=ALU.mult), reads=['lamv'], writes=['lamt0'])
        S.op('dve', lambda e: e.tensor_tensor(out=lamt[:, 1, :], in0=lamv[:, 2, :], in1=lamv[:, 3, :], op=ALU.mult), reads=['lamv'], writes=['lamt1'])
        S.op('dve', lambda e: e.reduce_sum(out=lsc[:, 0:1], in_=lamt[:, 0, :], axis=AX.X), reads=['lamt0'], writes=['lsc0'])
        S.op('dve', lambda e: e.reduce_sum(out=lsc[:, 1:2], in_=lamt[:, 1, :], axis=AX.X), reads=['lamt1'], writes=['lsc1'])
        S.op('act', lambda e: e.activation(out=lsc[:, 2:4], in_=lsc[:, 0:2], func=AF.Exp), reads=['lsc0', 'lsc1'], writes=['lsc23'])
        S.op('dve', lambda e: e.tensor_tensor(out=lsc[:, 4:5], in0=lsc[:, 2:3], in1=lsc[:, 3:4], op=ALU.subtract), reads=['lsc23'], writes=['lsc4'])
        S.op('dve', lambda e: e.tensor_scalar(out=lsc[:, 4:5], in0=lsc[:, 4:5], scalar1=0.2, scalar2=-1.0, op0=ALU.add, op1=ALU.mult), reads=['lsc4'], writes=['lsc4'])
        S.op('dve', lambda e: e.tensor_scalar(out=SG, in0=SG, scalar1=0.8, scalar2=None, op0=ALU.mult), reads=['SG'], writes=['SG'])
        pmark = ar.mark()

        W = ar.alloc([KC, 1280], BF16)
        hT = ar.alloc([KC, T], BF16)
        NXS = 3
        xs = [ar.alloc([2, T], F32) for i in range(NXS)]
        pw = ar.alloc([4, 256], BF16)
        rc = ar.alloc([2, T], F32)
        ue = [ar.alloc([4, 528], F32) for i in range(2)]
        NPT = 4
        pA = [ar.alloc([528], F32) for i in range(NPT)]
        pB = [ar.alloc([528], F32) for i in range(NPT)]
        pS = [ar.alloc([T], F32) for i in range(NPT)]
        z = ar.alloc([4, T], BF16)
        NST = 3
        qst = [ar.alloc([T], BF16) for i in range(NST)]
        vst = [ar.alloc([2, 130], BF16) for i in range(2)]
        ost = [ar.alloc([T], BF16) for i in range(2)]

        for k in range(KC):
            S.dma('pool', lambda e, k=k: e.dma_start(out=W[:, k, :], in_=wq[k * 128:(k + 1) * 128, :]), slot='W%d' % k, writes=[('W', k)])
        S.dma('pool', lambda e: e.dma_start(out=pw, in_=pw_d.rearrange("(c p) n -> p c n", p=128)), slot='pw', writes=['pw'])
        S.dma('sp', lambda e: e.dma_start(out=rc, in_=rc_d), slot='rc', writes=['rc'])
        for i in range(2):
            S.op('dve', lambda e, i=i: e.memset(vst[i][:, :, 128:130], 1.0), writes=['vst%d' % i])
        S.op('dve', lambda e: e.memset(ue[0][:, :, 0:16], 0.0), writes=[('ue', 0, j) for j in range(4)])
        WR = [('W', k) for k in range(KC)]
        xv = xT.rearrange("(c q) t -> q c t", q=128)
        xit = 0
        qit = 0
        for tg in range(NTG_RUN):
            tsl = slice(tg * T, (tg + 1) * T)
            for c2 in range(KC // 2):
                b = xit % NXS
                xit += 1
                S.dma('sp', lambda e, b=b, c2=c2, tsl=tsl: e.dma_start(out=xs[b], in_=xv[:, c2 * 2:c2 * 2 + 2, tsl]), slot='xs%d' % b, writes=['xs%d' % b])
                for i in range(2):
                    c = c2 * 2 + i
                    S.op('act', lambda e, b=b, i=i, c=c: e.activation(out=hT[:, c, :], in_=xs[b][:, i, :], func=AF.Identity,
                                                                   scale=sc1p[:, c:c + 1], bias=mvs[:, 0, c:c + 1]),
                         reads=['xs%d' % b, 'sc1p', 'mvs'], writes=[('hT', c)])
            for k in range(KC):
                for j in range(4):
                    S.op('pe', lambda e, k=k, j=j: e.matmul(psf[j][:], lhsT=W[:, k, j * 128:(j + 1) * 128], rhs=hT[:, k, :], start=(k == 0), stop=(k == KC - 1)),
                         reads=[('W', k), ('hT', k)], writes=['ps%d' % j], inc=(k == KC - 1))
            for j in range(4):
                b = qit % NST
                qit += 1
                if j % 2 == 0:
                    S.op('act', lambda e, b=b, j=j: e.activation(out=qst[b], in_=psf[j][:], func=AF.Copy), reads=['ps%d' % j], writes=['qst%d' % b])
                else:
                    S.op('dve', lambda e, b=b, j=j: e.tensor_copy(out=qst[b], in_=psf[j][:]), reads=['ps%d' % j], writes=['qst%d' % b])
                S.dma('sp', lambda e, b=b, j=j, tsl=tsl: e.dma_start(out=qk_scr[j, :, tsl], in_=qst[b]), slot='qst%d' % b, reads=['qst%d' % b], writes=[])
            U = ue[tg % 2]
            Un = ue[(tg + 1) % 2]
            for k in range(KC):
                for j in range(4):
                    S.op('pe', lambda e, k=k, j=j: e.matmul(psf[4 + j][:], lhsT=W[:, k, 512 + j * 128:512 + (j + 1) * 128], rhs=hT[:, k, :], start=(k == 0), stop=(k == KC - 1)),
                         reads=[('W', k), ('hT', k)], writes=['ps%d' % (4 + j)], inc=(k == KC - 1))
            def chainp(j, tg=tg, U=U, Un=Un):
                ur = ('ue', tg % 2, j)
                urn = ('ue', (tg + 1) % 2, j)
                S.op('act', lambda e, j=j, U=U: e.activation(out=U[:, j, 16:528], in_=psf[4 + j][:], func=AF.Copy), reads=['ps%d' % (4 + j)], writes=[ur])
                S.op('pool', lambda e, j=j, U=U, Un=Un: e.tensor_copy(out=Un[:, j, 0:16], in_=U[:, j, 512:528]), reads=[ur], writes=[urn])
                eng = 'dve'
                i2 = j % NPT
                A, B, SS = pA[i2], pB[i2], pS[i2]
                Ar, Br, Sr = 'pA%d' % i2, 'pB%d' % i2, 'pS%d' % i2
                E_ = lambda lo, hi, U=U, j=j: U[:, j, lo:hi]
                S.op(eng, lambda e, A=A, E_=E_: e.tensor_tensor(out=A[:, 1:528], in0=E_(1, 528), in1=E_(0, 527), op=ALU.add), reads=[ur], writes=[Ar])
                S.op(eng, lambda e, A=A, SS=SS: e.tensor_scalar(out=SS, in0=A[:, 16:528], scalar1=sel4[:, 0:1], scalar2=None, op0=ALU.mult), reads=[Ar, 'sel4'], writes=[Sr])
                S.op(eng, lambda e, A=A, B=B: e.tensor_tensor(out=B[:, 3:528], in0=A[:, 3:528], in1=A[:, 1:526], op=ALU.add), reads=[Ar], writes=[Br])
                S.op(eng, lambda e, B=B, SS=SS: e.scalar_tensor_tensor(out=SS, in0=B[:, 16:528], scalar=sel4[:, 1:2], in1=SS, op0=ALU.mult, op1=ALU.add), reads=[Br, Sr, 'sel4'], writes=[Sr])
                S.op(eng, lambda e, A=A, B=B: e.tensor_tensor(out=A[:, 7:528], in0=B[:, 7:528], in1=B[:, 3:524], op=ALU.add), reads=[Br], writes=[Ar])
                S.op(eng, lambda e, A=A, SS=SS: e.scalar_tensor_tensor(out=SS, in0=A[:, 16:528], scalar=sel4[:, 2:3], in1=SS, op0=ALU.mult, op1=ALU.add), reads=[Ar, Sr, 'sel4'], writes=[Sr])
                S.op(eng, lambda e, A=A, B=B: e.tensor_tensor(out=B[:, 15:528], in0=A[:, 15:528], in1=A[:, 7:520], op=ALU.add), reads=[Ar], writes=[Br])
                S.op(eng, lambda e, B=B, SS=SS: e.scalar_tensor_tensor(out=SS, in0=B[:, 16:528], scalar=sel4[:, 3:4], in1=SS, op0=ALU.mult, op1=ALU.add), reads=[Br, Sr, 'sel4'], writes=[Sr])
                rci = 0 if tg == 0 else 1
                S.op(eng, lambda e, SS=SS, rci=rci: e.tensor_tensor(out=SS, in0=SS, in1=rc[:, rci, :], op=ALU.mult), reads=[Sr, 'rc'], writes=[Sr])
                S.op(eng, lambda e, SS=SS, j=j, E_=E_: e.tensor_tensor(out=z[:, j, :], in0=SS, in1=E_(16, 528), op=ALU.subtract), reads=[Sr, ur], writes=[('z', j)])
            S.replay([S.record(lambda j=j: chainp(j)) for j in range(4)])
            for tb in range(4):
                for k in range(KC):
                    S.op('pe', lambda e, k=k, tb=tb: e.matmul(psf[tb][:, 0:256], lhsT=hT[:, k, tb * 128:(tb + 1) * 128], rhs=W[:, k, 1024:1280], start=(k == 0), stop=(k == KC - 1)),
                         reads=[('W', k), ('hT', k)], writes=['ps%d' % tb], inc=(k == KC - 1))
            for tb in range(4):
                b = tb % 2
                S.op('dve', lambda e, b=b, tb=tb: e.tensor_copy(out=vst[b][:, :, 0:128], in_=psf[tb][:, 0:256].rearrange("p (h d) -> p h d", h=2)),
                     reads=['ps%d' % tb], writes=['vst%d' % b])
                r0 = tg * T + tb * 128
                S.dma('sp', lambda e, b=b, r0=r0: e.dma_start(out=v_scr[r0:r0 + 128, :, :], in_=vst[b]), slot='vst%d' % b, reads=['vst%d' % b], writes=[])
            for oc in range(2):
                for kc in range(4):
                    S.op('pe', lambda e, oc=oc, kc=kc: e.matmul(psf[4 + oc][:], lhsT=pw[:, kc, oc * 128:(oc + 1) * 128], rhs=z[:, kc, :], start=(kc == 0), stop=(kc == 3)),
                         reads=['pw', ('z', kc)], writes=['ps%d' % (4 + oc)], inc=(kc == 3))
                S.op('act', lambda e, oc=oc: e.activation(out=ost[oc], in_=psf[4 + oc][:], func=AF.Identity, scale=psc[:, oc:oc + 1]),
                     reads=['ps%d' % (4 + oc), 'psc'], writes=['ost%d' % oc])
                S.dma('sp', lambda e, oc=oc, tsl=tsl: e.dma_start(out=oTc[256 + oc * 128:256 + (oc + 1) * 128, tsl], in_=ost[oc]), slot='ost%d' % oc, reads=['ost%d' % oc], writes=[])

        S.barrier()
        ar.reset(pmark)
        QK = ar.alloc([4, S_], BF16)
        Vx = ar.alloc([64, 2, 130], BF16)
        bt = ar.alloc([2, 5, T], F32)
        offt = ar.alloc([2, 64], F32)
        NSB = 3
        Sb = [ar.alloc([T], F32) for i in range(NSB)]
        NPT2 = 4
        PT = [ar.alloc([T], BF16) for i in range(NPT2)]
        rl = ar.alloc([4, 4], F32)
        t2 = [ar.alloc([128], F32) for i in range(4)]
        o_ = [ar.alloc([128], F32) for i in range(4)]
        junk = ar.alloc([128], F32)
        on = [ar.alloc([128], BF16) for i in range(4)]
        ost2 = [ar.alloc([T], BF16) for i in range(2)]
        for j in range(4):
            S.dma('sp', lambda e, j=j: e.dma_start(out=QK[:, j, :], in_=qk_scr[j]), slot='QK%d' % j, writes=[('QK', j)])
        S.dma('sp', lambda e: e.dma_start(out=Vx, in_=v_scr.rearrange("(b p) h d -> p b h d", p=128)), slot='Vx', writes=['Vx'])
        S.dma('sp', lambda e: e.dma_start(out=bt, in_=bt_d), slot='bt', writes=['bt'])
        S.dma('sp', lambda e: e.dma_start(out=offt, in_=off_d), slot='offt', writes=['offt'])

        accpos = {}
        lst = [(m, s) for m in range(2) for s in range(4)]
        for i, (m, s) in enumerate(lst):
            accpos[(m, s)] = (4 + i // 3, (i % 3) * 130)
        tcount = 0
        epi = 0
        for h in heads:
            for ib in range(NTG_RUN):
                for bk in (4, 5, 6):
                    S.op('dve', lambda e, bk=bk: e.memset(psf[bk][:, 0:390], 0.0), writes=['ps%d' % bk])
                tiles = [(m, jb) for jb in range(4 * ib + 4) for m in range(2)]
                SKEW = 3
                def emit_qk(ti, h=h, ib=ib, tiles=tiles, tcount=tcount):
                    m, jb = tiles[ti]
                    bank = (tcount + ti) % 4
                    S.op('pe', lambda e: e.matmul(psf[bank][:], lhsT=QK[m * 64:(m + 1) * 64, 2 + h, jb * 128:(jb + 1) * 128],
                                                  rhs=QK[m * 64:(m + 1) * 64, h, ib * T:(ib + 1) * T], start=True, stop=True),
                         reads=[('QK', 2 + h), ('QK', h)], writes=['ps%d' % bank])
                    d = jb - 4 * ib
                    var = 0 if d < 0 else d + 1
                    n = 4 * ib - jb if d < 0 else 0
                    sbi = (tcount + ti) % NSB
                    pti = (tcount + ti) % NPT2
                    S.op('dve', lambda e: e.scalar_tensor_tensor(out=Sb[sbi], in0=psf[bank][:], scalar=0.125, in1=bt[:, h, var, :], op0=ALU.mult, op1=ALU.add),
                         reads=['ps%d' % bank, 'bt'], writes=['Sb%d' % sbi])
                    S.op('act', lambda e: e.activation(out=PT[pti], in_=Sb[sbi], func=AF.Exp, bias=offt[:, h, n:n + 1], scale=1.0),
                         reads=['Sb%d' % sbi, 'offt'], writes=['PT%d' % pti])
                def emit_pv(ti, h=h, ib=ib, tiles=tiles, tcount=tcount):
                    m, jb = tiles[ti]
                    d = jb - 4 * ib
                    pti = (tcount + ti) % NPT2
                    subs = [s for s in range(4) if not (d >= 0 and s < d)]
                    for s in subs:
                        bk, c0 = accpos[(m, s)]
                        S.op('pe', lambda e, s=s, bk=bk, c0=c0: e.matmul(psf[bk][:, c0:c0 + 129], lhsT=PT[pti][:, s * 128:(s + 1) * 128], rhs=Vx[:, jb, h, 0:129],
                                                                         start=False, stop=False, skip_group_check=True),
                             reads=['PT%d' % pti, 'Vx'], writes=['ps%d' % bk], inc=(s == subs[-1]))
                nt = len(tiles)
                for ti in range(nt + SKEW):
                    if ti < nt:
                        emit_qk(ti)
                    if ti - SKEW >= 0:
                        emit_pv(ti - SKEW)
                tcount += nt
                def chaine(s):
                    nonlocal epi
                    b1, c1 = accpos[(0, s)]
                    b2, c2 = accpos[(1, s)]
                    e2 = s
                    epi += 1
                    A1 = psf[b1][:, c1:c1 + 129]
                    A2 = psf[b2][:, c2:c2 + 129]
                    rr = ('rl', s)
                    S.op('dve', lambda e, s=s, A1=A1: e.reciprocal(out=rl[:, s, 0:1], in_=A1[:, 128:129]), reads=['ps%d' % b1], writes=[rr])
                    S.op('dve', lambda e, s=s, A2=A2: e.reciprocal(out=rl[:, s, 1:2], in_=A2[:, 128:129]), reads=['ps%d' % b2], writes=[rr])
                    S.op('dve', lambda e, s=s: e.tensor_tensor(out=rl[:, s, 1:2], in0=rl[:, s, 1:2], in1=lsc[:, 4:5], op=ALU.mult), reads=[rr, 'lsc4'], writes=[rr])
                    S.op('act', lambda e, s=s, A2=A2, e2=e2: e.activation(out=t2[e2], in_=A2[:, 0:128], func=AF.Identity, scale=rl[:, s, 1:2]),
                         reads=['ps%d' % b2, rr], writes=['t2%d' % e2])
                    S.op('dve', lambda e, s=s, A1=A1, e2=e2: e.scalar_tensor_tensor(out=o_[e2], in0=A1[:, 0:128], scalar=rl[:, s, 0:1], in1=t2[e2], op0=ALU.mult, op1=ALU.add),
                         reads=['ps%d' % b1, rr, 't2%d' % e2], writes=['o%d' % e2])
                    S.op('act', lambda e, s=s, e2=e2: e.activation(out=junk, in_=o_[e2], func=AF.Square, accum_out=rl[:, s, 2:3]),
                         reads=['o%d' % e2], writes=['junk', ('rl2', s)])
                    S.op('dve', lambda e, s=s: e.tensor_scalar(out=rl[:, s, 2:3], in0=rl[:, s, 2:3], scalar1=1.0 / 128, scalar2=RMS_EPS, op0=ALU.mult, op1=ALU.add),
                         reads=[('rl2', s)], writes=[('rl2', s)])
                    S.op('act', lambda e, s=s: e.activation(out=rl[:, s, 3:4], in_=rl[:, s, 2:3], func=AF.Sqrt), reads=[('rl2', s)], writes=[('rl3', s)])
                    S.op('dve', lambda e, s=s: e.reciprocal(out=rl[:, s, 3:4], in_=rl[:, s, 3:4]), reads=[('rl3', s)], writes=[('rl3', s)])
                    S.op('dve', lambda e, s=s, e2=e2: e.scalar_tensor_tensor(out=on[e2], in0=o_[e2], scalar=rl[:, s, 3:4], in1=SG, op0=ALU.mult, op1=ALU.mult),
                         reads=['o%d' % e2, ('rl3', s), 'SG'], writes=['on%d' % e2])
                    S.op('pe', lambda e, s=s, e2=e2: e.transpose(out=ps7b[:, s * 128:(s + 1) * 128], in_=on[e2], identity=identb),
                         reads=['on%d' % e2, 'identb'], writes=['ps7'])
                S.replay([S.record(lambda s=s: chaine(s)) for s in range(4)])
                ob = (epi // 4) % 2
                S.op('act', lambda e, ob=ob: e.activation(out=ost2[ob], in_=ps7b[:, 0:512], func=AF.Copy), reads=['ps7'], writes=['ost2%d' % ob])
                S.dma('sp', lambda e, ob=ob, h=h, ib=ib: e.dma_start(out=oTc[h * 128:(h + 1) * 128, ib * T:(ib + 1) * T], in_=ost2[ob]),
                      slot='ost2%d' % ob, reads=['ost2%d' % ob], writes=[])
        S.finish()
        print("stage b ninst", S.ninst)
    return nc


D = 4096
KC = 32
S_ = 8192
T = 512
NTG = S_ // T
RMS_EPS = 1e-6
CH = 128


def build_d(NTG_RUN=NTG, heads=(0, 1, 2, 3)):
    nc = bass.Bass("TRN2", target_bir_lowering=False)
    dt_in = lambda name, shape, dt=F32: nc.dram_tensor(name, shape, dt, kind="ExternalInput").ap()
    xT = dt_in("xT", [D, S_])
    mv = dt_in("mv", [128, 2, KC])
    wd = dt_in("wd", [D, 2048])
    lbr_d = dt_in("lbr", [128, 2, 4])
    gn_d = dt_in("gn4", [128, 512])
    mask_d = dt_in("mask01", [128, 128])
    idb_d = dt_in("identb", [128, 128], BF16)
    oTd = nc.dram_tensor("oTd", [512, S_], BF16, kind="ExternalOutput").ap()
    qt_scr = nc.dram_tensor("qt_scr", [4, 128, S_], BF16, kind="Internal").ap()
    kt_scr = nc.dram_tensor("kt_scr", [4, 128, S_], BF16, kind="Internal").ap()
    kh_scr = nc.dram_tensor("kh_scr", [S_, 4, 128], BF16, kind="Internal").ap()
    v_scr = nc.dram_tensor("v_scr", [S_, 4, 128], BF16, kind="Internal").ap()
    sg_scr = nc.dram_tensor("sg_scr", [S_, 4, 128], BF16, kind="Internal").ap()
    NCHK = S_ // CH

    with ExitStack() as es:
        S = Sched(nc, es)
        ar = Arena(nc, es, "arena", 200)
        psf = [es.enter_context(nc.psum_tensor("ps%d" % i, [128, 512], F32)) for i in range(8)]
        psb = [psf[i][:].bitcast(BF16) for i in range(8)]

        mvs = ar.alloc([2, KC], F32)
        sc1p = ar.alloc([KC], F32)
        lbr = ar.alloc([2, 4], F32)
        lbt = ar.alloc([3, 4], F32)
        GN4 = ar.alloc([512], F32)
        mask01 = ar.alloc([128], F32)
        identb = ar.alloc([128], BF16)
        ones = ar.alloc([128], F32)
        dtab = ar.alloc([2, 4, NCHK], F32)
        for (dst, src, nm) in [(mvs, mv, 'mvs'), (lbr, lbr_d, 'lbr'), (GN4, gn_d, 'GN4'), (mask01, mask_d, 'mask01'), (identb, idb_d, 'identb')]:
            S.dma('sp', lambda e, dst=dst, src=src: e.dma_start(out=dst, in_=src), slot=nm, writes=[nm])
        S.op('dve', lambda e: e.memset(ones, 1.0), writes=['ones'])
        S.op('dve', lambda e: e.tensor_scalar(out=sc1p, in0=mvs[:, 1, :], scalar1=1.0, scalar2=None, op0=ALU.add), reads=['mvs'], writes=['sc1p'])
        S.op('dve', lambda e: e.tensor_tensor(out=lbt[:, 2, :], in0=lbr[:, 0, :], in1=lbr[:, 1, :], op=ALU.subtract), reads=['lbr'], writes=['lbt2'])
        S.op('act', lambda e: e.activation(out=lbt[:, 2, :], in_=lbt[:, 2, :], func=AF.Exp), reads=['lbt2'], writes=['lbt2'])
        S.op('dve', lambda e: e.tensor_scalar(out=lbt[:, 2, :], in0=lbt[:, 2, :], scalar1=1.0, scalar2=None, op0=ALU.add), reads=['lbt2'], writes=['lbt2'])
        S.op('dve', lambda e: e.reciprocal(out=lbt[:, 0, :], in_=lbt[:, 2, :]), reads=['lbt2'], writes=['lbt0'])
        S.op('dve', lambda e: e.tensor_scalar(out=lbt[:, 1, :], in0=lbt[:, 0, :], scalar1=-1.0, scalar2=1.0, op0=ALU.mult, op1=ALU.add), reads=['lbt0'], writes=['lbt1'])
        LB = ['lbt0', 'lbt1']
        pmark = ar.mark()

        W = ar.alloc([KC, 1024], BF16)
        hT2 = [ar.alloc([KC, T], BF16) for i in range(2)]
        NXS = 3
        xs = [ar.alloc([2, T], F32) for i in range(NXS)]
        NTP = 2
        tmp = [[ar.alloc([T], F32) for i in range(6)] for p in range(NTP)]
        NST = 3
        qst = [ar.alloc([T], BF16) for i in range(NST)]
        khfm = [ar.alloc([T], BF16) for i in range(2)]
        khst = [ar.alloc([4, 128], BF16) for i in range(2)]
        vst = [ar.alloc([T], BF16) for i in range(4)]
        sgst = [ar.alloc([T], BF16) for i in range(4)]
        xv = xT.rearrange("(c q) t -> q c t", q=128)
        xit = [0]
        qit = [0]

        def load_W(col0):
            for k in range(KC):
                S.dma('pool', lambda e, k=k: e.dma_start(out=W[:, k, :], in_=wd[k * 128:(k + 1) * 128, col0:col0 + 1024]), slot='W%d' % k, writes=[('W', k)])

        hcnt = [0]

        def load_mod(tg):
            hb_ = hcnt[0] % 2
            hcnt[0] += 1
            hT = hT2[hb_]
            tsl = slice(tg * T, (tg + 1) * T)
            for c2 in range(KC // 2):
                b = xit[0] % NXS
                xit[0] += 1
                S.dma('sp', lambda e, b=b, c2=c2, tsl=tsl: e.dma_start(out=xs[b], in_=xv[:, c2 * 2:c2 * 2 + 2, tsl]), slot='xs%d' % b, writes=['xs%d' % b])
                for i in range(2):
                    c = c2 * 2 + i
                    S.op('act', lambda e, b=b, i=i, c=c, hT=hT: e.activation(out=hT[:, c, :], in_=xs[b][:, i, :], func=AF.Identity,
                                                                   scale=sc1p[:, c:c + 1], bias=mvs[:, 0, c:c + 1]),
                         reads=['xs%d' % b, 'sc1p', 'mvs'], writes=[('hT', hb_, c)])
            return hT, hb_

        load_W(0)
        hcount = 0
        nxt = load_mod(0)
        for tg in range(NTG_RUN):
            tsl = slice(tg * T, (tg + 1) * T)
            hT, hbi = nxt
            if tg + 1 < NTG_RUN:
                nxt = load_mod(tg + 1)
            QB = lambda j: (j // 2) * 4 + (j % 2)
            FB = lambda j: (j // 2) * 4 + 2 + (j % 2)
            for half in range(2):
                for k in range(KC):
                    for jj in range(2):
                        j = half * 2 + jj
                        for grp in range(2):
                            bank = QB(j) if grp == 0 else FB(j)
                            S.op('pe', lambda e, k=k, j=j, grp=grp, bank=bank, hT=hT: e.matmul(psf[bank][:], lhsT=W[:, k, grp * 512 + j * 128:grp * 512 + (j + 1) * 128], rhs=hT[:, k, :],
                                                                                    start=(k == 0), stop=(k == KC - 1)),
                                 reads=[('W', k), ('hT', hbi, k)], writes=['ps%d' % bank], inc=(k == KC - 1))
            def chain1a(j, tg=tg, tsl=tsl):
                nonlocal hcount
                pp = j % NTP
                hcount += 1
                t0, t1, t2, t3, t4, t5 = tmp[pp]
                R = lambda i, pp=pp: 't%d_%d' % (pp, i)
                qb, fb = 'ps%d' % QB(j), 'ps%d' % FB(j)
                qps, fps = psf[QB(j)], psf[FB(j)]
                psbq = psb[QB(j)]
                S.op('act', lambda e, t0=t0, fps=fps: e.activation(out=t0, in_=fps[:], func=AF.Exp, scale=-1.0), reads=[fb], writes=[R(0)])
                S.op('dve', lambda e, t0=t0: e.tensor_scalar(out=t0, in0=t0, scalar1=1.0, scalar2=None, op0=ALU.add), reads=[R(0)], writes=[R(0)])
                S.op('dve', lambda e, t0=t0: e.reciprocal(out=t0, in_=t0), reads=[R(0)], writes=[R(0)])
                S.op('dve', lambda e, t0=t0, t1=t1, j=j: e.tensor_scalar(out=t1, in0=t0, scalar1=lbt[:, 1, j:j + 1], scalar2=lbt[:, 0, j:j + 1], op0=ALU.mult, op1=ALU.add),
                     reads=[R(0)] + LB, writes=[R(1)])
                S.op('dve', lambda e, t1=t1, t2=t2: e.tensor_scalar(out=t2, in0=t1, scalar1=-1.0, scalar2=1.0, op0=ALU.mult, op1=ALU.add), reads=[R(1)], writes=[R(2)])
                S.op('act', lambda e, t0=t0, t1=t1: e.activation(out=t0, in_=t1, func=AF.Ln), reads=[R(1)], writes=[R(0)])
                for c in range(4):
                    cs = slice(c * CH, (c + 1) * CH)
                    S.op('dve', lambda e, t0=t0, t3=t3, cs=cs: e.tensor_tensor_scan(out=t3[:, cs], data0=ones, data1=t0[:, cs], initial=0.0, op0=ALU.mult, op1=ALU.add),
                         reads=[R(0), 'ones'], writes=[R(3)])
                S.op('act', lambda e, t4=t4, qps=qps: e.activation(out=t4, in_=qps[:], func=AF.Exp, scale=-1.0), reads=[qb], writes=[R(4)])
                S.op('dve', lambda e, t4=t4: e.tensor_scalar(out=t4, in0=t4, scalar1=1.0, scalar2=None, op0=ALU.add), reads=[R(4)], writes=[R(4)])
                S.op('dve', lambda e, t4=t4: e.reciprocal(out=t4, in_=t4), reads=[R(4)], writes=[R(4)])
                S.op('dve', lambda e, t4=t4, qps=qps: e.tensor_tensor(out=t4, in0=qps[:], in1=t4, op=ALU.mult), reads=[R(4), qb], writes=[R(4)])
                B3 = t3.rearrange("p (c t) -> p c t", c=4)
                Bref = t3[:, 63:T:CH].unsqueeze(2).to_broadcast([128, 4, CH])
                Blast = t3[:, CH - 1:T:CH].unsqueeze(2).to_broadcast([128, 4, CH])
                t53 = t5.rearrange("p (c t) -> p c t", c=4)
                S.op('dve', lambda e, B3=B3, Bref=Bref, t53=t53: e.tensor_tensor(out=t53, in0=B3, in1=Bref, op=ALU.subtract), reads=[R(3)], writes=[R(5)])
                S.op('act', lambda e, t5=t5: e.activation(out=t5, in_=t5, func=AF.Exp), reads=[R(5)], writes=[R(5)])
                b = qit[0] % NST
                qit[0] += 1
                S.op('dve', lambda e, t4=t4, t5=t5, b=b: e.tensor_tensor(out=qst[b], in0=t4, in1=t5, op=ALU.mult), reads=[R(4), R(5)], writes=['qst%d' % b])
                S.dma('sp', lambda e, b=b, j=j, tsl=tsl: e.dma_start(out=qt_scr[j, :, tsl], in_=qst[b]), slot='qst%d' % b, reads=['qst%d' % b], writes=[])
                S.op('dve', lambda e, B3=B3, Bref=Bref, t53=t53: e.tensor_tensor(out=t53, in0=Bref, in1=B3, op=ALU.subtract), reads=[R(3)], writes=[R(5)])
                S.op('act', lambda e, t5=t5: e.activation(out=t5, in_=t5, func=AF.Exp), reads=[R(5)], writes=[R(5)])
                b = qit[0] % NST
                qit[0] += 1
                S.op('dve', lambda e, t2=t2, t5=t5, b=b: e.scalar_tensor_tensor(out=qst[b], in0=t5, scalar=1e30, in1=t2, op0=ALU.min, op1=ALU.mult), reads=[R(2), R(5)], writes=['qst%d' % b])
                S.dma('sp', lambda e, b=b, j=j, tsl=tsl: e.dma_start(out=kt_scr[j, :, tsl], in_=qst[b]), slot='qst%d' % b, reads=['qst%d' % b], writes=[])
                S.op('dve', lambda e, B3=B3, Blast=Blast, t53=t53: e.tensor_tensor(out=t53, in0=Blast, in1=B3, op=ALU.subtract), reads=[R(3)], writes=[R(5)])
                S.op('act', lambda e, t5=t5: e.activation(out=t5, in_=t5, func=AF.Exp), reads=[R(5)], writes=[R(5)])
                kb = j % 2
                S.op('dve', lambda e, t2=t2, t5=t5, kb=kb: e.tensor_tensor(out=khfm[kb], in0=t2, in1=t5, op=ALU.mult), reads=[R(2), R(5)], writes=['khfm%d' % kb])
                for c in range(4):
                    S.op('pe', lambda e, c=c, kb=kb, psbq=psbq: e.transpose(out=psbq[:, c * 128:(c + 1) * 128], in_=khfm[kb][:, c * CH:(c + 1) * CH], identity=identb),
                         reads=['khfm%d' % kb, 'identb'], writes=[qb])
                S.op('act', lambda e, kb=kb, psbq=psbq: e.activation(out=khst[kb], in_=psbq[:, 0:512].rearrange("p (c d) -> p c d", c=4), func=AF.Copy),
                     reads=[qb], writes=['khst%d' % kb])
                S.dma('sp', lambda e, kb=kb, j=j, tg=tg: e.dma_start(out=kh_scr.rearrange("(b p) h d -> p b h d", p=128)[:, tg * 4:(tg + 1) * 4, j, :], in_=khst[kb]),
                      slot='khst%d' % kb, reads=['khst%d' % kb], writes=[])
                S.op('act', lambda e, t3=t3, j=j, tg=tg: e.activation(out=dtab[:, 0, j, tg * 4:(tg + 1) * 4], in_=t3[:, CH - 1:T:CH], func=AF.Exp), reads=[R(3)], writes=[('dtab', j, tg)])
                S.op('act', lambda e, t3=t3, j=j, tg=tg: e.activation(out=dtab[:, 1, j, tg * 4:(tg + 1) * 4], in_=t3[:, 63:T:CH], func=AF.Exp), reads=[R(3)], writes=[('dtab', j, tg)])
            for pair in ((0, 1), (2, 3)):
                S.replay([S.record(lambda j=j: chain1a(j)) for j in pair])

        load_W(1024)
        vit = 0
        nxt = load_mod(0)
        for tg in range(NTG_RUN):
            hT, hbi = nxt
            if tg + 1 < NTG_RUN:
                nxt = load_mod(tg + 1)
            for k in range(KC):
                for tb in range(4):
                    for grp in range(2):
                        bank = grp * 4 + tb
                        S.op('pe', lambda e, k=k, tb=tb, grp=grp, bank=bank, hT=hT: e.matmul(psf[bank][:], lhsT=hT[:, k, tb * 128:(tb + 1) * 128], rhs=W[:, k, grp * 512:(grp + 1) * 512],
                                                                                 start=(k == 0), stop=(k == KC - 1)),
                             reads=[('W', k), ('hT', hbi, k)], writes=['ps%d' % bank], inc=(k == KC - 1))
            def chain1b(tb, tg=tg):
                nonlocal vit
                vb = tb
                vit += 1
                r0 = tg * T + tb * 128
                S.op('act', lambda e, vb=vb, tb=tb: e.activation(out=vst[vb], in_=psf[tb][:], func=AF.Copy), reads=['ps%d' % tb], writes=['vst%d' % vb])
                S.dma('sp', lambda e, vb=vb, r0=r0: e.dma_start(out=v_scr[r0:r0 + 128, :, :].rearrange("p h d -> p (h d)"), in_=vst[vb]), slot='vst%d' % vb, reads=['vst%d' % vb], writes=[])
                t0 = tmp[tb % 2][tb // 2]
                tr = 't%d_%d' % (tb % 2, tb // 2)
                gb = 'ps%d' % (4 + tb)
                S.op('act', lambda e, t0=t0, tb=tb: e.activation(out=t0, in_=psf[4 + tb][:], func=AF.Exp, scale=-1.0), reads=[gb], writes=[tr])
                S.op('dve', lambda e, t0=t0: e.tensor_scalar(out=t0, in0=t0, scalar1=1.0, scalar2=None, op0=ALU.add), reads=[tr], writes=[tr])
                S.op('dve', lambda e, t0=t0: e.reciprocal(out=t0, in_=t0), reads=[tr], writes=[tr])
                S.op('dve', lambda e, t0=t0, tb=tb: e.tensor_tensor(out=t0, in0=psf[4 + tb][:], in1=t0, op=ALU.mult), reads=[tr, gb], writes=[tr])
                S.op('dve', lambda e, t0=t0, vb=vb: e.tensor_tensor(out=sgst[vb], in0=t0, in1=GN4, op=ALU.mult), reads=[tr, 'GN4'], writes=['sgst%d' % vb])
                S.dma('sp', lambda e, vb=vb, r0=r0: e.dma_start(out=sg_scr[r0:r0 + 128, :, :].rearrange("p h d -> p (h d)"), in_=sgst[vb]), slot='sgst%d' % vb, reads=['sgst%d' % vb], writes=[])
            S.replay([S.record(lambda tb=tb: chain1b(tb)) for tb in range(4)])

        S.barrier()
        ar.reset(pmark)
        NC_RUN = NTG_RUN * 4
        QT = [ar.alloc([S_], BF16) for i in range(2)]
        KT = [ar.alloc([S_], BF16) for i in range(2)]
        KH = [ar.alloc([NCHK, 128], BF16) for i in range(2)]
        VV = [ar.alloc([NCHK, 128], BF16) for i in range(2)]
        SG = [ar.alloc([NCHK, 128], BF16) for i in range(2)]
        St = [ar.alloc([128], F32) for i in range(2)]
        Sbf = [[ar.alloc([128], BF16) for i in range(2)] for hb in range(2)]
        ATs = [[ar.alloc([128], BF16) for i in range(2)] for hb in range(2)]
        sc = [ar.alloc([4, 4], F32) for hb in range(2)]
        junk = [ar.alloc([128], F32) for hb in range(2)]
        on = [[ar.alloc([128], BF16) for i in range(2)] for hb in range(2)]
        ost = [ar.alloc([T], BF16) for i in range(4)]
        for hb in range(2):
            for i in range(2):
                S.op('dve', lambda e, hb=hb, i=i: e.memset(ATs[hb][i], 0.0), writes=['ATs%d_%d' % (hb, i)])
        tokv = lambda scr: scr.rearrange("(b p) h d -> p b h d", p=128)
        oi = [0]
        BA = lambda hb: hb * 4 + 0
        BU = lambda hb: hb * 4 + 1
        BO = lambda hb: hb * 4 + 2
        BT = lambda hb: hb * 4 + 3

        def emit_AU(c, hb):
            cs = slice(c * CH, (c + 1) * CH)
            a = c % 2
            S.op('pe', lambda e: e.matmul(psf[BA(hb)][:, 0:128], lhsT=KT[hb][:, cs], rhs=QT[hb][:, cs], start=True, stop=True),
                 reads=['KT%d' % hb, 'QT%d' % hb], writes=['ps%d' % BA(hb)])
            S.op('dve', lambda e: e.copy_predicated(out=ATs[hb][a], mask=mask01.bitcast(mybir.dt.uint32), data=psf[BA(hb)][:, 0:128]),
                 reads=['ps%d' % BA(hb), 'mask01', 'ATs%d_%d' % (hb, a)], writes=['ATs%d_%d' % (hb, a)])
            S.op('pe', lambda e: e.matmul(psf[BU(hb)][:, 0:128], lhsT=KH[hb][:, c, :], rhs=VV[hb][:, c, :], start=True, stop=True),
                 reads=['KH%d' % hb, 'VV%d' % hb], writes=['ps%d' % BU(hb)])

        def emit_chunk(c, hb, h):
            a = c % 2
            cs = slice(c * CH, (c + 1) * CH)
            ob = 'ps%d' % BO(hb)
            S.op('pe', lambda e: e.matmul(psf[BO(hb)][:, 0:128], lhsT=ATs[hb][a], rhs=VV[hb][:, c, :], start=True, stop=False),
                 reads=['ATs%d_%d' % (hb, a), 'VV%d' % hb], writes=[ob], inc=False)
            S.op('pe', lambda e: e.matmul(psf[BO(hb)][:, 0:128], lhsT=QT[hb][:, cs], rhs=Sbf[hb][a], start=False, stop=True),
                 reads=['QT%d' % hb, 'Sbf%d_%d' % (hb, a)], writes=[ob])
            S.op('dve', lambda e: e.scalar_tensor_tensor(out=St[hb], in0=St[hb], scalar=dtab[:, 0, h, c:c + 1], in1=psf[BU(hb)][:, 0:128], op0=ALU.mult, op1=ALU.add),
                 reads=['St%d' % hb, 'ps%d' % BU(hb)], writes=['St%d' % hb])
            if c + 1 < NC_RUN:
                S.op('act', lambda e: e.activation(out=Sbf[hb][1 - a], in_=St[hb], func=AF.Identity, scale=dtab[:, 1, h, c + 1:c + 2]),
                     reads=['St%d' % hb], writes=['Sbf%d_%d' % (hb, 1 - a)])
            s4 = c % 4
            S.op('act', lambda e: e.activation(out=junk[hb], in_=psf[BO(hb)][:, 0:128], func=AF.Square, accum_out=sc[hb][:, s4, 0:1]), reads=[ob], writes=['junk%d' % hb, ('sc0', hb, s4)])
            S.op('dve', lambda e: e.tensor_scalar(out=sc[hb][:, s4, 0:1], in0=sc[hb][:, s4, 0:1], scalar1=1.0 / 128, scalar2=RMS_EPS, op0=ALU.mult, op1=ALU.add),
                 reads=[('sc0', hb, s4)], writes=[('sc0', hb, s4)])
            S.op('act', lambda e: e.activation(out=sc[hb][:, s4, 1:2], in_=sc[hb][:, s4, 0:1], func=AF.Sqrt), reads=[('sc0', hb, s4)], writes=[('sc1', hb, s4)])
            S.op('dve', lambda e: e.reciprocal(out=sc[hb][:, s4, 1:2], in_=sc[hb][:, s4, 1:2]), reads=[('sc1', hb, s4)], writes=[('sc1', hb, s4)])
            S.op('dve', lambda e: e.scalar_tensor_tensor(out=on[hb][a], in0=psf[BO(hb)][:, 0:128], scalar=sc[hb][:, s4, 1:2], in1=SG[hb][:, c, :], op0=ALU.mult, op1=ALU.mult),
                 reads=[ob, ('sc1', hb, s4), 'SG%d' % hb], writes=['on%d_%d' % (hb, a)])
            S.op('pe', lambda e: e.transpose(out=psb[BT(hb)][:, s4 * 128:(s4 + 1) * 128], in_=on[hb][a], identity=identb),
                 reads=['on%d_%d' % (hb, a), 'identb'], writes=['ps%d' % BT(hb)])
            if s4 == 3:
                o2 = oi[0] % 4
                oi[0] += 1
                g4 = c // 4
                S.op('act', lambda e: e.activation(out=ost[o2], in_=psb[BT(hb)][:, 0:512], func=AF.Copy), reads=['ps%d' % BT(hb)], writes=['ost%d' % o2])
                S.dma('sp', lambda e: e.dma_start(out=oTd[h * 128:(h + 1) * 128, g4 * T:(g4 + 1) * T], in_=ost[o2]),
                      slot='ost%d' % o2, reads=['ost%d' % o2], writes=[])

        heads = list(heads)
        for pi in range(0, len(heads), 2):
            hs = heads[pi:pi + 2]
            for hb, h in enumerate(hs):
                S.dma('sp', lambda e, h=h, hb=hb: e.dma_start(out=QT[hb], in_=qt_scr[h]), slot='QT%d' % hb, writes=['QT%d' % hb])
                S.dma('sp', lambda e, h=h, hb=hb: e.dma_start(out=KT[hb], in_=kt_scr[h]), slot='KT%d' % hb, writes=['KT%d' % hb])
                S.dma('sp', lambda e, h=h, hb=hb: e.dma_start(out=KH[hb], in_=tokv(kh_scr)[:, :, h, :]), slot='KH%d' % hb, writes=['KH%d' % hb])
                S.dma('sp', lambda e, h=h, hb=hb: e.dma_start(out=VV[hb], in_=tokv(v_scr)[:, :, h, :]), slot='VV%d' % hb, writes=['VV%d' % hb])
                S.dma('sp', lambda e, h=h, hb=hb: e.dma_start(out=SG[hb], in_=tokv(sg_scr)[:, :, h, :]), slot='SG%d' % hb, writes=['SG%d' % hb])
                S.op('dve', lambda e, hb=hb: e.memset(St[hb], 0.0), writes=['St%d' % hb])
                S.op('dve', lambda e, hb=hb: e.memset(Sbf[hb][0], 0.0), writes=['Sbf%d_0' % hb])
            for hb, h in enumerate(hs):
                emit_AU(0, hb)
            for c in range(NC_RUN):
                for hb, h in enumerate(hs):
                    emit_chunk(c, hb, h)
                    if c + 1 < NC_RUN:
                        emit_AU(c + 1, hb)
        S.finish()
        print("stage d ninst", S.ninst)
    return nc


D = 4096
KC = 32
T = 512
ALPHA = 4 ** 0.25
LN_EPS = 1e-5


def build_ff(moe, TOK=1024, n_groups_limit=None, mode='full'):
    nc = bass.Bass("TRN2", target_bir_lowering=False)
    NPASS = TOK // T
    oT = nc.dram_tensor("oT", [D, TOK], BF16, kind="ExternalInput").ap()
    xT = nc.dram_tensor("xT", [D, TOK], F32, kind="ExternalInput").ap()
    pv = nc.dram_tensor("pv", [128, 8, KC], F32, kind="ExternalInput").ap()
    w_o = nc.dram_tensor("w_o", [D, D], F32, kind="ExternalInput").ap()
    ones_d = nc.dram_tensor("ones", [128, 128], F32, kind="ExternalInput").ap()
    if moe:
        NE, H = 8, 4096
        rw_d = nc.dram_tensor("rw", [128, KC, 8], F32, kind="ExternalInput").ap()
        groups = []
        if mode == 'full':
            w_in = nc.dram_tensor("w_in", [NE, D, 2 * H], F32, kind="ExternalInput").ap()
            w_o2 = nc.dram_tensor("w_o2", [NE, H, D], F32, kind="ExternalInput").ap()
            sel_d = nc.dram_tensor("sel", [8, 8, 128], F32, kind="ExternalInput").ap()
            ident_d = nc.dram_tensor("ident", [128, 128], F32, kind="ExternalInput").ap()
            for e in range(NE):
                groups.append(dict(win=w_in[e].rearrange("k (s c) -> k s c", s=2), wo=w_o2[e],
                                   pairs=list(range(16)), gate=e))
    else:
        H = 11008
        w_in = nc.dram_tensor("w_in", [D, 2 * H], F32, kind="ExternalInput").ap()
        w_o2 = nc.dram_tensor("w_o2", [H, D], F32, kind="ExternalInput").ap()
        win_v = w_in.rearrange("k (s c) -> k s c", s=2)
        groups = [dict(win=win_v, wo=w_o2, pairs=list(range(0, 16)), gate=None),
                  dict(win=win_v, wo=w_o2, pairs=list(range(16, 32)), gate=None),
                  dict(win=win_v, wo=w_o2, pairs=list(range(32, 43)), gate=None)]
    if n_groups_limit is not None:
        groups = groups[:n_groups_limit]
    if mode == 'full':
        yT = nc.dram_tensor("yT", [D, TOK], F32, kind="ExternalOutput").ap()
    else:
        h2o = nc.dram_tensor("h2o", [D, TOK], BF16, kind="ExternalOutput").ap()
        acco = nc.dram_tensor("acco", [D, TOK], F32, kind="ExternalOutput").ap()
        gout = nc.dram_tensor("gout", [TOK, 8], F32, kind="ExternalOutput").ap()

    with ExitStack() as es:
        S = Sched(nc, es)
        sb = lambda name, shape, dt: es.enter_context(nc.sbuf_tensor(name, shape, dt))
        acc = sb("acc", [128, KC, T], F32)
        actT = sb("actT", [128, KC, T], BF16)
        h2T = sb("h2T", [128, KC, T], BF16)
        NB = 8
        wt = [sb("wt%d" % i, [128, 512], BF16) for i in range(NB)]
        pvs = sb("pvs", [128, 8, KC], F32)
        dv = sb("dv", [128, 6, KC], F32)
        ones = sb("ones_s", [128, 128], F32)
        s1 = sb("s1", [128, T], F32)
        s2 = sb("s2", [128, T], F32)
        mean = sb("mean", [128, T], F32)
        msq = sb("msq", [128, T], F32)
        rstd = sb("rstd", [128, T], F32)
        NTB = 3
        tb_ = [sb("tb%d" % i, [128, T], F32) for i in range(NTB)]
        sa_ = [sb("sa%d" % i, [128, T], F32) for i in range(NTB)]
        yo_ = [sb("yo%d" % i, [128, T], F32) for i in range(NTB)]
        ps = [es.enter_context(nc.psum_tensor("ps%d" % i, [128, 512], F32)) for i in range(8)]
        if moe:
            rw = sb("rw_s", [128, KC, 8], F32)
            if mode == 'full':
                sel = sb("sel_s", [8, 8, 128], F32)
                ident = sb("ident_s", [128, 128], F32)
            h2f_ = [sb("h2f%d" % i, [128, T], F32) for i in range(2)]
            G_ = [sb("G%d" % i, [128, T], F32) for i in range(2)]
            bg_ = [sb("bg%d" % i, [128, T], F32) for i in range(NTB)]
            gT = sb("gT", [8, T], F32)
            Lsb = sb("Lsb", [128, 4, 8], F32)
            m8 = sb("m8", [128, 4, 8], F32)
            nv1 = sb("nv1", [128, 4], F32)
            msk = sb("msk", [128, 4, 8], F32)
            ex = sb("ex", [128, 4, 8], F32)
            me = sb("me", [128, 4, 8], F32)
            den = sb("den", [128, 4], F32)
            gsb = sb("gsb", [128, 4, 8], F32)

        S.dma('sp', lambda e: e.dma_start(out=pvs[:], in_=pv), slot='pvs', writes=['pvs'])
        S.dma('sp', lambda e: e.dma_start(out=ones[:], in_=ones_d), slot='ones', writes=['ones'])
        if moe:
            S.dma('sp', lambda e: e.dma_start(out=rw[:], in_=rw_d), slot='rw', writes=['rw'])
            if mode == 'full':
                S.dma('sp', lambda e: e.dma_start(out=sel[:], in_=sel_d), slot='sel', writes=['sel'])
                S.dma('sp', lambda e: e.dma_start(out=ident[:], in_=ident_d), slot='ident', writes=['ident'])
        V = lambda i: pvs[:, i, :]
        S.op('dve', lambda e: e.tensor_scalar(out=dv[:, 0, :], in0=V(0), scalar1=1.0, scalar2=None, op0=ALU.add), reads=['pvs'], writes=['dv0'])
        S.op('dve', lambda e: e.tensor_scalar(out=dv[:, 5, :], in0=V(2), scalar1=1.0, scalar2=None, op0=ALU.add), reads=['pvs'], writes=['dv5'])
        S.op('dve', lambda e: e.tensor_tensor(out=dv[:, 1, :], in0=V(4), in1=dv[:, 5, :], op=ALU.mult), reads=['pvs', 'dv5'], writes=['dv1'])
        S.op('dve', lambda e: e.tensor_tensor(out=dv[:, 2, :], in0=V(5), in1=dv[:, 5, :], op=ALU.mult), reads=['pvs', 'dv5'], writes=['dv2'])
        S.op('dve', lambda e: e.tensor_tensor(out=dv[:, 2, :], in0=dv[:, 2, :], in1=V(1), op=ALU.add), reads=['pvs', 'dv2'], writes=['dv2'])
        S.op('dve', lambda e: e.tensor_scalar(out=dv[:, 3, :], in0=V(4), scalar1=ALPHA, scalar2=None, op0=ALU.mult), reads=['pvs'], writes=['dv3'])
        S.op('dve', lambda e: e.tensor_scalar(out=dv[:, 4, :], in0=V(5), scalar1=ALPHA, scalar2=None, op0=ALU.mult), reads=['pvs'], writes=['dv4'])
        S.op('dve', lambda e: e.tensor_scalar(out=dv[:, 5, :], in0=V(3), scalar1=1.0, scalar2=None, op0=ALU.add), reads=['pvs', 'dv1', 'dv2'], writes=['dv5'])
        DVR = ['dv0', 'dv1', 'dv2', 'dv3', 'dv4', 'dv5', 'pvs']

        witer = [0]

        def wtile_load(src_ap, three=False):
            b = witer[0] % NB
            witer[0] += 1
            if three:
                S.dma('pool', lambda e: e.dma_start(out=wt[b][:].rearrange("p (s c) -> p s c", s=2), in_=src_ap),
                      slot='wt%d' % b, writes=['wt%d' % b])
            else:
                S.dma('pool', lambda e: e.dma_start(out=wt[b][:], in_=src_ap), slot='wt%d' % b, writes=['wt%d' % b])
            return b

        bankset = [0]

        def gemm_group(tiles, rhs_of, rhs_res, evac):
            base = (bankset[0] % 2) * 4
            bankset[0] += 1
            nk = len(tiles)
            for ki, (src, three) in enumerate(tiles):
                b = wtile_load(src, three)
                for j in range(4):
                    bank = base + j
                    S.op('pe', lambda e, b=b, j=j, ki=ki, bank=bank: e.matmul(
                        ps[bank][:], lhsT=wt[b][:, j * 128:(j + 1) * 128], rhs=rhs_of(ki),
                        start=(ki == 0), stop=(ki == nk - 1)),
                        reads=['wt%d' % b] + rhs_res(ki), writes=['ps%d' % bank], inc=(j == 3))
            for j in range(4):
                evac(j, base + j)

        def layernorm(final, ps_pass):
            for c in range(KC):
                if c == 0:
                    S.op('dve', lambda e: e.tensor_copy(out=s1[:], in_=acc[:, 0, :]), reads=[('acc', 0)], writes=['s1'])
                    S.op('act', lambda e: e.activation(out=s2[:], in_=acc[:, 0, :], func=AF.Square), reads=[('acc', 0)], writes=['s2'])
                else:
                    t = tb_[c % NTB]
                    tr = 'tb%d' % (c % NTB)
                    S.op('act', lambda e, c=c, t=t: e.activation(out=t[:], in_=acc[:, c, :], func=AF.Square), reads=[('acc', c)], writes=[tr])
                    S.op('dve', lambda e, c=c: e.tensor_tensor(out=s1[:], in0=s1[:], in1=acc[:, c, :], op=ALU.add), reads=[('acc', c), 's1'], writes=['s1'])
                    S.op('dve', lambda e, t=t: e.tensor_tensor(out=s2[:], in0=s2[:], in1=t[:], op=ALU.add), reads=[tr, 's2'], writes=['s2'])
            S.op('pe', lambda e: e.matmul(ps[4][:], lhsT=ones[:], rhs=s1[:], start=True, stop=True), reads=['ones', 's1'], writes=['ps4'])
            S.op('pe', lambda e: e.matmul(ps[5][:], lhsT=ones[:], rhs=s2[:], start=True, stop=True), reads=['ones', 's2'], writes=['ps5'])
            S.op('act', lambda e: e.activation(out=mean[:], in_=ps[4][:], func=AF.Copy, scale=1.0 / D), reads=['ps4'], writes=['mean'])
            S.op('dve', lambda e: e.tensor_tensor(out=msq[:], in0=mean[:], in1=mean[:], op=ALU.mult), reads=['mean'], writes=['msq'])
            S.op('dve', lambda e: e.scalar_tensor_tensor(out=msq[:], in0=ps[5][:], scalar=1.0 / D, in1=msq[:], op0=ALU.mult, op1=ALU.subtract),
                 reads=['ps5', 'msq'], writes=['msq'])
            S.op('dve', lambda e: e.tensor_scalar(out=msq[:], in0=msq[:], scalar1=LN_EPS, scalar2=None, op0=ALU.add), reads=['msq'], writes=['msq'])
            S.op('act', lambda e: e.activation(out=rstd[:], in_=msq[:], func=AF.Sqrt), reads=['msq'], writes=['rstd'])
            S.op('dve', lambda e: e.reciprocal(out=rstd[:], in_=rstd[:]), reads=['rstd'], writes=['rstd'])
            for c in range(KC):
                t = tb_[c % NTB]
                tr = 'tb%d' % (c % NTB)
                S.op('dve', lambda e, c=c, t=t: e.tensor_tensor(out=t[:], in0=acc[:, c, :], in1=mean[:], op=ALU.subtract), reads=[('acc', c), 'mean'], writes=[tr])
                S.op('dve', lambda e, t=t: e.tensor_tensor(out=t[:], in0=t[:], in1=rstd[:], op=ALU.mult), reads=[tr, 'rstd'], writes=[tr])
                if not final:
                    S.op('act', lambda e, c=c, t=t: e.activation(out=h2T[:, c, :], in_=t[:], func=AF.Identity, scale=dv[:, 1, c:c + 1], bias=dv[:, 2, c:c + 1]),
                         reads=[tr] + DVR, writes=[('h2T', c)])
                    if moe:
                        hf = h2f_[c % 2]
                        hr = 'h2f%d' % (c % 2)
                        S.op('dve', lambda e, c=c, t=t, hf=hf: e.tensor_scalar(out=hf[:], in0=t[:], scalar1=dv[:, 1, c:c + 1], scalar2=dv[:, 2, c:c + 1], op0=ALU.mult, op1=ALU.add),
                             reads=[tr] + DVR, writes=[hr])
                        for q in range(4):
                            S.op('pe', lambda e, c=c, q=q, hf=hf: e.matmul(ps[q][:, 0:8], lhsT=hf[:, q * 128:(q + 1) * 128], rhs=rw[:, c, :],
                                                                     start=(c == 0), stop=(c == KC - 1)),
                                 reads=[hr, 'rw'], writes=['ps%d' % q])
                    S.op('act', lambda e, c=c, t=t: e.activation(out=acc[:, c, :], in_=t[:], func=AF.Identity, scale=dv[:, 3, c:c + 1], bias=dv[:, 4, c:c + 1]),
                         reads=[tr] + DVR, writes=[('acc', c)])
                else:
                    yo = yo_[c % NTB]
                    yr = 'yo%d' % (c % NTB)
                    S.op('act', lambda e, c=c, t=t, yo=yo: e.activation(out=yo[:], in_=t[:], func=AF.Identity, scale=pvs[:, 6, c:c + 1], bias=pvs[:, 7, c:c + 1]),
                         reads=[tr] + DVR, writes=[yr])
                    S.dma('sp', lambda e, c=c, yo=yo: e.dma_start(out=yT[c * 128:(c + 1) * 128, ps_pass * T:(ps_pass + 1) * T], in_=yo[:]),
                          slot=yr, reads=[yr], writes=[])

        for p in range(NPASS):
            tsl = slice(p * T, (p + 1) * T)
            xv = xT.rearrange("(c q) t -> q c t", q=128)
            ov = oT.rearrange("(c q) t -> q c t", q=128)
            for g4 in range(4):
                cs = slice(g4 * 8, (g4 + 1) * 8)
                S.dma('sp', lambda e, cs=cs, tsl=tsl, xv=xv: e.dma_start(out=acc[:, cs, :], in_=xv[:, cs, tsl]), slot='accld%d' % g4,
                      writes=[('acc', c) for c in range(g4 * 8, g4 * 8 + 8)])
                S.dma('sp', lambda e, cs=cs, tsl=tsl, ov=ov: e.dma_start(out=actT[:, cs, :], in_=ov[:, cs, tsl]), slot='actld%d' % g4,
                      writes=[('actT', c) for c in range(g4 * 8, g4 * 8 + 8)])
            for c in range(KC):
                S.op('act', lambda e, c=c: e.activation(out=acc[:, c, :], in_=acc[:, c, :], func=AF.Copy, scale=ALPHA),
                     reads=[('acc', c)], writes=[('acc', c)])
            for ng in range(D // 512):
                tiles = [(w_o[k * 128:(k + 1) * 128, ng * 512:(ng + 1) * 512], False) for k in range(KC)]

                def evac(j, bank, ng=ng):
                    n = ng * 4 + j
                    S.op('dve', lambda e: e.scalar_tensor_tensor(out=acc[:, n, :], in0=ps[bank][:], scalar=dv[:, 0, n:n + 1], in1=acc[:, n, :],
                                                                 op0=ALU.mult, op1=ALU.add),
                         reads=['ps%d' % bank, ('acc', n)] + DVR, writes=[('acc', n)])
                gemm_group(tiles, lambda ki: actT[:, ki, :], lambda ki: [('actT', ki)], evac)
            layernorm(False, p)
            if moe:
                for q in range(4):
                    S.op('dve', lambda e, q=q: e.tensor_copy(out=Lsb[:, q, :], in_=ps[q][:, 0:8]), reads=['ps%d' % q], writes=[('L', q)])
                    S.op('dve', lambda e, q=q: e.max(out=m8[:, q, :], in_=Lsb[:, q, :]), reads=[('L', q)], writes=[('m8', q)])
                    S.op('dve', lambda e, q=q: e.tensor_scalar(out=nv1[:, q:q + 1], in0=m8[:, q, 0:1], scalar1=-1.0, scalar2=None, op0=ALU.mult),
                         reads=[('m8', q)], writes=[('nv1', q)])
                    S.op('dve', lambda e, q=q: e.tensor_scalar(out=msk[:, q, :], in0=Lsb[:, q, :], scalar1=m8[:, q, 1:2], scalar2=None, op0=ALU.is_ge),
                         reads=[('m8', q), ('L', q)], writes=[('msk', q)])
                    S.op('act', lambda e, q=q: e.activation(out=ex[:, q, :], in_=Lsb[:, q, :], func=AF.Exp, bias=nv1[:, q:q + 1], scale=1.0),
                         reads=[('L', q), ('nv1', q)], writes=[('ex', q)])
                    S.op('dve', lambda e, q=q: e.tensor_tensor(out=me[:, q, :], in0=msk[:, q, :], in1=ex[:, q, :], op=ALU.mult),
                         reads=[('msk', q), ('ex', q)], writes=[('me', q)])
                    S.op('dve', lambda e, q=q: e.reduce_sum(out=den[:, q:q + 1], in_=me[:, q, :], axis=AX.X),
                         reads=[('me', q)], writes=[('den', q)])
                    S.op('dve', lambda e, q=q: e.reciprocal(out=den[:, q:q + 1], in_=den[:, q:q + 1]), reads=[('den', q)], writes=[('den', q)])
                    S.op('dve', lambda e, q=q: e.tensor_scalar(out=gsb[:, q, :], in0=me[:, q, :], scalar1=den[:, q:q + 1], scalar2=None, op0=ALU.mult),
                         reads=[('me', q), ('den', q)], writes=[('gsb', q)])
                    if mode == 'full':
                        S.op('pe', lambda e, q=q: e.transpose(out=ps[6][0:8, q * 128:(q + 1) * 128], in_=gsb[:, q, :], identity=ident[:]),
                             reads=[('gsb', q), 'ident'], writes=['ps6'])
                if mode == 'full':
                    S.op('act', lambda e: e.activation(out=gT[:], in_=ps[6][0:8, :], func=AF.Copy), reads=['ps6'], writes=['gT'])
            if mode == 'e1':
                hv = h2o.rearrange("(c q) t -> q c t", q=128)
                av = acco.rearrange("(c q) t -> q c t", q=128)
                for g4 in range(4):
                    cs = slice(g4 * 8, (g4 + 1) * 8)
                    S.dma('sp', lambda e, cs=cs, tsl=tsl, hv=hv: e.dma_start(out=hv[:, cs, tsl], in_=h2T[:, cs, :]), slot='h2o%d' % g4,
                          reads=[('h2T', c) for c in range(g4 * 8, g4 * 8 + 8)], writes=[])
                    S.dma('sp', lambda e, cs=cs, tsl=tsl, av=av: e.dma_start(out=av[:, cs, tsl], in_=acc[:, cs, :]), slot='acco%d' % g4,
                          reads=[('acc', c) for c in range(g4 * 8, g4 * 8 + 8)], writes=[])
                S.dma('sp', lambda e, p=p: e.dma_start(out=gout.rearrange("(q r) x -> r q x", r=128)[:, p * 4:(p + 1) * 4, :], in_=gsb[:]), slot='gout',
                      reads=[('gsb', q) for q in range(4)], writes=[])
                continue
            for gi, g in enumerate(groups):
                if g['gate'] is not None:
                    G = G_[gi % 2]
                    Gr = 'G%d' % (gi % 2)
                    ge = g['gate']
                    S.op('pe', lambda e, ge=ge: e.matmul(ps[7][:], lhsT=sel[:, ge, :], rhs=gT[:], start=True, stop=True),
                         reads=['sel', 'gT'], writes=['ps7'])
                    S.op('act', lambda e, G=G: e.activation(out=G[:], in_=ps[7][:], func=AF.Copy), reads=['ps7'], writes=[Gr])
                pairs = g['pairs']
                chunks = []
                for pi, P in enumerate(pairs):
                    tiles = [(g['win'][k * 128:(k + 1) * 128, :, P * 256:(P + 1) * 256], True) for k in range(KC)]
                    lj0 = pi * 2

                    def evac(j, bank, lj0=lj0, g=g):
                        if j >= 2:
                            return
                        lj = lj0 + j
                        abank, bbank = bank, bank + 2
                        i3 = lj % NTB
                        sa = sa_[i3]
                        S.op('act', lambda e: e.activation(out=sa[:], in_=ps[abank][:], func=AF.Silu), reads=['ps%d' % abank], writes=['sa%d' % i3])
                        if g['gate'] is not None:
                            bg = bg_[i3]
                            Gg = G_[gi % 2]
                            S.op('dve', lambda e: e.tensor_tensor(out=bg[:], in0=ps[bbank][:], in1=Gg[:], op=ALU.mult),
                                 reads=['ps%d' % bbank, 'G%d' % (gi % 2)], writes=['bg%d' % i3])
                            S.op('dve', lambda e: e.tensor_tensor(out=actT[:, lj, :], in0=sa[:], in1=bg[:], op=ALU.mult),
                                 reads=['sa%d' % i3, 'bg%d' % i3], writes=[('actT', lj)])
                        else:
                            S.op('dve', lambda e: e.tensor_tensor(out=actT[:, lj, :], in0=sa[:], in1=ps[bbank][:], op=ALU.mult),
                                 reads=['sa%d' % i3, 'ps%d' % bbank], writes=[('actT', lj)])
                    gemm_group(tiles, lambda ki: h2T[:, ki, :], lambda ki: [('h2T', ki)], evac)
                    chunks += [(lj0, P * 2), (lj0 + 1, P * 2 + 1)]
                for ng in range(D // 512):
                    tiles = [(g['wo'][gj * 128:(gj + 1) * 128, ng * 512:(ng + 1) * 512], False) for (lj, gj) in chunks]

                    def evac2(j, bank, ng=ng):
                        n = ng * 4 + j
                        S.op('dve', lambda e: e.scalar_tensor_tensor(out=acc[:, n, :], in0=ps[bank][:], scalar=dv[:, 5, n:n + 1], in1=acc[:, n, :],
                                                                     op0=ALU.mult, op1=ALU.add),
                             reads=['ps%d' % bank, ('acc', n)] + DVR, writes=[('acc', n)])
                    gemm_group(tiles, lambda ki, chunks=chunks: actT[:, chunks[ki][0], :], lambda ki, chunks=chunks: [('actT', chunks[ki][0])], evac2)
            layernorm(True, p)
        S.finish()
        print("ff ninst", S.ninst)
    return nc


def build_e2(NP):
    nc = bass.Bass("TRN2", target_bir_lowering=False)
    H = 4096
    hT_d = nc.dram_tensor("hT", [D, NP], BF16, kind="ExternalInput").ap()
    gb_d = nc.dram_tensor("gb", [128, NP], F32, kind="ExternalInput").ap()
    w_in = nc.dram_tensor("w_in", [D, 2 * H], F32, kind="ExternalInput").ap()
    w_o2 = nc.dram_tensor("w_o2", [H, D], F32, kind="ExternalInput").ap()
    yT = nc.dram_tensor("yT", [D, NP], F32, kind="ExternalOutput").ap()
    win_v = w_in.rearrange("k (s c) -> k s c", s=2)
    with ExitStack() as es:
        S = Sched(nc, es)
        sb = lambda name, shape, dt: es.enter_context(nc.sbuf_tensor(name, shape, dt))
        actT = sb("actT", [128, KC, T], BF16)
        h2T = [sb("h2T%d" % i, [128, KC, T], BF16) for i in range(2)]
        NB = 8
        wt = [sb("wt%d" % i, [128, 512], BF16) for i in range(NB)]
        NTB = 3
        sa_ = [sb("sa%d" % i, [128, T], F32) for i in range(NTB)]
        bg_ = [sb("bg%d" % i, [128, T], F32) for i in range(NTB)]
        yo_ = [sb("yo%d" % i, [128, T], F32) for i in range(4)]
        G_ = [sb("G%d" % i, [128, T], F32) for i in range(2)]
        ps = [es.enter_context(nc.psum_tensor("ps%d" % i, [128, 512], F32)) for i in range(8)]
        witer = [0]
        bankset = [0]
        yit = [0]

        def wtile_load(src_ap, three):
            b = witer[0] % NB
            witer[0] += 1
            if three:
                S.dma('pool', lambda e: e.dma_start(out=wt[b][:].rearrange("p (s c) -> p s c", s=2), in_=src_ap), slot='wt%d' % b, writes=['wt%d' % b])
            else:
                S.dma('pool', lambda e: e.dma_start(out=wt[b][:], in_=src_ap), slot='wt%d' % b, writes=['wt%d' % b])
            return b

        def gemm_group(tiles, rhs_of, rhs_res, evac):
            base = (bankset[0] % 2) * 4
            bankset[0] += 1
            nk = len(tiles)
            for ki, (src, three) in enumerate(tiles):
                b = wtile_load(src, three)
                for j in range(4):
                    bank = base + j
                    S.op('pe', lambda e, b=b, j=j, ki=ki, bank=bank: e.matmul(ps[bank][:], lhsT=wt[b][:, j * 128:(j + 1) * 128], rhs=rhs_of(ki),
                                                                          start=(ki == 0), stop=(ki == nk - 1)),
                         reads=['wt%d' % b] + rhs_res(ki), writes=['ps%d' % bank], inc=(j == 3))
            for j in range(4):
                evac(j, base + j)

        hv = hT_d.rearrange("(c q) t -> q c t", q=128)
        for p in range(NP // T):
            tsl = slice(p * T, (p + 1) * T)
            hb = p % 2
            hT = h2T[hb]
            G = G_[hb]
            for g4 in range(4):
                cs = slice(g4 * 8, (g4 + 1) * 8)
                S.dma('sp', lambda e, cs=cs, tsl=tsl, hT=hT: e.dma_start(out=hT[:, cs, :], in_=hv[:, cs, tsl]), slot='hld%d_%d' % (hb, g4),
                      writes=[('h2T', hb, c) for c in range(g4 * 8, g4 * 8 + 8)])
            S.dma('sp', lambda e, tsl=tsl, G=G: e.dma_start(out=G[:], in_=gb_d[:, tsl]), slot='G%d' % hb, writes=['G%d' % hb])
            chunks = []
            for P in range(16):
                tiles = [(win_v[k * 128:(k + 1) * 128, :, P * 256:(P + 1) * 256], True) for k in range(KC)]

                def evac(j, bank, P=P, hb=hb, G=G):
                    if j >= 2:
                        return
                    lj = P * 2 + j
                    abank, bbank = bank, bank + 2
                    i3 = lj % NTB
                    sa, bg = sa_[i3], bg_[i3]
                    S.op('act', lambda e: e.activation(out=sa[:], in_=ps[abank][:], func=AF.Silu), reads=['ps%d' % abank], writes=['sa%d' % i3])
                    S.op('dve', lambda e: e.tensor_tensor(out=bg[:], in0=ps[bbank][:], in1=G[:], op=ALU.mult), reads=['ps%d' % bbank, 'G%d' % hb], writes=['bg%d' % i3])
                    S.op('dve', lambda e: e.tensor_tensor(out=actT[:, lj, :], in0=sa[:], in1=bg[:], op=ALU.mult), reads=['sa%d' % i3, 'bg%d' % i3], writes=[('actT', lj)])
                gemm_group(tiles, lambda ki, hT=hT: hT[:, ki, :], lambda ki, hb=hb: [('h2T', hb, ki)], evac)
            for ng in range(D // 512):
                tiles = [(w_o2[j * 128:(j + 1) * 128, ng * 512:(ng + 1) * 512], False) for j in range(KC)]

                def evac2(j, bank, ng=ng, tsl=tsl):
                    n = ng * 4 + j
                    yi = yit[0] % 4
                    yit[0] += 1
                    yo = yo_[yi]
                    if j % 2 == 0:
                        S.op('act', lambda e: e.activation(out=yo[:], in_=ps[bank][:], func=AF.Copy), reads=['ps%d' % bank], writes=['yo%d' % yi])
                    else:
                        S.op('dve', lambda e: e.tensor_copy(out=yo[:], in_=ps[bank][:]), reads=['ps%d' % bank], writes=['yo%d' % yi])
                    S.dma('sp', lambda e: e.dma_start(out=yT[n * 128:(n + 1) * 128, tsl], in_=yo[:]), slot='yo%d' % yi, reads=['yo%d' % yi], writes=[])
                gemm_group(tiles, lambda ki: actT[:, ki, :], lambda ki: [('actT', ki)], evac2)
        S.finish()
        print("e2 ninst", S.ninst)
    return nc


def build_e3(TOK=1024):
    nc = bass.Bass("TRN2", target_bir_lowering=False)
    acc_d = nc.dram_tensor("accT", [D, TOK], F32, kind="ExternalInput").ap()
    ya_d = nc.dram_tensor("yA", [D, TOK], F32, kind="ExternalInput").ap()
    yb_d = nc.dram_tensor("yB", [D, TOK], F32, kind="ExternalInput").ap()
    pv = nc.dram_tensor("pv", [128, 8, KC], F32, kind="ExternalInput").ap()
    ones_d = nc.dram_tensor("ones", [128, 128], F32, kind="ExternalInput").ap()
    yT = nc.dram_tensor("yT", [D, TOK], F32, kind="ExternalOutput").ap()
    with ExitStack() as es:
        S = Sched(nc, es)
        sb = lambda name, shape, dt: es.enter_context(nc.sbuf_tensor(name, shape, dt))
        acc = sb("acc", [128, KC, T], F32)
        pvs = sb("pvs", [128, 8, KC], F32)
        g2p = sb("g2p", [128, KC], F32)
        ones = sb("ones_s", [128, 128], F32)
        s1 = sb("s1", [128, T], F32)
        s2 = sb("s2", [128, T], F32)
        mean = sb("mean", [128, T], F32)
        msq = sb("msq", [128, T], F32)
        rstd = sb("rstd", [128, T], F32)
        NTB = 3
        tb_ = [sb("tb%d" % i, [128, T], F32) for i in range(NTB)]
        yo_ = [sb("yo%d" % i, [128, T], F32) for i in range(NTB)]
        ya_ = [sb("ya%d" % i, [128, T], F32) for i in range(NTB)]
        yb_ = [sb("yb%d" % i, [128, T], F32) for i in range(NTB)]
        ps = [es.enter_context(nc.psum_tensor("ps%d" % i, [128, 512], F32)) for i in range(2)]
        S.dma('sp', lambda e: e.dma_start(out=pvs[:], in_=pv), slot='pvs', writes=['pvs'])
        S.dma('sp', lambda e: e.dma_start(out=ones[:], in_=ones_d), slot='ones', writes=['ones'])
        S.op('dve', lambda e: e.tensor_scalar(out=g2p[:], in0=pvs[:, 3, :], scalar1=1.0, scalar2=None, op0=ALU.add), reads=['pvs'], writes=['g2p'])
        av = acc_d.rearrange("(c q) t -> q c t", q=128)
        for p in range(TOK // T):
            tsl = slice(p * T, (p + 1) * T)
            for g4 in range(4):
                cs = slice(g4 * 8, (g4 + 1) * 8)
                S.dma('sp', lambda e, cs=cs, tsl=tsl: e.dma_start(out=acc[:, cs, :], in_=av[:, cs, tsl]), slot='accld%d' % g4,
                      writes=[('acc', c) for c in range(g4 * 8, g4 * 8 + 8)])
            for c in range(KC):
                i3 = c % NTB
                ya, yb = ya_[i3], yb_[i3]
                S.dma('sp', lambda e, c=c, tsl=tsl, ya=ya: e.dma_start(out=ya[:], in_=ya_d[c * 128:(c + 1) * 128, tsl]), slot='ya%d' % i3, writes=['ya%d' % i3])
                S.dma('sp', lambda e, c=c, tsl=tsl, yb=yb: e.dma_start(out=yb[:], in_=yb_d[c * 128:(c + 1) * 128, tsl]), slot='yb%d' % i3, writes=['yb%d' % i3])
                S.op('dve', lambda e, ya=ya, yb=yb: e.tensor_tensor(out=ya[:], in0=ya[:], in1=yb[:], op=ALU.add), reads=['ya%d' % i3, 'yb%d' % i3], writes=['ya%d' % i3])
                S.op('dve', lambda e, c=c, ya=ya: e.scalar_tensor_tensor(out=acc[:, c, :], in0=ya[:], scalar=g2p[:, c:c + 1], in1=acc[:, c, :], op0=ALU.mult, op1=ALU.add),
                     reads=['ya%d' % i3, 'g2p', ('acc', c)], writes=[('acc', c)])
            for c in range(KC):
                if c == 0:
                    S.op('dve', lambda e: e.tensor_copy(out=s1[:], in_=acc[:, 0, :]), reads=[('acc', 0)], writes=['s1'])
                    S.op('act', lambda e: e.activation(out=s2[:], in_=acc[:, 0, :], func=AF.Square), reads=[('acc', 0)], writes=['s2'])
                else:
                    t = tb_[c % NTB]
                    tr = 'tb%d' % (c % NTB)
                    S.op('act', lambda e, c=c, t=t: e.activation(out=t[:], in_=acc[:, c, :], func=AF.Square), reads=[('acc', c)], writes=[tr])
                    S.op('dve', lambda e, c=c: e.tensor_tensor(out=s1[:], in0=s1[:], in1=acc[:, c, :], op=ALU.add), reads=[('acc', c), 's1'], writes=['s1'])
                    S.op('dve', lambda e, t=t: e.tensor_tensor(out=s2[:], in0=s2[:], in1=t[:], op=ALU.add), reads=[tr, 's2'], writes=['s2'])
            S.op('pe', lambda e: e.matmul(ps[0][:], lhsT=ones[:], rhs=s1[:], start=True, stop=True), reads=['ones', 's1'], writes=['ps0'])
            S.op('pe', lambda e: e.matmul(ps[1][:], lhsT=ones[:], rhs=s2[:], start=True, stop=True), reads=['ones', 's2'], writes=['ps1'])
            S.op('act', lambda e: e.activation(out=mean[:], in_=ps[0][:], func=AF.Copy, scale=1.0 / D), reads=['ps0'], writes=['mean'])
            S.op('dve', lambda e: e.tensor_tensor(out=msq[:], in0=mean[:], in1=mean[:], op=ALU.mult), reads=['mean'], writes=['msq'])
            S.op('dve', lambda e: e.scalar_tensor_tensor(out=msq[:], in0=ps[1][:], scalar=1.0 / D, in1=msq[:], op0=ALU.mult, op1=ALU.subtract), reads=['ps1', 'msq'], writes=['msq'])
            S.op('dve', lambda e: e.tensor_scalar(out=msq[:], in0=msq[:], scalar1=LN_EPS, scalar2=None, op0=ALU.add), reads=['msq'], writes=['msq'])
            S.op('act', lambda e: e.activation(out=rstd[:], in_=msq[:], func=AF.Sqrt), reads=['msq'], writes=['rstd'])
            S.op('dve', lambda e: e.reciprocal(out=rstd[:], in_=rstd[:]), reads=['rstd'], writes=['rstd'])
            for c in range(KC):
                t = tb_[c % NTB]
                tr = 'tb%d' % (c % NTB)
                yo = yo_[c % NTB]
                yr = 'yo%d' % (c % NTB)
                S.op('dve', lambda e, c=c, t=t: e.tensor_tensor(out=t[:], in0=acc[:, c, :], in1=mean[:], op=ALU.subtract), reads=[('acc', c), 'mean'], writes=[tr])
                S.op('dve', lambda e, t=t: e.tensor_tensor(out=t[:], in0=t[:], in1=rstd[:], op=ALU.mult), reads=[tr, 'rstd'], writes=[tr])
                S.op('act', lambda e, c=c, t=t, yo=yo: e.activation(out=yo[:], in_=t[:], func=AF.Identity, scale=pvs[:, 6, c:c + 1], bias=pvs[:, 7, c:c + 1]),
                     reads=[tr, 'pvs'], writes=[yr])
                S.dma('sp', lambda e, c=c, yo=yo, tsl=tsl: e.dma_start(out=yT[c * 128:(c + 1) * 128, tsl], in_=yo[:]), slot=yr, reads=[yr], writes=[])
        S.finish()
    return nc

POOL_WINDOWS = (2, 4, 8, 16)

def pc(v):
    return np.ascontiguousarray(np.asarray(v, np.float32).reshape(-1, 128).T)

def stage_b_inputs(core, xT, sh1, sc1, even_w_in, lam_q1, lam_k1, lam_q2, lam_k2, subln_g, pool_w, pool_scale):
    hA, hB = 2 * core, 2 * core + 1
    g, half = core // 2, core % 2
    w = even_w_in
    cols = np.concatenate([np.arange(hA * 128, hA * 128 + 128), np.arange(hB * 128, hB * 128 + 128),
                           2048 + np.arange(hA * 128, hA * 128 + 128), 2048 + np.arange(hB * 128, hB * 128 + 128),
                           6144 + g * 512 + np.arange(512),
                           4096 + np.arange(hA * 128, hA * 128 + 128), 4096 + np.arange(hB * 128, hB * 128 + 128)])
    wq = np.ascontiguousarray(w[:, cols])
    pw = np.ascontiguousarray(pool_w[g][:, half * 256:(half + 1) * 256])
    ps = pool_scale[g * 512 + half * 256: g * 512 + (half + 1) * 256]
    psc = np.ascontiguousarray(ps.reshape(2, 128).T)
    sel4 = np.zeros((128, 4), np.float32); sel4[:, g] = 1
    wdw = POOL_WINDOWS[g]
    t = np.arange(512)
    rc = np.zeros((128, 2, 512), np.float32)
    rc[:, 0, :] = (1.0 / np.minimum(t + 1, wdw)).astype(np.float32)
    rc[:, 1, :] = np.float32(1.0 / wdw)
    slopes = (2.0 ** (-8.0 * np.arange(1, 17, dtype=np.float32) / 16)).astype(np.float32)
    bt = np.zeros((128, 2, 5, 512), np.float32)
    offt = np.zeros((128, 2, 64), np.float32)
    j = np.arange(128)[:, None].astype(np.float32); i = np.arange(512)[None, :].astype(np.float32)
    for hl, h in enumerate((hA, hB)):
        sl = slopes[h]
        bt[:, hl, 0, :] = -sl * (i - j)
        for d in range(4):
            dist = i - j - 128 * d
            bt[:, hl, d + 1, :] = np.where(dist >= 0, -sl * dist, np.float32(-1e30))
        offt[:, hl, :] = (-sl * 128 * np.arange(64, dtype=np.float32))[None, :]
    lamv = np.stack([np.broadcast_to(v, (128, 64)) for v in (lam_q1, lam_k1, lam_q2, lam_k2)], axis=1).astype(np.float32)
    sg = np.broadcast_to(subln_g, (128, 128)).astype(np.float32)
    return dict(xT=xT, mv=np.ascontiguousarray(np.stack([pc(sh1), pc(sc1)], axis=1)), wq=wq, pw=pw, psc=psc, sel4=sel4, rc=rc,
                bt=bt, offt=offt, lamv=np.ascontiguousarray(lamv), sg=np.ascontiguousarray(sg),
                identb=np.eye(128, dtype=np.float32).astype(ml_dtypes.bfloat16))

def stage_d_inputs(core, xT, sh1, sc1, odd_w_in, lb_raw, gnorm_g):
    c0 = core * 512
    cols = np.concatenate([k * 4096 + c0 + np.arange(512) for k in range(4)])
    wd = np.ascontiguousarray(odd_w_in[:, cols])
    lbr = np.ascontiguousarray(lb_raw[:, c0:c0 + 512].reshape(2, 4, 128).transpose(2, 0, 1)).astype(np.float32)
    gn4 = np.ascontiguousarray(np.broadcast_to(np.tile(gnorm_g, 4), (128, 512))).astype(np.float32)
    s = np.arange(128)[:, None]; t = np.arange(128)[None, :]
    mask01 = (s <= t).astype(np.float32)
    return dict(xT=xT, mv=np.ascontiguousarray(np.stack([pc(sh1), pc(sc1)], axis=1)), wd=wd, lbr=lbr, gn4=gn4, mask01=mask01,
                identb=np.eye(128, dtype=np.float32).astype(ml_dtypes.bfloat16))

def moe_layout(gate, h2T, PASS=512):
    sel = gate != 0
    ok = bool((sel.sum(1) == 2).all())
    idx = [np.nonzero(sel[:, e])[0] for e in range(gate.shape[1])]
    nmax = max(1, max(len(i) for i in idx))
    NP = ((nmax + PASS - 1) // PASS) * PASS
    per = []
    for e, ie in enumerate(idx):
        n = len(ie)
        hT = np.zeros((h2T.shape[0], NP), h2T.dtype)
        hT[:, :n] = h2T[:, ie]
        gb = np.zeros((128, NP), np.float32)
        gb[:, :n] = gate[ie, e][None, :]
        per.append(dict(hT=hT, gb=gb))
    rank = np.cumsum(sel, axis=1) - 1
    return ok, idx, NP, per, rank

def moe_scatter(ys, idx, rank, N):
    Dm = ys[0].shape[0]
    YA = np.zeros((Dm, N), np.float32)
    YB = np.zeros((Dm, N), np.float32)
    for e, ie in enumerate(idx):
        n = len(ie)
        r = rank[ie, e]
        y = ys[e][:, :n]
        YA[:, ie[r == 0]] = y[:, r == 0]
        YB[:, ie[r == 1]] = y[:, r == 1]
    return YA, YB

def _run(nc, in_maps):
    return run_bass_kernel_spmd(nc, in_maps, core_ids=list(range(8))).results


def kernel(**inputs):
    g = lambda k: np.asarray(inputs[k])
    x = np.asarray(g('x'), np.float32)[0]
    c = np.asarray(g('c'), np.float32)[0]
    ada_w, ada_b = g('ada_w'), g('ada_b')
    ln_g, ln_b = g('ln_g'), g('ln_b')
    NCORE = 8
    in_maps = []
    cvp = pc(c)
    for core in range(NCORE):
        l, cb = core // 4, core % 4
        in_maps.append(dict(aw=np.ascontiguousarray(ada_w[l][:, cb * 6144:(cb + 1) * 6144]),
                            ab=np.ascontiguousarray(ada_b[l][cb * 6144:(cb + 1) * 6144][None, :]), cv=cvp))
    res = _run(build_a(), in_maps)
    mod = np.concatenate([res[core]['mo'][0] for core in range(NCORE)]).reshape(2, 6, 4096)
    del in_maps
    ones = np.ones((128, 128), np.float32)
    xT = np.ascontiguousarray(x.T)
    in_maps = [stage_b_inputs(core, xT, mod[0, 0], mod[0, 1], g('even_w_in')[0], g('lam_q1')[0], g('lam_k1')[0], g('lam_q2')[0],
                              g('lam_k2')[0], g('subln_g')[0], g('pool_w')[0], g('pool_scale')[0]) for core in range(NCORE)]
    res = _run(build_b(), in_maps)
    ocT = np.empty((4096, 8192), ml_dtypes.bfloat16)
    for core in range(NCORE):
        o = res[core]['oTc']
        ocT[core * 256:(core + 1) * 256] = o[0:256]
        ocT[2048 + core * 256:2048 + (core + 1) * 256] = o[256:512]
    del in_maps, res

    def ff_stage(moe, l, ocT, xT_in, w_o, extra):
        pv = np.ascontiguousarray(np.stack([pc(mod[l, 2]), pc(mod[l, 3]), pc(mod[l, 4]), pc(mod[l, 5]),
                                            pc(ln_g[l, 0]), pc(ln_b[l, 0]), pc(ln_g[l, 1]), pc(ln_b[l, 1])], axis=1))
        in_maps = []
        for core in range(NCORE):
            sl = slice(core * 1024, (core + 1) * 1024)
            m = dict(oT=np.ascontiguousarray(ocT[:, sl]), xT=np.ascontiguousarray(xT_in[:, sl]), pv=pv, w_o=w_o, ones=ones)
            m.update(extra)
            in_maps.append(m)
        res = _run(build_ff(moe), in_maps)
        return np.concatenate([res[core]['yT'] for core in range(NCORE)], axis=1)

    x1T = ff_stage(False, 0, ocT, xT, np.ascontiguousarray(g('even_w_out')[0]),
                   dict(w_in=np.ascontiguousarray(g('ffn_w_in')[0]), w_o2=np.ascontiguousarray(g('ffn_w_out')[0])))
    del xT
    in_maps = [stage_d_inputs(core, x1T, mod[1, 0], mod[1, 1], g('odd_w_in')[0], g('lb_raw'), g('gnorm_g')[0]) for core in range(NCORE)]
    res = _run(build_d(), in_maps)
    oT = np.concatenate([res[core]['oTd'] for core in range(NCORE)], axis=0)
    del in_maps, res
    rw = np.ascontiguousarray(np.asarray(g('router_w')[0], np.float32).reshape(32, 128, 8).transpose(1, 0, 2))
    w_o1 = np.ascontiguousarray(g('odd_w_out')[0])
    pv1 = np.ascontiguousarray(np.stack([pc(mod[1, 2]), pc(mod[1, 3]), pc(mod[1, 4]), pc(mod[1, 5]),
                                         pc(ln_g[1, 0]), pc(ln_b[1, 0]), pc(ln_g[1, 1]), pc(ln_b[1, 1])], axis=1))
    in_maps = []
    for core in range(NCORE):
        sl = slice(core * 1024, (core + 1) * 1024)
        in_maps.append(dict(oT=np.ascontiguousarray(oT[:, sl]), xT=np.ascontiguousarray(x1T[:, sl]), pv=pv1, w_o=w_o1, ones=ones, rw=rw))
    res = _run(build_ff(True, mode='e1'), in_maps)
    h2T = np.concatenate([res[core]['h2o'] for core in range(NCORE)], axis=1)
    accT = np.concatenate([res[core]['acco'] for core in range(NCORE)], axis=1)
    gate = np.concatenate([res[core]['gout'] for core in range(NCORE)], axis=0)
    del in_maps, res
    ok, idx, NP, per, rank = moe_layout(gate, h2T)
    if not ok:
        sel = np.zeros((8, 8, 128), np.float32)
        for e in range(8):
            sel[e, e, :] = 1
        x2T = ff_stage(True, 1, oT, x1T, w_o1,
                       dict(w_in=np.ascontiguousarray(g('exp_w_in')[0]), w_o2=np.ascontiguousarray(g('exp_w_out')[0]), rw=rw, sel=sel,
                            ident=np.eye(128, dtype=np.float32)))
        return np.ascontiguousarray(x2T.T)[None].astype(np.float32)
    ewi, ewo = g('exp_w_in')[0], g('exp_w_out')[0]
    in_maps = [dict(hT=per[e]['hT'], gb=per[e]['gb'], w_in=np.ascontiguousarray(ewi[e]), w_o2=np.ascontiguousarray(ewo[e])) for e in range(NCORE)]
    res = _run(build_e2(NP), in_maps)
    YA, YB = moe_scatter([res[e]['yT'] for e in range(NCORE)], idx, rank, 8192)
    del in_maps, res, per
    in_maps = []
    for core in range(NCORE):
        sl = slice(core * 1024, (core + 1) * 1024)
        in_maps.append(dict(accT=np.ascontiguousarray(accT[:, sl]), yA=np.ascontiguousarray(YA[:, sl]), yB=np.ascontiguousarray(YB[:, sl]), pv=pv1, ones=ones))
    res = _run(build_e3(), in_maps)
    x2T = np.concatenate([res[core]['yT'] for core in range(NCORE)], axis=1)
    return np.ascontiguousarray(x2T.T)[None].astype(np.float32)
```

```python
import numpy as np
import ml_dtypes
import concourse.bass as bass
import concourse.mybir as mybir
from concourse.bass_utils import run_bass_kernel_spmd
from contextlib import ExitStack

F32 = mybir.dt.float32
BF16 = mybir.dt.bfloat16
AF = mybir.ActivationFunctionType
ALU = mybir.AluOpType
AX = mybir.AxisListType


class Sched:
    ENG = ('pe', 'act', 'dve', 'pool', 'sp')

    def __init__(self, nc, es, immediate=False):
        self.immediate = immediate
        self.nc = nc
        self.es = es
        self.eng = {'pe': nc.tensor, 'act': nc.scalar, 'dve': nc.vector,
                    'pool': nc.gpsimd, 'sp': nc.sync}
        self.sem = {e: es.enter_context(nc.semaphore("s_" + e)) for e in self.ENG}
        self.cnt = {e: 0 for e in self.ENG}
        self.prog = {e: [] for e in self.ENG}
        self.waited = {}
        self.lastw = {}
        self.reads = {}
        self.dsem = {}
        self.semobj = {e: self.sem[e] for e in self.ENG}
        self.ninst = 0
        self._rec = None

    def record(self, fn):
        saved = self._rec
        self._rec = []
        try:
            fn()
            out = self._rec
        finally:
            self._rec = saved
        return out

    def replay(self, lists):
        n = max([len(l) for l in lists] + [0])
        for i in range(n):
            for l in lists:
                if i < len(l):
                    kind, a, kw = l[i]
                    getattr(self, kind)(*a, **kw)

    def _dma_sem(self, key):
        if key not in self.dsem:
            s = self.es.enter_context(self.nc.semaphore("d_%d" % len(self.dsem)))
            k = ('d', key)
            self.semobj[k] = s
            self.dsem[key] = [k, 0]
        return self.dsem[key]

    def _deps(self, eng, reads, writes):
        deps = {}
        def add(d):
            if d is None:
                return
            k, v, e = d
            if e == 'pe' and eng == 'pe':
                return
            if deps.get(k, 0) < v:
                deps[k] = v
        for r in reads:
            add(self.lastw.get(r))
        for w in writes:
            add(self.lastw.get(w))
            for d in self.reads.get(w, ()):
                add(d)
        out = []
        for k, v in deps.items():
            if self.waited.get((eng, k), 0) >= v:
                continue
            self.waited[(eng, k)] = v
            out.append((self.semobj[k], v))
        return out

    def _commit(self, ident, reads, writes):
        for w in writes:
            self.lastw[w] = ident
            self.reads[w] = []
        for r in reads:
            if r in writes:
                continue
            self.reads.setdefault(r, []).append(ident)

    def op(self, eng, fn, reads=(), writes=(), inc=True):
        if self._rec is not None:
            self._rec.append(('op', (eng, fn), dict(reads=reads, writes=writes, inc=inc)))
            return
        waits = self._deps(eng, reads, writes)
        if inc:
            self.cnt[eng] += 1
            val = self.cnt[eng]
        else:
            val = self.cnt[eng] + 1
        sem = self.sem[eng]
        e = self.eng[eng]
        def emit():
            for s, v in waits:
                e.wait_ge(s, v)
            i = fn(e)
            if inc:
                i.then_inc(sem, 1)
        if self.immediate:
            emit()
        else:
            self.prog[eng].append(emit)
        self._commit((eng, val, eng), reads, writes)
        self.ninst += 1

    def dma(self, eng, fn, slot, reads=(), writes=()):
        if self._rec is not None:
            self._rec.append(('dma', (eng, fn, slot), dict(reads=reads, writes=writes)))
            return
        waits = self._deps(eng, reads, writes)
        ds = self._dma_sem(slot)
        ds[1] += 16
        k, val = ds[0], ds[1]
        sem = self.semobj[k]
        e = self.eng[eng]
        def emit():
            for s, v in waits:
                e.wait_ge(s, v)
            fn(e).then_inc(sem, 16)
        if self.immediate:
            emit()
        else:
            self.prog[eng].append(emit)
        self._commit((k, val, 'dma'), reads, writes)
        self.ninst += 1

    def finish(self, final_res=None):
        waits = [(self.sem[k], self.cnt[k]) for k in self.ENG if self.cnt[k] > 0]
        waits += [(self.semobj[k], v) for (k, v) in self.dsem.values()]
        e = self.eng['sp']
        def emit():
            for s, v in waits:
                e.wait_ge(s, v)
        self.prog['sp'].append(emit)
        nc = self.nc
        allsems = [self.sem[k] for k in self.ENG] + [self.semobj[k] for (k, v) in self.dsem.values()]
        with nc.Block() as b0:
            @b0.sync
            def _(t):
                for s_ in allsems:
                    t.sem_clear(s_)
        with nc.Block() as block:
            @block.tensor
            def _(t):
                for f in self.prog['pe']:
                    f()
            @block.scalar
            def _(t):
                for f in self.prog['act']:
                    f()
            @block.vector
            def _(t):
                for f in self.prog['dve']:
                    f()
            @block.gpsimd
            def _(t):
                for f in self.prog['pool']:
                    f()
            @block.sync
            def _(t):
                for f in self.prog['sp']:
                    f()


    def barrier(self):
        waits = [(k, self.cnt[k]) for k in self.ENG if self.cnt[k] > 0]
        waits += [(k, v) for (k, v) in self.dsem.values()]
        for eng in self.ENG:
            ws = []
            for k, v in waits:
                if self.waited.get((eng, k), 0) >= v:
                    continue
                self.waited[(eng, k)] = v
                ws.append((self.semobj[k], v))
            e = self.eng[eng]
            def emit(ws=ws, e=e):
                for s, v in ws:
                    e.wait_ge(s, v)
            self.prog[eng].append(emit)
        self.lastw = {}
        self.reads = {}


class Arena:
    def __init__(self, nc, es, name, kbytes):
        self.t = es.enter_context(nc.sbuf_tensor(name, [128, kbytes * 256], F32))
        self.n = kbytes * 256
        self.off = 0
        self.marks = []

    def alloc(self, shape, dt, parts=128):
        nel = 1
        for s_ in shape:
            nel *= s_
        esz = 4 if dt == F32 else 2
        nw = (nel * esz + 3) // 4
        nw = (nw + 7) // 8 * 8
        assert self.off + nw <= self.n, "arena overflow %d + %d > %d" % (self.off, nw, self.n)
        v = self.t[0:parts, self.off:self.off + nw]
        self.off += nw
        if dt != F32:
            v = v.bitcast(dt)
        v = v[:, 0:nel]
        if len(shape) == 2:
            v = v.rearrange("p (a b) -> p a b", a=shape[0])
        elif len(shape) == 3:
            v = v.rearrange("p (a b c) -> p a b c", a=shape[0], b=shape[1])
        return v

    def mark(self):
        return self.off

    def reset(self, mark):
        self.off = mark


IMM = False
def build_a(NCOL=6144):
    nc = bass.Bass("TRN2", target_bir_lowering=False)
    D = 4096
    aw = nc.dram_tensor("aw", [D, NCOL], F32, kind="ExternalInput").ap()
    ab = nc.dram_tensor("ab", [1, NCOL], F32, kind="ExternalInput").ap()
    cv = nc.dram_tensor("cv", [128, 32], F32, kind="ExternalInput").ap()
    mo = nc.dram_tensor("mo", [1, NCOL], F32, kind="ExternalOutput").ap()
    with ExitStack() as es:
        S = Sched(nc, es, immediate=IMM)
        sb = lambda name, shape, dt: es.enter_context(nc.sbuf_tensor(name, shape, dt))
        NB = 8
        wt = [sb("wt%d" % i, [128, 512], F32) for i in range(NB)]
        cs = sb("cs", [128, 32], F32)
        ca = sb("ca", [128, 32], F32)
        abs_ = sb("abs", [1, NCOL], F32)
        mos = sb("mos", [1, NCOL], F32)
        ps = [es.enter_context(nc.psum_tensor("ps%d" % i, [128, 512], F32)) for i in range(2)]
        S.dma('sp', lambda e: e.dma_start(out=cs[:], in_=cv), slot='cs', writes=['cs'])
        S.dma('sp', lambda e: e.dma_start(out=abs_[:], in_=ab), slot='abs', writes=['abs'])
        S.op('act', lambda e: e.activation(out=ca[:], in_=cs[:], func=AF.Silu), reads=['cs'], writes=['ca'])
        it = 0
        for n in range(NCOL // 512):
            bank = n % 2
            for k in range(32):
                b = it % NB
                it += 1
                S.dma('sp', lambda e, b=b, k=k, n=n: e.dma_start(out=wt[b][:], in_=aw[k * 128:(k + 1) * 128, n * 512:(n + 1) * 512]),
                      slot='wt%d' % b, writes=['wt%d' % b])
                S.op('pe', lambda e, b=b, k=k, bank=bank: e.matmul(ps[bank][0:1, :], lhsT=ca[:, k:k + 1], rhs=wt[b][:], start=(k == 0), stop=(k == 31)),
                     reads=['wt%d' % b, 'ca'], writes=['ps%d' % bank])
            S.op('dve', lambda e, n=n, bank=bank: e.tensor_tensor(out=mos[0:1, n * 512:(n + 1) * 512], in0=ps[bank][0:1, :], in1=abs_[0:1, n * 512:(n + 1) * 512], op=ALU.add),
                 reads=['ps%d' % bank, 'abs'], writes=[('mos', n)])
        S.dma('sp', lambda e: e.dma_start(out=mo, in_=mos[:]), slot='out', reads=[('mos', n) for n in range(NCOL // 512)], writes=[])
        S.finish()
    return nc


D = 4096
KC = 32
S_ = 8192
T = 512
NTG = S_ // T
RMS_EPS = 1e-6


def build_b(NTG_RUN=NTG, heads=(0, 1)):
    nc = bass.Bass("TRN2", target_bir_lowering=False)
    dt_in = lambda name, shape, dt=F32: nc.dram_tensor(name, shape, dt, kind="ExternalInput").ap()
    xT = dt_in("xT", [D, S_])
    mv = dt_in("mv", [128, 2, KC])
    wq = dt_in("wq", [D, 1280])
    pw_d = dt_in("pw", [512, 256])
    psc_d = dt_in("psc", [128, 2])
    sel_d = dt_in("sel4", [128, 4])
    rc_d = dt_in("rc", [128, 2, T])
    bt_d = dt_in("bt", [128, 2, 5, T])
    off_d = dt_in("offt", [128, 2, 64])
    lam_d = dt_in("lamv", [128, 4, 64])
    sg_d = dt_in("sg", [128, 128])
    idb_d = dt_in("identb", [128, 128], BF16)
    oTc = nc.dram_tensor("oTc", [512, S_], BF16, kind="ExternalOutput").ap()
    qk_scr = nc.dram_tensor("qk_scr", [4, 128, S_], BF16, kind="Internal").ap()
    v_scr = nc.dram_tensor("v_scr", [S_, 2, 130], BF16, kind="Internal").ap()

    with ExitStack() as es:
        S = Sched(nc, es)
        ar = Arena(nc, es, "arena", 204)
        psf = [es.enter_context(nc.psum_tensor("ps%d" % i, [128, 512], F32)) for i in range(8)]
        ps7b = psf[7][:].bitcast(BF16)

        mvs = ar.alloc([2, KC], F32)
        sc1p = ar.alloc([KC], F32)
        psc = ar.alloc([2], F32)
        sel4 = ar.alloc([4], F32)
        lamv = ar.alloc([4, 64], F32)
        lamt = ar.alloc([2, 64], F32)
        lsc = ar.alloc([8], F32)
        SG = ar.alloc([128], F32)
        identb = ar.alloc([128], BF16)
        for (dst, src, nm) in [(mvs, mv, 'mvs'), (psc, psc_d, 'psc'), (sel4, sel_d, 'sel4'), (lamv, lam_d, 'lamv'),
                               (SG, sg_d, 'SG'), (identb, idb_d, 'identb')]:
            S.dma('sp', lambda e, dst=dst, src=src: e.dma_start(out=dst, in_=src), slot=nm, writes=[nm])
        S.op('dve', lambda e: e.tensor_scalar(out=sc1p, in0=mvs[:, 1, :], scalar1=1.0, scalar2=None, op0=ALU.add), reads=['mvs'], writes=['sc1p'])
        S.op('dve', lambda e: e.tensor_tensor(out=lamt[:, 0, :], in0=lamv[:, 0, :], in1=lamv[:, 1, :], op=ALU.mult), reads=['lamv'], writes=['lamt0'])
        S.op('dve', lambda e: e.tensor_tensor(out=lamt[:, 1, :], in0=lamv[:, 2, :], in1=lamv[:, 3, :], op=ALU.mult), reads=['lamv'], writes=['lamt1'])
        S.op('dve', lambda e: e.reduce_sum(out=lsc[:, 0:1], in_=lamt[:, 0, :], axis=AX.X), reads=['lamt0'], writes=['lsc0'])
        S.op('dve', lambda e: e.reduce_sum(out=lsc[:, 1:2], in_=lamt[:, 1, :], axis=AX.X), reads=['lamt1'], writes=['lsc1'])
        S.op('act', lambda e: e.activation(out=lsc[:, 2:4], in_=lsc[:, 0:2], func=AF.Exp), reads=['lsc0', 'lsc1'], writes=['lsc23'])
        S.op('dve', lambda e: e.tensor_tensor(out=lsc[:, 4:5], in0=lsc[:, 2:3], in1=lsc[:, 3:4], op=ALU.subtract), reads=['lsc23'], writes=['lsc4'])
        S.op('dve', lambda e: e.tensor_scalar(out=lsc[:, 4:5], in0=lsc[:, 4:5], scalar1=0.2, scalar2=-1.0, op0=ALU.add, op1=ALU.mult), reads=['lsc4'], writes=['lsc4'])
        S.op('dve', lambda e: e.tensor_scalar(out=SG, in0=SG, scalar1=0.8, scalar2=None, op0=ALU.mult), reads=['SG'], writes=['SG'])
        pmark = ar.mark()

        W = ar.alloc([KC, 1280], BF16)
        hT2 = [ar.alloc([KC, T], BF16) for i in range(2)]
        NXS = 2
        xs = [ar.alloc([2, T], F32) for i in range(NXS)]
        pw = ar.alloc([4, 256], BF16)
        rc = ar.alloc([2, T], F32)
        ue = [ar.alloc([4, 528], F32) for i in range(2)]
        NPT = 2
        pA = [ar.alloc([528], F32) for i in range(NPT)]
        pB = [ar.alloc([528], F32) for i in range(NPT)]
        pS = [ar.alloc([T], F32) for i in range(NPT)]
        z = ar.alloc([4, T], BF16)
        NST = 3
        qst = [ar.alloc([T], BF16) for i in range(NST)]
        vst = [ar.alloc([2, 130], BF16) for i in range(2)]
        ost = [ar.alloc([T], BF16) for i in range(2)]

        for k in range(KC):
            S.dma('pool', lambda e, k=k: e.dma_start(out=W[:, k, :], in_=wq[k * 128:(k + 1) * 128, :]), slot='W%d' % k, writes=[('W', k)])
        S.dma('pool', lambda e: e.dma_start(out=pw, in_=pw_d.rearrange("(c p) n -> p c n", p=128)), slot='pw', writes=['pw'])
        S.dma('sp', lambda e: e.dma_start(out=rc, in_=rc_d), slot='rc', writes=['rc'])
        for i in range(2):
            S.op('dve', lambda e, i=i: e.memset(vst[i][:, :, 128:130], 1.0), writes=['vst%d' % i])
        S.op('dve', lambda e: e.memset(ue[0][:, :, 0:16], 0.0), writes=[('ue', 0, j) for j in range(4)])
        WR = [('W', k) for k in range(KC)]
        xv = xT.rearrange("(c q) t -> q c t", q=128)
        xit = [0]
        qit = 0
        hcnt = [0]

        def load_mod(tg):
            hb_ = hcnt[0] % 2
            hcnt[0] += 1
            hT = hT2[hb_]
            tsl = slice(tg * T, (tg + 1) * T)
            for c2 in range(KC // 2):
                b = xit[0] % NXS
                xit[0] += 1
                S.dma('sp', lambda e, b=b, c2=c2, tsl=tsl: e.dma_start(out=xs[b], in_=xv[:, c2 * 2:c2 * 2 + 2, tsl]), slot='xs%d' % b, writes=['xs%d' % b])
                for i in range(2):
                    c = c2 * 2 + i
                    S.op('act', lambda e, b=b, i=i, c=c, hT=hT: e.activation(out=hT[:, c, :], in_=xs[b][:, i, :], func=AF.Identity,
                                                                   scale=sc1p[:, c:c + 1], bias=mvs[:, 0, c:c + 1]),
                         reads=['xs%d' % b, 'sc1p', 'mvs'], writes=[('hT', hb_, c)])
            return hT, hb_

        nxt = load_mod(0)
        for tg in range(NTG_RUN):
            tsl = slice(tg * T, (tg + 1) * T)
            hT, hbi = nxt
            if tg + 1 < NTG_RUN:
                nxt = load_mod(tg + 1)
            for k in range(KC):
                for j in range(4):
                    S.op('pe', lambda e, k=k, j=j, hT=hT: e.matmul(psf[j][:], lhsT=W[:, k, j * 128:(j + 1) * 128], rhs=hT[:, k, :], start=(k == 0), stop=(k == KC - 1)),
                         reads=[('W', k), ('hT', hbi, k)], writes=['ps%d' % j], inc=(k == KC - 1))
            for j in range(4):
                b = qit % NST
                qit += 1
                if j % 2 == 0:
                    S.op('act', lambda e, b=b, j=j: e.activation(out=qst[b], in_=psf[j][:], func=AF.Copy), reads=['ps%d' % j], writes=['qst%d' % b])
                else:
                    S.op('dve', lambda e, b=b, j=j: e.tensor_copy(out=qst[b], in_=psf[j][:]), reads=['ps%d' % j], writes=['qst%d' % b])
                S.dma('sp', lambda e, b=b, j=j, tsl=tsl: e.dma_start(out=qk_scr[j, :, tsl], in_=qst[b]), slot='qst%d' % b, reads=['qst%d' % b], writes=[])
            U = ue[tg % 2]
            Un = ue[(tg + 1) % 2]
            for k in range(KC):
                for j in range(4):
                    S.op('pe', lambda e, k=k, j=j, hT=hT: e.matmul(psf[4 + j][:], lhsT=W[:, k, 512 + j * 128:512 + (j + 1) * 128], rhs=hT[:, k, :], start=(k == 0), stop=(k == KC - 1)),
                         reads=[('W', k), ('hT', hbi, k)], writes=['ps%d' % (4 + j)], inc=(k == KC - 1))
            def chainp(j, tg=tg, U=U, Un=Un):
                ur = ('ue', tg % 2, j)
                urn = ('ue', (tg + 1) % 2, j)
                S.op('act', lambda e, j=j, U=U: e.activation(out=U[:, j, 16:528], in_=psf[4 + j][:], func=AF.Copy), reads=['ps%d' % (4 + j)], writes=[ur])
                S.op('pool', lambda e, j=j, U=U, Un=Un: e.tensor_copy(out=Un[:, j, 0:16], in_=U[:, j, 512:528]), reads=[ur], writes=[urn])
                eng = 'dve'
                i2 = j % NPT
                A, B, SS = pA[i2], pB[i2], pS[i2]
                Ar, Br, Sr = 'pA%d' % i2, 'pB%d' % i2, 'pS%d' % i2
                E_ = lambda lo, hi, U=U, j=j: U[:, j, lo:hi]
                S.op(eng, lambda e, A=A, E_=E_: e.tensor_tensor(out=A[:, 1:528], in0=E_(1, 528), in1=E_(0, 527), op=ALU.add), reads=[ur], writes=[Ar])
                S.op(eng, lambda e, A=A, SS=SS: e.tensor_scalar(out=SS, in0=A[:, 16:528], scalar1=sel4[:, 0:1], scalar2=None, op0=ALU.mult), reads=[Ar, 'sel4'], writes=[Sr])
                S.op(eng, lambda e, A=A, B=B: e.tensor_tensor(out=B[:, 3:528], in0=A[:, 3:528], in1=A[:, 1:526], op=ALU.add), reads=[Ar], writes=[Br])
                S.op(eng, lambda e, B=B, SS=SS: e.scalar_tensor_tensor(out=SS, in0=B[:, 16:528], scalar=sel4[:, 1:2], in1=SS, op0=ALU.mult, op1=ALU.add), reads=[Br, Sr, 'sel4'], writes=[Sr])
                S.op(eng, lambda e, A=A, B=B: e.tensor_tensor(out=A[:, 7:528], in0=B[:, 7:528], in1=B[:, 3:524], op=ALU.add), reads=[Br], writes=[Ar])
                S.op(eng, lambda e, A=A, SS=SS: e.scalar_tensor_tensor(out=SS, in0=A[:, 16:528], scalar=sel4[:, 2:3], in1=SS, op0=ALU.mult, op1=ALU.add), reads=[Ar, Sr, 'sel4'], writes=[Sr])
                S.op(eng, lambda e, A=A, B=B: e.tensor_tensor(out=B[:, 15:528], in0=A[:, 15:528], in1=A[:, 7:520], op=ALU.add), reads=[Ar], writes=[Br])
                S.op(eng, lambda e, B=B, SS=SS: e.scalar_tensor_tensor(out=SS, in0=B[:, 16:528], scalar=sel4[:, 3:4], in1=SS, op0=ALU.mult, op1=ALU.add), reads=[Br, Sr, 'sel4'], writes=[Sr])
                rci = 0 if tg == 0 else 1
                S.op(eng, lambda e, SS=SS, rci=rci: e.tensor_tensor(out=SS, in0=SS, in1=rc[:, rci, :], op=ALU.mult), reads=[Sr, 'rc'], writes=[Sr])
                S.op(eng, lambda e, SS=SS, j=j, E_=E_: e.tensor_tensor(out=z[:, j, :], in0=SS, in1=E_(16, 528), op=ALU.subtract), reads=[Sr, ur], writes=[('z', j)])
            for pair in ((0, 1), (2, 3)):
                S.replay([S.record(lambda j=j: chainp(j)) for j in pair])
            for tb in range(4):
                for k in range(KC):
                    S.op('pe', lambda e, k=k, tb=tb, hT=hT: e.matmul(psf[tb][:, 0:256], lhsT=hT[:, k, tb * 128:(tb + 1) * 128], rhs=W[:, k, 1024:1280], start=(k == 0), stop=(k == KC - 1)),
                         reads=[('W', k), ('hT', hbi, k)], writes=['ps%d' % tb], inc=(k == KC - 1))
            for tb in range(4):
                b = tb % 2
                S.op('dve', lambda e, b=b, tb=tb: e.tensor_copy(out=vst[b][:, :, 0:128], in_=psf[tb][:, 0:256].rearrange("p (h d) -> p h d", h=2)),
                     reads=['ps%d' % tb], writes=['vst%d' % b])
                r0 = tg * T + tb * 128
                S.dma('sp', lambda e, b=b, r0=r0: e.dma_start(out=v_scr[r0:r0 + 128, :, :], in_=vst[b]), slot='vst%d' % b, reads=['vst%d' % b], writes=[])
            for oc in range(2):
                for kc in range(4):
                    S.op('pe', lambda e, oc=oc, kc=kc: e.matmul(psf[4 + oc][:], lhsT=pw[:, kc, oc * 128:(oc + 1) * 128], rhs=z[:, kc, :], start=(kc == 0), stop=(kc == 3)),
                         reads=['pw', ('z', kc)], writes=['ps%d' % (4 + oc)], inc=(kc == 3))
                S.op('act', lambda e, oc=oc: e.activation(out=ost[oc], in_=psf[4 + oc][:], func=AF.Identity, scale=psc[:, oc:oc + 1]),
                     reads=['ps%d' % (4 + oc), 'psc'], writes=['ost%d' % oc])
                S.dma('sp', lambda e, oc=oc, tsl=tsl: e.dma_start(out=oTc[256 + oc * 128:256 + (oc + 1) * 128, tsl], in_=ost[oc]), slot='ost%d' % oc, reads=['ost%d' % oc], writes=[])

        S.barrier()
        ar.reset(pmark)
        QK = ar.alloc([4, S_], BF16)
        Vx = ar.alloc([64, 2, 130], BF16)
        bt = ar.alloc([2, 5, T], F32)
        offt = ar.alloc([2, 64], F32)
        NSB = 3
        Sb = [ar.alloc([T], F32) for i in range(NSB)]
        NPT2 = 6
        PT = [ar.alloc([T], BF16) for i in range(NPT2)]
        rl = ar.alloc([4, 4], F32)
        t2 = [ar.alloc([128], F32) for i in range(4)]
        o_ = [ar.alloc([128], F32) for i in range(4)]
        junk = ar.alloc([128], F32)
        on = [ar.alloc([128], BF16) for i in range(4)]
        ost2 = [ar.alloc([T], BF16) for i in range(2)]
        for j in range(4):
            S.dma('sp', lambda e, j=j: e.dma_start(out=QK[:, j, :], in_=qk_scr[j]), slot='QK%d' % j, writes=[('QK', j)])
        S.dma('sp', lambda e: e.dma_start(out=Vx, in_=v_scr.rearrange("(b p) h d -> p b h d", p=128)), slot='Vx', writes=['Vx'])
        S.dma('sp', lambda e: e.dma_start(out=bt, in_=bt_d), slot='bt', writes=['bt'])
        S.dma('sp', lambda e: e.dma_start(out=offt, in_=off_d), slot='offt', writes=['offt'])

        accpos = {}
        lst = [(m, s) for m in range(2) for s in range(4)]
        for i, (m, s) in enumerate(lst):
            accpos[(m, s)] = (4 + i // 3, (i % 3) * 130)
        tcount = 0
        epi = 0
        for h in heads:
            for ib in range(NTG_RUN):
                for bk in (4, 5, 6):
                    S.op('dve', lambda e, bk=bk: e.memset(psf[bk][:, 0:390], 0.0), writes=['ps%d' % bk])
                tiles = [(m, jb) for jb in range(4 * ib + 4) for m in range(2)]
                SKEW = 3
                def emit_qk(ti, h=h, ib=ib, tiles=tiles, tcount=tcount):
                    m, jb = tiles[ti]
                    bank = (tcount + ti) % 4
                    S.op('pe', lambda e: e.matmul(psf[bank][:], lhsT=QK[m * 64:(m + 1) * 64, 2 + h, jb * 128:(jb + 1) * 128],
                                                  rhs=QK[m * 64:(m + 1) * 64, h, ib * T:(ib + 1) * T], start=True, stop=True),
                         reads=[('QK', 2 + h), ('QK', h)], writes=['ps%d' % bank])
                    d = jb - 4 * ib
                    var = 0 if d < 0 else d + 1
                    n = 4 * ib - jb if d < 0 else 0
                    sbi = (tcount + ti) % NSB
                    pti = (tcount + ti) % NPT2
                    S.op('dve', lambda e: e.scalar_tensor_tensor(out=Sb[sbi], in0=psf[bank][:], scalar=0.125, in1=bt[:, h, var, :], op0=ALU.mult, op1=ALU.add),
                         reads=['ps%d' % bank, 'bt'], writes=['Sb%d' % sbi])
                    S.op('act', lambda e: e.activation(out=PT[pti], in_=Sb[sbi], func=AF.Exp, bias=offt[:, h, n:n + 1], scale=1.0),
                         reads=['Sb%d' % sbi, 'offt'], writes=['PT%d' % pti])
                def emit_pv(ti, h=h, ib=ib, tiles=tiles, tcount=tcount):
                    m, jb = tiles[ti]
                    d = jb - 4 * ib
                    pti = (tcount + ti) % NPT2
                    subs = [s for s in range(4) if not (d >= 0 and s < d)]
                    for s in subs:
                        bk, c0 = accpos[(m, s)]
                        S.op('pe', lambda e, s=s, bk=bk, c0=c0: e.matmul(psf[bk][:, c0:c0 + 129], lhsT=PT[pti][:, s * 128:(s + 1) * 128], rhs=Vx[:, jb, h, 0:129],
                                                                         start=False, stop=False, skip_group_check=True),
                             reads=['PT%d' % pti, 'Vx'], writes=['ps%d' % bk], inc=(s == subs[-1]))
                nt = len(tiles)
                SKEW = 4
                for ti in range(0, nt + SKEW, 2):
                    for t2_ in (ti, ti + 1):
                        if t2_ < nt:
                            emit_qk(t2_)
                    for t2_ in (ti - SKEW, ti + 1 - SKEW):
                        if 0 <= t2_ < nt:
                            emit_pv(t2_)
                tcount += nt
                def chaine(s):
                    nonlocal epi
                    b1, c1 = accpos[(0, s)]
                    b2, c2 = accpos[(1, s)]
                    e2 = s
                    epi += 1
                    A1 = psf[b1][:, c1:c1 + 129]
                    A2 = psf[b2][:, c2:c2 + 129]
                    rr = ('rl', s)
                    S.op('dve', lambda e, s=s, A1=A1: e.reciprocal(out=rl[:, s, 0:1], in_=A1[:, 128:129]), reads=['ps%d' % b1], writes=[rr])
                    S.op('dve', lambda e, s=s, A2=A2: e.reciprocal(out=rl[:, s, 1:2], in_=A2[:, 128:129]), reads=['ps%d' % b2], writes=[rr])
                    S.op('dve', lambda e, s=s: e.tensor_tensor(out=rl[:, s, 1:2], in0=rl[:, s, 1:2], in1=lsc[:, 4:5], op=ALU.mult), reads=[rr, 'lsc4'], writes=[rr])
                    S.op('act', lambda e, s=s, A2=A2, e2=e2: e.activation(out=t2[e2], in_=A2[:, 0:128], func=AF.Identity, scale=rl[:, s, 1:2]),
                         reads=['ps%d' % b2, rr], writes=['t2%d' % e2])
                    S.op('dve', lambda e, s=s, A1=A1, e2=e2: e.scalar_tensor_tensor(out=o_[e2], in0=A1[:, 0:128], scalar=rl[:, s, 0:1], in1=t2[e2], op0=ALU.mult, op1=ALU.add),
                         reads=['ps%d' % b1, rr, 't2%d' % e2], writes=['o%d' % e2])
                    S.op('act', lambda e, s=s, e2=e2: e.activation(out=junk, in_=o_[e2], func=AF.Square, accum_out=rl[:, s, 2:3]),
                         reads=['o%d' % e2], writes=['junk', ('rl2', s)])
                    S.op('dve', lambda e, s=s: e.tensor_scalar(out=rl[:, s, 2:3], in0=rl[:, s, 2:3], scalar1=1.0 / 128, scalar2=RMS_EPS, op0=ALU.mult, op1=ALU.add),
                         reads=[('rl2', s)], writes=[('rl2', s)])
                    S.op('act', lambda e, s=s: e.activation(out=rl[:, s, 3:4], in_=rl[:, s, 2:3], func=AF.Sqrt), reads=[('rl2', s)], writes=[('rl3', s)])
                    S.op('dve', lambda e, s=s: e.reciprocal(out=rl[:, s, 3:4], in_=rl[:, s, 3:4]), reads=[('rl3', s)], writes=[('rl3', s)])
                    S.op('dve', lambda e, s=s, e2=e2: e.scalar_tensor_tensor(out=on[e2], in0=o_[e2], scalar=rl[:, s, 3:4], in1=SG, op0=ALU.mult, op1=ALU.mult),
                         reads=['o%d' % e2, ('rl3', s), 'SG'], writes=['on%d' % e2])
                    S.op('pe', lambda e, s=s, e2=e2: e.transpose(out=ps7b[:, s * 128:(s + 1) * 128], in_=on[e2], identity=identb),
                         reads=['on%d' % e2, 'identb'], writes=['ps7'])
                S.replay([S.record(lambda s=s: chaine(s)) for s in range(4)])
                ob = (epi // 4) % 2
                S.op('act', lambda e, ob=ob: e.activation(out=ost2[ob], in_=ps7b[:, 0:512], func=AF.Copy), reads=['ps7'], writes=['ost2%d' % ob])
                S.dma('sp', lambda e, ob=ob, h=h, ib=ib: e.dma_start(out=oTc[h * 128:(h + 1) * 128, ib * T:(ib + 1) * T], in_=ost2[ob]),
                      slot='ost2%d' % ob, reads=['ost2%d' % ob], writes=[])
        S.finish()
        print("stage b ninst", S.ninst)
    return nc


D = 4096
KC = 32
S_ = 8192
T = 512
NTG = S_ // T
RMS_EPS = 1e-6
CH = 128


def build_d(NTG_RUN=NTG, heads=(0, 1, 2, 3)):
    nc = bass.Bass("TRN2", target_bir_lowering=False)
    dt_in = lambda name, shape, dt=F32: nc.dram_tensor(name, shape, dt, kind="ExternalInput").ap()
    xT = dt_in("xT", [D, S_])
    mv = dt_in("mv", [128, 2, KC])
    wd = dt_in("wd", [D, 2048])
    lbr_d = dt_in("lbr", [128, 2, 4])
    gn_d = dt_in("gn4", [128, 512])
    mask_d = dt_in("mask01", [128, 128])
    idb_d = dt_in("identb", [128, 128], BF16)
    oTd = nc.dram_tensor("oTd", [512, S_], BF16, kind="ExternalOutput").ap()
    qt_scr = nc.dram_tensor("qt_scr", [4, 128, S_], BF16, kind="Internal").ap()
    kt_scr = nc.dram_tensor("kt_scr", [4, 128, S_], BF16, kind="Internal").ap()
    kh_scr = nc.dram_tensor("kh_scr", [S_, 4, 128], BF16, kind="Internal").ap()
    v_scr = nc.dram_tensor("v_scr", [S_, 4, 128], BF16, kind="Internal").ap()
    sg_scr = nc.dram_tensor("sg_scr", [S_, 4, 128], BF16, kind="Internal").ap()
    NCHK = S_ // CH

    with ExitStack() as es:
        S = Sched(nc, es)
        ar = Arena(nc, es, "arena", 200)
        psf = [es.enter_context(nc.psum_tensor("ps%d" % i, [128, 512], F32)) for i in range(8)]
        psb = [psf[i][:].bitcast(BF16) for i in range(8)]

        mvs = ar.alloc([2, KC], F32)
        sc1p = ar.alloc([KC], F32)
        lbr = ar.alloc([2, 4], F32)
        lbt = ar.alloc([3, 4], F32)
        GN4 = ar.alloc([512], F32)
        mask01 = ar.alloc([128], F32)
        identb = ar.alloc([128], BF16)
        ones = ar.alloc([128], F32)
        dtab = ar.alloc([2, 4, NCHK], F32)
        for (dst, src, nm) in [(mvs, mv, 'mvs'), (lbr, lbr_d, 'lbr'), (GN4, gn_d, 'GN4'), (mask01, mask_d, 'mask01'), (identb, idb_d, 'identb')]:
            S.dma('sp', lambda e, dst=dst, src=src: e.dma_start(out=dst, in_=src), slot=nm, writes=[nm])
        S.op('dve', lambda e: e.memset(ones, 1.0), writes=['ones'])
        S.op('dve', lambda e: e.tensor_scalar(out=sc1p, in0=mvs[:, 1, :], scalar1=1.0, scalar2=None, op0=ALU.add), reads=['mvs'], writes=['sc1p'])
        S.op('dve', lambda e: e.tensor_tensor(out=lbt[:, 2, :], in0=lbr[:, 0, :], in1=lbr[:, 1, :], op=ALU.subtract), reads=['lbr'], writes=['lbt2'])
        S.op('act', lambda e: e.activation(out=lbt[:, 2, :], in_=lbt[:, 2, :], func=AF.Exp), reads=['lbt2'], writes=['lbt2'])
        S.op('dve', lambda e: e.tensor_scalar(out=lbt[:, 2, :], in0=lbt[:, 2, :], scalar1=1.0, scalar2=None, op0=ALU.add), reads=['lbt2'], writes=['lbt2'])
        S.op('dve', lambda e: e.reciprocal(out=lbt[:, 0, :], in_=lbt[:, 2, :]), reads=['lbt2'], writes=['lbt0'])
        S.op('dve', lambda e: e.tensor_scalar(out=lbt[:, 1, :], in0=lbt[:, 0, :], scalar1=-1.0, scalar2=1.0, op0=ALU.mult, op1=ALU.add), reads=['lbt0'], writes=['lbt1'])
        LB = ['lbt0', 'lbt1']
        pmark = ar.mark()

        W = ar.alloc([KC, 1024], BF16)
        hT2 = [ar.alloc([KC, T], BF16) for i in range(2)]
        NXS = 3
        xs = [ar.alloc([2, T], F32) for i in range(NXS)]
        NTP = 2
        tmp = [[ar.alloc([T], F32) for i in range(6)] for p in range(NTP)]
        NST = 3
        qst = [ar.alloc([T], BF16) for i in range(NST)]
        khfm = [ar.alloc([T], BF16) for i in range(2)]
        khst = [ar.alloc([4, 128], BF16) for i in range(2)]
        vst = [ar.alloc([T], BF16) for i in range(4)]
        sgst = [ar.alloc([T], BF16) for i in range(4)]
        xv = xT.rearrange("(c q) t -> q c t", q=128)
        xit = [0]
        qit = [0]

        def load_W(col0):
            for k in range(KC):
                S.dma('pool', lambda e, k=k: e.dma_start(out=W[:, k, :], in_=wd[k * 128:(k + 1) * 128, col0:col0 + 1024]), slot='W%d' % k, writes=[('W', k)])

        hcnt = [0]

        def load_mod(tg):
            hb_ = hcnt[0] % 2
            hcnt[0] += 1
            hT = hT2[hb_]
            tsl = slice(tg * T, (tg + 1) * T)
            for c2 in range(KC // 2):
                b = xit[0] % NXS
                xit[0] += 1
                S.dma('sp', lambda e, b=b, c2=c2, tsl=tsl: e.dma_start(out=xs[b], in_=xv[:, c2 * 2:c2 * 2 + 2, tsl]), slot='xs%d' % b, writes=['xs%d' % b])
                for i in range(2):
                    c = c2 * 2 + i
                    S.op('act', lambda e, b=b, i=i, c=c, hT=hT: e.activation(out=hT[:, c, :], in_=xs[b][:, i, :], func=AF.Identity,
                                                                   scale=sc1p[:, c:c + 1], bias=mvs[:, 0, c:c + 1]),
                         reads=['xs%d' % b, 'sc1p', 'mvs'], writes=[('hT', hb_, c)])
            return hT, hb_

        load_W(0)
        hcount = 0
        nxt = load_mod(0)
        for tg in range(NTG_RUN):
            tsl = slice(tg * T, (tg + 1) * T)
            hT, hbi = nxt
            if tg + 1 < NTG_RUN:
                nxt = load_mod(tg + 1)
            QB = lambda j: (j // 2) * 4 + (j % 2)
            FB = lambda j: (j // 2) * 4 + 2 + (j % 2)
            for half in range(2):
                for k in range(KC):
                    for jj in range(2):
                        j = half * 2 + jj
                        for grp in range(2):
                            bank = QB(j) if grp == 0 else FB(j)
                            S.op('pe', lambda e, k=k, j=j, grp=grp, bank=bank, hT=hT: e.matmul(psf[bank][:], lhsT=W[:, k, grp * 512 + j * 128:grp * 512 + (j + 1) * 128], rhs=hT[:, k, :],
                                                                                    start=(k == 0), stop=(k == KC - 1)),
                                 reads=[('W', k), ('hT', hbi, k)], writes=['ps%d' % bank], inc=(k == KC - 1))
            def chain1a(j, tg=tg, tsl=tsl):
                nonlocal hcount
                pp = j % NTP
                hcount += 1
                t0, t1, t2, t3, t4, t5 = tmp[pp]
                R = lambda i, pp=pp: 't%d_%d' % (pp, i)
                qb, fb = 'ps%d' % QB(j), 'ps%d' % FB(j)
                qps, fps = psf[QB(j)], psf[FB(j)]
                psbq = psb[QB(j)]
                S.op('act', lambda e, t0=t0, fps=fps: e.activation(out=t0, in_=fps[:], func=AF.Exp, scale=-1.0), reads=[fb], writes=[R(0)])
                S.op('dve', lambda e, t0=t0: e.tensor_scalar(out=t0, in0=t0, scalar1=1.0, scalar2=None, op0=ALU.add), reads=[R(0)], writes=[R(0)])
                S.op('dve', lambda e, t0=t0: e.reciprocal(out=t0, in_=t0), reads=[R(0)], writes=[R(0)])
                S.op('dve', lambda e, t0=t0, t1=t1, j=j: e.tensor_scalar(out=t1, in0=t0, scalar1=lbt[:, 1, j:j + 1], scalar2=lbt[:, 0, j:j + 1], op0=ALU.mult, op1=ALU.add),
                     reads=[R(0)] + LB, writes=[R(1)])
                S.op('dve', lambda e, t1=t1, t2=t2: e.tensor_scalar(out=t2, in0=t1, scalar1=-1.0, scalar2=1.0, op0=ALU.mult, op1=ALU.add), reads=[R(1)], writes=[R(2)])
                S.op('act', lambda e, t0=t0, t1=t1: e.activation(out=t0, in_=t1, func=AF.Ln), reads=[R(1)], writes=[R(0)])
                for c in range(4):
                    cs = slice(c * CH, (c + 1) * CH)
                    S.op('dve', lambda e, t0=t0, t3=t3, cs=cs: e.tensor_tensor_scan(out=t3[:, cs], data0=ones, data1=t0[:, cs], initial=0.0, op0=ALU.mult, op1=ALU.add),
                         reads=[R(0), 'ones'], writes=[R(3)])
                S.op('act', lambda e, t4=t4, qps=qps: e.activation(out=t4, in_=qps[:], func=AF.Exp, scale=-1.0), reads=[qb], writes=[R(4)])
                S.op('dve', lambda e, t4=t4: e.tensor_scalar(out=t4, in0=t4, scalar1=1.0, scalar2=None, op0=ALU.add), reads=[R(4)], writes=[R(4)])
                S.op('dve', lambda e, t4=t4: e.reciprocal(out=t4, in_=t4), reads=[R(4)], writes=[R(4)])
                S.op('dve', lambda e, t4=t4, qps=qps: e.tensor_tensor(out=t4, in0=qps[:], in1=t4, op=ALU.mult), reads=[R(4), qb], writes=[R(4)])
                B3 = t3.rearrange("p (c t) -> p c t", c=4)
                Bref = t3[:, 63:T:CH].unsqueeze(2).to_broadcast([128, 4, CH])
                Blast = t3[:, CH - 1:T:CH].unsqueeze(2).to_broadcast([128, 4, CH])
                t53 = t5.rearrange("p (c t) -> p c t", c=4)
                S.op('dve', lambda e, B3=B3, Bref=Bref, t53=t53: e.tensor_tensor(out=t53, in0=B3, in1=Bref, op=ALU.subtract), reads=[R(3)], writes=[R(5)])
                S.op('act', lambda e, t5=t5: e.activation(out=t5, in_=t5, func=AF.Exp), reads=[R(5)], writes=[R(5)])
                b = qit[0] % NST
                qit[0] += 1
                S.op('dve', lambda e, t4=t4, t5=t5, b=b: e.tensor_tensor(out=qst[b], in0=t4, in1=t5, op=ALU.mult), reads=[R(4), R(5)], writes=['qst%d' % b])
                S.dma('sp', lambda e, b=b, j=j, tsl=tsl: e.dma_start(out=qt_scr[j, :, tsl], in_=qst[b]), slot='qst%d' % b, reads=['qst%d' % b], writes=[])
                S.op('dve', lambda e, B3=B3, Bref=Bref, t53=t53: e.tensor_tensor(out=t53, in0=Bref, in1=B3, op=ALU.subtract), reads=[R(3)], writes=[R(5)])
                S.op('act', lambda e, t5=t5: e.activation(out=t5, in_=t5, func=AF.Exp), reads=[R(5)], writes=[R(5)])
                b = qit[0] % NST
                qit[0] += 1
                S.op('dve', lambda e, t2=t2, t5=t5, b=b: e.scalar_tensor_tensor(out=qst[b], in0=t5, scalar=1e30, in1=t2, op0=ALU.min, op1=ALU.mult), reads=[R(2), R(5)], writes=['qst%d' % b])
                S.dma('sp', lambda e, b=b, j=j, tsl=tsl: e.dma_start(out=kt_scr[j, :, tsl], in_=qst[b]), slot='qst%d' % b, reads=['qst%d' % b], writes=[])
                S.op('dve', lambda e, B3=B3, Blast=Blast, t53=t53: e.tensor_tensor(out=t53, in0=Blast, in1=B3, op=ALU.subtract), reads=[R(3)], writes=[R(5)])
                S.op('act', lambda e, t5=t5: e.activation(out=t5, in_=t5, func=AF.Exp), reads=[R(5)], writes=[R(5)])
                kb = j % 2
                S.op('dve', lambda e, t2=t2, t5=t5, kb=kb: e.tensor_tensor(out=khfm[kb], in0=t2, in1=t5, op=ALU.mult), reads=[R(2), R(5)], writes=['khfm%d' % kb])
                for c in range(4):
                    S.op('pe', lambda e, c=c, kb=kb, psbq=psbq: e.transpose(out=psbq[:, c * 128:(c + 1) * 128], in_=khfm[kb][:, c * CH:(c + 1) * CH], identity=identb),
                         reads=['khfm%d' % kb, 'identb'], writes=[qb])
                S.op('act', lambda e, kb=kb, psbq=psbq: e.activation(out=khst[kb], in_=psbq[:, 0:512].rearrange("p (c d) -> p c d", c=4), func=AF.Copy),
                     reads=[qb], writes=['khst%d' % kb])
                S.dma('sp', lambda e, kb=kb, j=j, tg=tg: e.dma_start(out=kh_scr.rearrange("(b p) h d -> p b h d", p=128)[:, tg * 4:(tg + 1) * 4, j, :], in_=khst[kb]),
                      slot='khst%d' % kb, reads=['khst%d' % kb], writes=[])
                S.op('act', lambda e, t3=t3, j=j, tg=tg: e.activation(out=dtab[:, 0, j, tg * 4:(tg + 1) * 4], in_=t3[:, CH - 1:T:CH], func=AF.Exp), reads=[R(3)], writes=[('dtab', j, tg)])
                S.op('act', lambda e, t3=t3, j=j, tg=tg: e.activation(out=dtab[:, 1, j, tg * 4:(tg + 1) * 4], in_=t3[:, 63:T:CH], func=AF.Exp), reads=[R(3)], writes=[('dtab', j, tg)])
            for pair in ((0, 1), (2, 3)):
                S.replay([S.record(lambda j=j: chain1a(j)) for j in pair])

        load_W(1024)
        vit = 0
        nxt = load_mod(0)
        for tg in range(NTG_RUN):
            hT, hbi = nxt
            if tg + 1 < NTG_RUN:
                nxt = load_mod(tg + 1)
            for k in range(KC):
                for tb in range(4):
                    for grp in range(2):
                        bank = grp * 4 + tb
                        S.op('pe', lambda e, k=k, tb=tb, grp=grp, bank=bank, hT=hT: e.matmul(psf[bank][:], lhsT=hT[:, k, tb * 128:(tb + 1) * 128], rhs=W[:, k, grp * 512:(grp + 1) * 512],
                                                                                 start=(k == 0), stop=(k == KC - 1)),
                             reads=[('W', k), ('hT', hbi, k)], writes=['ps%d' % bank], inc=(k == KC - 1))
            def chain1b(tb, tg=tg):
                nonlocal vit
                vb = tb
                vit += 1
                r0 = tg * T + tb * 128
                S.op('act', lambda e, vb=vb, tb=tb: e.activation(out=vst[vb], in_=psf[tb][:], func=AF.Copy), reads=['ps%d' % tb], writes=['vst%d' % vb])
                S.dma('sp', lambda e, vb=vb, r0=r0: e.dma_start(out=v_scr[r0:r0 + 128, :, :].rearrange("p h d -> p (h d)"), in_=vst[vb]), slot='vst%d' % vb, reads=['vst%d' % vb], writes=[])
                t0 = tmp[tb % 2][tb // 2]
                tr = 't%d_%d' % (tb % 2, tb // 2)
                gb = 'ps%d' % (4 + tb)
                S.op('act', lambda e, t0=t0, tb=tb: e.activation(out=t0, in_=psf[4 + tb][:], func=AF.Exp, scale=-1.0), reads=[gb], writes=[tr])
                S.op('dve', lambda e, t0=t0: e.tensor_scalar(out=t0, in0=t0, scalar1=1.0, scalar2=None, op0=ALU.add), reads=[tr], writes=[tr])
                S.op('dve', lambda e, t0=t0: e.reciprocal(out=t0, in_=t0), reads=[tr], writes=[tr])
                S.op('dve', lambda e, t0=t0, tb=tb: e.tensor_tensor(out=t0, in0=psf[4 + tb][:], in1=t0, op=ALU.mult), reads=[tr, gb], writes=[tr])
                S.op('dve', lambda e, t0=t0, vb=vb: e.tensor_tensor(out=sgst[vb], in0=t0, in1=GN4, op=ALU.mult), reads=[tr, 'GN4'], writes=['sgst%d' % vb])
                S.dma('sp', lambda e, vb=vb, r0=r0: e.dma_start(out=sg_scr[r0:r0 + 128, :, :].rearrange("p h d -> p (h d)"), in_=sgst[vb]), slot='sgst%d' % vb, reads=['sgst%d' % vb], writes=[])
            S.replay([S.record(lambda tb=tb: chain1b(tb)) for tb in range(4)])

        S.barrier()
        ar.reset(pmark)
        NC_RUN = NTG_RUN * 4
        QT = [ar.alloc([S_], BF16) for i in range(2)]
        KT = [ar.alloc([S_], BF16) for i in range(2)]
        KH = [ar.alloc([NCHK, 128], BF16) for i in range(2)]
        VV = [ar.alloc([NCHK, 128], BF16) for i in range(2)]
        SG = [ar.alloc([NCHK, 128], BF16) for i in range(2)]
        St = [ar.alloc([128], F32) for i in range(2)]
        Sbf = [[ar.alloc([128], BF16) for i in range(2)] for hb in range(2)]
        ATs = [[ar.alloc([128], BF16) for i in range(2)] for hb in range(2)]
        sc = [ar.alloc([4, 4], F32) for hb in range(2)]
        junk = [ar.alloc([128], F32) for hb in range(2)]
        on = [[ar.alloc([128], BF16) for i in range(2)] for hb in range(2)]
        ost = [ar.alloc([T], BF16) for i in range(4)]
        for hb in range(2):
            for i in range(2):
                S.op('dve', lambda e, hb=hb, i=i: e.memset(ATs[hb][i], 0.0), writes=['ATs%d_%d' % (hb, i)])
        tokv = lambda scr: scr.rearrange("(b p) h d -> p b h d", p=128)
        oi = [0]
        BA = lambda hb: hb * 4 + 0
        BU = lambda hb: hb * 4 + 1
        BO = lambda hb: hb * 4 + 2
        BT = lambda hb: hb * 4 + 3

        def emit_AU(c, hb):
            cs = slice(c * CH, (c + 1) * CH)
            a = c % 2
            S.op('pe', lambda e: e.matmul(psf[BA(hb)][:, 0:128], lhsT=KT[hb][:, cs], rhs=QT[hb][:, cs], start=True, stop=True),
                 reads=['KT%d' % hb, 'QT%d' % hb], writes=['ps%d' % BA(hb)])
            S.op('dve', lambda e: e.copy_predicated(out=ATs[hb][a], mask=mask01.bitcast(mybir.dt.uint32), data=psf[BA(hb)][:, 0:128]),
                 reads=['ps%d' % BA(hb), 'mask01', 'ATs%d_%d' % (hb, a)], writes=['ATs%d_%d' % (hb, a)])
            S.op('pe', lambda e: e.matmul(psf[BU(hb)][:, 0:128], lhsT=KH[hb][:, c, :], rhs=VV[hb][:, c, :], start=True, stop=True),
                 reads=['KH%d' % hb, 'VV%d' % hb], writes=['ps%d' % BU(hb)])

        def emit_chunk(c, hb, h):
            a = c % 2
            cs = slice(c * CH, (c + 1) * CH)
            ob = 'ps%d' % BO(hb)
            S.op('pe', lambda e: e.matmul(psf[BO(hb)][:, 0:128], lhsT=ATs[hb][a], rhs=VV[hb][:, c, :], start=True, stop=False),
                 reads=['ATs%d_%d' % (hb, a), 'VV%d' % hb], writes=[ob], inc=False)
            S.op('pe', lambda e: e.matmul(psf[BO(hb)][:, 0:128], lhsT=QT[hb][:, cs], rhs=Sbf[hb][a], start=False, stop=True),
                 reads=['QT%d' % hb, 'Sbf%d_%d' % (hb, a)], writes=[ob])
            S.op('dve', lambda e: e.scalar_tensor_tensor(out=St[hb], in0=St[hb], scalar=dtab[:, 0, h, c:c + 1], in1=psf[BU(hb)][:, 0:128], op0=ALU.mult, op1=ALU.add),
                 reads=['St%d' % hb, 'ps%d' % BU(hb)], writes=['St%d' % hb])
            if c + 1 < NC_RUN:
                S.op('act', lambda e: e.activation(out=Sbf[hb][1 - a], in_=St[hb], func=AF.Identity, scale=dtab[:, 1, h, c + 1:c + 2]),
                     reads=['St%d' % hb], writes=['Sbf%d_%d' % (hb, 1 - a)])
            s4 = c % 4
            S.op('act', lambda e: e.activation(out=junk[hb], in_=psf[BO(hb)][:, 0:128], func=AF.Square, accum_out=sc[hb][:, s4, 0:1]), reads=[ob], writes=['junk%d' % hb, ('sc0', hb, s4)])
            S.op('dve', lambda e: e.tensor_scalar(out=sc[hb][:, s4, 0:1], in0=sc[hb][:, s4, 0:1], scalar1=1.0 / 128, scalar2=RMS_EPS, op0=ALU.mult, op1=ALU.add),
                 reads=[('sc0', hb, s4)], writes=[('sc0', hb, s4)])
            S.op('act', lambda e: e.activation(out=sc[hb][:, s4, 1:2], in_=sc[hb][:, s4, 0:1], func=AF.Sqrt), reads=[('sc0', hb, s4)], writes=[('sc1', hb, s4)])
            S.op('dve', lambda e: e.reciprocal(out=sc[hb][:, s4, 1:2], in_=sc[hb][:, s4, 1:2]), reads=[('sc1', hb, s4)], writes=[('sc1', hb, s4)])
            S.op('dve', lambda e: e.scalar_tensor_tensor(out=on[hb][a], in0=psf[BO(hb)][:, 0:128], scalar=sc[hb][:, s4, 1:2], in1=SG[hb][:, c, :], op0=ALU.mult, op1=ALU.mult),
                 reads=[ob, ('sc1', hb, s4), 'SG%d' % hb], writes=['on%d_%d' % (hb, a)])
            S.op('pe', lambda e: e.transpose(out=psb[BT(hb)][:, s4 * 128:(s4 + 1) * 128], in_=on[hb][a], identity=identb),
                 reads=['on%d_%d' % (hb, a), 'identb'], writes=['ps%d' % BT(hb)])
            if s4 == 3:
                o2 = oi[0] % 4
                oi[0] += 1
                g4 = c // 4
                S.op('act', lambda e: e.activation(out=ost[o2], in_=psb[BT(hb)][:, 0:512], func=AF.Copy), reads=['ps%d' % BT(hb)], writes=['ost%d' % o2])
                S.dma('sp', lambda e: e.dma_start(out=oTd[h * 128:(h + 1) * 128, g4 * T:(g4 + 1) * T], in_=ost[o2]),
                      slot='ost%d' % o2, reads=['ost%d' % o2], writes=[])

        heads = list(heads)
        for pi in range(0, len(heads), 2):
            hs = heads[pi:pi + 2]
            for hb, h in enumerate(hs):
                S.dma('sp', lambda e, h=h, hb=hb: e.dma_start(out=QT[hb], in_=qt_scr[h]), slot='QT%d' % hb, writes=['QT%d' % hb])
                S.dma('sp', lambda e, h=h, hb=hb: e.dma_start(out=KT[hb], in_=kt_scr[h]), slot='KT%d' % hb, writes=['KT%d' % hb])
                S.dma('sp', lambda e, h=h, hb=hb: e.dma_start(out=KH[hb], in_=tokv(kh_scr)[:, :, h, :]), slot='KH%d' % hb, writes=['KH%d' % hb])
                S.dma('sp', lambda e, h=h, hb=hb: e.dma_start(out=VV[hb], in_=tokv(v_scr)[:, :, h, :]), slot='VV%d' % hb, writes=['VV%d' % hb])
                S.dma('sp', lambda e, h=h, hb=hb: e.dma_start(out=SG[hb], in_=tokv(sg_scr)[:, :, h, :]), slot='SG%d' % hb, writes=['SG%d' % hb])
                S.op('dve', lambda e, hb=hb: e.memset(St[hb], 0.0), writes=['St%d' % hb])
                S.op('dve', lambda e, hb=hb: e.memset(Sbf[hb][0], 0.0), writes=['Sbf%d_0' % hb])
            for hb, h in enumerate(hs):
                emit_AU(0, hb)
            for c in range(NC_RUN):
                for hb, h in enumerate(hs):
                    emit_chunk(c, hb, h)
                    if c + 1 < NC_RUN:
                        emit_AU(c + 1, hb)
        S.finish()
        print("stage d ninst", S.ninst)
    return nc


D = 4096
KC = 32
T = 512
ALPHA = 4 ** 0.25
LN_EPS = 1e-5


def build_ff(moe, TOK=1024, n_groups_limit=None, mode='full'):
    nc = bass.Bass("TRN2", target_bir_lowering=False)
    NPASS = TOK // T
    oT = nc.dram_tensor("oT", [D, TOK], BF16, kind="ExternalInput").ap()
    xT = nc.dram_tensor("xT", [D, TOK], F32, kind="ExternalInput").ap()
    pv = nc.dram_tensor("pv", [128, 8, KC], F32, kind="ExternalInput").ap()
    w_o = nc.dram_tensor("w_o", [D, D], F32, kind="ExternalInput").ap()
    ones_d = nc.dram_tensor("ones", [128, 128], F32, kind="ExternalInput").ap()
    if moe:
        NE, H = 8, 4096
        rw_d = nc.dram_tensor("rw", [128, KC, 8], F32, kind="ExternalInput").ap()
        groups = []
        if mode == 'full':
            w_in = nc.dram_tensor("w_in", [NE, D, 2 * H], F32, kind="ExternalInput").ap()
            w_o2 = nc.dram_tensor("w_o2", [NE, H, D], F32, kind="ExternalInput").ap()
            sel_d = nc.dram_tensor("sel", [8, 8, 128], F32, kind="ExternalInput").ap()
            ident_d = nc.dram_tensor("ident", [128, 128], F32, kind="ExternalInput").ap()
            for e in range(NE):
                groups.append(dict(win=w_in[e].rearrange("k (s c) -> k s c", s=2), wo=w_o2[e],
                                   pairs=list(range(16)), gate=e))
    else:
        H = 11008
        w_in = nc.dram_tensor("w_in", [D, 2 * H], F32, kind="ExternalInput").ap()
        w_o2 = nc.dram_tensor("w_o2", [H, D], F32, kind="ExternalInput").ap()
        win_v = w_in.rearrange("k (s c) -> k s c", s=2)
        groups = [dict(win=win_v, wo=w_o2, pairs=list(range(0, 16)), gate=None),
                  dict(win=win_v, wo=w_o2, pairs=list(range(16, 32)), gate=None),
                  dict(win=win_v, wo=w_o2, pairs=list(range(32, 43)), gate=None)]
    if n_groups_limit is not None:
        groups = groups[:n_groups_limit]
    if mode == 'full':
        yT = nc.dram_tensor("yT", [D, TOK], F32, kind="ExternalOutput").ap()
    else:
        h2o = nc.dram_tensor("h2o", [D, TOK], BF16, kind="ExternalOutput").ap()
        acco = nc.dram_tensor("acco", [D, TOK], F32, kind="ExternalOutput").ap()
        gout = nc.dram_tensor("gout", [TOK, 8], F32, kind="ExternalOutput").ap()

    with ExitStack() as es:
        S = Sched(nc, es)
        sb = lambda name, shape, dt: es.enter_context(nc.sbuf_tensor(name, shape, dt))
        acc = sb("acc", [128, KC, T], F32)
        actT = sb("actT", [128, KC, T], BF16)
        h2T = sb("h2T", [128, KC, T], BF16)
        NB = 8
        wt = [sb("wt%d" % i, [128, 512], BF16) for i in range(NB)]
        pvs = sb("pvs", [128, 8, KC], F32)
        dv = sb("dv", [128, 6, KC], F32)
        ones = sb("ones_s", [128, 128], F32)
        s1 = sb("s1", [128, T], F32)
        s2 = sb("s2", [128, T], F32)
        mean = sb("mean", [128, T], F32)
        msq = sb("msq", [128, T], F32)
        rstd = sb("rstd", [128, T], F32)
        NTB = 3
        tb_ = [sb("tb%d" % i, [128, T], F32) for i in range(NTB)]
        sa_ = [sb("sa%d" % i, [128, T], F32) for i in range(NTB)]
        yo_ = [sb("yo%d" % i, [128, T], F32) for i in range(NTB)]
        ps = [es.enter_context(nc.psum_tensor("ps%d" % i, [128, 512], F32)) for i in range(8)]
        if moe:
            rw = sb("rw_s", [128, KC, 8], F32)
            if mode == 'full':
                sel = sb("sel_s", [8, 8, 128], F32)
                ident = sb("ident_s", [128, 128], F32)
            h2f_ = [sb("h2f%d" % i, [128, T], F32) for i in range(2)]
            G_ = [sb("G%d" % i, [128, T], F32) for i in range(2)]
            bg_ = [sb("bg%d" % i, [128, T], F32) for i in range(NTB)]
            gT = sb("gT", [8, T], F32)
            Lsb = sb("Lsb", [128, 4, 8], F32)
            m8 = sb("m8", [128, 4, 8], F32)
            nv1 = sb("nv1", [128, 4], F32)
            msk = sb("msk", [128, 4, 8], F32)
            ex = sb("ex", [128, 4, 8], F32)
            me = sb("me", [128, 4, 8], F32)
            den = sb("den", [128, 4], F32)
            gsb = sb("gsb", [128, 4, 8], F32)

        S.dma('sp', lambda e: e.dma_start(out=pvs[:], in_=pv), slot='pvs', writes=['pvs'])
        S.dma('sp', lambda e: e.dma_start(out=ones[:], in_=ones_d), slot='ones', writes=['ones'])
        if moe:
            S.dma('sp', lambda e: e.dma_start(out=rw[:], in_=rw_d), slot='rw', writes=['rw'])
            if mode == 'full':
                S.dma('sp', lambda e: e.dma_start(out=sel[:], in_=sel_d), slot='sel', writes=['sel'])
                S.dma('sp', lambda e: e.dma_start(out=ident[:], in_=ident_d), slot='ident', writes=['ident'])
        V = lambda i: pvs[:, i, :]
        S.op('dve', lambda e: e.tensor_scalar(out=dv[:, 0, :], in0=V(0), scalar1=1.0, scalar2=None, op0=ALU.add), reads=['pvs'], writes=['dv0'])
        S.op('dve', lambda e: e.tensor_scalar(out=dv[:, 5, :], in0=V(2), scalar1=1.0, scalar2=None, op0=ALU.add), reads=['pvs'], writes=['dv5'])
        S.op('dve', lambda e: e.tensor_tensor(out=dv[:, 1, :], in0=V(4), in1=dv[:, 5, :], op=ALU.mult), reads=['pvs', 'dv5'], writes=['dv1'])
        S.op('dve', lambda e: e.tensor_tensor(out=dv[:, 2, :], in0=V(5), in1=dv[:, 5, :], op=ALU.mult), reads=['pvs', 'dv5'], writes=['dv2'])
        S.op('dve', lambda e: e.tensor_tensor(out=dv[:, 2, :], in0=dv[:, 2, :], in1=V(1), op=ALU.add), reads=['pvs', 'dv2'], writes=['dv2'])
        S.op('dve', lambda e: e.tensor_scalar(out=dv[:, 3, :], in0=V(4), scalar1=ALPHA, scalar2=None, op0=ALU.mult), reads=['pvs'], writes=['dv3'])
        S.op('dve', lambda e: e.tensor_scalar(out=dv[:, 4, :], in0=V(5), scalar1=ALPHA, scalar2=None, op0=ALU.mult), reads=['pvs'], writes=['dv4'])
        S.op('dve', lambda e: e.tensor_scalar(out=dv[:, 5, :], in0=V(3), scalar1=1.0, scalar2=None, op0=ALU.add), reads=['pvs', 'dv1', 'dv2'], writes=['dv5'])
        DVR = ['dv0', 'dv1', 'dv2', 'dv3', 'dv4', 'dv5', 'pvs']

        witer = [0]

        def wtile_load(src_ap, three=False):
            b = witer[0] % NB
            witer[0] += 1
            if three:
                S.dma('pool', lambda e: e.dma_start(out=wt[b][:].rearrange("p (s c) -> p s c", s=2), in_=src_ap),
                      slot='wt%d' % b, writes=['wt%d' % b])
            else:
                S.dma('pool', lambda e: e.dma_start(out=wt[b][:], in_=src_ap), slot='wt%d' % b, writes=['wt%d' % b])
            return b

        bankset = [0]

        def gemm_group(tiles, rhs_of, rhs_res, evac):
            base = (bankset[0] % 2) * 4
            bankset[0] += 1
            nk = len(tiles)
            for ki, (src, three) in enumerate(tiles):
                b = wtile_load(src, three)
                for j in range(4):
                    bank = base + j
                    S.op('pe', lambda e, b=b, j=j, ki=ki, bank=bank: e.matmul(
                        ps[bank][:], lhsT=wt[b][:, j * 128:(j + 1) * 128], rhs=rhs_of(ki),
                        start=(ki == 0), stop=(ki == nk - 1)),
                        reads=['wt%d' % b] + rhs_res(ki), writes=['ps%d' % bank], inc=(j == 3))
            for j in range(4):
                evac(j, base + j)

        def layernorm(final, ps_pass):
            for c in range(KC):
                if c == 0:
                    S.op('dve', lambda e: e.tensor_copy(out=s1[:], in_=acc[:, 0, :]), reads=[('acc', 0)], writes=['s1'])
                    S.op('act', lambda e: e.activation(out=s2[:], in_=acc[:, 0, :], func=AF.Square), reads=[('acc', 0)], writes=['s2'])
                else:
                    t = tb_[c % NTB]
                    tr = 'tb%d' % (c % NTB)
                    S.op('act', lambda e, c=c, t=t: e.activation(out=t[:], in_=acc[:, c, :], func=AF.Square), reads=[('acc', c)], writes=[tr])
                    S.op('dve', lambda e, c=c: e.tensor_tensor(out=s1[:], in0=s1[:], in1=acc[:, c, :], op=ALU.add), reads=[('acc', c), 's1'], writes=['s1'])
                    S.op('dve', lambda e, t=t: e.tensor_tensor(out=s2[:], in0=s2[:], in1=t[:], op=ALU.add), reads=[tr, 's2'], writes=['s2'])
            S.op('pe', lambda e: e.matmul(ps[4][:], lhsT=ones[:], rhs=s1[:], start=True, stop=True), reads=['ones', 's1'], writes=['ps4'])
            S.op('pe', lambda e: e.matmul(ps[5][:], lhsT=ones[:], rhs=s2[:], start=True, stop=True), reads=['ones', 's2'], writes=['ps5'])
            S.op('act', lambda e: e.activation(out=mean[:], in_=ps[4][:], func=AF.Copy, scale=1.0 / D), reads=['ps4'], writes=['mean'])
            S.op('dve', lambda e: e.tensor_tensor(out=msq[:], in0=mean[:], in1=mean[:], op=ALU.mult), reads=['mean'], writes=['msq'])
            S.op('dve', lambda e: e.scalar_tensor_tensor(out=msq[:], in0=ps[5][:], scalar=1.0 / D, in1=msq[:], op0=ALU.mult, op1=ALU.subtract),
                 reads=['ps5', 'msq'], writes=['msq'])
            S.op('dve', lambda e: e.tensor_scalar(out=msq[:], in0=msq[:], scalar1=LN_EPS, scalar2=None, op0=ALU.add), reads=['msq'], writes=['msq'])
            S.op('act', lambda e: e.activation(out=rstd[:], in_=msq[:], func=AF.Sqrt), reads=['msq'], writes=['rstd'])
            S.op('dve', lambda e: e.reciprocal(out=rstd[:], in_=rstd[:]), reads=['rstd'], writes=['rstd'])
            for c in range(KC):
                t = tb_[c % NTB]
                tr = 'tb%d' % (c % NTB)
                S.op('dve', lambda e, c=c, t=t: e.tensor_tensor(out=t[:], in0=acc[:, c, :], in1=mean[:], op=ALU.subtract), reads=[('acc', c), 'mean'], writes=[tr])
                S.op('dve', lambda e, t=t: e.tensor_tensor(out=t[:], in0=t[:], in1=rstd[:], op=ALU.mult), reads=[tr, 'rstd'], writes=[tr])
                if not final:
                    S.op('act', lambda e, c=c, t=t: e.activation(out=h2T[:, c, :], in_=t[:], func=AF.Identity, scale=dv[:, 1, c:c + 1], bias=dv[:, 2, c:c + 1]),
                         reads=[tr] + DVR, writes=[('h2T', c)])
                    if moe:
                        hf = h2f_[c % 2]
                        hr = 'h2f%d' % (c % 2)
                        S.op('dve', lambda e, c=c, t=t, hf=hf: e.tensor_scalar(out=hf[:], in0=t[:], scalar1=dv[:, 1, c:c + 1], scalar2=dv[:, 2, c:c + 1], op0=ALU.mult, op1=ALU.add),
                             reads=[tr] + DVR, writes=[hr])
                        for q in range(4):
                            S.op('pe', lambda e, c=c, q=q, hf=hf: e.matmul(ps[q][:, 0:8], lhsT=hf[:, q * 128:(q + 1) * 128], rhs=rw[:, c, :],
                                                                     start=(c == 0), stop=(c == KC - 1)),
                                 reads=[hr, 'rw'], writes=['ps%d' % q])
                    S.op('act', lambda e, c=c, t=t: e.activation(out=acc[:, c, :], in_=t[:], func=AF.Identity, scale=dv[:, 3, c:c + 1], bias=dv[:, 4, c:c + 1]),
                         reads=[tr] + DVR, writes=[('acc', c)])
                else:
                    yo = yo_[c % NTB]
                    yr = 'yo%d' % (c % NTB)
                    S.op('act', lambda e, c=c, t=t, yo=yo: e.activation(out=yo[:], in_=t[:], func=AF.Identity, scale=pvs[:, 6, c:c + 1], bias=pvs[:, 7, c:c + 1]),
                         reads=[tr] + DVR, writes=[yr])
                    S.dma('sp', lambda e, c=c, yo=yo: e.dma_start(out=yT[c * 128:(c + 1) * 128, ps_pass * T:(ps_pass + 1) * T], in_=yo[:]),
                          slot=yr, reads=[yr], writes=[])

        for p in range(NPASS):
            tsl = slice(p * T, (p + 1) * T)
            xv = xT.rearrange("(c q) t -> q c t", q=128)
            ov = oT.rearrange("(c q) t -> q c t", q=128)
            for g4 in range(4):
                cs = slice(g4 * 8, (g4 + 1) * 8)
                S.dma('sp', lambda e, cs=cs, tsl=tsl, xv=xv: e.dma_start(out=acc[:, cs, :], in_=xv[:, cs, tsl]), slot='accld%d' % g4,
                      writes=[('acc', c) for c in range(g4 * 8, g4 * 8 + 8)])
                S.dma('sp', lambda e, cs=cs, tsl=tsl, ov=ov: e.dma_start(out=actT[:, cs, :], in_=ov[:, cs, tsl]), slot='actld%d' % g4,
                      writes=[('actT', c) for c in range(g4 * 8, g4 * 8 + 8)])
            for c in range(KC):
                S.op('act', lambda e, c=c: e.activation(out=acc[:, c, :], in_=acc[:, c, :], func=AF.Copy, scale=ALPHA),
                     reads=[('acc', c)], writes=[('acc', c)])
            for ng in range(D // 512):
                tiles = [(w_o[k * 128:(k + 1) * 128, ng * 512:(ng + 1) * 512], False) for k in range(KC)]

                def evac(j, bank, ng=ng):
                    n = ng * 4 + j
                    S.op('dve', lambda e: e.scalar_tensor_tensor(out=acc[:, n, :], in0=ps[bank][:], scalar=dv[:, 0, n:n + 1], in1=acc[:, n, :],
                                                                 op0=ALU.mult, op1=ALU.add),
                         reads=['ps%d' % bank, ('acc', n)] + DVR, writes=[('acc', n)])
                gemm_group(tiles, lambda ki: actT[:, ki, :], lambda ki: [('actT', ki)], evac)
            layernorm(False, p)
            if moe:
                for q in range(4):
                    S.op('dve', lambda e, q=q: e.tensor_copy(out=Lsb[:, q, :], in_=ps[q][:, 0:8]), reads=['ps%d' % q], writes=[('L', q)])
                    S.op('dve', lambda e, q=q: e.max(out=m8[:, q, :], in_=Lsb[:, q, :]), reads=[('L', q)], writes=[('m8', q)])
                    S.op('dve', lambda e, q=q: e.tensor_scalar(out=nv1[:, q:q + 1], in0=m8[:, q, 0:1], scalar1=-1.0, scalar2=None, op0=ALU.mult),
                         reads=[('m8', q)], writes=[('nv1', q)])
                    S.op('dve', lambda e, q=q: e.tensor_scalar(out=msk[:, q, :], in0=Lsb[:, q, :], scalar1=m8[:, q, 1:2], scalar2=None, op0=ALU.is_ge),
                         reads=[('m8', q), ('L', q)], writes=[('msk', q)])
                    S.op('act', lambda e, q=q: e.activation(out=ex[:, q, :], in_=Lsb[:, q, :], func=AF.Exp, bias=nv1[:, q:q + 1], scale=1.0),
                         reads=[('L', q), ('nv1', q)], writes=[('ex', q)])
                    S.op('dve', lambda e, q=q: e.tensor_tensor(out=me[:, q, :], in0=msk[:, q, :], in1=ex[:, q, :], op=ALU.mult),
                         reads=[('msk', q), ('ex', q)], writes=[('me', q)])
                    S.op('dve', lambda e, q=q: e.reduce_sum(out=den[:, q:q + 1], in_=me[:, q, :], axis=AX.X),
                         reads=[('me', q)], writes=[('den', q)])
                    S.op('dve', lambda e, q=q: e.reciprocal(out=den[:, q:q + 1], in_=den[:, q:q + 1]), reads=[('den', q)], writes=[('den', q)])
                    S.op('dve', lambda e, q=q: e.tensor_scalar(out=gsb[:, q, :], in0=me[:, q, :], scalar1=den[:, q:q + 1], scalar2=None, op0=ALU.mult),
                         reads=[('me', q), ('den', q)], writes=[('gsb', q)])
                    if mode == 'full':
                        S.op('pe', lambda e, q=q: e.transpose(out=ps[6][0:8, q * 128:(q + 1) * 128], in_=gsb[:, q, :], identity=ident[:]),
                             reads=[('gsb', q), 'ident'], writes=['ps6'])
                if mode == 'full':
                    S.op('act', lambda e: e.activation(out=gT[:], in_=ps[6][0:8, :], func=AF.Copy), reads=['ps6'], writes=['gT'])
            if mode == 'e1':
                hv = h2o.rearrange("(c q) t -> q c t", q=128)
                av = acco.rearrange("(c q) t -> q c t", q=128)
                for g4 in range(4):
                    cs = slice(g4 * 8, (g4 + 1) * 8)
                    S.dma('sp', lambda e, cs=cs, tsl=tsl, hv=hv: e.dma_start(out=hv[:, cs, tsl], in_=h2T[:, cs, :]), slot='h2o%d' % g4,
                          reads=[('h2T', c) for c in range(g4 * 8, g4 * 8 + 8)], writes=[])
                    S.dma('sp', lambda e, cs=cs, tsl=tsl, av=av: e.dma_start(out=av[:, cs, tsl], in_=acc[:, cs, :]), slot='acco%d' % g4,
                          reads=[('acc', c) for c in range(g4 * 8, g4 * 8 + 8)], writes=[])
                S.dma('sp', lambda e, p=p: e.dma_start(out=gout.rearrange("(q r) x -> r q x", r=128)[:, p * 4:(p + 1) * 4, :], in_=gsb[:]), slot='gout',
                      reads=[('gsb', q) for q in range(4)], writes=[])
                continue
            for gi, g in enumerate(groups):
                if g['gate'] is not None:
                    G = G_[gi % 2]
                    Gr = 'G%d' % (gi % 2)
                    ge = g['gate']
                    S.op('pe', lambda e, ge=ge: e.matmul(ps[7][:], lhsT=sel[:, ge, :], rhs=gT[:], start=True, stop=True),
                         reads=['sel', 'gT'], writes=['ps7'])
                    S.op('act', lambda e, G=G: e.activation(out=G[:], in_=ps[7][:], func=AF.Copy), reads=['ps7'], writes=[Gr])
                pairs = g['pairs']
                chunks = []
                for pi, P in enumerate(pairs):
                    tiles = [(g['win'][k * 128:(k + 1) * 128, :, P * 256:(P + 1) * 256], True) for k in range(KC)]
                    lj0 = pi * 2

                    def evac(j, bank, lj0=lj0, g=g):
                        if j >= 2:
                            return
                        lj = lj0 + j
                        abank, bbank = bank, bank + 2
                        i3 = lj % NTB
                        sa = sa_[i3]
                        S.op('act', lambda e: e.activation(out=sa[:], in_=ps[abank][:], func=AF.Silu), reads=['ps%d' % abank], writes=['sa%d' % i3])
                        if g['gate'] is not None:
                            bg = bg_[i3]
                            Gg = G_[gi % 2]
                            S.op('dve', lambda e: e.tensor_tensor(out=bg[:], in0=ps[bbank][:], in1=Gg[:], op=ALU.mult),
                                 reads=['ps%d' % bbank, 'G%d' % (gi % 2)], writes=['bg%d' % i3])
                            S.op('dve', lambda e: e.tensor_tensor(out=actT[:, lj, :], in0=sa[:], in1=bg[:], op=ALU.mult),
                                 reads=['sa%d' % i3, 'bg%d' % i3], writes=[('actT', lj)])
                        else:
                            S.op('dve', lambda e: e.tensor_tensor(out=actT[:, lj, :], in0=sa[:], in1=ps[bbank][:], op=ALU.mult),
                                 reads=['sa%d' % i3, 'ps%d' % bbank], writes=[('actT', lj)])
                    gemm_group(tiles, lambda ki: h2T[:, ki, :], lambda ki: [('h2T', ki)], evac)
                    chunks += [(lj0, P * 2), (lj0 + 1, P * 2 + 1)]
                for ng in range(D // 512):
                    tiles = [(g['wo'][gj * 128:(gj + 1) * 128, ng * 512:(ng + 1) * 512], False) for (lj, gj) in chunks]

                    def evac2(j, bank, ng=ng):
                        n = ng * 4 + j
                        S.op('dve', lambda e: e.scalar_tensor_tensor(out=acc[:, n, :], in0=ps[bank][:], scalar=dv[:, 5, n:n + 1], in1=acc[:, n, :],
                                                                     op0=ALU.mult, op1=ALU.add),
                             reads=['ps%d' % bank, ('acc', n)] + DVR, writes=[('acc', n)])
                    gemm_group(tiles, lambda ki, chunks=chunks: actT[:, chunks[ki][0], :], lambda ki, chunks=chunks: [('actT', chunks[ki][0])], evac2)
            layernorm(True, p)
        S.finish()
        print("ff ninst", S.ninst)
    return nc


def build_e2(NP):
    nc = bass.Bass("TRN2", target_bir_lowering=False)
    H = 4096
    hT_d = nc.dram_tensor("hT", [D, NP], BF16, kind="ExternalInput").ap()
    gb_d = nc.dram_tensor("gb", [128, NP], F32, kind="ExternalInput").ap()
    w_in = nc.dram_tensor("w_in", [D, 2 * H], F32, kind="ExternalInput").ap()
    w_o2 = nc.dram_tensor("w_o2", [H, D], F32, kind="ExternalInput").ap()
    yT = nc.dram_tensor("yT", [D, NP], F32, kind="ExternalOutput").ap()
    win_v = w_in.rearrange("k (s c) -> k s c", s=2)
    with ExitStack() as es:
        S = Sched(nc, es)
        sb = lambda name, shape, dt: es.enter_context(nc.sbuf_tensor(name, shape, dt))
        actT = sb("actT", [128, KC, T], BF16)
        h2T = [sb("h2T%d" % i, [128, KC, T], BF16) for i in range(2)]
        NB = 8
        wt = [sb("wt%d" % i, [128, 512], BF16) for i in range(NB)]
        NTB = 3
        sa_ = [sb("sa%d" % i, [128, T], F32) for i in range(NTB)]
        bg_ = [sb("bg%d" % i, [128, T], F32) for i in range(NTB)]
        yo_ = [sb("yo%d" % i, [128, T], F32) for i in range(4)]
        G_ = [sb("G%d" % i, [128, T], F32) for i in range(2)]
        ps = [es.enter_context(nc.psum_tensor("ps%d" % i, [128, 512], F32)) for i in range(8)]
        witer = [0]
        bankset = [0]
        yit = [0]

        def wtile_load(src_ap, three):
            b = witer[0] % NB
            witer[0] += 1
            if three:
                S.dma('pool', lambda e: e.dma_start(out=wt[b][:].rearrange("p (s c) -> p s c", s=2), in_=src_ap), slot='wt%d' % b, writes=['wt%d' % b])
            else:
                S.dma('pool', lambda e: e.dma_start(out=wt[b][:], in_=src_ap), slot='wt%d' % b, writes=['wt%d' % b])
            return b

        def gemm_group(tiles, rhs_of, rhs_res, evac, Tp):
            base = (bankset[0] % 2) * 4
            bankset[0] += 1
            nk = len(tiles)
            for ki, (src, three) in enumerate(tiles):
                b = wtile_load(src, three)
                for j in range(4):
                    bank = base + j
                    S.op('pe', lambda e, b=b, j=j, ki=ki, bank=bank: e.matmul(ps[bank][:, 0:Tp], lhsT=wt[b][:, j * 128:(j + 1) * 128], rhs=rhs_of(ki),
                                                                          start=(ki == 0), stop=(ki == nk - 1)),
                         reads=['wt%d' % b] + rhs_res(ki), writes=['ps%d' % bank], inc=(j == 3))
            for j in range(4):
                evac(j, base + j)

        hv = hT_d.rearrange("(c q) t -> q c t", q=128)
        sizes = [T] * (NP // T) + ([NP % T] if NP % T else [])
        tok0 = 0
        for p, Tp in enumerate(sizes):
            tsl = slice(tok0, tok0 + Tp)
            tok0 += Tp
            hb = p % 2
            hT = h2T[hb]
            G = G_[hb]
            for g4 in range(4):
                cs = slice(g4 * 8, (g4 + 1) * 8)
                S.dma('sp', lambda e, cs=cs, tsl=tsl, hT=hT, Tp=Tp: e.dma_start(out=hT[:, cs, 0:Tp], in_=hv[:, cs, tsl]), slot='hld%d_%d' % (hb, g4),
                      writes=[('h2T', hb, c) for c in range(g4 * 8, g4 * 8 + 8)])
            S.dma('sp', lambda e, tsl=tsl, G=G, Tp=Tp: e.dma_start(out=G[:, 0:Tp], in_=gb_d[:, tsl]), slot='G%d' % hb, writes=['G%d' % hb])
            chunks = []
            for P in range(16):
                tiles = [(win_v[k * 128:(k + 1) * 128, :, P * 256:(P + 1) * 256], True) for k in range(KC)]

                def evac(j, bank, P=P, hb=hb, G=G, Tp=Tp):
                    if j >= 2:
                        return
                    lj = P * 2 + j
                    abank, bbank = bank, bank + 2
                    i3 = lj % NTB
                    sa, bg = sa_[i3], bg_[i3]
                    S.op('act', lambda e: e.activation(out=sa[:, 0:Tp], in_=ps[abank][:, 0:Tp], func=AF.Silu), reads=['ps%d' % abank], writes=['sa%d' % i3])
                    S.op('dve', lambda e: e.tensor_tensor(out=bg[:, 0:Tp], in0=ps[bbank][:, 0:Tp], in1=G[:, 0:Tp], op=ALU.mult), reads=['ps%d' % bbank, 'G%d' % hb], writes=['bg%d' % i3])
                    S.op('dve', lambda e: e.tensor_tensor(out=actT[:, lj, 0:Tp], in0=sa[:, 0:Tp], in1=bg[:, 0:Tp], op=ALU.mult), reads=['sa%d' % i3, 'bg%d' % i3], writes=[('actT', lj)])
                gemm_group(tiles, lambda ki, hT=hT, Tp=Tp: hT[:, ki, 0:Tp], lambda ki, hb=hb: [('h2T', hb, ki)], evac, Tp)
            for ng in range(D // 512):
                tiles = [(w_o2[j * 128:(j + 1) * 128, ng * 512:(ng + 1) * 512], False) for j in range(KC)]

                def evac2(j, bank, ng=ng, tsl=tsl, Tp=Tp):
                    n = ng * 4 + j
                    yi = yit[0] % 4
                    yit[0] += 1
                    yo = yo_[yi]
                    if j % 2 == 0:
                        S.op('act', lambda e: e.activation(out=yo[:, 0:Tp], in_=ps[bank][:, 0:Tp], func=AF.Copy), reads=['ps%d' % bank], writes=['yo%d' % yi])
                    else:
                        S.op('dve', lambda e: e.tensor_copy(out=yo[:, 0:Tp], in_=ps[bank][:, 0:Tp]), reads=['ps%d' % bank], writes=['yo%d' % yi])
                    S.dma('sp', lambda e: e.dma_start(out=yT[n * 128:(n + 1) * 128, tsl], in_=yo[:, 0:Tp]), slot='yo%d' % yi, reads=['yo%d' % yi], writes=[])
                gemm_group(tiles, lambda ki, Tp=Tp: actT[:, ki, 0:Tp], lambda ki: [('actT', ki)], evac2, Tp)
        S.finish()
        print("e2 ninst", S.ninst)
    return nc


def build_e3(TOK=1024):
    nc = bass.Bass("TRN2", target_bir_lowering=False)
    acc_d = nc.dram_tensor("accT", [D, TOK], F32, kind="ExternalInput").ap()
    ya_d = nc.dram_tensor("yA", [D, TOK], F32, kind="ExternalInput").ap()
    yb_d = nc.dram_tensor("yB", [D, TOK], F32, kind="ExternalInput").ap()
    pv = nc.dram_tensor("pv", [128, 8, KC], F32, kind="ExternalInput").ap()
    ones_d = nc.dram_tensor("ones", [128, 128], F32, kind="ExternalInput").ap()
    yT = nc.dram_tensor("yT", [D, TOK], F32, kind="ExternalOutput").ap()
    with ExitStack() as es:
        S = Sched(nc, es)
        sb = lambda name, shape, dt: es.enter_context(nc.sbuf_tensor(name, shape, dt))
        acc = sb("acc", [128, KC, T], F32)
        pvs = sb("pvs", [128, 8, KC], F32)
        g2p = sb("g2p", [128, KC], F32)
        ones = sb("ones_s", [128, 128], F32)
        s1 = sb("s1", [128, T], F32)
        s2 = sb("s2", [128, T], F32)
        mean = sb("mean", [128, T], F32)
        msq = sb("msq", [128, T], F32)
        rstd = sb("rstd", [128, T], F32)
        NTB = 3
        tb_ = [sb("tb%d" % i, [128, T], F32) for i in range(NTB)]
        yo_ = [sb("yo%d" % i, [128, T], F32) for i in range(NTB)]
        ya_ = [sb("ya%d" % i, [128, T], F32) for i in range(NTB)]
        yb_ = [sb("yb%d" % i, [128, T], F32) for i in range(NTB)]
        ps = [es.enter_context(nc.psum_tensor("ps%d" % i, [128, 512], F32)) for i in range(2)]
        S.dma('sp', lambda e: e.dma_start(out=pvs[:], in_=pv), slot='pvs', writes=['pvs'])
        S.dma('sp', lambda e: e.dma_start(out=ones[:], in_=ones_d), slot='ones', writes=['ones'])
        S.op('dve', lambda e: e.tensor_scalar(out=g2p[:], in0=pvs[:, 3, :], scalar1=1.0, scalar2=None, op0=ALU.add), reads=['pvs'], writes=['g2p'])
        av = acc_d.rearrange("(c q) t -> q c t", q=128)
        for p in range(TOK // T):
            tsl = slice(p * T, (p + 1) * T)
            for g4 in range(4):
                cs = slice(g4 * 8, (g4 + 1) * 8)
                S.dma('sp', lambda e, cs=cs, tsl=tsl: e.dma_start(out=acc[:, cs, :], in_=av[:, cs, tsl]), slot='accld%d' % g4,
                      writes=[('acc', c) for c in range(g4 * 8, g4 * 8 + 8)])
            for c in range(KC):
                i3 = c % NTB
                ya, yb = ya_[i3], yb_[i3]
                S.dma('sp', lambda e, c=c, tsl=tsl, ya=ya: e.dma_start(out=ya[:], in_=ya_d[c * 128:(c + 1) * 128, tsl]), slot='ya%d' % i3, writes=['ya%d' % i3])
                S.dma('sp', lambda e, c=c, tsl=tsl, yb=yb: e.dma_start(out=yb[:], in_=yb_d[c * 128:(c + 1) * 128, tsl]), slot='yb%d' % i3, writes=['yb%d' % i3])
                S.op('dve', lambda e, ya=ya, yb=yb: e.tensor_tensor(out=ya[:], in0=ya[:], in1=yb[:], op=ALU.add), reads=['ya%d' % i3, 'yb%d' % i3], writes=['ya%d' % i3])
                S.op('dve', lambda e, c=c, ya=ya: e.scalar_tensor_tensor(out=acc[:, c, :], in0=ya[:], scalar=g2p[:, c:c + 1], in1=acc[:, c, :], op0=ALU.mult, op1=ALU.add),
                     reads=['ya%d' % i3, 'g2p', ('acc', c)], writes=[('acc', c)])
            for c in range(KC):
                if c == 0:
                    S.op('dve', lambda e: e.tensor_copy(out=s1[:], in_=acc[:, 0, :]), reads=[('acc', 0)], writes=['s1'])
                    S.op('act', lambda e: e.activation(out=s2[:], in_=acc[:, 0, :], func=AF.Square), reads=[('acc', 0)], writes=['s2'])
                else:
                    t = tb_[c % NTB]
                    tr = 'tb%d' % (c % NTB)
                    S.op('act', lambda e, c=c, t=t: e.activation(out=t[:], in_=acc[:, c, :], func=AF.Square), reads=[('acc', c)], writes=[tr])
                    S.op('dve', lambda e, c=c: e.tensor_tensor(out=s1[:], in0=s1[:], in1=acc[:, c, :], op=ALU.add), reads=[('acc', c), 's1'], writes=['s1'])
                    S.op('dve', lambda e, t=t: e.tensor_tensor(out=s2[:], in0=s2[:], in1=t[:], op=ALU.add), reads=[tr, 's2'], writes=['s2'])
            S.op('pe', lambda e: e.matmul(ps[0][:], lhsT=ones[:], rhs=s1[:], start=True, stop=True), reads=['ones', 's1'], writes=['ps0'])
            S.op('pe', lambda e: e.matmul(ps[1][:], lhsT=ones[:], rhs=s2[:], start=True, stop=True), reads=['ones', 's2'], writes=['ps1'])
            S.op('act', lambda e: e.activation(out=mean[:], in_=ps[0][:], func=AF.Copy, scale=1.0 / D), reads=['ps0'], writes=['mean'])
            S.op('dve', lambda e: e.tensor_tensor(out=msq[:], in0=mean[:], in1=mean[:], op=ALU.mult), reads=['mean'], writes=['msq'])
            S.op('dve', lambda e: e.scalar_tensor_tensor(out=msq[:], in0=ps[1][:], scalar=1.0 / D, in1=msq[:], op0=ALU.mult, op1=ALU.subtract), reads=['ps1', 'msq'], writes=['msq'])
            S.op('dve', lambda e: e.tensor_scalar(out=msq[:], in0=msq[:], scalar1=LN_EPS, scalar2=None, op0=ALU.add), reads=['msq'], writes=['msq'])
            S.op('act', lambda e: e.activation(out=rstd[:], in_=msq[:], func=AF.Sqrt), reads=['msq'], writes=['rstd'])
            S.op('dve', lambda e: e.reciprocal(out=rstd[:], in_=rstd[:]), reads=['rstd'], writes=['rstd'])
            for c in range(KC):
                t = tb_[c % NTB]
                tr = 'tb%d' % (c % NTB)
                yo = yo_[c % NTB]
                yr = 'yo%d' % (c % NTB)
                S.op('dve', lambda e, c=c, t=t: e.tensor_tensor(out=t[:], in0=acc[:, c, :], in1=mean[:], op=ALU.subtract), reads=[('acc', c), 'mean'], writes=[tr])
                S.op('dve', lambda e, t=t: e.tensor_tensor(out=t[:], in0=t[:], in1=rstd[:], op=ALU.mult), reads=[tr, 'rstd'], writes=[tr])
                S.op('act', lambda e, c=c, t=t, yo=yo: e.activation(out=yo[:], in_=t[:], func=AF.Identity, scale=pvs[:, 6, c:c + 1], bias=pvs[:, 7, c:c + 1]),
                     reads=[tr, 'pvs'], writes=[yr])
                S.dma('sp', lambda e, c=c, yo=yo, tsl=tsl: e.dma_start(out=yT[c * 128:(c + 1) * 128, tsl], in_=yo[:]), slot=yr, reads=[yr], writes=[])
        S.finish()
    return nc

POOL_WINDOWS = (2, 4, 8, 16)

def pc(v):
    return np.ascontiguousarray(np.asarray(v, np.float32).reshape(-1, 128).T)

def stage_b_inputs(core, xT, sh1, sc1, even_w_in, lam_q1, lam_k1, lam_q2, lam_k2, subln_g, pool_w, pool_scale):
    hA, hB = 2 * core, 2 * core + 1
    g, half = core // 2, core % 2
    w = even_w_in
    cols = np.concatenate([np.arange(hA * 128, hA * 128 + 128), np.arange(hB * 128, hB * 128 + 128),
                           2048 + np.arange(hA * 128, hA * 128 + 128), 2048 + np.arange(hB * 128, hB * 128 + 128),
                           6144 + g * 512 + np.arange(512),
                           4096 + np.arange(hA * 128, hA * 128 + 128), 4096 + np.arange(hB * 128, hB * 128 + 128)])
    wq = np.ascontiguousarray(w[:, cols])
    pw = np.ascontiguousarray(pool_w[g][:, half * 256:(half + 1) * 256])
    ps = pool_scale[g * 512 + half * 256: g * 512 + (half + 1) * 256]
    psc = np.ascontiguousarray(ps.reshape(2, 128).T)
    sel4 = np.zeros((128, 4), np.float32); sel4[:, g] = 1
    wdw = POOL_WINDOWS[g]
    t = np.arange(512)
    rc = np.zeros((128, 2, 512), np.float32)
    rc[:, 0, :] = (1.0 / np.minimum(t + 1, wdw)).astype(np.float32)
    rc[:, 1, :] = np.float32(1.0 / wdw)
    slopes = (2.0 ** (-8.0 * np.arange(1, 17, dtype=np.float32) / 16)).astype(np.float32)
    bt = np.zeros((128, 2, 5, 512), np.float32)
    offt = np.zeros((128, 2, 64), np.float32)
    j = np.arange(128)[:, None].astype(np.float32); i = np.arange(512)[None, :].astype(np.float32)
    for hl, h in enumerate((hA, hB)):
        sl = slopes[h]
        bt[:, hl, 0, :] = -sl * (i - j)
        for d in range(4):
            dist = i - j - 128 * d
            bt[:, hl, d + 1, :] = np.where(dist >= 0, -sl * dist, np.float32(-1e30))
        offt[:, hl, :] = (-sl * 128 * np.arange(64, dtype=np.float32))[None, :]
    lamv = np.stack([np.broadcast_to(v, (128, 64)) for v in (lam_q1, lam_k1, lam_q2, lam_k2)], axis=1).astype(np.float32)
    sg = np.broadcast_to(subln_g, (128, 128)).astype(np.float32)
    return dict(xT=xT, mv=np.ascontiguousarray(np.stack([pc(sh1), pc(sc1)], axis=1)), wq=wq, pw=pw, psc=psc, sel4=sel4, rc=rc,
                bt=bt, offt=offt, lamv=np.ascontiguousarray(lamv), sg=np.ascontiguousarray(sg),
                identb=np.eye(128, dtype=np.float32).astype(ml_dtypes.bfloat16))

def stage_d_inputs(core, xT, sh1, sc1, odd_w_in, lb_raw, gnorm_g):
    c0 = core * 512
    cols = np.concatenate([k * 4096 + c0 + np.arange(512) for k in range(4)])
    wd = np.ascontiguousarray(odd_w_in[:, cols])
    lbr = np.ascontiguousarray(lb_raw[:, c0:c0 + 512].reshape(2, 4, 128).transpose(2, 0, 1)).astype(np.float32)
    gn4 = np.ascontiguousarray(np.broadcast_to(np.tile(gnorm_g, 4), (128, 512))).astype(np.float32)
    s = np.arange(128)[:, None]; t = np.arange(128)[None, :]
    mask01 = (s <= t).astype(np.float32)
    return dict(xT=xT, mv=np.ascontiguousarray(np.stack([pc(sh1), pc(sc1)], axis=1)), wd=wd, lbr=lbr, gn4=gn4, mask01=mask01,
                identb=np.eye(128, dtype=np.float32).astype(ml_dtypes.bfloat16))

def moe_layout(gate, h2T, PASS=256):
    sel = gate != 0
    ok = bool((sel.sum(1) == 2).all())
    idx = [np.nonzero(sel[:, e])[0] for e in range(gate.shape[1])]
    nmax = max(1, max(len(i) for i in idx))
    NP = ((nmax + PASS - 1) // PASS) * PASS
    per = []
    for e, ie in enumerate(idx):
        n = len(ie)
        hT = np.zeros((h2T.shape[0], NP), h2T.dtype)
        hT[:, :n] = h2T[:, ie]
        gb = np.zeros((128, NP), np.float32)
        gb[:, :n] = gate[ie, e][None, :]
        per.append(dict(hT=hT, gb=gb))
    rank = np.cumsum(sel, axis=1) - 1
    return ok, idx, NP, per, rank

def moe_scatter(ys, idx, rank, N):
    Dm = ys[0].shape[0]
    YA = np.zeros((Dm, N), np.float32)
    YB = np.zeros((Dm, N), np.float32)
    for e, ie in enumerate(idx):
        n = len(ie)
        r = rank[ie, e]
        y = ys[e][:, :n]
        YA[:, ie[r == 0]] = y[:, r == 0]
        YB[:, ie[r == 1]] = y[:, r == 1]
    return YA, YB

def _run(nc, in_maps):
    return run_bass_kernel_spmd(nc, in_maps, core_ids=list(range(8))).results


def kernel(**inputs):
    g = lambda k: np.asarray(inputs[k])
    x = np.asarray(g('x'), np.float32)[0]
    c = np.asarray(g('c'), np.float32)[0]
    ada_w, ada_b = g('ada_w'), g('ada_b')
    ln_g, ln_b = g('ln_g'), g('ln_b')
    NCORE = 8
    in_maps = []
    cvp = pc(c)
    for core in range(NCORE):
        l, cb = core // 4, core % 4
        in_maps.append(dict(aw=np.ascontiguousarray(ada_w[l][:, cb * 6144:(cb + 1) * 6144]),
                            ab=np.ascontiguousarray(ada_b[l][cb * 6144:(cb + 1) * 6144][None, :]), cv=cvp))
    res = _run(build_a(), in_maps)
    mod = np.concatenate([res[core]['mo'][0] for core in range(NCORE)]).reshape(2, 6, 4096)
    del in_maps
    ones = np.ones((128, 128), np.float32)
    xT = np.ascontiguousarray(x.T)
    in_maps = [stage_b_inputs(core, xT, mod[0, 0], mod[0, 1], g('even_w_in')[0], g('lam_q1')[0], g('lam_k1')[0], g('lam_q2')[0],
                              g('lam_k2')[0], g('subln_g')[0], g('pool_w')[0], g('pool_scale')[0]) for core in range(NCORE)]
    res = _run(build_b(), in_maps)
    ocT = np.empty((4096, 8192), ml_dtypes.bfloat16)
    for core in range(NCORE):
        o = res[core]['oTc']
        ocT[core * 256:(core + 1) * 256] = o[0:256]
        ocT[2048 + core * 256:2048 + (core + 1) * 256] = o[256:512]
    del in_maps, res

    def ff_stage(moe, l, ocT, xT_in, w_o, extra):
        pv = np.ascontiguousarray(np.stack([pc(mod[l, 2]), pc(mod[l, 3]), pc(mod[l, 4]), pc(mod[l, 5]),
                                            pc(ln_g[l, 0]), pc(ln_b[l, 0]), pc(ln_g[l, 1]), pc(ln_b[l, 1])], axis=1))
        in_maps = []
        for core in range(NCORE):
            sl = slice(core * 1024, (core + 1) * 1024)
            m = dict(oT=np.ascontiguousarray(ocT[:, sl]), xT=np.ascontiguousarray(xT_in[:, sl]), pv=pv, w_o=w_o, ones=ones)
            m.update(extra)
            in_maps.append(m)
        res = _run(build_ff(moe), in_maps)
        return np.concatenate([res[core]['yT'] for core in range(NCORE)], axis=1)

    x1T = ff_stage(False, 0, ocT, xT, np.ascontiguousarray(g('even_w_out')[0]),
                   dict(w_in=np.ascontiguousarray(g('ffn_w_in')[0]), w_o2=np.ascontiguousarray(g('ffn_w_out')[0])))
    del xT
    in_maps = [stage_d_inputs(core, x1T, mod[1, 0], mod[1, 1], g('odd_w_in')[0], g('lb_raw'), g('gnorm_g')[0]) for core in range(NCORE)]
    res = _run(build_d(), in_maps)
    oT = np.concatenate([res[core]['oTd'] for core in range(NCORE)], axis=0)
    del in_maps, res
    rw = np.ascontiguousarray(np.asarray(g('router_w')[0], np.float32).reshape(32, 128, 8).transpose(1, 0, 2))
    w_o1 = np.ascontiguousarray(g('odd_w_out')[0])
    pv1 = np.ascontiguousarray(np.stack([pc(mod[1, 2]), pc(mod[1, 3]), pc(mod[1, 4]), pc(mod[1, 5]),
                                         pc(ln_g[1, 0]), pc(ln_b[1, 0]), pc(ln_g[1, 1]), pc(ln_b[1, 1])], axis=1))
    in_maps = []
    for core in range(NCORE):
        sl = slice(core * 1024, (core + 1) * 1024)
        in_maps.append(dict(oT=np.ascontiguousarray(oT[:, sl]), xT=np.ascontiguousarray(x1T[:, sl]), pv=pv1, w_o=w_o1, ones=ones, rw=rw))
    res = _run(build_ff(True, mode='e1'), in_maps)
    h2T = np.concatenate([res[core]['h2o'] for core in range(NCORE)], axis=1)
    accT = np.concatenate([res[core]['acco'] for core in range(NCORE)], axis=1)
    gate = np.concatenate([res[core]['gout'] for core in range(NCORE)], axis=0)
    del in_maps, res
    ok, idx, NP, per, rank = moe_layout(gate, h2T)
    if not ok:
        sel = np.zeros((8, 8, 128), np.float32)
        for e in range(8):
            sel[e, e, :] = 1
        x2T = ff_stage(True, 1, oT, x1T, w_o1,
                       dict(w_in=np.ascontiguousarray(g('exp_w_in')[0]), w_o2=np.ascontiguousarray(g('exp_w_out')[0]), rw=rw, sel=sel,
                            ident=np.eye(128, dtype=np.float32)))
        return np.ascontiguousarray(x2T.T)[None].astype(np.float32)
    ewi, ewo = g('exp_w_in')[0], g('exp_w_out')[0]
    in_maps = [dict(hT=per[e]['hT'], gb=per[e]['gb'], w_in=np.ascontiguousarray(ewi[e]), w_o2=np.ascontiguousarray(ewo[e])) for e in range(NCORE)]
    res = _run(build_e2(NP), in_maps)
    YA, YB = moe_scatter([res[e]['yT'] for e in range(NCORE)], idx, rank, 8192)
    del in_maps, res, per
    in_maps = []
    for core in range(NCORE):
        sl = slice(core * 1024, (core + 1) * 1024)
        in_maps.append(dict(accT=np.ascontiguousarray(accT[:, sl]), yA=np.ascontiguousarray(YA[:, sl]), yB=np.ascontiguousarray(YB[:, sl]), pv=pv1, ones=ones))
    res = _run(build_e3(), in_maps)
    x2T = np.concatenate([res[core]['yT'] for core in range(NCORE)], axis=1)
    return np.ascontiguousarray(x2T.T)[None].astype(np.float32)
```
